# Optimizing a Trainium2 kernel written in Bass

```python
import jax
import jax.numpy as jnp
from jax import lax
import numpy as np

D_MODEL = 1024
BATCH = 4
SEQ = 4096
DEPTH = 1

GRID_W = 64
CTX_LEN = 256
HEAD_DIM = 64
A_HEADS = 8
A_KV_HEADS = 2
A_GROUP = A_HEADS // A_KV_HEADS
WINDOW = 128
BLOCK = 128
A_SCALE = HEAD_DIM ** -0.5
MLA_HEADS = 8
MLA_NOPE = 64
MLA_ROPE = 32
MLA_V = 64
MLA_Q_LORA = 384
MLA_KV_LORA = 256
MLA_SCALE = (MLA_NOPE + MLA_ROPE) ** -0.5
A_Q_W = A_HEADS * HEAD_DIM
A_KV_W = A_KV_HEADS * HEAD_DIM
PROJ_SPLITS = (A_Q_W, A_Q_W + A_KV_W, A_Q_W + 2 * A_KV_W, A_Q_W + 2 * A_KV_W + MLA_Q_LORA,
               A_Q_W + 2 * A_KV_W + MLA_Q_LORA + MLA_KV_LORA)
PROJ_WIDTH = A_Q_W + 2 * A_KV_W + MLA_Q_LORA + MLA_KV_LORA + MLA_ROPE
MIX_WIDTH = A_HEADS * HEAD_DIM + MLA_HEADS * MLA_V
N_EXPERTS = 32
TOP_K = 4
D_FF = 1024
SWIGLU_LIMIT = 7.0
SWIGLU_ALPHA = 1.702
MOE_BLOCK = 256
ROPE_BASE = 10000.0
EPS = 1e-6
NEG_INF = -1e30

kernel_name = 'hybrid_swa_mla_moe_dit_layer'


def rms_norm(x, g):
    xf = x.astype(jnp.float32)
    y = xf * lax.rsqrt(jnp.mean(xf * xf, axis=-1, keepdims=True) + EPS)
    return (y * g.astype(jnp.float32)).astype(x.dtype)


def modulate(h, shift, scale):
    return h * (1 + scale) + shift


def axial_rope_tables(rows, rot_dim):
    row = jnp.broadcast_to(jnp.arange(rows, dtype=jnp.float32)[:, None], (rows, GRID_W)).reshape(-1)
    col = jnp.broadcast_to(jnp.arange(GRID_W, dtype=jnp.float32)[None, :], (rows, GRID_W)).reshape(-1)
    quarter = rot_dim // 4
    inv_freq = ROPE_BASE ** (-jnp.arange(quarter, dtype=jnp.float32) / quarter)
    ang = jnp.concatenate([row[:, None] * inv_freq, col[:, None] * inv_freq], axis=-1)
    return jnp.cos(ang), jnp.sin(ang)


def apply_rope(x, cos, sin):
    xf = x.astype(jnp.float32)
    half = xf.shape[-1] // 2
    x1, x2 = xf[..., :half], xf[..., half:]
    c, s = cos[:, None, :], sin[:, None, :]
    return jnp.concatenate([x1 * c - x2 * s, x1 * s + x2 * c], axis=-1).astype(x.dtype)


def windowed_gqa(q, k, v, kc, vc, sink):
    B, S = q.shape[:2]
    C = kc.shape[1]
    nb = S // BLOCK
    qb = q.reshape(B, nb, BLOCK, A_KV_HEADS, A_GROUP, HEAD_DIM)
    pad = ((0, 0), (BLOCK, BLOCK), (0, 0), (0, 0))
    kp = jnp.pad(k, pad).reshape(B, nb + 2, BLOCK, A_KV_HEADS, HEAD_DIM)
    vp = jnp.pad(v, pad).reshape(B, nb + 2, BLOCK, A_KV_HEADS, HEAD_DIM)
    kb = jnp.concatenate([kp[:, :-2], kp[:, 1:-1], kp[:, 2:]], axis=2)
    vb = jnp.concatenate([vp[:, :-2], vp[:, 1:-1], vp[:, 2:]], axis=2)
    n_loc = 3 * BLOCK
    s_loc = jnp.einsum('bnqhgd,bnkhd->bnhgqk', qb, kb, preferred_element_type=jnp.float32) * A_SCALE
    qpos = jnp.arange(nb)[:, None, None] * BLOCK + jnp.arange(BLOCK)[None, :, None]
    kpos = jnp.arange(nb)[:, None, None] * BLOCK - BLOCK + jnp.arange(n_loc)[None, None, :]
    valid = (jnp.abs(qpos - kpos) <= WINDOW) & (kpos >= 0) & (kpos < S)
    s_loc = jnp.where(valid[None, :, None, None], s_loc, NEG_INF)
    s_ctx = jnp.einsum('bnqhgd,bchd->bnhgqc', qb, kc, preferred_element_type=jnp.float32) * A_SCALE
    s_sink = jnp.broadcast_to(sink.astype(jnp.float32).reshape(1, 1, A_KV_HEADS, A_GROUP, 1, 1),
                              s_loc.shape[:-1] + (1,))
    p = jax.nn.softmax(jnp.concatenate([s_loc, s_ctx, s_sink], axis=-1), axis=-1).astype(v.dtype)
    o = (jnp.einsum('bnhgqk,bnkhd->bnqhgd', p[..., :n_loc], vb)
         + jnp.einsum('bnhgqc,bchd->bnqhgd', p[..., n_loc:n_loc + C], vc))
    return o.reshape(B, S, A_HEADS * HEAD_DIM)


def context_gqa(q, k, v, sink):
    B, C = q.shape[:2]
    qg = q.reshape(B, C, A_KV_HEADS, A_GROUP, HEAD_DIM)
    s = jnp.einsum('bqhgd,bkhd->bhgqk', qg, k, preferred_element_type=jnp.float32) * A_SCALE
    s_sink = jnp.broadcast_to(sink.astype(jnp.float32).reshape(1, A_KV_HEADS, A_GROUP, 1, 1), s.shape[:-1] + (1,))
    p = jax.nn.softmax(jnp.concatenate([s, s_sink], axis=-1), axis=-1)[..., :-1].astype(v.dtype)
    return jnp.einsum('bhgqk,bkhd->bqhgd', p, v).reshape(B, C, A_HEADS * HEAD_DIM)


def mla_project(cq, ckv, g_q_a, w_uq, g_kv_a, w_ukv):
    B, N = cq.shape[:2]
    q = (rms_norm(cq, g_q_a) @ w_uq).reshape(B, N, MLA_HEADS, MLA_NOPE + MLA_ROPE)
    kv = (rms_norm(ckv, g_kv_a) @ w_ukv).reshape(B, N, MLA_HEADS, MLA_NOPE + MLA_V)
    return q[..., :MLA_NOPE], q[..., MLA_NOPE:], kv[..., :MLA_NOPE], kv[..., MLA_NOPE:]


def mla_attend(qn, qr, kn, kr, v):
    s = (jnp.einsum('bqhd,bkhd->bhqk', qn, kn, preferred_element_type=jnp.float32)
         + jnp.einsum('bqhd,bkd->bhqk', qr, kr, preferred_element_type=jnp.float32)) * MLA_SCALE
    p = jax.nn.softmax(s, axis=-1).astype(v.dtype)
    return jnp.einsum('bhqk,bkhd->bqhd', p, v)


def mla_latent(qn, qr, kn, kr, v, kn_c, kr_c, v_c):
    B, S = qn.shape[:2]
    nb = S // BLOCK
    kn_all = jnp.concatenate([kn_c, kn], axis=1)
    kr_all = jnp.concatenate([kr_c, kr], axis=1)
    v_all = jnp.concatenate([v_c, v], axis=1)
    qn_b = qn.reshape(B, nb, BLOCK, MLA_HEADS, MLA_NOPE).transpose(1, 0, 2, 3, 4)
    qr_b = qr.reshape(B, nb, BLOCK, MLA_HEADS, MLA_ROPE).transpose(1, 0, 2, 3, 4)
    o = lax.map(lambda qs: mla_attend(qs[0], qs[1], kn_all, kr_all, v_all), (qn_b, qr_b))
    return o.transpose(1, 0, 2, 3, 4).reshape(B, S, MLA_HEADS * MLA_V)


def token_mixing(h, hc, w_in, sink, g_q_a, w_uq, g_kv_a, w_ukv, w_o, rope_a, rope_b, ctx_out):
    B, S, _ = h.shape
    C = hc.shape[1]
    qa, ka, va, cq, ckv, kr = jnp.split(h @ w_in, PROJ_SPLITS, axis=-1)
    qa_c, ka_c, va_c, cq_c, ckv_c, kr_c = jnp.split(hc @ w_in, PROJ_SPLITS, axis=-1)
    qa = apply_rope(qa.reshape(B, S, A_HEADS, HEAD_DIM), *rope_a)
    ka = apply_rope(ka.reshape(B, S, A_KV_HEADS, HEAD_DIM), *rope_a)
    va = va.reshape(B, S, A_KV_HEADS, HEAD_DIM)
    ka_c = ka_c.reshape(B, C, A_KV_HEADS, HEAD_DIM)
    va_c = va_c.reshape(B, C, A_KV_HEADS, HEAD_DIM)
    out_a = windowed_gqa(qa, ka, va, ka_c, va_c, sink)
    qn, qr, kn, vb = mla_project(cq, ckv, g_q_a, w_uq, g_kv_a, w_ukv)
    qr = apply_rope(qr, *rope_b)
    kr = apply_rope(kr[:, :, None, :], *rope_b)[:, :, 0, :]
    qn_c, qr_c, kn_c, vb_c = mla_project(cq_c, ckv_c, g_q_a, w_uq, g_kv_a, w_ukv)
    out_b = mla_latent(qn, qr, kn, kr, vb, kn_c, kr_c, vb_c)
    y = jnp.concatenate([out_a, out_b], axis=-1) @ w_o
    if not ctx_out:
        return y, None
    out_a_c = context_gqa(qa_c.reshape(B, C, A_HEADS, HEAD_DIM), ka_c, va_c, sink)
    out_b_c = mla_attend(qn_c, qr_c, kn_c, kr_c, vb_c).reshape(B, C, MLA_HEADS * MLA_V)
    yc = jnp.concatenate([out_a_c, out_b_c], axis=-1) @ w_o
    return y, yc


def moe_ffn(h, w_router, b_router, w_gate_up, b_gate_up, w_down, b_down):
    shape = h.shape
    d = shape[-1]
    xt = h.reshape(-1, d)
    T = xt.shape[0]
    logits = jnp.einsum('td,de->te', xt, w_router, preferred_element_type=jnp.float32) + b_router.astype(jnp.float32)
    top_logit, top_idx = lax.top_k(logits, TOP_K)
    top_w = jax.nn.softmax(top_logit, axis=-1)
    n_assign = T * TOP_K
    flat_e = top_idx.reshape(-1)
    order = jnp.argsort(flat_e)
    sorted_e = flat_e[order]
    counts = jnp.bincount(flat_e, length=N_EXPERTS)
    padded = (counts + MOE_BLOCK - 1) // MOE_BLOCK * MOE_BLOCK
    pad_end = jnp.cumsum(padded)
    pad_start = pad_end - padded
    start = jnp.cumsum(counts) - counts
    dest = pad_start[sorted_e] + jnp.arange(n_assign) - start[sorted_e]
    n_blocks = -(-n_assign // MOE_BLOCK) + N_EXPERTS
    cap = n_blocks * MOE_BLOCK
    slot_tok = jnp.full((cap,), T, jnp.int32).at[dest].set((order // TOP_K).astype(jnp.int32))
    slot_w = jnp.zeros((cap,), jnp.float32).at[dest].set(top_w.reshape(-1)[order])
    block_e = jnp.minimum(jnp.searchsorted(pad_end, jnp.arange(n_blocks) * MOE_BLOCK, side='right'), N_EXPERTS - 1)
    x_pad = jnp.concatenate([xt, jnp.zeros((1, d), xt.dtype)], axis=0)
    xs = x_pad[slot_tok].reshape(n_blocks, MOE_BLOCK, d)

    def expert_block(args):
        xb, e = args
        gate, lin = jnp.split(xb @ w_gate_up[e] + b_gate_up[e], 2, axis=-1)
        gate = jnp.minimum(gate, SWIGLU_LIMIT)
        lin = jnp.clip(lin, -SWIGLU_LIMIT, SWIGLU_LIMIT)
        act = (lin + 1) * (gate * jax.nn.sigmoid(SWIGLU_ALPHA * gate))
        return act @ w_down[e] + b_down[e]

    ys = lax.map(expert_block, (xs, block_e)).reshape(cap, d)
    ys = ys * slot_w[:, None].astype(ys.dtype)
    out = jnp.zeros((T + 1, d), ys.dtype).at[slot_tok].add(ys)[:T]
    return out.reshape(shape)


def setup_inputs(seed: int = 0) -> dict:
    key = jax.random.key(seed)
    ks = jax.random.split(key, 24)
    f32 = jnp.float32
    L, D = DEPTH, D_MODEL

    def nrm(k, shape, scale):
        return jax.random.normal(k, shape, f32) * scale

    return {
        'x': nrm(ks[0], (BATCH, SEQ, D), 1.0),
        'c': nrm(ks[1], (BATCH, D), 1.0),
        'ctx': nrm(ks[2], (BATCH, CTX_LEN, D), 1.0),
        'c_ctx': nrm(ks[3], (D,), 1.0),
        'w_ada': nrm(ks[4], (L, D, 6 * D), 0.5 * D ** -0.5),
        'b_ada': nrm(ks[5], (L, 6 * D), 0.02),
        'g_mix_pre': 1.0 + nrm(ks[6], (L, D), 0.1),
        'g_mix_post': 1.0 + nrm(ks[7], (L, D), 0.1),
        'w_in': nrm(ks[8], (L, D, PROJ_WIDTH), D ** -0.5),
        'sink': nrm(ks[9], (L, A_HEADS), 1.0),
        'g_q_a': 1.0 + nrm(ks[10], (L, MLA_Q_LORA), 0.1),
        'w_uq': nrm(ks[11], (L, MLA_Q_LORA, MLA_HEADS * (MLA_NOPE + MLA_ROPE)), MLA_Q_LORA ** -0.5),
        'g_kv_a': 1.0 + nrm(ks[12], (L, MLA_KV_LORA), 0.1),
        'w_ukv': nrm(ks[13], (L, MLA_KV_LORA, MLA_HEADS * (MLA_NOPE + MLA_V)), MLA_KV_LORA ** -0.5),
        'w_o': nrm(ks[14], (L, MIX_WIDTH, D), MIX_WIDTH ** -0.5),
        'g_ffn_pre': 1.0 + nrm(ks[15], (L, D), 0.1),
        'g_ffn_post': 1.0 + nrm(ks[16], (L, D), 0.1),
        'w_router': nrm(ks[17], (L, D, N_EXPERTS), D ** -0.5),
        'b_router': nrm(ks[18], (L, N_EXPERTS), 0.01),
        'w_gate_up': nrm(ks[19], (L, N_EXPERTS, D, 2 * D_FF), D ** -0.5),
        'b_gate_up': nrm(ks[20], (L, N_EXPERTS, 2 * D_FF), 0.02),
        'w_down': nrm(ks[21], (L, N_EXPERTS, D_FF, D), D_FF ** -0.5),
        'b_down': nrm(ks[22], (L, N_EXPERTS, D), 0.02),
    }


def reference(x, c, ctx, c_ctx, w_ada, b_ada, g_mix_pre, g_mix_post, w_in, sink, g_q_a, w_uq, g_kv_a, w_ukv,
              w_o, g_ffn_pre, g_ffn_post, w_router, b_router, w_gate_up, b_gate_up, w_down, b_down):
    S = x.shape[1]
    ROWS = S // GRID_W
    rope_a = axial_rope_tables(ROWS, HEAD_DIM)
    rope_b = axial_rope_tables(ROWS, MLA_ROPE)
    silu_c = jax.nn.silu(c)
    silu_cc = jax.nn.silu(c_ctx)
    for l in range(DEPTH):
        ctx_out = l < DEPTH - 1
        mod = jnp.split((silu_c @ w_ada[l] + b_ada[l])[:, None, :], 6, axis=-1)
        mod_c = jnp.split(silu_cc @ w_ada[l] + b_ada[l], 6, axis=-1)
        h = modulate(rms_norm(x, g_mix_pre[l]), mod[0], mod[1])
        hc = modulate(rms_norm(ctx, g_mix_pre[l]), mod_c[0], mod_c[1])
        y, yc = token_mixing(h, hc, w_in[l], sink[l], g_q_a[l], w_uq[l], g_kv_a[l], w_ukv[l], w_o[l],
                             rope_a, rope_b, ctx_out)
        x = x + mod[2] * rms_norm(y, g_mix_post[l])
        h2 = modulate(rms_norm(x, g_ffn_pre[l]), mod[3], mod[4])
        f = moe_ffn(h2, w_router[l], b_router[l], w_gate_up[l], b_gate_up[l], w_down[l], b_down[l])
        x = x + mod[5] * rms_norm(f, g_ffn_post[l])
        if ctx_out:
            ctx = ctx + mod_c[2] * rms_norm(yc, g_mix_post[l])
            hc2 = modulate(rms_norm(ctx, g_ffn_pre[l]), mod_c[3], mod_c[4])
            fc = moe_ffn(hc2, w_router[l], b_router[l], w_gate_up[l], b_gate_up[l], w_down[l], b_down[l])
            ctx = ctx + mod_c[5] * rms_norm(fc, g_ffn_post[l])
    return x
```

```python
import numpy as np
from contextlib import ExitStack
from collections import deque
import concourse.bass as bass
import concourse.mybir as mybir
from concourse.bass_utils import run_bass_kernel_spmd

F32 = mybir.dt.float32
BF16 = mybir.dt.bfloat16
AF = mybir.ActivationFunctionType
ALU = mybir.AluOpType

import os
DBG_SKIP_ROUTER = os.environ.get("DBG_SKIP_ROUTER") == "1"
EPS = 1e-6
A_SCALE = 64 ** -0.5
MLA_SCALE = 96 ** -0.5
C_QA, C_KA, C_VA, C_CQ, C_CKV, C_KR2, C_QAP, C_KAP, C_END = 0, 512, 640, 768, 1152, 1408, 1600, 2112, 2240


class Buf:
    __slots__ = ("name", "w", "r")

    def __init__(self, name):
        self.name = name
        self.w = None
        self.r = []


class Eng:
    def __init__(self, S, name, obj, is_pe=False, n_dma=0):
        self.name = name
        self.obj = obj
        self.sem = S.new_sem("e_" + name)
        self.count = 0
        self.seen = {}
        self.is_pe = is_pe
        self.pend_r = []
        self.pend_w = []
        self.dma_sems = [[S.new_sem("d_%s%d" % (name, i)), 0] for i in range(n_dma)]
        self.dma_rr = 0


class Sched:
    def __init__(self, nc, es):
        self.nc = nc
        self.es = es
        self.pe = Eng(self, "pe", nc.tensor, is_pe=True)
        self.dve = Eng(self, "dve", nc.vector)
        self.act = Eng(self, "act", nc.scalar)
        self.pool = Eng(self, "pool", nc.gpsimd, n_dma=16)
        self.sp = Eng(self, "sp", nc.sync, n_dma=24)
        self.engs = [self.pe, self.dve, self.act, self.pool, self.sp]
        self.n_inst = 0

    def new_sem(self, name):
        return self.es.enter_context(self.nc.semaphore(name))

    def _wait(self, eng, ev):
        sem, val = ev
        k = id(sem)
        if eng.seen.get(k, 0) >= val:
            return
        eng.obj.wait_ge(sem, val)
        eng.seen[k] = val
        self.n_inst += 1

    def _deps(self, eng, reads, writes):
        for b in reads:
            if b.w is not None and not (eng.is_pe and b.w[0] is eng.sem):
                self._wait(eng, b.w)
        for b in writes:
            if b.w is not None and not (eng.is_pe and b.w[0] is eng.sem):
                self._wait(eng, b.w)
            for ev in b.r:
                if not (eng.is_pe and ev[0] is eng.sem):
                    self._wait(eng, ev)

    def _commit(self, ev, reads, writes):
        for b in reads:
            b.r.append(ev)
            if len(b.r) > 48:
                last = {}
                for e in b.r:
                    k = id(e[0])
                    if k not in last or last[k][1] < e[1]:
                        last[k] = e
                b.r = list(last.values())
        for b in writes:
            b.w = ev
            b.r = []

    def op(self, eng, fn, reads=(), writes=()):
        self._deps(eng, reads, writes)
        ins = fn(eng.obj)
        eng.count += 1
        ev = (eng.sem, eng.count)
        ins.then_inc(eng.sem, 1)
        self._commit(ev, reads, writes)
        self.n_inst += 1
        return ev

    def mm(self, fn, reads=(), writes=(), signal=True, first=True):
        eng = self.pe
        self._deps(eng, reads, writes if first else ())
        ins = fn(eng.obj)
        eng.pend_r.extend(reads)
        for b in writes:
            if b not in eng.pend_w:
                eng.pend_w.append(b)
        self.n_inst += 1
        if signal:
            eng.count += 1
            ev = (eng.sem, eng.count)
            ins.then_inc(eng.sem, 1)
            self._commit(ev, eng.pend_r, eng.pend_w)
            eng.pend_r = []
            eng.pend_w = []
            return ev
        return None

    def dma(self, q, out, in_, reads=(), writes=()):
        slot = q.dma_sems[q.dma_rr % len(q.dma_sems)]
        q.dma_rr += 1
        sem, cur = slot
        if cur > 0:
            self._wait(q, (sem, cur))
        self._deps(q, reads, writes)
        ins = q.obj.dma_start(out=out, in_=in_)
        slot[1] = cur + 16
        ev = (sem, cur + 16)
        ins.then_inc(sem, 16)
        self._commit(ev, reads, writes)
        self.n_inst += 1
        return ev

    def barrier(self):
        assert not self.pe.pend_r and not self.pe.pend_w
        evs = [(e.sem, e.count) for e in self.engs if e.count > 0]
        for e in self.engs:
            evs += [(s, v) for s, v in e.dma_sems if v > 0]
        for e in self.engs:
            for ev in evs:
                if ev[0] is e.sem and e.is_pe:
                    continue
                self._wait(e, ev)


class Arena:
    def __init__(self, nc, base=20480, top=229312):
        self.nc = nc
        self.ptr = base
        self.top = top
        self.n = 0
        self.peak = base
        self.limit = top

    def alloc_at(self, name, shape, dtype, off):
        self.n += 1
        return self.nc.alloc_sbuf_tensor_at("%s_%d" % (name, self.n), list(shape), dtype, offset=off)

    def alloc(self, name, shape, dtype):
        esz = 4 if dtype == F32 else 2
        nbytes = int(np.prod(shape[1:])) * esz
        off = (self.ptr + 31) // 32 * 32
        assert off + nbytes <= self.limit, ("SBUF overflow", name, off, nbytes, self.limit)
        self.ptr = off + nbytes
        self.peak = max(self.peak, self.ptr)
        self.n += 1
        return self.nc.alloc_sbuf_tensor_at("%s_%d" % (name, self.n), list(shape), dtype, offset=off)

    def mark(self):
        return self.ptr

    def release(self, m):
        self.ptr = m


def build_program(stage=99, dbg=False, n_experts=32):
    nc = bass.Bass("TRN2", target_bir_lowering=False)

    def din(name, shape, dt=F32):
        return nc.dram_tensor(name, list(shape), dt, kind="ExternalInput").ap()

    def dout(name, shape, dt=F32):
        return nc.dram_tensor(name, list(shape), dt, kind="ExternalOutput").ap()

    def dscr(name, shape, dt):
        return nc.dram_tensor(name, list(shape), dt, kind="Internal").ap()

    xin = din("xin", [4352, 1024])
    cvec = din("cvec", [128, 16])
    w_ada = din("w_ada", [1024, 6144])
    rows_in = din("rows", [1, 10240])
    w_inx = din("w_inx", [1024, C_END])
    tabc = din("tabc", [96, 4096])
    tabs = din("tabs", [96, 4096])
    masks = din("masks", [128, 4, 512])
    sinkr = din("sinkr", [1, 1024])
    gq = din("gq", [128, 3])
    gkv = din("gkv", [128, 2])
    w_uq = din("w_uq", [384, 768])
    w_uqp = din("w_uqp", [384, 768])
    w_ukv = din("w_ukv", [256, 1024])
    w_o = din("w_o", [1024, 1024])
    w_r = din("w_r", [1024, 32])
    b_r16 = din("b_r16", [1, 512])
    w_gu = din("w_gu", [n_experts, 1024, 2048])
    b_gu = din("b_gu", [32, 2048])
    w_dn = din("w_dn", [n_experts, 1024, 1024])
    b_dn = din("b_dn", [32, 1024])
    ident = din("ident", [128, 128])
    out = dout("out", [2048, 1024])

    cq_s = dscr("cq_s", [128, 3, 2048], BF16)
    ckv_s = dscr("ckv_s", [128, 2, 4352], BF16)
    kr_s = dscr("kr_s", [96, 4352], BF16)
    x1_s = dscr("x1_s", [2048, 1024], F32)

    dbg_outs = {}

    def ddump(S, name, src_ap, shape, reads, cast=True):
        if not dbg:
            return
        o = dout(name, shape)
        dbg_outs[name] = o
        S.dma(S.pool if cast else S.sp, o, src_ap, reads=reads)

    with ExitStack() as es:
        S = Sched(nc, es)
        A = Arena(nc)
        pe, dve, act, pool, sp = S.pe, S.dve, S.act, S.pool, S.sp

        ident_b = A.alloc("ident_b", [128, 128], BF16)
        ident_f = A.alloc("ident_f", [128, 128], F32)
        ones_b = A.alloc("ones_b", [128, 128], BF16)
        ones_f = A.alloc("ones_f", [128, 128], F32)
        e64 = A.alloc("e64", [1, 65], BF16)
        gf_b = A.alloc("gf_b", [128, 1024], F32)
        gm_b = A.alloc("gm_b", [128, 1024], F32)
        gs2_b = A.alloc("gs2_b", [128, 1024], F32)
        sh2_b = A.alloc("sh2_b", [128, 1024], F32)
        Bc = {k: Buf(k) for k in ["ident_b", "ident_f", "ones_b", "ones_f", "e64", "gf_b", "gm_b", "gs2_b", "sh2_b"]}
        S.dma(pool, ident_b[:], ident, writes=[Bc["ident_b"]])
        S.dma(sp, ident_f[:], ident, writes=[Bc["ident_f"]])
        S.op(pool, lambda e: e.memset(ones_b[:], 1.0), writes=[Bc["ones_b"]])
        S.op(pool, lambda e: e.memset(ones_f[:], 1.0), writes=[Bc["ones_f"]])
        S.op(pool, lambda e: e.memset(e64[:], 0.0), writes=[Bc["e64"]])
        S.op(pool, lambda e: e.memset(e64[0:1, 64:65], 1.0), writes=[Bc["e64"]])
        m_persist = A.mark()

        gs1_b = A.alloc("gs1_b", [128, 1024], F32)
        sh1_b = A.alloc("sh1_b", [128, 1024], F32)
        gsc_b = A.alloc("gsc_b", [128, 1024], F32)
        shc_b = A.alloc("shc_b", [128, 1024], F32)
        for k in ["gs1_b", "sh1_b", "gsc_b", "shc_b"]:
            Bc[k] = Buf(k)
        m_bc1 = A.mark()
        with ExitStack() as pes:
            rows_t = A.alloc("rows_t", [1, 10240], F32)
            cv = A.alloc("cv", [128, 16], F32)
            sg = A.alloc("sg", [128, 16], F32)
            sl = A.alloc("sl", [128, 16], F32)
            rep = A.alloc("rep", [128, 16, 128], BF16)
            wa = [A.alloc("wa%d" % i, [128, 8, 512], BF16) for i in range(2)]
            gb = [A.alloc("gb%d" % i, [128, 512], F32) for i in range(2)]
            pm = [pes.enter_context(nc.psum_tensor("pm%d" % i, [128, 512], F32)) for i in range(2)]
            pmc = [pes.enter_context(nc.psum_tensor("pmc%d" % i, [128, 512], F32)) for i in range(2)]
            pg = [pes.enter_context(nc.psum_tensor("pg%d" % i, [128, 512], F32)) for i in range(2)]
            B0 = {k: Buf(k) for k in ["rows", "cv", "sg", "sl", "rep", "wa0", "wa1", "gb0", "gb1", "pm0", "pm1",
                                      "pmc0", "pmc1", "pg0", "pg1"]}
            S.dma(sp, rows_t[:], rows_in, writes=[B0["rows"]])
            S.dma(sp, cv[:], cvec, writes=[B0["cv"]])
            S.op(act, lambda e: e.activation(out=sg[:], in_=cv[:], func=AF.Sigmoid), reads=[B0["cv"]], writes=[B0["sg"]])
            S.op(dve, lambda e: e.tensor_tensor(out=sl[:], in0=cv[:], in1=sg[:], op=ALU.mult),
                 reads=[B0["cv"], B0["sg"]], writes=[B0["sl"]])
            for j in range(16):
                S.op(dve, lambda e, j=j: e.tensor_scalar(rep[:, j, :], ones_f[:], sl[:, j:j + 1], None, op0=ALU.mult),
                     reads=[B0["sl"], Bc["ones_f"]], writes=[B0["rep"]])
            w_ada_v = w_ada.rearrange("(c p) n -> p c n", p=128)
            g_off = {1: 0, 2: 1024, 4: 2048, 5: 3072}
            dests = {0: sh1_b, 1: gs1_b, 2: gm_b, 3: sh2_b, 4: gs2_b, 5: gf_b}
            destB = {0: "sh1_b", 1: "gs1_b", 2: "gm_b", 3: "sh2_b", 4: "gs2_b", 5: "gf_b"}
            for j in range(12):
                m, half = j // 2, j % 2
                k = j % 2
                cs = slice(half * 512, (half + 1) * 512)
                S.dma(pool, wa[k][:], w_ada_v[:, :, j * 512:(j + 1) * 512], writes=[B0["wa%d" % k]])
                vecs = [(0, pm[k], B0["pm%d" % k], dests[m], Bc[destB[m]])]
                if m < 2:
                    vecs.append((8, pmc[k], B0["pmc%d" % k], (shc_b if m == 0 else gsc_b),
                                 Bc["shc_b" if m == 0 else "gsc_b"]))
                if m in g_off:
                    go = g_off[m] + half * 512
                    S.mm(lambda e, k=k, go=go: e.matmul(pg[k][:], ones_f[0:1, :], rows_t[0:1, go:go + 512],
                                                        start=True, stop=True),
                         reads=[Bc["ones_f"], B0["rows"]], writes=[B0["pg%d" % k]])
                    S.op(act, lambda e, k=k: e.copy(gb[k][:], pg[k][:]), reads=[B0["pg%d" % k]], writes=[B0["gb%d" % k]])
                for (v0, pt_, pB, dst, dB) in vecs:
                    for c in range(8):
                        S.mm(lambda e, c=c, v0=v0, pt_=pt_, k=k: e.matmul(pt_[:], rep[:, v0 + c, :], wa[k][:, c, :],
                                                                          start=(c == 0), stop=False),
                             reads=[B0["rep"], B0["wa%d" % k]], writes=[pB], signal=False, first=(c == 0))
                    bo = 4096 + j * 512
                    S.mm(lambda e, pt_=pt_, bo=bo: e.matmul(pt_[:], ones_f[0:1, :], rows_t[0:1, bo:bo + 512],
                                                            start=False, stop=True),
                         reads=[Bc["ones_f"], B0["rows"]], writes=[pB], signal=True, first=False)
                    if m in (0, 3):
                        S.op(act, lambda e, dst=dst, pt_=pt_, cs=cs: e.copy(dst[:, cs], pt_[:]), reads=[pB], writes=[dB])
                    elif m in (1, 4):
                        S.op(dve, lambda e, dst=dst, pt_=pt_, cs=cs, k=k: e.scalar_tensor_tensor(
                            out=dst[:, cs], in0=pt_[:], scalar=1.0, in1=gb[k][:], op0=ALU.add, op1=ALU.mult),
                            reads=[pB, B0["gb%d" % k]], writes=[dB])
                    else:
                        S.op(dve, lambda e, dst=dst, pt_=pt_, cs=cs, k=k: e.tensor_tensor(
                            out=dst[:, cs], in0=pt_[:], in1=gb[k][:], op=ALU.mult),
                            reads=[pB, B0["gb%d" % k]], writes=[dB])
            if dbg and stage == 0:
                for nm, t in [("d_gs1", gs1_b), ("d_sh1", sh1_b), ("d_gsc", gsc_b), ("d_shc", shc_b), ("d_gm", gm_b),
                              ("d_gs2", gs2_b), ("d_sh2", sh2_b), ("d_gf", gf_b)]:
                    ddump(S, nm, t[:], [128, 1024], [Bc[k] for k in Bc], cast=False)
            S.barrier()
        A.release(m_bc1)
        if stage == 0:
            return nc, dbg_outs

        qaT = A.alloc("qaT", [64, 8, 2048], BF16)
        kaT = A.alloc("kaT", [64, 2, 4352], BF16)
        va = A.alloc("va", [128, 34, 130], BF16)
        Bq = {k: Buf(k) for k in ["qaT", "kaT", "va", "cq_s", "ckv_s", "kr_s"]}
        m_attnA = A.mark()
        with ExitStack() as pes:
            W = A.alloc("W", [128, 8, C_END], BF16)
            xt = [A.alloc("xt%d" % i, [128, 1024], F32) for i in range(2)]
            junk = A.alloc("junk", [128, 1024], BF16)
            tt_ = A.alloc("tt_", [128, 1024], F32)
            hb = [A.alloc("hb%d" % i, [128, 1024], BF16) for i in range(2)]
            hTb = [A.alloc("hTb%d" % i, [128, 8, 512], BF16) for i in range(2)]
            tcb = [A.alloc("tcb%d" % i, [96, 512], F32) for i in range(2)]
            tsb = [A.alloc("tsb%d" % i, [96, 512], F32) for i in range(2)]
            r1 = [A.alloc("r1_%d" % i, [96, 512], F32) for i in range(2)]
            r2 = [A.alloc("r2_%d" % i, [96, 512], F32) for i in range(2)]
            ss = A.alloc("ss", [128, 34], F32)
            lnv = A.alloc("lnv", [128, 34], F32)
            rs = A.alloc("rs", [128, 34], F32)
            sq = A.alloc("sq", [128, 3, 512], BF16)
            rl0 = A.alloc("rl0", [128, 512], F32)
            rl1 = A.alloc("rl1", [128, 512], F32)
            cqs = [A.alloc("cqs%d" % i, [128, 3, 512], BF16) for i in range(2)]
            ckvs = [A.alloc("ckvs%d" % i, [128, 2, 512], BF16) for i in range(2)]
            krs = [A.alloc("krs%d" % i, [96, 512], BF16) for i in range(2)]
            pT = [pes.enter_context(nc.psum_tensor("pT%d" % i, [128, 1024], BF16)) for i in range(2)]
            pp = [pes.enter_context(nc.psum_tensor("pp%d" % i, [128, 512], F32)) for i in range(5)]
            pr = pes.enter_context(nc.psum_tensor("pr", [128, 512], F32))
            B1 = {k: Buf(k) for k in ["W", "xt0", "xt1", "tt_", "hb0", "hb1", "hTb0", "hTb1", "tcb0", "tcb1", "tsb0",
                                      "tsb1", "r1_0", "r1_1", "r2_0", "r2_1", "sq", "rl0", "rl1", "cqs0", "cqs1",
                                      "ckvs0", "ckvs1", "krs0", "krs1", "pT0", "pT1", "pp0", "pp1", "pp2", "pp3", "pp4",
                                      "pr"]}
            Bss = [Buf("ss%d" % i) for i in range(34)]
            Bln = [Buf("ln%d" % i) for i in range(34)]
            Brs = [Buf("rs%d" % i) for i in range(34)]
            S.dma(pool, W[:], w_inx.rearrange("(c p) n -> p c n", p=128), writes=[B1["W"]])
            S.op(pool, lambda e: e.memset(va[:, :, 64:65], 1.0), writes=[Bq["va"]])
            S.op(pool, lambda e: e.memset(va[:, :, 129:130], 1.0), writes=[Bq["va"]])
            ppi = [0]
            ropei = [0]

            def next_pp():
                i = ppi[0] % 5
                ppi[0] += 1
                return pp[i], B1["pp%d" % i]

            def projT(dst, dB, col0, M, hT, hB, ntok):
                for c in range(8):
                    S.mm(lambda e, c=c: e.matmul(dst[0:M, 0:ntok], W[:, c, col0:col0 + M], hT[:, c, 0:ntok],
                                                 start=(c == 0), stop=(c == 7)),
                         reads=[B1["W"], hB], writes=[dB], signal=(c == 7), first=(c == 0))

            def rope_evac(pa, pBa, pb, pBb, r0, r1_, ntok, k, dst_ap, dstB):
                i = ropei[0] % 2
                ropei[0] += 1
                S.op(dve, lambda e: e.tensor_tensor(out=r1[i][r0:r1_, 0:ntok], in0=pa[r0:r1_, 0:ntok],
                                                    in1=tcb[k][r0:r1_, 0:ntok], op=ALU.mult),
                     reads=[pBa, B1["tcb%d" % k]], writes=[B1["r1_%d" % i]])
                S.op(dve, lambda e: e.tensor_tensor(out=r2[i][r0:r1_, 0:ntok], in0=pb[r0:r1_, 0:ntok],
                                                    in1=tsb[k][r0:r1_, 0:ntok], op=ALU.mult),
                     reads=[pBb, B1["tsb%d" % k]], writes=[B1["r2_%d" % i]])
                S.op(pool, lambda e: e.tensor_tensor(out=dst_ap, in0=r1[i][r0:r1_, 0:ntok], in1=r2[i][r0:r1_, 0:ntok],
                                                     op=ALU.add),
                     reads=[B1["r1_%d" % i], B1["r2_%d" % i]], writes=[dstB])

            def latent_norm(pcs, nfeat, ntok, dst, dstB):
                nj = len(pcs)
                for j, (pc, pB) in enumerate(pcs):
                    S.op(act, lambda e, j=j, pc=pc: e.activation(out=sq[:, j, 0:ntok], in_=pc[:, 0:ntok], func=AF.Square),
                         reads=[pB], writes=[B1["sq"]])
                for j in range(nj):
                    S.mm(lambda e, j=j: e.matmul(pr[:, 0:ntok], ones_b[:], sq[:, j, 0:ntok], start=(j == 0),
                                                 stop=(j == nj - 1)),
                         reads=[Bc["ones_b"], B1["sq"]], writes=[B1["pr"]], signal=(j == nj - 1), first=(j == 0))
                S.op(act, lambda e: e.activation(out=rl0[:, 0:ntok], in_=pr[:, 0:ntok], func=AF.Ln, bias=EPS,
                                                 scale=1.0 / nfeat), reads=[B1["pr"]], writes=[B1["rl0"]])
                S.op(act, lambda e: e.activation(out=rl1[:, 0:ntok], in_=rl0[:, 0:ntok], func=AF.Exp, scale=-0.5),
                     reads=[B1["rl0"]], writes=[B1["rl1"]])
                for j, (pc, pB) in enumerate(pcs):
                    S.op(dve, lambda e, j=j, pc=pc: e.tensor_tensor(out=dst[:, j, 0:ntok], in0=pc[:, 0:ntok],
                                                                     in1=rl1[:, 0:ntok], op=ALU.mult),
                         reads=[pB, B1["rl1"]], writes=[dstB])

            for bi in range(9):
                ntile = 4 if bi < 8 else 2
                ntok = ntile * 128
                t0 = bi * 4
                k = bi % 2
                hT, hB = hTb[k], B1["hTb%d" % k]
                tok = slice(bi * 512, bi * 512 + ntok)
                if bi < 8:
                    S.dma(sp, tcb[k][:], tabc[:, tok], writes=[B1["tcb%d" % k]])
                    S.dma(sp, tsb[k][:], tabs[:, tok], writes=[B1["tsb%d" % k]])
                gsb, shb = (gs1_b, sh1_b) if bi < 8 else (gsc_b, shc_b)
                gsB, shB = (Bc["gs1_b"], Bc["sh1_b"]) if bi < 8 else (Bc["gsc_b"], Bc["shc_b"])
                for ti in range(ntile):
                    tt = t0 + ti
                    x_, xB = xt[tt % 2], B1["xt%d" % (tt % 2)]
                    h_, hbB = hb[tt % 2], B1["hb%d" % (tt % 2)]
                    pT_, pTB = pT[tt % 2], B1["pT%d" % (tt % 2)]
                    S.dma(sp, x_[:], xin[tt * 128:(tt + 1) * 128, :], writes=[xB])
                    S.op(act, lambda e, x_=x_, tt=tt: e.activation(out=junk[:], in_=x_[:], func=AF.Square,
                                                                   accum_out=ss[:, tt:tt + 1]),
                         reads=[xB], writes=[Bss[tt]])
                    S.op(act, lambda e, tt=tt: e.activation(out=lnv[:, tt:tt + 1], in_=ss[:, tt:tt + 1], func=AF.Ln,
                                                            bias=EPS, scale=1.0 / 1024), reads=[Bss[tt]], writes=[Bln[tt]])
                    S.op(act, lambda e, tt=tt: e.activation(out=rs[:, tt:tt + 1], in_=lnv[:, tt:tt + 1], func=AF.Exp,
                                                            scale=-0.5), reads=[Bln[tt]], writes=[Brs[tt]])
                    S.op(dve, lambda e, x_=x_, tt=tt: e.scalar_tensor_tensor(out=tt_[:], in0=x_[:], scalar=rs[:, tt:tt + 1],
                                                                             in1=gsb[:], op0=ALU.mult, op1=ALU.mult),
                         reads=[xB, Brs[tt], gsB], writes=[B1["tt_"]])
                    S.op(pool, lambda e, h_=h_: e.tensor_tensor(out=h_[:], in0=tt_[:], in1=shb[:], op=ALU.add),
                         reads=[B1["tt_"], shB], writes=[hbB])
                    for c in range(8):
                        S.mm(lambda e, c=c, h_=h_, pT_=pT_: e.transpose(pT_[:, c * 128:(c + 1) * 128],
                                                                       h_[:, c * 128:(c + 1) * 128], ident_b[:]),
                             reads=[hbB, Bc["ident_b"]], writes=[pTB], signal=(c == 7), first=(c == 0))
                    S.op(act, lambda e, pT_=pT_, ti=ti: e.copy(hT[:, :, ti * 128:(ti + 1) * 128],
                                                               pT_[:].rearrange("p (c t) -> p c t", c=8)),
                         reads=[pTB], writes=[hB])
                if bi < 4:
                    for h in range(8):
                        pa, pBa = next_pp()
                        pb, pBb = next_pp()
                        projT(pa, pBa, C_QA + h * 64, 64, hT, hB, ntok)
                        projT(pb, pBb, C_QAP + h * 64, 64, hT, hB, ntok)
                        rope_evac(pa, pBa, pb, pBb, 0, 64, ntok, k, qaT[:, h, tok], Bq["qaT"])
                for kh in range(2):
                    pa, pBa = next_pp()
                    projT(pa, pBa, C_KA + kh * 64, 64, hT, hB, ntok)
                    if bi < 8:
                        pb, pBb = next_pp()
                        projT(pb, pBb, C_KAP + kh * 64, 64, hT, hB, ntok)
                        rope_evac(pa, pBa, pb, pBb, 0, 64, ntok, k, kaT[:, kh, tok], Bq["kaT"])
                    else:
                        S.op(dve, lambda e, pa=pa, kh=kh: e.tensor_copy(kaT[:, kh, tok], pa[0:64, 0:ntok]),
                             reads=[pBa], writes=[Bq["kaT"]])
                pv, pBv = next_pp()
                for ti in range(ntile):
                    for c in range(8):
                        S.mm(lambda e, c=c, ti=ti: e.matmul(pv[:, ti * 128:(ti + 1) * 128],
                                                           hT[:, c, ti * 128:(ti + 1) * 128], W[:, c, C_VA:C_VA + 128],
                                                           start=(c == 0), stop=(c == 7)),
                             reads=[B1["W"], hB], writes=[pBv], signal=(c == 7 and ti == ntile - 1),
                             first=(c == 0 and ti == 0))
                for kh in range(2):
                    S.op(dve, lambda e, kh=kh: e.tensor_copy(
                        va[:, t0:t0 + ntile, kh * 65:kh * 65 + 64],
                        pv[:, 0:ntile * 128].rearrange("p (t x) -> p t x", t=ntile)[:, :, kh * 64:(kh + 1) * 64]),
                        reads=[pBv], writes=[Bq["va"]])
                if bi < 4:
                    pcs = []
                    for j in range(3):
                        pc, pB = next_pp()
                        projT(pc, pB, C_CQ + j * 128, 128, hT, hB, ntok)
                        pcs.append((pc, pB))
                    latent_norm(pcs, 384, ntok, cqs[k], B1["cqs%d" % k])
                    S.dma(sp, cq_s[:, :, tok], cqs[k][:], reads=[B1["cqs%d" % k]], writes=[Bq["cq_s"]])
                pcs = []
                for j in range(2):
                    pc, pB = next_pp()
                    projT(pc, pB, C_CKV + j * 128, 128, hT, hB, ntok)
                    pcs.append((pc, pB))
                latent_norm(pcs, 256, ntok, ckvs[k], B1["ckvs%d" % k])
                S.dma(sp, ckv_s[:, :, tok], ckvs[k][:, :, 0:ntok], reads=[B1["ckvs%d" % k]], writes=[Bq["ckv_s"]])
                pa, pBa = next_pp()
                projT(pa, pBa, C_KR2, 96, hT, hB, ntok)
                if bi < 8:
                    pb, pBb = next_pp()
                    projT(pb, pBb, C_KR2 + 96, 96, hT, hB, ntok)
                    rope_evac(pa, pBa, pb, pBb, 64, 96, ntok, k, krs[k][64:96, 0:ntok], B1["krs%d" % k])
                else:
                    S.op(dve, lambda e, pa=pa: e.tensor_copy(krs[k][64:96, 0:ntok], pa[64:96, 0:ntok]),
                         reads=[pBa], writes=[B1["krs%d" % k]])
                S.dma(sp, kr_s[64:96, tok], krs[k][64:96, 0:ntok], reads=[B1["krs%d" % k]], writes=[Bq["kr_s"]])
            if dbg and stage == 1:
                allB = [Bq[k] for k in Bq]
                ddump(S, "d_qaT", qaT[:], [64, 8, 2048], allB)
                ddump(S, "d_kaT", kaT[:], [64, 2, 4352], allB)
                ddump(S, "d_va", va[:], [128, 34, 130], allB)
                ddump(S, "d_cq", cq_s, [128, 3, 2048], allB)
                ddump(S, "d_ckv", ckv_s, [128, 2, 4352], allB)
                ddump(S, "d_kr", kr_s[64:96, :], [32, 4352], allB)
            S.barrier()
        A.release(m_attnA)
        if stage == 1:
            return nc, dbg_outs

        def run_attention(iters, st, stB, ptb, ptB, ot, otB, bc, bcB, rden, rdB, bcs, bcsB, fillers=None, fill_every=1):
            flat = []
            for ii, it in enumerate(iters):
                ng = len(it["groups"])
                for gi, g in enumerate(it["groups"]):
                    flat.append((ii, gi, gi == ng - 1, g))
            n = len(flat)
            nst, npt, nbc = len(st), len(ptb), len(bc)

            def QK(i):
                ii, gi, last, g = flat[i]
                it = iters[ii]
                s_, sB = st[i % nst], stB[i % nst]
                nmm = sum(1 + (1 if m is not None else 0) for (_, _, m, _, _) in g)
                cnt = 0
                for slot, (kap, kB, mask, vap, vB) in enumerate(g):
                    cnt += 1
                    S.mm(lambda e, slot=slot, kap=kap, it=it, mask=mask: e.matmul(
                        s_[:, slot * 512:(slot + 1) * 512], kap, it["q"], start=True, stop=(mask is None)),
                        reads=[kB, it["qB"]], writes=[sB], signal=(cnt == nmm), first=(cnt == 1))
                    if mask is not None:
                        cnt += 1
                        S.mm(lambda e, slot=slot, mask=mask: e.matmul(s_[:, slot * 512:(slot + 1) * 512], ident_b[:],
                                                                      mask[0], start=False, stop=True),
                             reads=[Bc["ident_b"], mask[1]], writes=[sB], signal=(cnt == nmm), first=False)

            def EXP(i):
                ii, gi, last, g = flat[i]
                it = iters[ii]
                wdt = 512 * len(g)
                s_, sB = st[i % nst], stB[i % nst]
                p_, pB = ptb[i % npt], ptB[i % npt]
                S.op(act, lambda e: e.activation(out=p_[:, 0:wdt], in_=s_[:, 0:wdt], func=AF.Exp, scale=it["scale"]),
                     reads=[sB], writes=[pB])

            def PV(i):
                ii, gi, last, g = flat[i]
                it = iters[ii]
                o_, oB = ot[ii % len(ot)], otB[ii % len(ot)]
                p_, pB = ptb[i % npt], ptB[i % npt]
                for slot, (kap, kB, mask, vap, vB) in enumerate(g):
                    lastmm = last and slot == len(g) - 1 and it.get("sink") is None
                    S.mm(lambda e, slot=slot, vap=vap: e.matmul(o_[0:65, :], vap, p_[:, slot * 512:(slot + 1) * 512],
                                                                start=(gi == 0 and slot == 0), stop=lastmm),
                         reads=[vB, pB], writes=[oB], signal=(slot == len(g) - 1), first=(gi == 0 and slot == 0))
                if last:
                    if it.get("sink") is not None:
                        sl_, sr_, sB_ = it["sink"]
                        S.mm(lambda e: e.matmul(o_[0:65, :], sl_, sr_, start=False, stop=True),
                             reads=[Bc["e64"], sB_], writes=[oB], signal=True, first=False)
                    j = ii % nbc
                    S.op(dve, lambda e: e.reciprocal(rden[j][64:65, :], o_[64:65, :]), reads=[oB], writes=[rdB[j]])
                    S.mm(lambda e: e.matmul(bc[j][0:64, :], ones_f[64:65, 0:64], rden[j][64:65, :], start=True, stop=True),
                         reads=[Bc["ones_f"], rdB[j]], writes=[bcB[j]])
                    S.op(dve, lambda e: e.tensor_copy(bcs[j][:], bc[j][0:64, :]), reads=[bcB[j]], writes=[bcsB[j]])
                    S.op(dve, lambda e: e.tensor_tensor(out=it["out"], in0=it["ovw"](o_[0:64, :]), in1=it["ovw"](bcs[j][:]),
                                                        op=ALU.mult),
                         reads=[oB, bcsB[j]], writes=[it["outB"]])

            for i in range(n + 2):
                if i < n:
                    QK(i)
                    EXP(i)
                if 0 <= i - 2 < n:
                    PV(i - 2)
                if fillers and (i % fill_every == 0):
                    if fillers:
                        fillers.popleft()()
            while fillers:
                fillers.popleft()()

        TOP = 229312
        out_aT = A.alloc_at("out_aT", [64, 8, 2048], BF16, TOP - 32768)
        A.limit = TOP - 32768
        B_oa = [Buf("oa%d" % i) for i in range(16)]
        m_attnB = A.mark()
        with ExitStack() as pes:
            maskb = A.alloc("maskb", [128, 4, 512], BF16)
            sinkf = A.alloc("sinkf", [1, 1024], F32)
            sinkb = A.alloc("sinkb", [1, 1024], BF16)
            ptb = [A.alloc("ptb%d" % i, [128, 1024], BF16) for i in range(3)]
            rden = [A.alloc("rden%d" % i, [65, 512], F32) for i in range(2)]
            bcs = [A.alloc("bcs%d" % i, [64, 512], F32) for i in range(2)]
            st = [pes.enter_context(nc.psum_tensor("st%d" % i, [128, 1024], F32)) for i in range(2)]
            ot = [pes.enter_context(nc.psum_tensor("ot%d" % i, [128, 512], F32)) for i in range(2)]
            bc = [pes.enter_context(nc.psum_tensor("bc%d" % i, [64, 512], F32)) for i in range(2)]
            B3 = {k: Buf(k) for k in ["maskb", "sinkf", "sinkb"]}
            stB = [Buf("st%d" % i) for i in range(2)]
            ptB = [Buf("pt%d" % i) for i in range(3)]
            otB = [Buf("ot%d" % i) for i in range(2)]
            bcB = [Buf("bc%d" % i) for i in range(2)]
            rdB = [Buf("rd%d" % i) for i in range(2)]
            bcsB = [Buf("bcs%d" % i) for i in range(2)]
            S.dma(pool, maskb[:], masks, writes=[B3["maskb"]])
            S.dma(sp, sinkf[:], sinkr, writes=[B3["sinkf"]])
            S.op(act, lambda e: e.activation(out=sinkb[:], in_=sinkf[:], func=AF.Exp), reads=[B3["sinkf"]],
                 writes=[B3["sinkb"]])
            iters = []
            for n_ in range(16):
                for kh in range(2):
                    Lt = n_ - 1 if n_ >= 1 else 31
                    Rt = n_ + 1 if n_ <= 14 else 16
                    mL = 0 if n_ >= 1 else 2
                    mR = 1 if n_ <= 14 else 3

                    def kt(j, m=None):
                        return (kaT[:, kh, j * 128:(j + 1) * 128], Bq["kaT"],
                                None if m is None else (maskb[:, m, :], B3["maskb"]),
                                va[:, j, kh * 65:(kh + 1) * 65], Bq["va"])
                    groups = [[kt(Lt, mL), kt(n_)], [kt(Rt, mR), kt(32)], [kt(33)]]
                    qs = slice(n_ * 128, (n_ + 1) * 128)
                    iters.append(dict(
                        q=qaT[:, 4 * kh:4 * kh + 4, qs], qB=Bq["qaT"], groups=groups, scale=A_SCALE,
                        sink=(e64[0:1, 0:65], sinkb[0:1, kh * 512:(kh + 1) * 512], B3["sinkb"]),
                        out=out_aT[:, 4 * kh:4 * kh + 4, qs], outB=B_oa[n_],
                        ovw=lambda ap: ap.rearrange("p (h q) -> p h q", h=4)))
            run_attention(iters, st, stB, ptb, ptB, ot, otB, bc, bcB, rden, rdB, bcs, bcsB)
            if dbg and stage == 2:
                ddump(S, "d_oaT", out_aT[:], [64, 8, 2048], B_oa)
            S.barrier()
        A.release(m_persist)
        if stage == 2:
            return nc, dbg_outs

        out_bT = A.alloc_at("out_bT", [64, 8, 2048], BF16, TOP - 65536)
        A.limit = TOP - 65536
        B_ob = [Buf("ob%d" % i) for i in range(4)]
        m_mla = A.mark()
        with ExitStack() as pes:
            cqT = A.alloc("cqT", [128, 3, 2048], BF16)
            ckvT = A.alloc("ckvT", [128, 2, 4352], BF16)
            krT = A.alloc("krT", [96, 4352], BF16)
            wuq = A.alloc("wuq", [128, 3, 768], BF16)
            wuqp = A.alloc("wuqp", [128, 3, 768], BF16)
            wukv = A.alloc("wukv", [128, 2, 1024], BF16)
            gq_t = A.alloc("gq_t", [128, 3], F32)
            gkv_t = A.alloc("gkv_t", [128, 2], F32)
            m_wst = A.mark()
            wst = A.alloc("wst", [128, 3, 768], F32)
            B4 = {k: Buf(k) for k in ["cqT", "ckvT", "krT", "wst", "wuq", "wuqp", "wukv", "gq", "gkv", "tcq", "tsq", "QT0",
                                      "QT1", "KT0", "KT1", "Vh0", "Vh1", "q1", "q2", "ppr"]}
            S.dma(sp, cqT[:], cq_s, reads=[Bq["cq_s"]], writes=[B4["cqT"]])
            S.dma(sp, ckvT[:], ckv_s, reads=[Bq["ckv_s"]], writes=[B4["ckvT"]])
            S.dma(sp, krT[64:96, :], kr_s[64:96, :], reads=[Bq["kr_s"]], writes=[B4["krT"]])
            S.dma(sp, gq_t[:], gq, writes=[B4["gq"]])
            S.dma(sp, gkv_t[:], gkv, writes=[B4["gkv"]])
            for (src, dstw, dB) in [(w_uq, wuq, "wuq"), (w_uqp, wuqp, "wuqp")]:
                S.dma(sp, wst[:, 0:3, :], src.rearrange("(c p) n -> p c n", p=128), writes=[B4["wst"]])
                for c in range(3):
                    S.op(dve, lambda e, c=c, dstw=dstw: e.tensor_scalar(dstw[:, c, :], wst[:, c, :], gq_t[:, c:c + 1], None,
                                                                        op0=ALU.mult),
                         reads=[B4["wst"], B4["gq"]], writes=[B4[dB]])
            for c in range(2):
                for (c0, c1) in [(0, 768), (768, 1024)]:
                    S.dma(sp, wst[:, 0, 0:c1 - c0], w_ukv[c * 128:(c + 1) * 128, c0:c1], writes=[B4["wst"]])
                    S.op(dve, lambda e, c=c, c0=c0, c1=c1: e.tensor_scalar(wukv[:, c, c0:c1], wst[:, 0, 0:c1 - c0],
                                                                           gkv_t[:, c:c + 1], None, op0=ALU.mult),
                         reads=[B4["wst"], B4["gkv"]], writes=[B4["wukv"]])
            S.barrier()
            A.release(m_wst)
            tcq = A.alloc("tcq", [96, 512], F32)
            tsq = A.alloc("tsq", [96, 512], F32)
            QT = [A.alloc("QT%d" % i, [96, 2048], BF16) for i in range(2)]
            KT = [A.alloc("KT%d" % i, [96, 4352], BF16) for i in range(2)]
            Vh = [A.alloc("Vh%d" % i, [128, 34, 65], BF16) for i in range(2)]
            q1 = A.alloc("q1", [96, 512], F32)
            q2 = A.alloc("q2", [96, 512], F32)
            ptb = [A.alloc("ptb%d" % i, [128, 1024], BF16) for i in range(3)]
            rden = [A.alloc("rden%d" % i, [65, 512], F32) for i in range(1)]
            bcs = [A.alloc("bcs%d" % i, [64, 512], F32) for i in range(1)]
            st = [pes.enter_context(nc.psum_tensor("mst%d" % i, [128, 1024], F32)) for i in range(2)]
            ot = [pes.enter_context(nc.psum_tensor("mot%d" % i, [128, 512], F32)) for i in range(2)]
            bc = [pes.enter_context(nc.psum_tensor("mbc%d" % i, [64, 512], F32)) for i in range(1)]
            ppr = pes.enter_context(nc.psum_tensor("ppr", [128, 512], F32))
            stB = [Buf("st%d" % i) for i in range(2)]
            ptB = [Buf("pt%d" % i) for i in range(3)]
            otB = [Buf("ot%d" % i) for i in range(2)]
            bcB = [Buf("bc%d" % i) for i in range(1)]
            rdB = [Buf("rd%d" % i) for i in range(1)]
            bcsB = [Buf("bcs%d" % i) for i in range(1)]
            for i in range(2):
                S.op(pool, lambda e, i=i: e.memset(Vh[i][:, :, 64:65], 1.0), writes=[B4["Vh%d" % i]])

            def prep_closures(h, hp):
                cl = []
                KTh, KB = KT[hp], B4["KT%d" % hp]
                Vhh, VB = Vh[hp], B4["Vh%d" % hp]
                QTh, QB = QT[hp], B4["QT%d" % hp]

                def kcopy():
                    S.op(pool, lambda e: e.tensor_copy(KTh[64:96, :], krT[64:96, :]), reads=[B4["krT"]], writes=[KB])
                cl.append(kcopy)
                for bi in range(9):
                    ntok = 512 if bi < 8 else 256
                    tok = slice(bi * 512, bi * 512 + ntok)

                    def kblk(tok=tok, ntok=ntok):
                        for c in range(2):
                            S.mm(lambda e, c=c: e.matmul(ppr[0:64, 0:ntok], wukv[:, c, h * 128:h * 128 + 64],
                                                         ckvT[:, c, tok], start=(c == 0), stop=(c == 1)),
                                 reads=[B4["wukv"], B4["ckvT"]], writes=[B4["ppr"]], signal=(c == 1), first=(c == 0))
                        S.op(dve, lambda e: e.tensor_copy(KTh[0:64, tok], ppr[0:64, 0:ntok]), reads=[B4["ppr"]],
                             writes=[KB])
                    cl.append(kblk)
                for g0 in range(0, 34, 8):
                    nt = min(8, 34 - g0)

                    def vblk(g0=g0, nt=nt):
                        for t in range(nt):
                            tsl = slice((g0 + t) * 128, (g0 + t + 1) * 128)
                            for c in range(2):
                                S.mm(lambda e, c=c, t=t, tsl=tsl: e.matmul(
                                    ppr[:, t * 64:(t + 1) * 64], ckvT[:, c, tsl], wukv[:, c, h * 128 + 64:h * 128 + 128],
                                    start=(c == 0), stop=(c == 1)),
                                    reads=[B4["wukv"], B4["ckvT"]], writes=[B4["ppr"]],
                                    signal=(c == 1 and t == nt - 1), first=(c == 0 and t == 0))
                        S.op(dve, lambda e: e.tensor_copy(Vhh[:, g0:g0 + nt, 0:64],
                                                          ppr[:, 0:nt * 64].rearrange("p (t d) -> p t d", t=nt)),
                             reads=[B4["ppr"]], writes=[VB])
                    cl.append(vblk)
                for qb in range(4):
                    tok = slice(qb * 512, (qb + 1) * 512)

                    def qa_(tok=tok):
                        S.dma(sp, tcq[64:96, :], tabc[64:96, tok], writes=[B4["tcq"]])
                        S.dma(sp, tsq[64:96, :], tabs[64:96, tok], writes=[B4["tsq"]])
                        for c in range(3):
                            S.mm(lambda e, c=c: e.matmul(ppr[0:96, :], wuq[:, c, h * 96:(h + 1) * 96], cqT[:, c, tok],
                                                         start=(c == 0), stop=(c == 2)),
                                 reads=[B4["wuq"], B4["cqT"]], writes=[B4["ppr"]], signal=(c == 2), first=(c == 0))
                        S.op(dve, lambda e: e.tensor_copy(QTh[0:64, tok], ppr[0:64, :]), reads=[B4["ppr"]], writes=[QB])
                        S.op(dve, lambda e: e.tensor_tensor(out=q1[64:96, :], in0=ppr[64:96, :], in1=tcq[64:96, :],
                                                            op=ALU.mult), reads=[B4["ppr"], B4["tcq"]], writes=[B4["q1"]])

                    def qb_(tok=tok):
                        for c in range(3):
                            S.mm(lambda e, c=c: e.matmul(ppr[0:96, :], wuqp[:, c, h * 96:(h + 1) * 96], cqT[:, c, tok],
                                                         start=(c == 0), stop=(c == 2)),
                                 reads=[B4["wuqp"], B4["cqT"]], writes=[B4["ppr"]], signal=(c == 2), first=(c == 0))
                        S.op(dve, lambda e: e.tensor_tensor(out=q2[64:96, :], in0=ppr[64:96, :], in1=tsq[64:96, :],
                                                            op=ALU.mult), reads=[B4["ppr"], B4["tsq"]], writes=[B4["q2"]])
                        S.op(pool, lambda e: e.tensor_tensor(out=QTh[64:96, tok], in0=q1[64:96, :], in1=q2[64:96, :],
                                                             op=ALU.add), reads=[B4["q1"], B4["q2"]], writes=[QB])
                    cl.append(qa_)
                    cl.append(qb_)
                return cl

            for f in prep_closures(0, 0):
                f()
            for h in range(8):
                hp = h % 2
                iters = []
                for qb in range(4):
                    tok = slice(qb * 512, (qb + 1) * 512)
                    groups = []
                    for g in range(17):
                        groups.append([(KT[hp][:, j * 128:(j + 1) * 128], B4["KT%d" % hp], None,
                                        Vh[hp][:, j, 0:65], B4["Vh%d" % hp]) for j in (2 * g, 2 * g + 1)])
                    iters.append(dict(q=QT[hp][:, tok], qB=B4["QT%d" % hp], groups=groups, scale=MLA_SCALE, sink=None,
                                      out=out_bT[:, h, tok], outB=B_ob[qb], ovw=lambda ap: ap))
                fl = deque(prep_closures(h + 1, 1 - hp)) if h < 7 else None
                run_attention(iters, st, stB, ptb, ptB, ot, otB, bc, bcB, rden, rdB, bcs, bcsB, fillers=fl, fill_every=2)
            if dbg and stage == 3:
                ddump(S, "d_obT", out_bT[:], [64, 8, 2048], B_ob)
            S.barrier()
        A.release(m_mla)
        if stage == 3:
            return nc, dbg_outs

        h2T = A.alloc("h2T", [128, 8, 2048], BF16)
        Wr = A.alloc("Wr", [128, 16, 32], F32)
        B_h2 = [Buf("h2T%d" % i) for i in range(4)]
        B_wr = [Buf("Wr%d" % i) for i in range(16)]
        B_x1 = [Buf("x1s%d" % i) for i in range(16)]
        m_p5 = A.mark()
        with ExitStack() as pes:
            wo = A.alloc("wo", [64, 16, 1024], BF16)
            wrb = A.alloc("wrb", [128, 8, 32], BF16)
            xt = [A.alloc("xt%d" % i, [128, 1024], F32) for i in range(2)]
            x1t = [A.alloc("x1t%d" % i, [128, 1024], F32) for i in range(2)]
            tt_ = A.alloc("tt5", [128, 1024], F32)
            junk = A.alloc("junk5", [128, 1024], BF16)
            hb = [A.alloc("h2b%d" % i, [128, 1024], BF16) for i in range(2)]
            sm = A.alloc("sm5", [128, 16, 8], F32)
            lg = A.alloc("lg", [128, 512], F32)
            rk = A.alloc("rk", [128, 512], F32)
            mk = A.alloc("mk", [128, 512], F32)
            ex = A.alloc("ex", [128, 512], F32)
            m1 = A.alloc("m1", [128, 16], F32)
            thr = A.alloc("thr", [128, 16], F32)
            br16 = A.alloc("br16", [1, 512], BF16)
            py = [pes.enter_context(nc.psum_tensor("py%d" % i, [128, 1024], F32)) for i in range(2)]
            pT = [pes.enter_context(nc.psum_tensor("pT5_%d" % i, [128, 1024], BF16)) for i in range(2)]
            plg = pes.enter_context(nc.psum_tensor("plg", [128, 512], F32))
            B5 = {k: Buf(k) for k in ["wo", "wrb", "brb", "xt0", "xt1", "x1t0", "x1t1", "tt", "hb0", "hb1", "lg", "rk", "m1", "thr",
                                      "mk", "ex", "py0", "py1", "pT0", "pT1", "plg"]}
            Bsm = [[Buf("sm%d_%d" % (i, j)) for j in range(8)] for i in range(16)]
            S.dma(pool, wo[:], w_o.rearrange("(h p) n -> p h n", p=64), writes=[B5["wo"]])
            S.dma(pool, wrb[:], w_r.rearrange("(c p) n -> p c n", p=128), writes=[B5["wrb"]])
            S.dma(pool, br16[:], b_r16, writes=[B5["brb"]])
            S.mm(lambda e: e.matmul(plg[:], ones_b[0:1, :], br16[0:1, :], start=True, stop=False),
                 reads=[Bc["ones_b"], B5["brb"]], writes=[B5["plg"]], signal=True, first=True)
            for tt in range(16):
                k = tt % 2
                tsl = slice(tt * 128, (tt + 1) * 128)
                y_, yB = py[k], B5["py%d" % k]
                x_, xB = xt[k], B5["xt%d" % k]
                x1_, x1B = x1t[k], B5["x1t%d" % k]
                h_, hbB = hb[k], B5["hb%d" % k]
                pT_, pTB = pT[k], B5["pT%d" % k]
                smt = sm[:, tt, :]
                S.dma(sp, x_[:], xin[tsl, :], writes=[xB])
                for cb in range(2):
                    for h in range(16):
                        src = out_aT if h < 8 else out_bT
                        sB = B_oa[tt] if h < 8 else B_ob[tt // 4]
                        S.mm(lambda e, cb=cb, h=h, src=src: e.matmul(y_[:, cb * 512:(cb + 1) * 512], src[:, h % 8, tsl],
                                                                     wo[:, h, cb * 512:(cb + 1) * 512],
                                                                     start=(h == 0), stop=(h == 15)),
                             reads=[sB, B5["wo"]], writes=[yB], signal=(h == 15 and cb == 1), first=(h == 0 and cb == 0))
                S.op(act, lambda e, y_=y_, tt=tt: e.activation(out=junk[:], in_=y_[:], func=AF.Square,
                                                               accum_out=sm[:, tt, 0:1]), reads=[yB], writes=[Bsm[tt][0]])
                S.op(act, lambda e, tt=tt: e.activation(out=sm[:, tt, 1:2], in_=sm[:, tt, 0:1], func=AF.Ln, bias=EPS,
                                                        scale=1.0 / 1024), reads=[Bsm[tt][0]], writes=[Bsm[tt][1]])
                S.op(act, lambda e, tt=tt: e.activation(out=sm[:, tt, 2:3], in_=sm[:, tt, 1:2], func=AF.Exp, scale=-0.5),
                     reads=[Bsm[tt][1]], writes=[Bsm[tt][2]])
                S.op(dve, lambda e, y_=y_, tt=tt: e.scalar_tensor_tensor(out=tt_[:], in0=y_[:], scalar=sm[:, tt, 2:3],
                                                                         in1=gm_b[:], op0=ALU.mult, op1=ALU.mult),
                     reads=[yB, Bsm[tt][2], Bc["gm_b"]], writes=[B5["tt"]])
                S.op(pool, lambda e, x_=x_, x1_=x1_: e.tensor_tensor(out=x1_[:], in0=tt_[:], in1=x_[:], op=ALU.add),
                     reads=[B5["tt"], xB], writes=[x1B])
                S.dma(sp, x1_s[tsl, :], x1_[:], reads=[x1B], writes=[B_x1[tt]])
                S.op(act, lambda e, x1_=x1_, tt=tt: e.activation(out=junk[:], in_=x1_[:], func=AF.Square,
                                                                 accum_out=sm[:, tt, 3:4]), reads=[x1B], writes=[Bsm[tt][3]])
                S.op(act, lambda e, tt=tt: e.activation(out=sm[:, tt, 4:5], in_=sm[:, tt, 3:4], func=AF.Ln, bias=EPS,
                                                        scale=1.0 / 1024), reads=[Bsm[tt][3]], writes=[Bsm[tt][4]])
                S.op(act, lambda e, tt=tt: e.activation(out=sm[:, tt, 5:6], in_=sm[:, tt, 4:5], func=AF.Exp, scale=-0.5),
                     reads=[Bsm[tt][4]], writes=[Bsm[tt][5]])
                S.op(dve, lambda e, x1_=x1_, tt=tt: e.scalar_tensor_tensor(out=tt_[:], in0=x1_[:], scalar=sm[:, tt, 5:6],
                                                                           in1=gs2_b[:], op0=ALU.mult, op1=ALU.mult),
                     reads=[x1B, Bsm[tt][5], Bc["gs2_b"]], writes=[B5["tt"]])
                S.op(pool, lambda e, h_=h_: e.tensor_tensor(out=h_[:], in0=tt_[:], in1=sh2_b[:], op=ALU.add),
                     reads=[B5["tt"], Bc["sh2_b"]], writes=[hbB])
                for c in range(8):
                    S.mm(lambda e, c=c, h_=h_, pT_=pT_: e.transpose(pT_[:, c * 128:(c + 1) * 128],
                                                                   h_[:, c * 128:(c + 1) * 128], ident_b[:]),
                         reads=[hbB, Bc["ident_b"]], writes=[pTB], signal=(c == 7), first=(c == 0))
                S.op(dve, lambda e, pT_=pT_: e.tensor_copy(h2T[:, :, tsl], pT_[:].rearrange("p (c t) -> p c t", c=8)),
                     reads=[pTB], writes=[B_h2[tt // 4]])
                for c in range(8):
                    S.mm(lambda e, c=c, tt=tt: e.matmul(plg[:, tt * 32:(tt + 1) * 32], h2T[:, c, tsl], wrb[:, c, :],
                                                        start=False, stop=(c == 7 and tt == 15)),
                         reads=[B_h2[tt // 4], B5["wrb"]], writes=[B5["plg"]], signal=(c == 7), first=False)
            v3 = lambda t: t[:].rearrange("p (t x) -> p t x", t=16)
            bc3 = lambda t: t[:].unsqueeze(2).to_broadcast([128, 16, 32])
            S.op(dve, lambda e: e.tensor_copy(lg[:], plg[:]), reads=[B5["plg"]], writes=[B5["lg"]])
            S.op(dve, lambda e: e.tensor_copy(rk[:], lg[:]), reads=[B5["lg"]], writes=[B5["rk"]])
            for kk in range(4):
                mdst = m1 if kk == 0 else thr
                mB = B5["m1"] if kk == 0 else B5["thr"]
                S.op(dve, lambda e, mdst=mdst: e.tensor_reduce(out=mdst[:], in_=v3(rk), axis=mybir.AxisListType.X,
                                                               op=ALU.max), reads=[B5["rk"]], writes=[mB])
                if kk < 3:
                    S.op(dve, lambda e, mdst=mdst: e.tensor_tensor(out=v3(mk), in0=v3(rk), in1=bc3(mdst), op=ALU.is_equal),
                         reads=[B5["rk"], mB], writes=[B5["mk"]])
                    S.op(dve, lambda e: e.scalar_tensor_tensor(out=rk[:], in0=mk[:], scalar=-1.0e9, in1=rk[:],
                                                               op0=ALU.mult, op1=ALU.add),
                         reads=[B5["mk"], B5["rk"]], writes=[B5["rk"]])
            S.op(dve, lambda e: e.tensor_tensor(out=v3(mk), in0=v3(lg), in1=bc3(thr), op=ALU.is_ge),
                 reads=[B5["lg"], B5["thr"]], writes=[B5["mk"]])
            S.op(dve, lambda e: e.tensor_tensor(out=v3(rk), in0=v3(lg), in1=bc3(m1), op=ALU.subtract),
                 reads=[B5["lg"], B5["m1"]], writes=[B5["rk"]])
            S.op(act, lambda e: e.activation(out=ex[:], in_=rk[:], func=AF.Exp), reads=[B5["rk"]], writes=[B5["ex"]])
            S.op(dve, lambda e: e.tensor_tensor(out=ex[:], in0=ex[:], in1=mk[:], op=ALU.mult),
                 reads=[B5["ex"], B5["mk"]], writes=[B5["ex"]])
            S.op(dve, lambda e: e.tensor_reduce(out=thr[:], in_=v3(ex), axis=mybir.AxisListType.X, op=ALU.add),
                 reads=[B5["ex"]], writes=[B5["thr"]])
            S.op(dve, lambda e: e.reciprocal(m1[:], thr[:]), reads=[B5["thr"]], writes=[B5["m1"]])
            S.op(dve, lambda e: e.tensor_tensor(out=Wr[:], in0=v3(ex), in1=bc3(m1), op=ALU.mult),
                 reads=[B5["ex"], B5["m1"]], writes=B_wr)
            if dbg and stage == 4:
                ddump(S, "d_h2T", h2T[:], [128, 8, 2048], B_h2)
                ddump(S, "d_Wr", Wr[:], [128, 16, 32], B_wr, cast=False)
                ddump(S, "d_x1", x1_s, [2048, 1024], B_x1, cast=False)
            S.barrier()
        A.release(m_p5)
        if stage == 4:
            return nc, dbg_outs

        A.limit = TOP
        facc = A.alloc("facc", [128, 16, 1024], F32)
        B_fa = [Buf("fa%d" % i) for i in range(16)]
        m_moe = A.mark()
        with ExitStack() as pes:
            bguT = A.alloc("bguT", [128, 16, 32], F32)
            B6 = {k: Buf(k) for k in ["GU", "DN", "bd0", "bd1", "actT0", "actT1", "T1_0", "T1_1", "T2_0", "T2_1", "T3_0",
                                      "T3_1", "bgu_t", "bguT", "pgl0", "pgl1", "pgl2", "pgl3", "po0", "po1"]}
            pgl = [pes.enter_context(nc.psum_tensor("pgl%d" % i, [128, 512], F32)) for i in range(4)]
            po = [pes.enter_context(nc.psum_tensor("po%d" % i, [128, 1024], F32)) for i in range(2)]
            m_bgu = A.mark()
            bgu_t = A.alloc("bgu_t", [32, 2048], F32)
            S.dma(sp, bgu_t[:], b_gu, writes=[B6["bgu_t"]])
            for c in range(16):
                S.mm(lambda e, c=c: e.transpose(pgl[0][:, c * 32:(c + 1) * 32], bgu_t[:, c * 128:(c + 1) * 128],
                                                ident_f[0:32, 0:32]),
                     reads=[B6["bgu_t"], Bc["ident_f"]], writes=[B6["pgl0"]], signal=(c == 15), first=(c == 0))
            S.op(dve, lambda e: e.tensor_copy(bguT[:], pgl[0][:].rearrange("p (c x) -> p c x", c=16)),
                 reads=[B6["pgl0"]], writes=[B6["bguT"]])
            S.barrier()
            A.release(m_bgu)
            GU = A.alloc("GU", [128, 8, 2048], BF16)
            DN = A.alloc("DN", [128, 8, 1024], BF16)
            bd = [A.alloc("bd%d" % i, [1, 1024], BF16) for i in range(2)]
            actT = [A.alloc("actT%d" % i, [128, 8, 512], BF16) for i in range(2)]
            T1 = [A.alloc("T1_%d" % i, [128, 512], F32) for i in range(2)]
            T2 = [A.alloc("T2_%d" % i, [128, 512], F32) for i in range(2)]
            T3 = [A.alloc("T3_%d" % i, [128, 512], F32) for i in range(2)]
            pair_i = [0]

            def load_gu(e_):
                S.dma(pool, GU[:], w_gu[e_].rearrange("(c p) n -> p c n", p=128), writes=[B6["GU"]])

            def load_dn(e_):
                S.dma(pool, DN[:], w_dn[e_].rearrange("(c p) n -> p c n", p=128), writes=[B6["DN"]])
                S.dma(pool, bd[e_ % 2][:], b_dn[e_:e_ + 1, :], writes=[B6["bd%d" % (e_ % 2)]])

            def gu_step(e_, tb):
                tok = slice(tb * 512, (tb + 1) * 512)
                aT, aB = actT[tb % 2], B6["actT%d" % (tb % 2)]
                for j in range(8):
                    i = pair_i[0] % 2
                    pair_i[0] += 1
                    pg_, pgB = pgl[2 * i], B6["pgl%d" % (2 * i)]
                    pl_, plB = pgl[2 * i + 1], B6["pgl%d" % (2 * i + 1)]
                    for (dst, dB, col0) in [(pg_, pgB, j * 128), (pl_, plB, 1024 + j * 128)]:
                        for c in range(8):
                            S.mm(lambda e, c=c, dst=dst, col0=col0: e.matmul(dst[:], GU[:, c, col0:col0 + 128],
                                                                            h2T[:, c, tok], start=(c == 0), stop=(c == 7)),
                                 reads=[B6["GU"], B_h2[tb]], writes=[dB], signal=(c == 7), first=(c == 0))
                    t1, t2, t3 = T1[i], T2[i], T3[i]
                    b1, b2, b3 = B6["T1_%d" % i], B6["T2_%d" % i], B6["T3_%d" % i]
                    S.op(act, lambda e, t1=t1, pg_=pg_, j=j: e.activation(out=t1[:], in_=pg_[:], func=AF.Identity,
                                                                          bias=bguT[:, j, e_:e_ + 1], scale=1.0),
                         reads=[pgB, B6["bguT"]], writes=[b1])
                    S.op(dve, lambda e, t2=t2, pl_=pl_, j=j: e.tensor_scalar(t2[:], pl_[:], bguT[:, 8 + j, e_:e_ + 1], 7.0,
                                                                             op0=ALU.add, op1=ALU.min),
                         reads=[plB, B6["bguT"]], writes=[b2])
                    S.op(pool, lambda e, t1=t1: e.tensor_scalar(t1[:], t1[:], 7.0, None, op0=ALU.min),
                         reads=[b1], writes=[b1])
                    S.op(act, lambda e, t1=t1, t3=t3: e.activation(out=t3[:], in_=t1[:], func=AF.Sigmoid, scale=1.702),
                         reads=[b1], writes=[b3])
                    S.op(dve, lambda e, t2=t2: e.tensor_scalar(t2[:], t2[:], -7.0, 1.0, op0=ALU.max, op1=ALU.add),
                         reads=[b2], writes=[b2])
                    S.op(pool, lambda e, t1=t1, t2=t2: e.tensor_tensor(out=t2[:], in0=t2[:], in1=t1[:], op=ALU.mult),
                         reads=[b1, b2], writes=[b2])
                    S.op(dve, lambda e, t2=t2, t3=t3, j=j: e.tensor_tensor(out=aT[:, j, :], in0=t2[:], in1=t3[:],
                                                                           op=ALU.mult),
                         reads=[b2, b3], writes=[aB])

            def dn_step(e_, tb):
                aT, aB = actT[tb % 2], B6["actT%d" % (tb % 2)]
                bdt, bdB = bd[e_ % 2], B6["bd%d" % (e_ % 2)]
                for ti in range(4):
                    tt = tb * 4 + ti
                    o_, oB = po[tt % 2], B6["po%d" % (tt % 2)]
                    for cb in range(2):
                        cs = slice(cb * 512, (cb + 1) * 512)
                        for j in range(8):
                            S.mm(lambda e, j=j, cs=cs, ti=ti: e.matmul(o_[:, cs], aT[:, j, ti * 128:(ti + 1) * 128],
                                                                      DN[:, j, cs], start=(j == 0), stop=False),
                                 reads=[aB, B6["DN"]], writes=[oB], signal=False, first=(j == 0 and cb == 0))
                        S.mm(lambda e, cs=cs: e.matmul(o_[:, cs], ones_b[0:1, :], bdt[0:1, cs], start=False, stop=True),
                             reads=[Bc["ones_b"], bdB], writes=[oB], signal=(cb == 1), first=False)
                    if e_ == 0:
                        S.op(dve, lambda e, tt=tt: e.tensor_scalar(facc[:, tt, :], o_[:], Wr[:, tt, e_:e_ + 1], None,
                                                                   op0=ALU.mult),
                             reads=[oB, B_wr[tt]], writes=[B_fa[tt]])
                    else:
                        S.op(dve, lambda e, tt=tt: e.scalar_tensor_tensor(out=facc[:, tt, :], in0=o_[:],
                                                                          scalar=Wr[:, tt, e_:e_ + 1], in1=facc[:, tt, :],
                                                                          op0=ALU.mult, op1=ALU.add),
                             reads=[oB, B_wr[tt], B_fa[tt]], writes=[B_fa[tt]])

            load_gu(0)
            load_dn(0)
            for e_ in range(n_experts):
                gu_step(e_, 0)
                gu_step(e_, 1)
                dn_step(e_, 0)
                gu_step(e_, 2)
                dn_step(e_, 1)
                gu_step(e_, 3)
                if e_ + 1 < n_experts:
                    load_gu(e_ + 1)
                dn_step(e_, 2)
                dn_step(e_, 3)
                if e_ + 1 < n_experts:
                    load_dn(e_ + 1)
            S.barrier()
        A.release(m_moe)

        with ExitStack() as pes:
            x1t = [A.alloc("x1f%d" % i, [128, 1024], F32) for i in range(2)]
            ot_ = [A.alloc("of%d" % i, [128, 1024], F32) for i in range(2)]
            tt_ = A.alloc("tt7", [128, 1024], F32)
            junk = A.alloc("junk7", [128, 1024], BF16)
            sm = A.alloc("sm7", [128, 16, 4], F32)
            B7 = {k: Buf(k) for k in ["x1f0", "x1f1", "of0", "of1", "tt"]}
            Bsm = [[Buf("sm7_%d_%d" % (i, j)) for j in range(3)] for i in range(16)]
            outs = []
            for tt in range(16):
                k = tt % 2
                tsl = slice(tt * 128, (tt + 1) * 128)
                S.dma(sp, x1t[k][:], x1_s[tsl, :], reads=[B_x1[tt]], writes=[B7["x1f%d" % k]])
                S.op(act, lambda e, tt=tt: e.activation(out=junk[:], in_=facc[:, tt, :], func=AF.Square,
                                                        accum_out=sm[:, tt, 0:1]), reads=[B_fa[tt]], writes=[Bsm[tt][0]])
                S.op(act, lambda e, tt=tt: e.activation(out=sm[:, tt, 1:2], in_=sm[:, tt, 0:1], func=AF.Ln, bias=EPS,
                                                        scale=1.0 / 1024), reads=[Bsm[tt][0]], writes=[Bsm[tt][1]])
                S.op(act, lambda e, tt=tt: e.activation(out=sm[:, tt, 2:3], in_=sm[:, tt, 1:2], func=AF.Exp, scale=-0.5),
                     reads=[Bsm[tt][1]], writes=[Bsm[tt][2]])
                S.op(dve, lambda e, tt=tt: e.scalar_tensor_tensor(out=tt_[:], in0=facc[:, tt, :], scalar=sm[:, tt, 2:3],
                                                                  in1=gf_b[:], op0=ALU.mult, op1=ALU.mult),
                     reads=[B_fa[tt], Bsm[tt][2], Bc["gf_b"]], writes=[B7["tt"]])
                S.op(pool, lambda e, k=k: e.tensor_tensor(out=ot_[k][:], in0=tt_[:], in1=x1t[k][:], op=ALU.add),
                     reads=[B7["tt"], B7["x1f%d" % k]], writes=[B7["of%d" % k]])
                outs.append(S.dma(sp, out[tsl, :], ot_[k][:], reads=[B7["of%d" % k]]))
            S.barrier()
        build_program.stats = dict(n_inst=S.n_inst, sbuf_peak=A.peak, auto_sbuf_left=nc.sbuf_bytes_remaining)
    return nc, dbg_outs


def _rope_tables(rot_dim):
    rows = 64
    row = np.repeat(np.arange(rows, dtype=np.float32), 64)
    col = np.tile(np.arange(64, dtype=np.float32), rows)
    quarter = rot_dim // 4
    inv = (np.float32(10000.0) ** (-np.arange(quarter, dtype=np.float32) / np.float32(quarter))).astype(np.float32)
    ang = np.concatenate([row[:, None] * inv, col[:, None] * inv], axis=-1).astype(np.float32)
    return np.cos(ang).astype(np.float32), np.sin(ang).astype(np.float32)


def _const_tables():
    ca, sa = _rope_tables(64)
    cb, sb = _rope_tables(32)
    tc = np.zeros((96, 4096), np.float32)
    ts = np.zeros((96, 4096), np.float32)
    tc[0:32] = ca.T
    tc[32:64] = ca.T
    ts[0:32] = -sa.T
    ts[32:64] = sa.T
    tc[64:80] = cb.T
    tc[80:96] = cb.T
    ts[64:80] = -sb.T
    ts[80:96] = sb.T
    return tc, ts


def _masks(s):
    NEG = -30000.0
    kk = np.arange(128)[:, None]
    ii = np.arange(128)[None, :]
    mL = np.where(ii <= kk, 0.0, NEG).astype(np.float32)
    mR = np.where(kk <= ii, 0.0, NEG).astype(np.float32)
    allneg = np.full((128, 128), NEG, np.float32)
    mLe = allneg if s == 0 else mL
    mRe = mR if s == 0 else allneg
    m = np.stack([np.tile(x, (1, 4)) for x in (mL, mR, mLe, mRe)], axis=1)
    return np.ascontiguousarray(m)


def _swap_halves(w, width):
    n = w.shape[1] // width
    w3 = w.reshape(w.shape[0], n, width)
    h = width // 2
    return np.concatenate([w3[:, :, h:], w3[:, :, :h]], axis=2).reshape(w.shape[0], n * width)


def prep_inputs(I):
    f = lambda a: np.ascontiguousarray(np.asarray(a, dtype=np.float32))
    w_in = f(I["w_in"][0])
    qa, ka = w_in[:, 0:512], w_in[:, 512:640]
    kr = w_in[:, 1408:1440]
    z64 = np.zeros((1024, 64), np.float32)
    w_inx = np.concatenate([w_in[:, 0:1408], z64, kr, z64, _swap_halves(kr, 32), _swap_halves(qa, 64),
                            _swap_halves(ka, 64)], axis=1)
    assert w_inx.shape[1] == C_END
    w_uq = f(I["w_uq"][0])
    wq3 = w_uq.reshape(384, 8, 96)
    w_uqp = np.zeros_like(wq3)
    w_uqp[:, :, 64:96] = _swap_halves(wq3[:, :, 64:96].reshape(384, 256), 32).reshape(384, 8, 32)
    w_uqp = np.ascontiguousarray(w_uqp.reshape(384, 768))
    rows = np.concatenate([f(I["g_mix_pre"][0]), f(I["g_mix_post"][0]), f(I["g_ffn_pre"][0]), f(I["g_ffn_post"][0]),
                           f(I["b_ada"][0])])[None, :]
    tc, ts = _const_tables()
    shared = dict(
        w_ada=f(I["w_ada"][0]), rows=np.ascontiguousarray(rows), w_inx=np.ascontiguousarray(w_inx),
        sinkr=np.ascontiguousarray(np.repeat(f(I["sink"][0]), 128)[None, :]),
        gq=np.ascontiguousarray(f(I["g_q_a"][0]).reshape(3, 128).T), gkv=np.ascontiguousarray(f(I["g_kv_a"][0]).reshape(2, 128).T),
        w_uq=w_uq, w_uqp=w_uqp, w_ukv=f(I["w_ukv"][0]), w_o=f(I["w_o"][0]), w_r=f(I["w_router"][0]),
        b_r16=np.ascontiguousarray(np.tile(f(I["b_router"][0]), 16)[None, :]), w_gu=f(I["w_gate_up"][0]), b_gu=f(I["b_gate_up"][0]), w_dn=f(I["w_down"][0]),
        b_dn=f(I["b_down"][0]), ident=np.eye(128, dtype=np.float32))
    x = np.asarray(I["x"], dtype=np.float32)
    ctx = np.asarray(I["ctx"], dtype=np.float32)
    c = np.asarray(I["c"], dtype=np.float32)
    c_ctx = np.asarray(I["c_ctx"], dtype=np.float32)
    maps = []
    for core in range(8):
        b, s = core // 2, core % 2
        own = x[b, s * 2048:(s + 1) * 2048]
        oth = x[b, (1 - s) * 2048:(2 - s) * 2048]
        xin = np.ascontiguousarray(np.concatenate([own, oth, ctx[b]], axis=0))
        cvec = np.ascontiguousarray(np.concatenate([c[b].reshape(8, 128).T, c_ctx.reshape(8, 128).T], axis=1))
        order = np.concatenate([np.arange(s * 2048, (s + 1) * 2048), np.arange((1 - s) * 2048, (2 - s) * 2048)])
        m = dict(shared)
        m.update(xin=xin, cvec=cvec, tabc=np.ascontiguousarray(tc[:, order]), tabs=np.ascontiguousarray(ts[:, order]),
                 masks=_masks(s))
        maps.append(m)
    return maps


_CACHE = {}


def kernel(**inputs):
    if "nc" not in _CACHE:
        _CACHE["nc"] = build_program()[0]
    nc = _CACHE["nc"]
    maps = prep_inputs(inputs)
    res = run_bass_kernel_spmd(nc, maps, core_ids=list(range(8)))
    outp = np.zeros((4, 4096, 1024), np.float32)
    for core in range(8):
        b, s = core // 2, core % 2
        outp[b, s * 2048:(s + 1) * 2048] = res.results[core]["out"]
    return outp
```

```python
import numpy as np
from contextlib import ExitStack
from collections import deque
import concourse.bass as bass
import concourse.mybir as mybir
from concourse.bass_utils import run_bass_kernel_spmd

F32 = mybir.dt.float32
BF16 = mybir.dt.bfloat16
AF = mybir.ActivationFunctionType
ALU = mybir.AluOpType

import os
DBG_SKIP_ROUTER = os.environ.get("DBG_SKIP_ROUTER") == "1"
EPS = 1e-6
A_SCALE = 64 ** -0.5
MLA_SCALE = 96 ** -0.5
C_QA, C_KA, C_VA, C_CQ, C_CKV, C_KR2, C_QAP, C_KAP, C_END = 0, 512, 640, 768, 1152, 1408, 1600, 2112, 2240


class Buf:
    __slots__ = ("name", "w", "r")

    def __init__(self, name):
        self.name = name
        self.w = None
        self.r = []


class Eng:
    def __init__(self, S, name, obj, is_pe=False, n_dma=0):
        self.name = name
        self.obj = obj
        self.sem = S.new_sem("e_" + name)
        self.count = 0
        self.seen = {}
        self.is_pe = is_pe
        self.pend_r = []
        self.pend_w = []
        self.dma_sems = [[S.new_sem("d_%s%d" % (name, i)), 0] for i in range(n_dma)]
        self.dma_rr = 0


class Sched:
    def __init__(self, nc, es):
        self.nc = nc
        self.es = es
        self.pe = Eng(self, "pe", nc.tensor, is_pe=True)
        self.dve = Eng(self, "dve", nc.vector)
        self.act = Eng(self, "act", nc.scalar)
        self.pool = Eng(self, "pool", nc.gpsimd, n_dma=16)
        self.sp = Eng(self, "sp", nc.sync, n_dma=24)
        self.engs = [self.pe, self.dve, self.act, self.pool, self.sp]
        self.n_inst = 0

    def new_sem(self, name):
        return self.es.enter_context(self.nc.semaphore(name))

    def _wait(self, eng, ev):
        sem, val = ev
        k = id(sem)
        if eng.seen.get(k, 0) >= val:
            return
        eng.obj.wait_ge(sem, val)
        eng.seen[k] = val
        self.n_inst += 1

    def _deps(self, eng, reads, writes):
        for b in reads:
            if b.w is not None and not (eng.is_pe and b.w[0] is eng.sem):
                self._wait(eng, b.w)
        for b in writes:
            if b.w is not None and not (eng.is_pe and b.w[0] is eng.sem):
                self._wait(eng, b.w)
            for ev in b.r:
                if not (eng.is_pe and ev[0] is eng.sem):
                    self._wait(eng, ev)

    def _commit(self, ev, reads, writes):
        for b in reads:
            b.r.append(ev)
            if len(b.r) > 48:
                last = {}
                for e in b.r:
                    k = id(e[0])
                    if k not in last or last[k][1] < e[1]:
                        last[k] = e
                b.r = list(last.values())
        for b in writes:
            b.w = ev
            b.r = []

    def op(self, eng, fn, reads=(), writes=()):
        self._deps(eng, reads, writes)
        ins = fn(eng.obj)
        eng.count += 1
        ev = (eng.sem, eng.count)
        ins.then_inc(eng.sem, 1)
        self._commit(ev, reads, writes)
        self.n_inst += 1
        return ev

    def mm(self, fn, reads=(), writes=(), signal=True, first=True):
        eng = self.pe
        self._deps(eng, reads, writes if first else ())
        ins = fn(eng.obj)
        eng.pend_r.extend(reads)
        for b in writes:
            if b not in eng.pend_w:
                eng.pend_w.append(b)
        self.n_inst += 1
        if signal:
            eng.count += 1
            ev = (eng.sem, eng.count)
            ins.then_inc(eng.sem, 1)
            self._commit(ev, eng.pend_r, eng.pend_w)
            eng.pend_r = []
            eng.pend_w = []
            return ev
        return None

    def dma(self, q, out, in_, reads=(), writes=()):
        slot = q.dma_sems[q.dma_rr % len(q.dma_sems)]
        q.dma_rr += 1
        sem, cur = slot
        if cur > 0:
            self._wait(q, (sem, cur))
        self._deps(q, reads, writes)
        ins = q.obj.dma_start(out=out, in_=in_)
        slot[1] = cur + 16
        ev = (sem, cur + 16)
        ins.then_inc(sem, 16)
        self._commit(ev, reads, writes)
        self.n_inst += 1
        return ev

    def barrier(self):
        assert not self.pe.pend_r and not self.pe.pend_w
        evs = [(e.sem, e.count) for e in self.engs if e.count > 0]
        for e in self.engs:
            evs += [(s, v) for s, v in e.dma_sems if v > 0]
        for e in self.engs:
            for ev in evs:
                if ev[0] is e.sem and e.is_pe:
                    continue
                self._wait(e, ev)


class Arena:
    def __init__(self, nc, base=20480, top=229312):
        self.nc = nc
        self.ptr = base
        self.top = top
        self.n = 0
        self.peak = base
        self.limit = top

    def alloc_at(self, name, shape, dtype, off):
        self.n += 1
        return self.nc.alloc_sbuf_tensor_at("%s_%d" % (name, self.n), list(shape), dtype, offset=off)

    def alloc(self, name, shape, dtype):
        esz = 4 if dtype == F32 else 2
        nbytes = int(np.prod(shape[1:])) * esz
        off = (self.ptr + 31) // 32 * 32
        assert off + nbytes <= self.limit, ("SBUF overflow", name, off, nbytes, self.limit)
        self.ptr = off + nbytes
        self.peak = max(self.peak, self.ptr)
        self.n += 1
        return self.nc.alloc_sbuf_tensor_at("%s_%d" % (name, self.n), list(shape), dtype, offset=off)

    def mark(self):
        return self.ptr

    def release(self, m):
        self.ptr = m


def build_program(stage=99, dbg=False, n_experts=32):
    nc = bass.Bass("TRN2", target_bir_lowering=False)

    def din(name, shape, dt=F32):
        return nc.dram_tensor(name, list(shape), dt, kind="ExternalInput").ap()

    def dout(name, shape, dt=F32):
        return nc.dram_tensor(name, list(shape), dt, kind="ExternalOutput").ap()

    def dscr(name, shape, dt):
        return nc.dram_tensor(name, list(shape), dt, kind="Internal").ap()

    xin = din("xin", [4352, 1024])
    cvec = din("cvec", [128, 16])
    w_ada = din("w_ada", [1024, 6144])
    rows_in = din("rows", [1, 10240])
    w_inx = din("w_inx", [1024, C_END])
    tabc = din("tabc", [96, 4096])
    tabs = din("tabs", [96, 4096])
    masks = din("masks", [128, 4, 512])
    sinkr = din("sinkr", [1, 1024])
    gq = din("gq", [128, 3])
    gkv = din("gkv", [128, 2])
    w_uq = din("w_uq", [384, 768])
    w_uqp = din("w_uqp", [384, 768])
    w_ukv = din("w_ukv", [256, 1024])
    w_o = din("w_o", [1024, 1024])
    w_r = din("w_r", [1024, 32])
    b_r16 = din("b_r16", [1, 512])
    w_gu = din("w_gu", [n_experts, 1024, 2048])
    b_gu = din("b_gu", [32, 2048])
    w_dn = din("w_dn", [n_experts, 1024, 1024])
    b_dn = din("b_dn", [32, 1024])
    ident = din("ident", [128, 128])
    out = dout("out", [2048, 1024])

    cq_s = dscr("cq_s", [128, 3, 2048], BF16)
    ckv_s = dscr("ckv_s", [128, 2, 4352], BF16)
    kr_s = dscr("kr_s", [96, 4352], BF16)
    x1_s = dscr("x1_s", [2048, 1024], F32)

    dbg_outs = {}

    def ddump(S, name, src_ap, shape, reads, cast=True):
        if not dbg:
            return
        o = dout(name, shape)
        dbg_outs[name] = o
        S.dma(S.pool if cast else S.sp, o, src_ap, reads=reads)

    with ExitStack() as es:
        S = Sched(nc, es)
        A = Arena(nc)
        pe, dve, act, pool, sp = S.pe, S.dve, S.act, S.pool, S.sp

        ident_b = A.alloc("ident_b", [128, 128], BF16)
        ident_f = A.alloc("ident_f", [128, 128], F32)
        ones_b = A.alloc("ones_b", [128, 128], BF16)
        ones_f = A.alloc("ones_f", [128, 128], F32)
        e64 = A.alloc("e64", [1, 65], BF16)
        gf_b = A.alloc("gf_b", [128, 1024], F32)
        gm_b = A.alloc("gm_b", [128, 1024], F32)
        gs2_b = A.alloc("gs2_b", [128, 1024], F32)
        sh2_b = A.alloc("sh2_b", [128, 1024], F32)
        Bc = {k: Buf(k) for k in ["ident_b", "ident_f", "ones_b", "ones_f", "e64", "gf_b", "gm_b", "gs2_b", "sh2_b"]}
        S.dma(pool, ident_b[:], ident, writes=[Bc["ident_b"]])
        S.dma(sp, ident_f[:], ident, writes=[Bc["ident_f"]])
        S.op(pool, lambda e: e.memset(ones_b[:], 1.0), writes=[Bc["ones_b"]])
        S.op(pool, lambda e: e.memset(ones_f[:], 1.0), writes=[Bc["ones_f"]])
        S.op(pool, lambda e: e.memset(e64[:], 0.0), writes=[Bc["e64"]])
        S.op(pool, lambda e: e.memset(e64[0:1, 64:65], 1.0), writes=[Bc["e64"]])
        m_persist = A.mark()

        gs1_b = A.alloc("gs1_b", [128, 1024], F32)
        sh1_b = A.alloc("sh1_b", [128, 1024], F32)
        gsc_b = A.alloc("gsc_b", [128, 1024], F32)
        shc_b = A.alloc("shc_b", [128, 1024], F32)
        for k in ["gs1_b", "sh1_b", "gsc_b", "shc_b"]:
            Bc[k] = Buf(k)
        m_bc1 = A.mark()
        with ExitStack() as pes:
            rows_t = A.alloc("rows_t", [1, 10240], F32)
            cv = A.alloc("cv", [128, 16], F32)
            sg = A.alloc("sg", [128, 16], F32)
            sl = A.alloc("sl", [128, 16], F32)
            rep = A.alloc("rep", [128, 16, 128], BF16)
            wa = [A.alloc("wa%d" % i, [128, 8, 512], BF16) for i in range(2)]
            gb = [A.alloc("gb%d" % i, [128, 512], F32) for i in range(2)]
            pm = [pes.enter_context(nc.psum_tensor("pm%d" % i, [128, 512], F32)) for i in range(2)]
            pmc = [pes.enter_context(nc.psum_tensor("pmc%d" % i, [128, 512], F32)) for i in range(2)]
            pg = [pes.enter_context(nc.psum_tensor("pg%d" % i, [128, 512], F32)) for i in range(2)]
            B0 = {k: Buf(k) for k in ["rows", "cv", "sg", "sl", "rep", "wa0", "wa1", "gb0", "gb1", "pm0", "pm1",
                                      "pmc0", "pmc1", "pg0", "pg1"]}
            S.dma(sp, rows_t[:], rows_in, writes=[B0["rows"]])
            S.dma(sp, cv[:], cvec, writes=[B0["cv"]])
            S.op(act, lambda e: e.activation(out=sg[:], in_=cv[:], func=AF.Sigmoid), reads=[B0["cv"]], writes=[B0["sg"]])
            S.op(dve, lambda e: e.tensor_tensor(out=sl[:], in0=cv[:], in1=sg[:], op=ALU.mult),
                 reads=[B0["cv"], B0["sg"]], writes=[B0["sl"]])
            for j in range(16):
                S.op(dve, lambda e, j=j: e.tensor_scalar(rep[:, j, :], ones_f[:], sl[:, j:j + 1], None, op0=ALU.mult),
                     reads=[B0["sl"], Bc["ones_f"]], writes=[B0["rep"]])
            w_ada_v = w_ada.rearrange("(c p) n -> p c n", p=128)
            g_off = {1: 0, 2: 1024, 4: 2048, 5: 3072}
            dests = {0: sh1_b, 1: gs1_b, 2: gm_b, 3: sh2_b, 4: gs2_b, 5: gf_b}
            destB = {0: "sh1_b", 1: "gs1_b", 2: "gm_b", 3: "sh2_b", 4: "gs2_b", 5: "gf_b"}
            for j in range(12):
                m, half = j // 2, j % 2
                k = j % 2
                cs = slice(half * 512, (half + 1) * 512)
                S.dma(pool, wa[k][:], w_ada_v[:, :, j * 512:(j + 1) * 512], writes=[B0["wa%d" % k]])
                vecs = [(0, pm[k], B0["pm%d" % k], dests[m], Bc[destB[m]])]
                if m < 2:
                    vecs.append((8, pmc[k], B0["pmc%d" % k], (shc_b if m == 0 else gsc_b),
                                 Bc["shc_b" if m == 0 else "gsc_b"]))
                if m in g_off:
                    go = g_off[m] + half * 512
                    S.mm(lambda e, k=k, go=go: e.matmul(pg[k][:], ones_f[0:1, :], rows_t[0:1, go:go + 512],
                                                        start=True, stop=True),
                         reads=[Bc["ones_f"], B0["rows"]], writes=[B0["pg%d" % k]])
                    S.op(act, lambda e, k=k: e.copy(gb[k][:], pg[k][:]), reads=[B0["pg%d" % k]], writes=[B0["gb%d" % k]])
                for (v0, pt_, pB, dst, dB) in vecs:
                    for c in range(8):
                        S.mm(lambda e, c=c, v0=v0, pt_=pt_, k=k: e.matmul(pt_[:], rep[:, v0 + c, :], wa[k][:, c, :],
                                                                          start=(c == 0), stop=False),
                             reads=[B0["rep"], B0["wa%d" % k]], writes=[pB], signal=False, first=(c == 0))
                    bo = 4096 + j * 512
                    S.mm(lambda e, pt_=pt_, bo=bo: e.matmul(pt_[:], ones_f[0:1, :], rows_t[0:1, bo:bo + 512],
                                                            start=False, stop=True),
                         reads=[Bc["ones_f"], B0["rows"]], writes=[pB], signal=True, first=False)
                    if m in (0, 3):
                        S.op(act, lambda e, dst=dst, pt_=pt_, cs=cs: e.copy(dst[:, cs], pt_[:]), reads=[pB], writes=[dB])
                    elif m in (1, 4):
                        S.op(dve, lambda e, dst=dst, pt_=pt_, cs=cs, k=k: e.scalar_tensor_tensor(
                            out=dst[:, cs], in0=pt_[:], scalar=1.0, in1=gb[k][:], op0=ALU.add, op1=ALU.mult),
                            reads=[pB, B0["gb%d" % k]], writes=[dB])
                    else:
                        S.op(dve, lambda e, dst=dst, pt_=pt_, cs=cs, k=k: e.tensor_tensor(
                            out=dst[:, cs], in0=pt_[:], in1=gb[k][:], op=ALU.mult),
                            reads=[pB, B0["gb%d" % k]], writes=[dB])
            if dbg and stage == 0:
                for nm, t in [("d_gs1", gs1_b), ("d_sh1", sh1_b), ("d_gsc", gsc_b), ("d_shc", shc_b), ("d_gm", gm_b),
                              ("d_gs2", gs2_b), ("d_sh2", sh2_b), ("d_gf", gf_b)]:
                    ddump(S, nm, t[:], [128, 1024], [Bc[k] for k in Bc], cast=False)
            S.barrier()
        A.release(m_bc1)
        if stage == 0:
            return nc, dbg_outs

        qaT = A.alloc("qaT", [64, 8, 2048], BF16)
        kaT = A.alloc("kaT", [64, 2, 4352], BF16)
        va = A.alloc("va", [128, 34, 130], BF16)
        Bq = {k: Buf(k) for k in ["qaT", "kaT", "va", "cq_s", "ckv_s", "kr_s"]}
        m_attnA = A.mark()
        with ExitStack() as pes:
            W = A.alloc("W", [128, 8, C_END], BF16)
            xt = [A.alloc("xt%d" % i, [128, 1024], F32) for i in range(2)]
            junk = A.alloc("junk", [128, 1024], BF16)
            tt_ = A.alloc("tt_", [128, 1024], F32)
            hb = [A.alloc("hb%d" % i, [128, 1024], BF16) for i in range(2)]
            hTb = [A.alloc("hTb%d" % i, [128, 8, 512], BF16) for i in range(2)]
            tcb = [A.alloc("tcb%d" % i, [96, 512], F32) for i in range(2)]
            tsb = [A.alloc("tsb%d" % i, [96, 512], F32) for i in range(2)]
            r1 = [A.alloc("r1_%d" % i, [96, 512], F32) for i in range(2)]
            r2 = [A.alloc("r2_%d" % i, [96, 512], F32) for i in range(2)]
            ss = A.alloc("ss", [128, 34], F32)
            lnv = A.alloc("lnv", [128, 34], F32)
            rs = A.alloc("rs", [128, 34], F32)
            sq = A.alloc("sq", [128, 3, 512], BF16)
            rl0 = A.alloc("rl0", [128, 512], F32)
            rl1 = A.alloc("rl1", [128, 512], F32)
            cqs = [A.alloc("cqs%d" % i, [128, 3, 512], BF16) for i in range(2)]
            ckvs = [A.alloc("ckvs%d" % i, [128, 2, 512], BF16) for i in range(2)]
            krs = [A.alloc("krs%d" % i, [96, 512], BF16) for i in range(2)]
            pT = [pes.enter_context(nc.psum_tensor("pT%d" % i, [128, 1024], BF16)) for i in range(2)]
            pp = [pes.enter_context(nc.psum_tensor("pp%d" % i, [128, 512], F32)) for i in range(5)]
            pr = pes.enter_context(nc.psum_tensor("pr", [128, 512], F32))
            B1 = {k: Buf(k) for k in ["W", "xt0", "xt1", "tt_", "hb0", "hb1", "hTb0", "hTb1", "tcb0", "tcb1", "tsb0",
                                      "tsb1", "r1_0", "r1_1", "r2_0", "r2_1", "sq", "rl0", "rl1", "cqs0", "cqs1",
                                      "ckvs0", "ckvs1", "krs0", "krs1", "pT0", "pT1", "pp0", "pp1", "pp2", "pp3", "pp4",
                                      "pr"]}
            Bss = [Buf("ss%d" % i) for i in range(34)]
            Bln = [Buf("ln%d" % i) for i in range(34)]
            Brs = [Buf("rs%d" % i) for i in range(34)]
            S.dma(pool, W[:], w_inx.rearrange("(c p) n -> p c n", p=128), writes=[B1["W"]])
            S.op(pool, lambda e: e.memset(va[:, :, 64:65], 1.0), writes=[Bq["va"]])
            S.op(pool, lambda e: e.memset(va[:, :, 129:130], 1.0), writes=[Bq["va"]])
            ppi = [0]
            ropei = [0]

            def next_pp():
                i = ppi[0] % 5
                ppi[0] += 1
                return pp[i], B1["pp%d" % i]

            def projT(dst, dB, col0, M, hT, hB, ntok):
                for c in range(8):
                    S.mm(lambda e, c=c: e.matmul(dst[0:M, 0:ntok], W[:, c, col0:col0 + M], hT[:, c, 0:ntok],
                                                 start=(c == 0), stop=(c == 7)),
                         reads=[B1["W"], hB], writes=[dB], signal=(c == 7), first=(c == 0))

            def rope_evac(pa, pBa, pb, pBb, r0, r1_, ntok, k, dst_ap, dstB):
                i = ropei[0] % 2
                ropei[0] += 1
                S.op(dve, lambda e: e.tensor_tensor(out=r1[i][r0:r1_, 0:ntok], in0=pa[r0:r1_, 0:ntok],
                                                    in1=tcb[k][r0:r1_, 0:ntok], op=ALU.mult),
                     reads=[pBa, B1["tcb%d" % k]], writes=[B1["r1_%d" % i]])
                S.op(dve, lambda e: e.tensor_tensor(out=r2[i][r0:r1_, 0:ntok], in0=pb[r0:r1_, 0:ntok],
                                                    in1=tsb[k][r0:r1_, 0:ntok], op=ALU.mult),
                     reads=[pBb, B1["tsb%d" % k]], writes=[B1["r2_%d" % i]])
                S.op(dve, lambda e: e.tensor_tensor(out=dst_ap, in0=r1[i][r0:r1_, 0:ntok], in1=r2[i][r0:r1_, 0:ntok],
                                                     op=ALU.add),
                     reads=[B1["r1_%d" % i], B1["r2_%d" % i]], writes=[dstB])

            def latent_norm(pcs, nfeat, ntok, dst, dstB):
                nj = len(pcs)
                for j, (pc, pB) in enumerate(pcs):
                    S.op(act, lambda e, j=j, pc=pc: e.activation(out=sq[:, j, 0:ntok], in_=pc[:, 0:ntok], func=AF.Square),
                         reads=[pB], writes=[B1["sq"]])
                for j in range(nj):
                    S.mm(lambda e, j=j: e.matmul(pr[:, 0:ntok], ones_b[:], sq[:, j, 0:ntok], start=(j == 0),
                                                 stop=(j == nj - 1)),
                         reads=[Bc["ones_b"], B1["sq"]], writes=[B1["pr"]], signal=(j == nj - 1), first=(j == 0))
                S.op(act, lambda e: e.activation(out=rl0[:, 0:ntok], in_=pr[:, 0:ntok], func=AF.Ln, bias=EPS,
                                                 scale=1.0 / nfeat), reads=[B1["pr"]], writes=[B1["rl0"]])
                S.op(act, lambda e: e.activation(out=rl1[:, 0:ntok], in_=rl0[:, 0:ntok], func=AF.Exp, scale=-0.5),
                     reads=[B1["rl0"]], writes=[B1["rl1"]])
                for j, (pc, pB) in enumerate(pcs):
                    S.op(dve, lambda e, j=j, pc=pc: e.tensor_tensor(out=dst[:, j, 0:ntok], in0=pc[:, 0:ntok],
                                                                     in1=rl1[:, 0:ntok], op=ALU.mult),
                         reads=[pB, B1["rl1"]], writes=[dstB])

            for bi in range(9):
                ntile = 4 if bi < 8 else 2
                ntok = ntile * 128
                t0 = bi * 4
                k = bi % 2
                hT, hB = hTb[k], B1["hTb%d" % k]
                tok = slice(bi * 512, bi * 512 + ntok)
                if bi < 8:
                    S.dma(sp, tcb[k][:], tabc[:, tok], writes=[B1["tcb%d" % k]])
                    S.dma(sp, tsb[k][:], tabs[:, tok], writes=[B1["tsb%d" % k]])
                gsb, shb = (gs1_b, sh1_b) if bi < 8 else (gsc_b, shc_b)
                gsB, shB = (Bc["gs1_b"], Bc["sh1_b"]) if bi < 8 else (Bc["gsc_b"], Bc["shc_b"])
                for ti in range(ntile):
                    tt = t0 + ti
                    x_, xB = xt[tt % 2], B1["xt%d" % (tt % 2)]
                    h_, hbB = hb[tt % 2], B1["hb%d" % (tt % 2)]
                    pT_, pTB = pT[tt % 2], B1["pT%d" % (tt % 2)]
                    S.dma(sp, x_[:], xin[tt * 128:(tt + 1) * 128, :], writes=[xB])
                    S.op(act, lambda e, x_=x_, tt=tt: e.activation(out=junk[:], in_=x_[:], func=AF.Square,
                                                                   accum_out=ss[:, tt:tt + 1]),
                         reads=[xB], writes=[Bss[tt]])
                    S.op(act, lambda e, tt=tt: e.activation(out=lnv[:, tt:tt + 1], in_=ss[:, tt:tt + 1], func=AF.Ln,
                                                            bias=EPS, scale=1.0 / 1024), reads=[Bss[tt]], writes=[Bln[tt]])
                    S.op(act, lambda e, tt=tt: e.activation(out=rs[:, tt:tt + 1], in_=lnv[:, tt:tt + 1], func=AF.Exp,
                                                            scale=-0.5), reads=[Bln[tt]], writes=[Brs[tt]])
                    S.op(dve, lambda e, x_=x_, tt=tt: e.scalar_tensor_tensor(out=tt_[:], in0=x_[:], scalar=rs[:, tt:tt + 1],
                                                                             in1=gsb[:], op0=ALU.mult, op1=ALU.mult),
                         reads=[xB, Brs[tt], gsB], writes=[B1["tt_"]])
                    S.op(dve, lambda e, h_=h_: e.tensor_tensor(out=h_[:], in0=tt_[:], in1=shb[:], op=ALU.add),
                         reads=[B1["tt_"], shB], writes=[hbB])
                    for c in range(8):
                        S.mm(lambda e, c=c, h_=h_, pT_=pT_: e.transpose(pT_[:, c * 128:(c + 1) * 128],
                                                                       h_[:, c * 128:(c + 1) * 128], ident_b[:]),
                             reads=[hbB, Bc["ident_b"]], writes=[pTB], signal=(c == 7), first=(c == 0))
                    S.op(act, lambda e, pT_=pT_, ti=ti: e.copy(hT[:, :, ti * 128:(ti + 1) * 128],
                                                               pT_[:].rearrange("p (c t) -> p c t", c=8)),
                         reads=[pTB], writes=[hB])
                if bi < 4:
                    for h in range(8):
                        pa, pBa = next_pp()
                        pb, pBb = next_pp()
                        projT(pa, pBa, C_QA + h * 64, 64, hT, hB, ntok)
                        projT(pb, pBb, C_QAP + h * 64, 64, hT, hB, ntok)
                        rope_evac(pa, pBa, pb, pBb, 0, 64, ntok, k, qaT[:, h, tok], Bq["qaT"])
                for kh in range(2):
                    pa, pBa = next_pp()
                    projT(pa, pBa, C_KA + kh * 64, 64, hT, hB, ntok)
                    if bi < 8:
                        pb, pBb = next_pp()
                        projT(pb, pBb, C_KAP + kh * 64, 64, hT, hB, ntok)
                        rope_evac(pa, pBa, pb, pBb, 0, 64, ntok, k, kaT[:, kh, tok], Bq["kaT"])
                    else:
                        S.op(dve, lambda e, pa=pa, kh=kh: e.tensor_copy(kaT[:, kh, tok], pa[0:64, 0:ntok]),
                             reads=[pBa], writes=[Bq["kaT"]])
                pv, pBv = next_pp()
                for ti in range(ntile):
                    for c in range(8):
                        S.mm(lambda e, c=c, ti=ti: e.matmul(pv[:, ti * 128:(ti + 1) * 128],
                                                           hT[:, c, ti * 128:(ti + 1) * 128], W[:, c, C_VA:C_VA + 128],
                                                           start=(c == 0), stop=(c == 7)),
                             reads=[B1["W"], hB], writes=[pBv], signal=(c == 7 and ti == ntile - 1),
                             first=(c == 0 and ti == 0))
                for kh in range(2):
                    S.op(dve, lambda e, kh=kh: e.tensor_copy(
                        va[:, t0:t0 + ntile, kh * 65:kh * 65 + 64],
                        pv[:, 0:ntile * 128].rearrange("p (t x) -> p t x", t=ntile)[:, :, kh * 64:(kh + 1) * 64]),
                        reads=[pBv], writes=[Bq["va"]])
                if bi < 4:
                    pcs = []
                    for j in range(3):
                        pc, pB = next_pp()
                        projT(pc, pB, C_CQ + j * 128, 128, hT, hB, ntok)
                        pcs.append((pc, pB))
                    latent_norm(pcs, 384, ntok, cqs[k], B1["cqs%d" % k])
                    S.dma(sp, cq_s[:, :, tok], cqs[k][:], reads=[B1["cqs%d" % k]], writes=[Bq["cq_s"]])
                pcs = []
                for j in range(2):
                    pc, pB = next_pp()
                    projT(pc, pB, C_CKV + j * 128, 128, hT, hB, ntok)
                    pcs.append((pc, pB))
                latent_norm(pcs, 256, ntok, ckvs[k], B1["ckvs%d" % k])
                S.dma(sp, ckv_s[:, :, tok], ckvs[k][:, :, 0:ntok], reads=[B1["ckvs%d" % k]], writes=[Bq["ckv_s"]])
                pa, pBa = next_pp()
                projT(pa, pBa, C_KR2, 96, hT, hB, ntok)
                if bi < 8:
                    pb, pBb = next_pp()
                    projT(pb, pBb, C_KR2 + 96, 96, hT, hB, ntok)
                    rope_evac(pa, pBa, pb, pBb, 64, 96, ntok, k, krs[k][64:96, 0:ntok], B1["krs%d" % k])
                else:
                    S.op(dve, lambda e, pa=pa: e.tensor_copy(krs[k][64:96, 0:ntok], pa[64:96, 0:ntok]),
                         reads=[pBa], writes=[B1["krs%d" % k]])
                S.dma(sp, kr_s[64:96, tok], krs[k][64:96, 0:ntok], reads=[B1["krs%d" % k]], writes=[Bq["kr_s"]])
            if dbg and stage == 1:
                allB = [Bq[k] for k in Bq]
                ddump(S, "d_qaT", qaT[:], [64, 8, 2048], allB)
                ddump(S, "d_kaT", kaT[:], [64, 2, 4352], allB)
                ddump(S, "d_va", va[:], [128, 34, 130], allB)
                ddump(S, "d_cq", cq_s, [128, 3, 2048], allB)
                ddump(S, "d_ckv", ckv_s, [128, 2, 4352], allB)
                ddump(S, "d_kr", kr_s[64:96, :], [32, 4352], allB)
            S.barrier()
        A.release(m_attnA)
        if stage == 1:
            return nc, dbg_outs

        def run_attention(iters, st, stB, ptb, ptB, ot, otB, bc, bcB, rden, rdB, bcs, bcsB, fillers=None, fill_every=1):
            flat = []
            for ii, it in enumerate(iters):
                ng = len(it["groups"])
                for gi, g in enumerate(it["groups"]):
                    flat.append((ii, gi, gi == ng - 1, g))
            n = len(flat)
            nst, npt, nbc = len(st), len(ptb), len(bc)

            def QK(i):
                ii, gi, last, g = flat[i]
                it = iters[ii]
                s_, sB = st[i % nst], stB[i % nst]
                nmm = sum(1 + (1 if m is not None else 0) for (_, _, m, _, _) in g)
                cnt = 0
                for slot, (kap, kB, mask, vap, vB) in enumerate(g):
                    cnt += 1
                    S.mm(lambda e, slot=slot, kap=kap, it=it, mask=mask: e.matmul(
                        s_[:, slot * 512:(slot + 1) * 512], kap, it["q"], start=True, stop=(mask is None)),
                        reads=[kB, it["qB"]], writes=[sB], signal=(cnt == nmm), first=(cnt == 1))
                    if mask is not None:
                        cnt += 1
                        S.mm(lambda e, slot=slot, mask=mask: e.matmul(s_[:, slot * 512:(slot + 1) * 512], ident_b[:],
                                                                      mask[0], start=False, stop=True),
                             reads=[Bc["ident_b"], mask[1]], writes=[sB], signal=(cnt == nmm), first=False)

            def EXP(i):
                ii, gi, last, g = flat[i]
                it = iters[ii]
                wdt = 512 * len(g)
                s_, sB = st[i % nst], stB[i % nst]
                p_, pB = ptb[i % npt], ptB[i % npt]
                S.op(act, lambda e: e.activation(out=p_[:, 0:wdt], in_=s_[:, 0:wdt], func=AF.Exp, scale=it["scale"]),
                     reads=[sB], writes=[pB])

            def PV(i):
                ii, gi, last, g = flat[i]
                it = iters[ii]
                o_, oB = ot[ii % len(ot)], otB[ii % len(ot)]
                p_, pB = ptb[i % npt], ptB[i % npt]
                for slot, (kap, kB, mask, vap, vB) in enumerate(g):
                    lastmm = last and slot == len(g) - 1 and it.get("sink") is None
                    S.mm(lambda e, slot=slot, vap=vap: e.matmul(o_[0:65, :], vap, p_[:, slot * 512:(slot + 1) * 512],
                                                                start=(gi == 0 and slot == 0), stop=lastmm),
                         reads=[vB, pB], writes=[oB], signal=(slot == len(g) - 1), first=(gi == 0 and slot == 0))
                if last:
                    if it.get("sink") is not None:
                        sl_, sr_, sB_ = it["sink"]
                        S.mm(lambda e: e.matmul(o_[0:65, :], sl_, sr_, start=False, stop=True),
                             reads=[Bc["e64"], sB_], writes=[oB], signal=True, first=False)
                    j = ii % nbc
                    S.op(dve, lambda e: e.reciprocal(rden[j][64:65, :], o_[64:65, :]), reads=[oB], writes=[rdB[j]])
                    S.mm(lambda e: e.matmul(bc[j][0:64, :], ones_f[64:65, 0:64], rden[j][64:65, :], start=True, stop=True),
                         reads=[Bc["ones_f"], rdB[j]], writes=[bcB[j]])
                    S.op(dve, lambda e: e.tensor_copy(bcs[j][:], bc[j][0:64, :]), reads=[bcB[j]], writes=[bcsB[j]])
                    S.op(dve, lambda e: e.tensor_tensor(out=it["out"], in0=it["ovw"](o_[0:64, :]), in1=it["ovw"](bcs[j][:]),
                                                        op=ALU.mult),
                         reads=[oB, bcsB[j]], writes=[it["outB"]])

            for i in range(n + 2):
                if i < n:
                    QK(i)
                    EXP(i)
                if 0 <= i - 2 < n:
                    PV(i - 2)
                if fillers and (i % fill_every == 0):
                    if fillers:
                        fillers.popleft()()
            while fillers:
                fillers.popleft()()

        TOP = 229312
        out_aT = A.alloc_at("out_aT", [64, 8, 2048], BF16, TOP - 32768)
        A.limit = TOP - 32768
        B_oa = [Buf("oa%d" % i) for i in range(16)]
        m_attnB = A.mark()
        with ExitStack() as pes:
            maskb = A.alloc("maskb", [128, 4, 512], BF16)
            sinkf = A.alloc("sinkf", [1, 1024], F32)
            sinkb = A.alloc("sinkb", [1, 1024], BF16)
            ptb = [A.alloc("ptb%d" % i, [128, 1024], BF16) for i in range(3)]
            rden = [A.alloc("rden%d" % i, [65, 512], F32) for i in range(2)]
            bcs = [A.alloc("bcs%d" % i, [64, 512], F32) for i in range(2)]
            st = [pes.enter_context(nc.psum_tensor("st%d" % i, [128, 1024], F32)) for i in range(2)]
            ot = [pes.enter_context(nc.psum_tensor("ot%d" % i, [128, 512], F32)) for i in range(2)]
            bc = [pes.enter_context(nc.psum_tensor("bc%d" % i, [64, 512], F32)) for i in range(2)]
            B3 = {k: Buf(k) for k in ["maskb", "sinkf", "sinkb"]}
            stB = [Buf("st%d" % i) for i in range(2)]
            ptB = [Buf("pt%d" % i) for i in range(3)]
            otB = [Buf("ot%d" % i) for i in range(2)]
            bcB = [Buf("bc%d" % i) for i in range(2)]
            rdB = [Buf("rd%d" % i) for i in range(2)]
            bcsB = [Buf("bcs%d" % i) for i in range(2)]
            S.dma(pool, maskb[:], masks, writes=[B3["maskb"]])
            S.dma(sp, sinkf[:], sinkr, writes=[B3["sinkf"]])
            S.op(act, lambda e: e.activation(out=sinkb[:], in_=sinkf[:], func=AF.Exp), reads=[B3["sinkf"]],
                 writes=[B3["sinkb"]])
            iters = []
            for n_ in range(16):
                for kh in range(2):
                    Lt = n_ - 1 if n_ >= 1 else 31
                    Rt = n_ + 1 if n_ <= 14 else 16
                    mL = 0 if n_ >= 1 else 2
                    mR = 1 if n_ <= 14 else 3

                    def kt(j, m=None):
                        return (kaT[:, kh, j * 128:(j + 1) * 128], Bq["kaT"],
                                None if m is None else (maskb[:, m, :], B3["maskb"]),
                                va[:, j, kh * 65:(kh + 1) * 65], Bq["va"])
                    groups = [[kt(Lt, mL), kt(n_)], [kt(Rt, mR), kt(32)], [kt(33)]]
                    qs = slice(n_ * 128, (n_ + 1) * 128)
                    iters.append(dict(
                        q=qaT[:, 4 * kh:4 * kh + 4, qs], qB=Bq["qaT"], groups=groups, scale=A_SCALE,
                        sink=(e64[0:1, 0:65], sinkb[0:1, kh * 512:(kh + 1) * 512], B3["sinkb"]),
                        out=out_aT[:, 4 * kh:4 * kh + 4, qs], outB=B_oa[n_],
                        ovw=lambda ap: ap.rearrange("p (h q) -> p h q", h=4)))
            run_attention(iters, st, stB, ptb, ptB, ot, otB, bc, bcB, rden, rdB, bcs, bcsB)
            if dbg and stage == 2:
                ddump(S, "d_oaT", out_aT[:], [64, 8, 2048], B_oa)
            S.barrier()
        A.release(m_persist)
        if stage == 2:
            return nc, dbg_outs

        out_bT = A.alloc_at("out_bT", [64, 8, 2048], BF16, TOP - 65536)
        A.limit = TOP - 65536
        B_ob = [Buf("ob%d" % i) for i in range(4)]
        m_mla = A.mark()
        with ExitStack() as pes:
            cqT = A.alloc("cqT", [128, 3, 2048], BF16)
            ckvT = A.alloc("ckvT", [128, 2, 4352], BF16)
            krT = A.alloc("krT", [96, 4352], BF16)
            wuq = A.alloc("wuq", [128, 3, 768], BF16)
            wuqp = A.alloc("wuqp", [128, 3, 768], BF16)
            wukv = A.alloc("wukv", [128, 2, 1024], BF16)
            gq_t = A.alloc("gq_t", [128, 3], F32)
            gkv_t = A.alloc("gkv_t", [128, 2], F32)
            m_wst = A.mark()
            wst = A.alloc("wst", [128, 3, 768], F32)
            B4 = {k: Buf(k) for k in ["cqT", "ckvT", "krT", "wst", "wuq", "wuqp", "wukv", "gq", "gkv", "tcq", "tsq", "QT0",
                                      "QT1", "KT0", "KT1", "Vh0", "Vh1", "q1", "q2", "ppr"]}
            S.dma(sp, cqT[:], cq_s, reads=[Bq["cq_s"]], writes=[B4["cqT"]])
            S.dma(sp, ckvT[:], ckv_s, reads=[Bq["ckv_s"]], writes=[B4["ckvT"]])
            S.dma(sp, krT[64:96, :], kr_s[64:96, :], reads=[Bq["kr_s"]], writes=[B4["krT"]])
            S.dma(sp, gq_t[:], gq, writes=[B4["gq"]])
            S.dma(sp, gkv_t[:], gkv, writes=[B4["gkv"]])
            for (src, dstw, dB) in [(w_uq, wuq, "wuq"), (w_uqp, wuqp, "wuqp")]:
                S.dma(sp, wst[:, 0:3, :], src.rearrange("(c p) n -> p c n", p=128), writes=[B4["wst"]])
                for c in range(3):
                    S.op(dve, lambda e, c=c, dstw=dstw: e.tensor_scalar(dstw[:, c, :], wst[:, c, :], gq_t[:, c:c + 1], None,
                                                                        op0=ALU.mult),
                         reads=[B4["wst"], B4["gq"]], writes=[B4[dB]])
            for c in range(2):
                for (c0, c1) in [(0, 768), (768, 1024)]:
                    S.dma(sp, wst[:, 0, 0:c1 - c0], w_ukv[c * 128:(c + 1) * 128, c0:c1], writes=[B4["wst"]])
                    S.op(dve, lambda e, c=c, c0=c0, c1=c1: e.tensor_scalar(wukv[:, c, c0:c1], wst[:, 0, 0:c1 - c0],
                                                                           gkv_t[:, c:c + 1], None, op0=ALU.mult),
                         reads=[B4["wst"], B4["gkv"]], writes=[B4["wukv"]])
            S.barrier()
            A.release(m_wst)
            tcq = A.alloc("tcq", [96, 512], F32)
            tsq = A.alloc("tsq", [96, 512], F32)
            QT = [A.alloc("QT%d" % i, [96, 2048], BF16) for i in range(2)]
            KT = [A.alloc("KT%d" % i, [96, 4352], BF16) for i in range(2)]
            Vh = [A.alloc("Vh%d" % i, [128, 34, 65], BF16) for i in range(2)]
            q1 = A.alloc("q1", [96, 512], F32)
            q2 = A.alloc("q2", [96, 512], F32)
            ptb = [A.alloc("ptb%d" % i, [128, 1024], BF16) for i in range(3)]
            rden = [A.alloc("rden%d" % i, [65, 512], F32) for i in range(1)]
            bcs = [A.alloc("bcs%d" % i, [64, 512], F32) for i in range(1)]
            st = [pes.enter_context(nc.psum_tensor("mst%d" % i, [128, 1024], F32)) for i in range(2)]
            ot = [pes.enter_context(nc.psum_tensor("mot%d" % i, [128, 512], F32)) for i in range(2)]
            bc = [pes.enter_context(nc.psum_tensor("mbc%d" % i, [64, 512], F32)) for i in range(1)]
            ppr = pes.enter_context(nc.psum_tensor("ppr", [128, 512], F32))
            stB = [Buf("st%d" % i) for i in range(2)]
            ptB = [Buf("pt%d" % i) for i in range(3)]
            otB = [Buf("ot%d" % i) for i in range(2)]
            bcB = [Buf("bc%d" % i) for i in range(1)]
            rdB = [Buf("rd%d" % i) for i in range(1)]
            bcsB = [Buf("bcs%d" % i) for i in range(1)]
            for i in range(2):
                S.op(pool, lambda e, i=i: e.memset(Vh[i][:, :, 64:65], 1.0), writes=[B4["Vh%d" % i]])

            def prep_closures(h, hp):
                cl = []
                KTh, KB = KT[hp], B4["KT%d" % hp]
                Vhh, VB = Vh[hp], B4["Vh%d" % hp]
                QTh, QB = QT[hp], B4["QT%d" % hp]

                def kcopy():
                    S.op(dve, lambda e: e.tensor_copy(KTh[64:96, :], krT[64:96, :]), reads=[B4["krT"]], writes=[KB])
                cl.append(kcopy)
                for bi in range(9):
                    ntok = 512 if bi < 8 else 256
                    tok = slice(bi * 512, bi * 512 + ntok)

                    def kblk(tok=tok, ntok=ntok):
                        for c in range(2):
                            S.mm(lambda e, c=c: e.matmul(ppr[0:64, 0:ntok], wukv[:, c, h * 128:h * 128 + 64],
                                                         ckvT[:, c, tok], start=(c == 0), stop=(c == 1)),
                                 reads=[B4["wukv"], B4["ckvT"]], writes=[B4["ppr"]], signal=(c == 1), first=(c == 0))
                        S.op(dve, lambda e: e.tensor_copy(KTh[0:64, tok], ppr[0:64, 0:ntok]), reads=[B4["ppr"]],
                             writes=[KB])
                    cl.append(kblk)
                for g0 in range(0, 34, 8):
                    nt = min(8, 34 - g0)

                    def vblk(g0=g0, nt=nt):
                        for t in range(nt):
                            tsl = slice((g0 + t) * 128, (g0 + t + 1) * 128)
                            for c in range(2):
                                S.mm(lambda e, c=c, t=t, tsl=tsl: e.matmul(
                                    ppr[:, t * 64:(t + 1) * 64], ckvT[:, c, tsl], wukv[:, c, h * 128 + 64:h * 128 + 128],
                                    start=(c == 0), stop=(c == 1)),
                                    reads=[B4["wukv"], B4["ckvT"]], writes=[B4["ppr"]],
                                    signal=(c == 1 and t == nt - 1), first=(c == 0 and t == 0))
                        S.op(dve, lambda e: e.tensor_copy(Vhh[:, g0:g0 + nt, 0:64],
                                                          ppr[:, 0:nt * 64].rearrange("p (t d) -> p t d", t=nt)),
                             reads=[B4["ppr"]], writes=[VB])
                    cl.append(vblk)
                for qb in range(4):
                    tok = slice(qb * 512, (qb + 1) * 512)

                    def qa_(tok=tok):
                        S.dma(sp, tcq[64:96, :], tabc[64:96, tok], writes=[B4["tcq"]])
                        S.dma(sp, tsq[64:96, :], tabs[64:96, tok], writes=[B4["tsq"]])
                        for c in range(3):
                            S.mm(lambda e, c=c: e.matmul(ppr[0:96, :], wuq[:, c, h * 96:(h + 1) * 96], cqT[:, c, tok],
                                                         start=(c == 0), stop=(c == 2)),
                                 reads=[B4["wuq"], B4["cqT"]], writes=[B4["ppr"]], signal=(c == 2), first=(c == 0))
                        S.op(dve, lambda e: e.tensor_copy(QTh[0:64, tok], ppr[0:64, :]), reads=[B4["ppr"]], writes=[QB])
                        S.op(dve, lambda e: e.tensor_tensor(out=q1[64:96, :], in0=ppr[64:96, :], in1=tcq[64:96, :],
                                                            op=ALU.mult), reads=[B4["ppr"], B4["tcq"]], writes=[B4["q1"]])

                    def qb_(tok=tok):
                        for c in range(3):
                            S.mm(lambda e, c=c: e.matmul(ppr[0:96, :], wuqp[:, c, h * 96:(h + 1) * 96], cqT[:, c, tok],
                                                         start=(c == 0), stop=(c == 2)),
                                 reads=[B4["wuqp"], B4["cqT"]], writes=[B4["ppr"]], signal=(c == 2), first=(c == 0))
                        S.op(dve, lambda e: e.tensor_tensor(out=q2[64:96, :], in0=ppr[64:96, :], in1=tsq[64:96, :],
                                                            op=ALU.mult), reads=[B4["ppr"], B4["tsq"]], writes=[B4["q2"]])
                        S.op(dve, lambda e: e.tensor_tensor(out=QTh[64:96, tok], in0=q1[64:96, :], in1=q2[64:96, :],
                                                             op=ALU.add), reads=[B4["q1"], B4["q2"]], writes=[QB])
                    cl.append(qa_)
                    cl.append(qb_)
                return cl

            for f in prep_closures(0, 0):
                f()
            for h in range(8):
                hp = h % 2
                iters = []
                for qb in range(4):
                    tok = slice(qb * 512, (qb + 1) * 512)
                    groups = []
                    for g in range(17):
                        groups.append([(KT[hp][:, j * 128:(j + 1) * 128], B4["KT%d" % hp], None,
                                        Vh[hp][:, j, 0:65], B4["Vh%d" % hp]) for j in (2 * g, 2 * g + 1)])
                    iters.append(dict(q=QT[hp][:, tok], qB=B4["QT%d" % hp], groups=groups, scale=MLA_SCALE, sink=None,
                                      out=out_bT[:, h, tok], outB=B_ob[qb], ovw=lambda ap: ap))
                fl = deque(prep_closures(h + 1, 1 - hp)) if h < 7 else None
                run_attention(iters, st, stB, ptb, ptB, ot, otB, bc, bcB, rden, rdB, bcs, bcsB, fillers=fl, fill_every=2)
            if dbg and stage == 3:
                ddump(S, "d_obT", out_bT[:], [64, 8, 2048], B_ob)
            S.barrier()
        A.release(m_mla)
        if stage == 3:
            return nc, dbg_outs

        h2T = A.alloc("h2T", [128, 8, 2048], BF16)
        Wr = A.alloc("Wr", [128, 16, 32], F32)
        B_h2 = [Buf("h2T%d" % i) for i in range(4)]
        B_wr = [Buf("Wr%d" % i) for i in range(16)]
        B_x1 = [Buf("x1s%d" % i) for i in range(16)]
        m_p5 = A.mark()
        with ExitStack() as pes:
            wo = A.alloc("wo", [64, 16, 1024], BF16)
            wrb = A.alloc("wrb", [128, 8, 32], BF16)
            xt = [A.alloc("xt%d" % i, [128, 1024], F32) for i in range(2)]
            x1t = [A.alloc("x1t%d" % i, [128, 1024], F32) for i in range(2)]
            tt_ = A.alloc("tt5", [128, 1024], F32)
            junk = A.alloc("junk5", [128, 1024], BF16)
            hb = [A.alloc("h2b%d" % i, [128, 1024], BF16) for i in range(2)]
            sm = A.alloc("sm5", [128, 16, 8], F32)
            lg = A.alloc("lg", [128, 512], F32)
            rk = A.alloc("rk", [128, 512], F32)
            mk = A.alloc("mk", [128, 512], F32)
            ex = A.alloc("ex", [128, 512], F32)
            m1 = A.alloc("m1", [128, 16], F32)
            thr = A.alloc("thr", [128, 16], F32)
            br16 = A.alloc("br16", [1, 512], BF16)
            py = [pes.enter_context(nc.psum_tensor("py%d" % i, [128, 1024], F32)) for i in range(2)]
            pT = [pes.enter_context(nc.psum_tensor("pT5_%d" % i, [128, 1024], BF16)) for i in range(2)]
            plg = pes.enter_context(nc.psum_tensor("plg", [128, 512], F32))
            B5 = {k: Buf(k) for k in ["wo", "wrb", "brb", "xt0", "xt1", "x1t0", "x1t1", "tt", "hb0", "hb1", "lg", "rk", "m1", "thr",
                                      "mk", "ex", "py0", "py1", "pT0", "pT1", "plg"]}
            Bsm = [[Buf("sm%d_%d" % (i, j)) for j in range(8)] for i in range(16)]
            S.dma(pool, wo[:], w_o.rearrange("(h p) n -> p h n", p=64), writes=[B5["wo"]])
            S.dma(pool, wrb[:], w_r.rearrange("(c p) n -> p c n", p=128), writes=[B5["wrb"]])
            S.dma(pool, br16[:], b_r16, writes=[B5["brb"]])
            S.mm(lambda e: e.matmul(plg[:], ones_b[0:1, :], br16[0:1, :], start=True, stop=False),
                 reads=[Bc["ones_b"], B5["brb"]], writes=[B5["plg"]], signal=True, first=True)
            for tt in range(16):
                k = tt % 2
                tsl = slice(tt * 128, (tt + 1) * 128)
                y_, yB = py[k], B5["py%d" % k]
                x_, xB = xt[k], B5["xt%d" % k]
                x1_, x1B = x1t[k], B5["x1t%d" % k]
                h_, hbB = hb[k], B5["hb%d" % k]
                pT_, pTB = pT[k], B5["pT%d" % k]
                smt = sm[:, tt, :]
                S.dma(sp, x_[:], xin[tsl, :], writes=[xB])
                for cb in range(2):
                    for h in range(16):
                        src = out_aT if h < 8 else out_bT
                        sB = B_oa[tt] if h < 8 else B_ob[tt // 4]
                        S.mm(lambda e, cb=cb, h=h, src=src: e.matmul(y_[:, cb * 512:(cb + 1) * 512], src[:, h % 8, tsl],
                                                                     wo[:, h, cb * 512:(cb + 1) * 512],
                                                                     start=(h == 0), stop=(h == 15)),
                             reads=[sB, B5["wo"]], writes=[yB], signal=(h == 15 and cb == 1), first=(h == 0 and cb == 0))
                S.op(act, lambda e, y_=y_, tt=tt: e.activation(out=junk[:], in_=y_[:], func=AF.Square,
                                                               accum_out=sm[:, tt, 0:1]), reads=[yB], writes=[Bsm[tt][0]])
                S.op(act, lambda e, tt=tt: e.activation(out=sm[:, tt, 1:2], in_=sm[:, tt, 0:1], func=AF.Ln, bias=EPS,
                                                        scale=1.0 / 1024), reads=[Bsm[tt][0]], writes=[Bsm[tt][1]])
                S.op(act, lambda e, tt=tt: e.activation(out=sm[:, tt, 2:3], in_=sm[:, tt, 1:2], func=AF.Exp, scale=-0.5),
                     reads=[Bsm[tt][1]], writes=[Bsm[tt][2]])
                S.op(dve, lambda e, y_=y_, tt=tt: e.scalar_tensor_tensor(out=tt_[:], in0=y_[:], scalar=sm[:, tt, 2:3],
                                                                         in1=gm_b[:], op0=ALU.mult, op1=ALU.mult),
                     reads=[yB, Bsm[tt][2], Bc["gm_b"]], writes=[B5["tt"]])
                S.op(dve, lambda e, x_=x_, x1_=x1_: e.tensor_tensor(out=x1_[:], in0=tt_[:], in1=x_[:], op=ALU.add),
                     reads=[B5["tt"], xB], writes=[x1B])
                S.dma(sp, x1_s[tsl, :], x1_[:], reads=[x1B], writes=[B_x1[tt]])
                S.op(act, lambda e, x1_=x1_, tt=tt: e.activation(out=junk[:], in_=x1_[:], func=AF.Square,
                                                                 accum_out=sm[:, tt, 3:4]), reads=[x1B], writes=[Bsm[tt][3]])
                S.op(act, lambda e, tt=tt: e.activation(out=sm[:, tt, 4:5], in_=sm[:, tt, 3:4], func=AF.Ln, bias=EPS,
                                                        scale=1.0 / 1024), reads=[Bsm[tt][3]], writes=[Bsm[tt][4]])
                S.op(act, lambda e, tt=tt: e.activation(out=sm[:, tt, 5:6], in_=sm[:, tt, 4:5], func=AF.Exp, scale=-0.5),
                     reads=[Bsm[tt][4]], writes=[Bsm[tt][5]])
                S.op(dve, lambda e, x1_=x1_, tt=tt: e.scalar_tensor_tensor(out=tt_[:], in0=x1_[:], scalar=sm[:, tt, 5:6],
                                                                           in1=gs2_b[:], op0=ALU.mult, op1=ALU.mult),
                     reads=[x1B, Bsm[tt][5], Bc["gs2_b"]], writes=[B5["tt"]])
                S.op(dve, lambda e, h_=h_: e.tensor_tensor(out=h_[:], in0=tt_[:], in1=sh2_b[:], op=ALU.add),
                     reads=[B5["tt"], Bc["sh2_b"]], writes=[hbB])
                for c in range(8):
                    S.mm(lambda e, c=c, h_=h_, pT_=pT_: e.transpose(pT_[:, c * 128:(c + 1) * 128],
                                                                   h_[:, c * 128:(c + 1) * 128], ident_b[:]),
                         reads=[hbB, Bc["ident_b"]], writes=[pTB], signal=(c == 7), first=(c == 0))
                S.op(dve, lambda e, pT_=pT_: e.tensor_copy(h2T[:, :, tsl], pT_[:].rearrange("p (c t) -> p c t", c=8)),
                     reads=[pTB], writes=[B_h2[tt // 4]])
                for c in range(8):
                    S.mm(lambda e, c=c, tt=tt: e.matmul(plg[:, tt * 32:(tt + 1) * 32], h2T[:, c, tsl], wrb[:, c, :],
                                                        start=False, stop=(c == 7 and tt == 15)),
                         reads=[B_h2[tt // 4], B5["wrb"]], writes=[B5["plg"]], signal=(c == 7), first=False)
            v3 = lambda t: t[:].rearrange("p (t x) -> p t x", t=16)
            bc3 = lambda t: t[:].unsqueeze(2).to_broadcast([128, 16, 32])
            S.op(dve, lambda e: e.tensor_copy(lg[:], plg[:]), reads=[B5["plg"]], writes=[B5["lg"]])
            S.op(dve, lambda e: e.tensor_copy(rk[:], lg[:]), reads=[B5["lg"]], writes=[B5["rk"]])
            for kk in range(4):
                mdst = m1 if kk == 0 else thr
                mB = B5["m1"] if kk == 0 else B5["thr"]
                S.op(dve, lambda e, mdst=mdst: e.tensor_reduce(out=mdst[:], in_=v3(rk), axis=mybir.AxisListType.X,
                                                               op=ALU.max), reads=[B5["rk"]], writes=[mB])
                if kk < 3:
                    S.op(dve, lambda e, mdst=mdst: e.tensor_tensor(out=v3(mk), in0=v3(rk), in1=bc3(mdst), op=ALU.is_equal),
                         reads=[B5["rk"], mB], writes=[B5["mk"]])
                    S.op(dve, lambda e: e.scalar_tensor_tensor(out=rk[:], in0=mk[:], scalar=-1.0e9, in1=rk[:],
                                                               op0=ALU.mult, op1=ALU.add),
                         reads=[B5["mk"], B5["rk"]], writes=[B5["rk"]])
            S.op(dve, lambda e: e.tensor_tensor(out=v3(mk), in0=v3(lg), in1=bc3(thr), op=ALU.is_ge),
                 reads=[B5["lg"], B5["thr"]], writes=[B5["mk"]])
            S.op(dve, lambda e: e.tensor_tensor(out=v3(rk), in0=v3(lg), in1=bc3(m1), op=ALU.subtract),
                 reads=[B5["lg"], B5["m1"]], writes=[B5["rk"]])
            S.op(act, lambda e: e.activation(out=ex[:], in_=rk[:], func=AF.Exp), reads=[B5["rk"]], writes=[B5["ex"]])
            S.op(dve, lambda e: e.tensor_tensor(out=ex[:], in0=ex[:], in1=mk[:], op=ALU.mult),
                 reads=[B5["ex"], B5["mk"]], writes=[B5["ex"]])
            S.op(dve, lambda e: e.tensor_reduce(out=thr[:], in_=v3(ex), axis=mybir.AxisListType.X, op=ALU.add),
                 reads=[B5["ex"]], writes=[B5["thr"]])
            S.op(dve, lambda e: e.reciprocal(m1[:], thr[:]), reads=[B5["thr"]], writes=[B5["m1"]])
            S.op(dve, lambda e: e.tensor_tensor(out=Wr[:], in0=v3(ex), in1=bc3(m1), op=ALU.mult),
                 reads=[B5["ex"], B5["m1"]], writes=B_wr)
            if dbg and stage == 4:
                ddump(S, "d_h2T", h2T[:], [128, 8, 2048], B_h2)
                ddump(S, "d_Wr", Wr[:], [128, 16, 32], B_wr, cast=False)
                ddump(S, "d_x1", x1_s, [2048, 1024], B_x1, cast=False)
            S.barrier()
        A.release(m_p5)
        if stage == 4:
            return nc, dbg_outs

        A.limit = TOP
        facc = A.alloc("facc", [128, 16, 1024], F32)
        B_fa = [Buf("fa%d" % i) for i in range(16)]
        m_moe = A.mark()
        with ExitStack() as pes:
            bguT = A.alloc("bguT", [128, 16, 32], F32)
            bg7 = A.alloc("bg7", [128, 8, 32], F32)
            bln = A.alloc("bln", [128, 8, 32], F32)
            sgb = A.alloc("sgb", [128, 1], F32)
            B6 = {k: Buf(k) for k in ["GU", "DN", "bd0", "bd1", "actT0", "actT1", "T1_0", "T1_1", "T2_0", "T2_1", "T3_0",
                                      "T3_1", "bgu_t", "bguT", "pgl0", "pgl1", "pgl2", "pgl3", "po0", "po1"]}
            pgl = [pes.enter_context(nc.psum_tensor("pgl%d" % i, [128, 512], F32)) for i in range(4)]
            po = [pes.enter_context(nc.psum_tensor("po%d" % i, [128, 1024], F32)) for i in range(2)]
            m_bgu = A.mark()
            bgu_t = A.alloc("bgu_t", [32, 2048], F32)
            S.dma(sp, bgu_t[:], b_gu, writes=[B6["bgu_t"]])
            for c in range(16):
                S.mm(lambda e, c=c: e.transpose(pgl[0][:, c * 32:(c + 1) * 32], bgu_t[:, c * 128:(c + 1) * 128],
                                                ident_f[0:32, 0:32]),
                     reads=[B6["bgu_t"], Bc["ident_f"]], writes=[B6["pgl0"]], signal=(c == 15), first=(c == 0))
            S.op(dve, lambda e: e.tensor_copy(bguT[:], pgl[0][:].rearrange("p (c x) -> p c x", c=16)),
                 reads=[B6["pgl0"]], writes=[B6["bguT"]])
            S.op(dve, lambda e: e.tensor_scalar(bg7[:], bguT[:, 0:8, :], -1.0, 7.0, op0=ALU.mult, op1=ALU.add),
                 reads=[B6["bguT"]], writes=[B6["bguT"]])
            S.op(dve, lambda e: e.tensor_scalar(bln[:], bguT[:, 8:16, :], -1.0, None, op0=ALU.mult),
                 reads=[B6["bguT"]], writes=[B6["bguT"]])
            S.op(dve, lambda e: e.memset(sgb[:], 11.914), writes=[B6["bguT"]])
            S.barrier()
            A.release(m_bgu)
            GU = A.alloc("GU", [128, 8, 2048], BF16)
            DN = A.alloc("DN", [128, 8, 1024], BF16)
            bd = [A.alloc("bd%d" % i, [1, 1024], BF16) for i in range(2)]
            actT = [A.alloc("actT%d" % i, [128, 8, 512], BF16) for i in range(2)]
            T1 = [A.alloc("T1_%d" % i, [128, 512], F32) for i in range(2)]
            T2 = [A.alloc("T2_%d" % i, [128, 512], F32) for i in range(2)]
            T3 = [A.alloc("T3_%d" % i, [128, 512], F32) for i in range(2)]
            pair_i = [0]

            def load_gu(e_):
                S.dma(pool, GU[:], w_gu[e_].rearrange("(c p) n -> p c n", p=128), writes=[B6["GU"]])

            def load_dn(e_):
                S.dma(pool, DN[:], w_dn[e_].rearrange("(c p) n -> p c n", p=128), writes=[B6["DN"]])
                S.dma(pool, bd[e_ % 2][:], b_dn[e_:e_ + 1, :], writes=[B6["bd%d" % (e_ % 2)]])

            def gu_step(e_, tb):
                tok = slice(tb * 512, (tb + 1) * 512)
                aT, aB = actT[tb % 2], B6["actT%d" % (tb % 2)]
                for j in range(8):
                    i = pair_i[0] % 2
                    pair_i[0] += 1
                    pg_, pgB = pgl[2 * i], B6["pgl%d" % (2 * i)]
                    pl_, plB = pgl[2 * i + 1], B6["pgl%d" % (2 * i + 1)]
                    for (dst, dB, col0) in [(pg_, pgB, j * 128), (pl_, plB, 1024 + j * 128)]:
                        for c in range(8):
                            S.mm(lambda e, c=c, dst=dst, col0=col0: e.matmul(dst[:], GU[:, c, col0:col0 + 128],
                                                                            h2T[:, c, tok], start=(c == 0), stop=(c == 7)),
                                 reads=[B6["GU"], B_h2[tb]], writes=[dB], signal=(c == 7), first=(c == 0))
                    t1, t2, t3 = T1[i], T2[i], T3[i]
                    b1, b2, b3 = B6["T1_%d" % i], B6["T2_%d" % i], B6["T3_%d" % i]
                    S.op(act, lambda e, t1=t1, pg_=pg_, j=j: e.activation(out=t1[:], in_=pg_[:], func=AF.Relu,
                                                                          bias=bg7[:, j, e_:e_ + 1], scale=-1.0),
                         reads=[pgB, B6["bguT"]], writes=[b1])
                    S.op(act, lambda e, t1=t1, t3=t3: e.activation(out=t3[:], in_=t1[:], func=AF.Sigmoid, bias=sgb[:, 0:1],
                                                                   scale=-1.702),
                         reads=[b1, B6["bguT"]], writes=[b3])
                    S.op(act, lambda e, t2=t2, pl_=pl_, j=j: e.activation(out=t2[:], in_=pl_[:], func=AF.Identity,
                                                                          bias=bln[:, j, e_:e_ + 1], scale=-1.0),
                         reads=[plB, B6["bguT"]], writes=[b2])
                    S.op(dve, lambda e, t2=t2: e.tensor_scalar(t2[:], t2[:], 7.0, -7.0, op0=ALU.min, op1=ALU.max),
                         reads=[b2], writes=[b2])
                    S.op(dve, lambda e, t1=t1, t3=t3: e.scalar_tensor_tensor(out=t1[:], in0=t1[:], scalar=7.0, in1=t3[:],
                                                                             op0=ALU.subtract, op1=ALU.mult),
                         reads=[b1, b3], writes=[b1])
                    S.op(dve, lambda e, t1=t1, t2=t2, j=j: e.scalar_tensor_tensor(out=aT[:, j, :], in0=t2[:], scalar=1.0,
                                                                                  in1=t1[:], op0=ALU.subtract, op1=ALU.mult),
                         reads=[b1, b2], writes=[aB])

            def dn_step(e_, tb):
                aT, aB = actT[tb % 2], B6["actT%d" % (tb % 2)]
                bdt, bdB = bd[e_ % 2], B6["bd%d" % (e_ % 2)]
                for ti in range(4):
                    tt = tb * 4 + ti
                    o_, oB = po[tt % 2], B6["po%d" % (tt % 2)]
                    for cb in range(2):
                        cs = slice(cb * 512, (cb + 1) * 512)
                        for j in range(8):
                            S.mm(lambda e, j=j, cs=cs, ti=ti: e.matmul(o_[:, cs], aT[:, j, ti * 128:(ti + 1) * 128],
                                                                      DN[:, j, cs], start=(j == 0), stop=False),
                                 reads=[aB, B6["DN"]], writes=[oB], signal=False, first=(j == 0 and cb == 0))
                        S.mm(lambda e, cs=cs: e.matmul(o_[:, cs], ones_b[0:1, :], bdt[0:1, cs], start=False, stop=True),
                             reads=[Bc["ones_b"], bdB], writes=[oB], signal=(cb == 1), first=False)
                    if e_ == 0:
                        S.op(dve, lambda e, tt=tt: e.tensor_scalar(facc[:, tt, :], o_[:], Wr[:, tt, e_:e_ + 1], None,
                                                                   op0=ALU.mult),
                             reads=[oB, B_wr[tt]], writes=[B_fa[tt]])
                    else:
                        S.op(dve, lambda e, tt=tt: e.scalar_tensor_tensor(out=facc[:, tt, :], in0=o_[:],
                                                                          scalar=Wr[:, tt, e_:e_ + 1], in1=facc[:, tt, :],
                                                                          op0=ALU.mult, op1=ALU.add),
                             reads=[oB, B_wr[tt], B_fa[tt]], writes=[B_fa[tt]])

            load_gu(0)
            load_dn(0)
            for e_ in range(n_experts):
                gu_step(e_, 0)
                gu_step(e_, 1)
                dn_step(e_, 0)
                gu_step(e_, 2)
                dn_step(e_, 1)
                gu_step(e_, 3)
                if e_ + 1 < n_experts:
                    load_gu(e_ + 1)
                dn_step(e_, 2)
                dn_step(e_, 3)
                if e_ + 1 < n_experts:
                    load_dn(e_ + 1)
            S.barrier()
        A.release(m_moe)

        with ExitStack() as pes:
            x1t = [A.alloc("x1f%d" % i, [128, 1024], F32) for i in range(2)]
            ot_ = [A.alloc("of%d" % i, [128, 1024], F32) for i in range(2)]
            tt_ = A.alloc("tt7", [128, 1024], F32)
            junk = A.alloc("junk7", [128, 1024], BF16)
            sm = A.alloc("sm7", [128, 16, 4], F32)
            B7 = {k: Buf(k) for k in ["x1f0", "x1f1", "of0", "of1", "tt"]}
            Bsm = [[Buf("sm7_%d_%d" % (i, j)) for j in range(3)] for i in range(16)]
            outs = []
            for tt in range(16):
                k = tt % 2
                tsl = slice(tt * 128, (tt + 1) * 128)
                S.dma(sp, x1t[k][:], x1_s[tsl, :], reads=[B_x1[tt]], writes=[B7["x1f%d" % k]])
                S.op(act, lambda e, tt=tt: e.activation(out=junk[:], in_=facc[:, tt, :], func=AF.Square,
                                                        accum_out=sm[:, tt, 0:1]), reads=[B_fa[tt]], writes=[Bsm[tt][0]])
                S.op(act, lambda e, tt=tt: e.activation(out=sm[:, tt, 1:2], in_=sm[:, tt, 0:1], func=AF.Ln, bias=EPS,
                                                        scale=1.0 / 1024), reads=[Bsm[tt][0]], writes=[Bsm[tt][1]])
                S.op(act, lambda e, tt=tt: e.activation(out=sm[:, tt, 2:3], in_=sm[:, tt, 1:2], func=AF.Exp, scale=-0.5),
                     reads=[Bsm[tt][1]], writes=[Bsm[tt][2]])
                S.op(dve, lambda e, tt=tt: e.scalar_tensor_tensor(out=tt_[:], in0=facc[:, tt, :], scalar=sm[:, tt, 2:3],
                                                                  in1=gf_b[:], op0=ALU.mult, op1=ALU.mult),
                     reads=[B_fa[tt], Bsm[tt][2], Bc["gf_b"]], writes=[B7["tt"]])
                S.op(dve, lambda e, k=k: e.tensor_tensor(out=ot_[k][:], in0=tt_[:], in1=x1t[k][:], op=ALU.add),
                     reads=[B7["tt"], B7["x1f%d" % k]], writes=[B7["of%d" % k]])
                outs.append(S.dma(sp, out[tsl, :], ot_[k][:], reads=[B7["of%d" % k]]))
            S.barrier()
        build_program.stats = dict(n_inst=S.n_inst, sbuf_peak=A.peak, auto_sbuf_left=nc.sbuf_bytes_remaining)
    return nc, dbg_outs


def _rope_tables(rot_dim):
    rows = 64
    row = np.repeat(np.arange(rows, dtype=np.float32), 64)
    col = np.tile(np.arange(64, dtype=np.float32), rows)
    quarter = rot_dim // 4
    inv = (np.float32(10000.0) ** (-np.arange(quarter, dtype=np.float32) / np.float32(quarter))).astype(np.float32)
    ang = np.concatenate([row[:, None] * inv, col[:, None] * inv], axis=-1).astype(np.float32)
    return np.cos(ang).astype(np.float32), np.sin(ang).astype(np.float32)


def _const_tables():
    ca, sa = _rope_tables(64)
    cb, sb = _rope_tables(32)
    tc = np.zeros((96, 4096), np.float32)
    ts = np.zeros((96, 4096), np.float32)
    tc[0:32] = ca.T
    tc[32:64] = ca.T
    ts[0:32] = -sa.T
    ts[32:64] = sa.T
    tc[64:80] = cb.T
    tc[80:96] = cb.T
    ts[64:80] = -sb.T
    ts[80:96] = sb.T
    return tc, ts


def _masks(s):
    NEG = -30000.0
    kk = np.arange(128)[:, None]
    ii = np.arange(128)[None, :]
    mL = np.where(ii <= kk, 0.0, NEG).astype(np.float32)
    mR = np.where(kk <= ii, 0.0, NEG).astype(np.float32)
    allneg = np.full((128, 128), NEG, np.float32)
    mLe = allneg if s == 0 else mL
    mRe = mR if s == 0 else allneg
    m = np.stack([np.tile(x, (1, 4)) for x in (mL, mR, mLe, mRe)], axis=1)
    return np.ascontiguousarray(m)


def _swap_halves(w, width):
    n = w.shape[1] // width
    w3 = w.reshape(w.shape[0], n, width)
    h = width // 2
    return np.concatenate([w3[:, :, h:], w3[:, :, :h]], axis=2).reshape(w.shape[0], n * width)


def prep_inputs(I):
    f = lambda a: np.ascontiguousarray(np.asarray(a, dtype=np.float32))
    w_in = f(I["w_in"][0])
    qa, ka = w_in[:, 0:512], w_in[:, 512:640]
    kr = w_in[:, 1408:1440]
    z64 = np.zeros((1024, 64), np.float32)
    w_inx = np.concatenate([w_in[:, 0:1408], z64, kr, z64, _swap_halves(kr, 32), _swap_halves(qa, 64),
                            _swap_halves(ka, 64)], axis=1)
    assert w_inx.shape[1] == C_END
    w_uq = f(I["w_uq"][0])
    wq3 = w_uq.reshape(384, 8, 96)
    w_uqp = np.zeros_like(wq3)
    w_uqp[:, :, 64:96] = _swap_halves(wq3[:, :, 64:96].reshape(384, 256), 32).reshape(384, 8, 32)
    w_uqp = np.ascontiguousarray(w_uqp.reshape(384, 768))
    rows = np.concatenate([f(I["g_mix_pre"][0]), f(I["g_mix_post"][0]), f(I["g_ffn_pre"][0]), f(I["g_ffn_post"][0]),
                           f(I["b_ada"][0])])[None, :]
    tc, ts = _const_tables()
    shared = dict(
        w_ada=f(I["w_ada"][0]), rows=np.ascontiguousarray(rows), w_inx=np.ascontiguousarray(w_inx),
        sinkr=np.ascontiguousarray(np.repeat(f(I["sink"][0]), 128)[None, :]),
        gq=np.ascontiguousarray(f(I["g_q_a"][0]).reshape(3, 128).T), gkv=np.ascontiguousarray(f(I["g_kv_a"][0]).reshape(2, 128).T),
        w_uq=w_uq, w_uqp=w_uqp, w_ukv=f(I["w_ukv"][0]), w_o=f(I["w_o"][0]), w_r=f(I["w_router"][0]),
        b_r16=np.ascontiguousarray(np.tile(f(I["b_router"][0]), 16)[None, :]), w_gu=f(I["w_gate_up"][0]), b_gu=f(I["b_gate_up"][0]), w_dn=f(I["w_down"][0]),
        b_dn=f(I["b_down"][0]), ident=np.eye(128, dtype=np.float32))
    x = np.asarray(I["x"], dtype=np.float32)
    ctx = np.asarray(I["ctx"], dtype=np.float32)
    c = np.asarray(I["c"], dtype=np.float32)
    c_ctx = np.asarray(I["c_ctx"], dtype=np.float32)
    maps = []
    for core in range(8):
        b, s = core // 2, core % 2
        own = x[b, s * 2048:(s + 1) * 2048]
        oth = x[b, (1 - s) * 2048:(2 - s) * 2048]
        xin = np.ascontiguousarray(np.concatenate([own, oth, ctx[b]], axis=0))
        cvec = np.ascontiguousarray(np.concatenate([c[b].reshape(8, 128).T, c_ctx.reshape(8, 128).T], axis=1))
        order = np.concatenate([np.arange(s * 2048, (s + 1) * 2048), np.arange((1 - s) * 2048, (2 - s) * 2048)])
        m = dict(shared)
        m.update(xin=xin, cvec=cvec, tabc=np.ascontiguousarray(tc[:, order]), tabs=np.ascontiguousarray(ts[:, order]),
                 masks=_masks(s))
        maps.append(m)
    return maps


_CACHE = {}


def kernel(**inputs):
    if "nc" not in _CACHE:
        _CACHE["nc"] = build_program()[0]
    nc = _CACHE["nc"]
    maps = prep_inputs(inputs)
    res = run_bass_kernel_spmd(nc, maps, core_ids=list(range(8)))
    outp = np.zeros((4, 4096, 1024), np.float32)
    for core in range(8):
        b, s = core // 2, core % 2
        outp[b, s * 2048:(s + 1) * 2048] = res.results[core]["out"]
    return outp
```

```python
import numpy as np
from contextlib import ExitStack
from collections import deque
import concourse.bass as bass
import concourse.mybir as mybir
from concourse.bass_utils import run_bass_kernel_spmd

F32 = mybir.dt.float32
BF16 = mybir.dt.bfloat16
AF = mybir.ActivationFunctionType
ALU = mybir.AluOpType

import os
DBG_SKIP_ROUTER = os.environ.get("DBG_SKIP_ROUTER") == "1"
EPS = 1e-6
A_SCALE = 64 ** -0.5
MLA_SCALE = 96 ** -0.5
C_QA, C_KA, C_VA, C_CQ, C_CKV, C_KR2, C_QAP, C_KAP, C_END = 0, 512, 640, 768, 1152, 1408, 1600, 2112, 2240


class Buf:
    __slots__ = ("name", "w", "r")

    def __init__(self, name):
        self.name = name
        self.w = None
        self.r = []


class Eng:
    def __init__(self, S, name, obj, is_pe=False, n_dma=0):
        self.name = name
        self.obj = obj
        self.sem = S.new_sem("e_" + name)
        self.count = 0
        self.seen = {}
        self.is_pe = is_pe
        self.pend_r = []
        self.pend_w = []
        self.dma_sems = [[S.new_sem("d_%s%d" % (name, i)), 0] for i in range(n_dma)]
        self.dma_rr = 0


class Sched:
    def __init__(self, nc, es):
        self.nc = nc
        self.es = es
        self.pe = Eng(self, "pe", nc.tensor, is_pe=True)
        self.dve = Eng(self, "dve", nc.vector)
        self.act = Eng(self, "act", nc.scalar)
        self.pool = Eng(self, "pool", nc.gpsimd, n_dma=16)
        self.sp = Eng(self, "sp", nc.sync, n_dma=24)
        self.engs = [self.pe, self.dve, self.act, self.pool, self.sp]
        self.n_inst = 0

    def new_sem(self, name):
        return self.es.enter_context(self.nc.semaphore(name))

    def _wait(self, eng, ev):
        sem, val = ev
        k = id(sem)
        if eng.seen.get(k, 0) >= val:
            return
        eng.obj.wait_ge(sem, val)
        eng.seen[k] = val
        self.n_inst += 1

    def _deps(self, eng, reads, writes):
        for b in reads:
            if b.w is not None and not (eng.is_pe and b.w[0] is eng.sem):
                self._wait(eng, b.w)
        for b in writes:
            if b.w is not None and not (eng.is_pe and b.w[0] is eng.sem):
                self._wait(eng, b.w)
            for ev in b.r:
                if not (eng.is_pe and ev[0] is eng.sem):
                    self._wait(eng, ev)

    def _commit(self, ev, reads, writes):
        for b in reads:
            b.r.append(ev)
            if len(b.r) > 48:
                last = {}
                for e in b.r:
                    k = id(e[0])
                    if k not in last or last[k][1] < e[1]:
                        last[k] = e
                b.r = list(last.values())
        for b in writes:
            b.w = ev
            b.r = []

    def op(self, eng, fn, reads=(), writes=()):
        self._deps(eng, reads, writes)
        ins = fn(eng.obj)
        eng.count += 1
        ev = (eng.sem, eng.count)
        ins.then_inc(eng.sem, 1)
        self._commit(ev, reads, writes)
        self.n_inst += 1
        return ev

    def mm(self, fn, reads=(), writes=(), signal=True, first=True):
        eng = self.pe
        self._deps(eng, reads, writes if first else ())
        ins = fn(eng.obj)
        eng.pend_r.extend(reads)
        for b in writes:
            if b not in eng.pend_w:
                eng.pend_w.append(b)
        self.n_inst += 1
        if signal:
            eng.count += 1
            ev = (eng.sem, eng.count)
            ins.then_inc(eng.sem, 1)
            self._commit(ev, eng.pend_r, eng.pend_w)
            eng.pend_r = []
            eng.pend_w = []
            return ev
        return None

    def dma(self, q, out, in_, reads=(), writes=()):
        slot = q.dma_sems[q.dma_rr % len(q.dma_sems)]
        q.dma_rr += 1
        sem, cur = slot
        if cur > 0:
            self._wait(q, (sem, cur))
        self._deps(q, reads, writes)
        ins = q.obj.dma_start(out=out, in_=in_)
        slot[1] = cur + 16
        ev = (sem, cur + 16)
        ins.then_inc(sem, 16)
        self._commit(ev, reads, writes)
        self.n_inst += 1
        return ev

    def barrier(self):
        assert not self.pe.pend_r and not self.pe.pend_w
        evs = [(e.sem, e.count) for e in self.engs if e.count > 0]
        for e in self.engs:
            evs += [(s, v) for s, v in e.dma_sems if v > 0]
        for e in self.engs:
            for ev in evs:
                if ev[0] is e.sem and e.is_pe:
                    continue
                self._wait(e, ev)


class Arena:
    def __init__(self, nc, base=20480, top=229312):
        self.nc = nc
        self.ptr = base
        self.top = top
        self.n = 0
        self.peak = base
        self.limit = top

    def alloc_at(self, name, shape, dtype, off):
        self.n += 1
        return self.nc.alloc_sbuf_tensor_at("%s_%d" % (name, self.n), list(shape), dtype, offset=off)

    def alloc(self, name, shape, dtype):
        esz = 4 if dtype == F32 else 2
        nbytes = int(np.prod(shape[1:])) * esz
        off = (self.ptr + 31) // 32 * 32
        assert off + nbytes <= self.limit, ("SBUF overflow", name, off, nbytes, self.limit)
        self.ptr = off + nbytes
        self.peak = max(self.peak, self.ptr)
        self.n += 1
        return self.nc.alloc_sbuf_tensor_at("%s_%d" % (name, self.n), list(shape), dtype, offset=off)

    def mark(self):
        return self.ptr

    def release(self, m):
        self.ptr = m


def build_program(stage=99, dbg=False, n_experts=32):
    nc = bass.Bass("TRN2", target_bir_lowering=False)

    def din(name, shape, dt=F32):
        return nc.dram_tensor(name, list(shape), dt, kind="ExternalInput").ap()

    def dout(name, shape, dt=F32):
        return nc.dram_tensor(name, list(shape), dt, kind="ExternalOutput").ap()

    def dscr(name, shape, dt):
        return nc.dram_tensor(name, list(shape), dt, kind="Internal").ap()

    xin = din("xin", [4352, 1024])
    cvec = din("cvec", [128, 16])
    w_ada = din("w_ada", [1024, 6144])
    rows_in = din("rows", [1, 10240])
    w_inx = din("w_inx", [1024, C_END])
    tabc = din("tabc", [96, 4096])
    tabs = din("tabs", [96, 4096])
    masks = din("masks", [128, 4, 512])
    sinkr = din("sinkr", [1, 1024])
    gq = din("gq", [128, 3])
    gkv = din("gkv", [128, 2])
    w_uq = din("w_uq", [384, 768])
    w_uqp = din("w_uqp", [384, 768])
    w_ukv = din("w_ukv", [256, 1024])
    w_o = din("w_o", [1024, 1024])
    w_r = din("w_r", [1024, 32])
    b_r16 = din("b_r16", [1, 512])
    w_gu = din("w_gu", [n_experts, 1024, 2048])
    b_gu = din("b_gu", [32, 2048])
    w_dn = din("w_dn", [n_experts, 1024, 1024])
    b_dn = din("b_dn", [32, 1024])
    ident = din("ident", [128, 128])
    out = dout("out", [2048, 1024])

    cq_s = dscr("cq_s", [128, 3, 2048], BF16)
    ckv_s = dscr("ckv_s", [128, 2, 4352], BF16)
    kr_s = dscr("kr_s", [96, 4352], BF16)
    x1_s = dscr("x1_s", [2048, 1024], F32)

    dbg_outs = {}

    def ddump(S, name, src_ap, shape, reads, cast=True):
        if not dbg:
            return
        o = dout(name, shape)
        dbg_outs[name] = o
        S.dma(S.pool if cast else S.sp, o, src_ap, reads=reads)

    with ExitStack() as es:
        S = Sched(nc, es)
        A = Arena(nc)
        pe, dve, act, pool, sp = S.pe, S.dve, S.act, S.pool, S.sp

        ident_b = A.alloc("ident_b", [128, 128], BF16)
        ident_f = A.alloc("ident_f", [128, 128], F32)
        ones_b = A.alloc("ones_b", [128, 128], BF16)
        ones_f = A.alloc("ones_f", [128, 128], F32)
        e64 = A.alloc("e64", [1, 65], BF16)
        gf_b = A.alloc("gf_b", [128, 1024], F32)
        gm_b = A.alloc("gm_b", [128, 1024], F32)
        gs2_b = A.alloc("gs2_b", [128, 1024], F32)
        sh2_b = A.alloc("sh2_b", [128, 1024], F32)
        Bc = {k: Buf(k) for k in ["ident_b", "ident_f", "ones_b", "ones_f", "e64", "gf_b", "gm_b", "gs2_b", "sh2_b"]}
        S.dma(pool, ident_b[:], ident, writes=[Bc["ident_b"]])
        S.dma(sp, ident_f[:], ident, writes=[Bc["ident_f"]])
        S.op(pool, lambda e: e.memset(ones_b[:], 1.0), writes=[Bc["ones_b"]])
        S.op(pool, lambda e: e.memset(ones_f[:], 1.0), writes=[Bc["ones_f"]])
        S.op(pool, lambda e: e.memset(e64[:], 0.0), writes=[Bc["e64"]])
        S.op(pool, lambda e: e.memset(e64[0:1, 64:65], 1.0), writes=[Bc["e64"]])
        m_persist = A.mark()

        gs1_b = A.alloc("gs1_b", [128, 1024], F32)
        sh1_b = A.alloc("sh1_b", [128, 1024], F32)
        gsc_b = A.alloc("gsc_b", [128, 1024], F32)
        shc_b = A.alloc("shc_b", [128, 1024], F32)
        for k in ["gs1_b", "sh1_b", "gsc_b", "shc_b"]:
            Bc[k] = Buf(k)
        m_bc1 = A.mark()
        with ExitStack() as pes:
            rows_t = A.alloc("rows_t", [1, 10240], F32)
            cv = A.alloc("cv", [128, 16], F32)
            sg = A.alloc("sg", [128, 16], F32)
            sl = A.alloc("sl", [128, 16], F32)
            rep = A.alloc("rep", [128, 16, 128], BF16)
            wa = [A.alloc("wa%d" % i, [128, 8, 512], BF16) for i in range(2)]
            gb = [A.alloc("gb%d" % i, [128, 512], F32) for i in range(2)]
            pm = [pes.enter_context(nc.psum_tensor("pm%d" % i, [128, 512], F32)) for i in range(2)]
            pmc = [pes.enter_context(nc.psum_tensor("pmc%d" % i, [128, 512], F32)) for i in range(2)]
            pg = [pes.enter_context(nc.psum_tensor("pg%d" % i, [128, 512], F32)) for i in range(2)]
            B0 = {k: Buf(k) for k in ["rows", "cv", "sg", "sl", "rep", "wa0", "wa1", "gb0", "gb1", "pm0", "pm1",
                                      "pmc0", "pmc1", "pg0", "pg1"]}
            S.dma(sp, rows_t[:], rows_in, writes=[B0["rows"]])
            S.dma(sp, cv[:], cvec, writes=[B0["cv"]])
            S.op(act, lambda e: e.activation(out=sg[:], in_=cv[:], func=AF.Sigmoid), reads=[B0["cv"]], writes=[B0["sg"]])
            S.op(dve, lambda e: e.tensor_tensor(out=sl[:], in0=cv[:], in1=sg[:], op=ALU.mult),
                 reads=[B0["cv"], B0["sg"]], writes=[B0["sl"]])
            for j in range(16):
                S.op(dve, lambda e, j=j: e.tensor_scalar(rep[:, j, :], ones_f[:], sl[:, j:j + 1], None, op0=ALU.mult),
                     reads=[B0["sl"], Bc["ones_f"]], writes=[B0["rep"]])
            w_ada_v = w_ada.rearrange("(c p) n -> p c n", p=128)
            g_off = {1: 0, 2: 1024, 4: 2048, 5: 3072}
            dests = {0: sh1_b, 1: gs1_b, 2: gm_b, 3: sh2_b, 4: gs2_b, 5: gf_b}
            destB = {0: "sh1_b", 1: "gs1_b", 2: "gm_b", 3: "sh2_b", 4: "gs2_b", 5: "gf_b"}
            for j in range(12):
                m, half = j // 2, j % 2
                k = j % 2
                cs = slice(half * 512, (half + 1) * 512)
                S.dma(pool, wa[k][:], w_ada_v[:, :, j * 512:(j + 1) * 512], writes=[B0["wa%d" % k]])
                vecs = [(0, pm[k], B0["pm%d" % k], dests[m], Bc[destB[m]])]
                if m < 2:
                    vecs.append((8, pmc[k], B0["pmc%d" % k], (shc_b if m == 0 else gsc_b),
                                 Bc["shc_b" if m == 0 else "gsc_b"]))
                if m in g_off:
                    go = g_off[m] + half * 512
                    S.mm(lambda e, k=k, go=go: e.matmul(pg[k][:], ones_f[0:1, :], rows_t[0:1, go:go + 512],
                                                        start=True, stop=True),
                         reads=[Bc["ones_f"], B0["rows"]], writes=[B0["pg%d" % k]])
                    S.op(act, lambda e, k=k: e.copy(gb[k][:], pg[k][:]), reads=[B0["pg%d" % k]], writes=[B0["gb%d" % k]])
                for (v0, pt_, pB, dst, dB) in vecs:
                    for c in range(8):
                        S.mm(lambda e, c=c, v0=v0, pt_=pt_, k=k: e.matmul(pt_[:], rep[:, v0 + c, :], wa[k][:, c, :],
                                                                          start=(c == 0), stop=False),
                             reads=[B0["rep"], B0["wa%d" % k]], writes=[pB], signal=False, first=(c == 0))
                    bo = 4096 + j * 512
                    S.mm(lambda e, pt_=pt_, bo=bo: e.matmul(pt_[:], ones_f[0:1, :], rows_t[0:1, bo:bo + 512],
                                                            start=False, stop=True),
                         reads=[Bc["ones_f"], B0["rows"]], writes=[pB], signal=True, first=False)
                    if m in (0, 3):
                        S.op(act, lambda e, dst=dst, pt_=pt_, cs=cs: e.copy(dst[:, cs], pt_[:]), reads=[pB], writes=[dB])
                    elif m in (1, 4):
                        S.op(dve, lambda e, dst=dst, pt_=pt_, cs=cs, k=k: e.scalar_tensor_tensor(
                            out=dst[:, cs], in0=pt_[:], scalar=1.0, in1=gb[k][:], op0=ALU.add, op1=ALU.mult),
                            reads=[pB, B0["gb%d" % k]], writes=[dB])
                    else:
                        S.op(dve, lambda e, dst=dst, pt_=pt_, cs=cs, k=k: e.tensor_tensor(
                            out=dst[:, cs], in0=pt_[:], in1=gb[k][:], op=ALU.mult),
                            reads=[pB, B0["gb%d" % k]], writes=[dB])
            if dbg and stage == 0:
                for nm, t in [("d_gs1", gs1_b), ("d_sh1", sh1_b), ("d_gsc", gsc_b), ("d_shc", shc_b), ("d_gm", gm_b),
                              ("d_gs2", gs2_b), ("d_sh2", sh2_b), ("d_gf", gf_b)]:
                    ddump(S, nm, t[:], [128, 1024], [Bc[k] for k in Bc], cast=False)
            S.barrier()
        A.release(m_bc1)
        if stage == 0:
            return nc, dbg_outs

        qaT = A.alloc("qaT", [64, 8, 2048], BF16)
        kaT = A.alloc("kaT", [64, 2, 4352], BF16)
        va = A.alloc("va", [128, 34, 130], BF16)
        Bq = {k: Buf(k) for k in ["qaT", "kaT", "va", "cq_s", "ckv_s", "kr_s"]}
        m_attnA = A.mark()
        with ExitStack() as pes:
            W = A.alloc("W", [128, 8, C_END], BF16)
            xt = [A.alloc("xt%d" % i, [128, 1024], F32) for i in range(2)]
            junk = A.alloc("junk", [128, 1024], BF16)
            tt_ = A.alloc("tt_", [128, 1024], F32)
            hb = [A.alloc("hb%d" % i, [128, 1024], BF16) for i in range(2)]
            hTb = [A.alloc("hTb%d" % i, [128, 8, 512], BF16) for i in range(2)]
            tcb = [A.alloc("tcb%d" % i, [96, 512], F32) for i in range(2)]
            tsb = [A.alloc("tsb%d" % i, [96, 512], F32) for i in range(2)]
            r1 = [A.alloc("r1_%d" % i, [96, 512], F32) for i in range(2)]
            r2 = [A.alloc("r2_%d" % i, [96, 512], F32) for i in range(2)]
            ss = A.alloc("ss", [128, 34], F32)
            lnv = A.alloc("lnv", [128, 34], F32)
            rs = A.alloc("rs", [128, 34], F32)
            sq = A.alloc("sq", [128, 3, 512], BF16)
            rl0 = A.alloc("rl0", [128, 512], F32)
            rl1 = A.alloc("rl1", [128, 512], F32)
            cqs = [A.alloc("cqs%d" % i, [128, 3, 512], BF16) for i in range(2)]
            ckvs = [A.alloc("ckvs%d" % i, [128, 2, 512], BF16) for i in range(2)]
            krs = [A.alloc("krs%d" % i, [96, 512], BF16) for i in range(2)]
            pT = [pes.enter_context(nc.psum_tensor("pT%d" % i, [128, 1024], BF16)) for i in range(2)]
            pp = [pes.enter_context(nc.psum_tensor("pp%d" % i, [128, 512], F32)) for i in range(5)]
            pr = pes.enter_context(nc.psum_tensor("pr", [128, 512], F32))
            B1 = {k: Buf(k) for k in ["W", "xt0", "xt1", "tt_", "hb0", "hb1", "hTb0", "hTb1", "tcb0", "tcb1", "tsb0",
                                      "tsb1", "r1_0", "r1_1", "r2_0", "r2_1", "sq", "rl0", "rl1", "cqs0", "cqs1",
                                      "ckvs0", "ckvs1", "krs0", "krs1", "pT0", "pT1", "pp0", "pp1", "pp2", "pp3", "pp4",
                                      "pr"]}
            Bss = [Buf("ss%d" % i) for i in range(34)]
            Bln = [Buf("ln%d" % i) for i in range(34)]
            Brs = [Buf("rs%d" % i) for i in range(34)]
            S.dma(pool, W[:], w_inx.rearrange("(c p) n -> p c n", p=128), writes=[B1["W"]])
            S.op(pool, lambda e: e.memset(va[:, :, 64:65], 1.0), writes=[Bq["va"]])
            S.op(pool, lambda e: e.memset(va[:, :, 129:130], 1.0), writes=[Bq["va"]])
            ppi = [0]
            ropei = [0]

            def next_pp():
                i = ppi[0] % 5
                ppi[0] += 1
                return pp[i], B1["pp%d" % i]

            def projT(dst, dB, col0, M, hT, hB, ntok):
                for c in range(8):
                    S.mm(lambda e, c=c: e.matmul(dst[0:M, 0:ntok], W[:, c, col0:col0 + M], hT[:, c, 0:ntok],
                                                 start=(c == 0), stop=(c == 7)),
                         reads=[B1["W"], hB], writes=[dB], signal=(c == 7), first=(c == 0))

            def rope_evac(pa, pBa, pb, pBb, r0, r1_, ntok, k, dst_ap, dstB):
                i = ropei[0] % 2
                ropei[0] += 1
                S.op(dve, lambda e: e.tensor_tensor(out=r1[i][r0:r1_, 0:ntok], in0=pa[r0:r1_, 0:ntok],
                                                    in1=tcb[k][r0:r1_, 0:ntok], op=ALU.mult),
                     reads=[pBa, B1["tcb%d" % k]], writes=[B1["r1_%d" % i]])
                S.op(dve, lambda e: e.tensor_tensor(out=r2[i][r0:r1_, 0:ntok], in0=pb[r0:r1_, 0:ntok],
                                                    in1=tsb[k][r0:r1_, 0:ntok], op=ALU.mult),
                     reads=[pBb, B1["tsb%d" % k]], writes=[B1["r2_%d" % i]])
                S.op(dve, lambda e: e.tensor_tensor(out=dst_ap, in0=r1[i][r0:r1_, 0:ntok], in1=r2[i][r0:r1_, 0:ntok],
                                                     op=ALU.add),
                     reads=[B1["r1_%d" % i], B1["r2_%d" % i]], writes=[dstB])

            def latent_norm(pcs, nfeat, ntok, dst, dstB):
                nj = len(pcs)
                for j, (pc, pB) in enumerate(pcs):
                    S.op(act, lambda e, j=j, pc=pc: e.activation(out=sq[:, j, 0:ntok], in_=pc[:, 0:ntok], func=AF.Square),
                         reads=[pB], writes=[B1["sq"]])
                for j in range(nj):
                    S.mm(lambda e, j=j: e.matmul(pr[:, 0:ntok], ones_b[:], sq[:, j, 0:ntok], start=(j == 0),
                                                 stop=(j == nj - 1)),
                         reads=[Bc["ones_b"], B1["sq"]], writes=[B1["pr"]], signal=(j == nj - 1), first=(j == 0))
                S.op(act, lambda e: e.activation(out=rl0[:, 0:ntok], in_=pr[:, 0:ntok], func=AF.Ln, bias=EPS,
                                                 scale=1.0 / nfeat), reads=[B1["pr"]], writes=[B1["rl0"]])
                S.op(act, lambda e: e.activation(out=rl1[:, 0:ntok], in_=rl0[:, 0:ntok], func=AF.Exp, scale=-0.5),
                     reads=[B1["rl0"]], writes=[B1["rl1"]])
                for j, (pc, pB) in enumerate(pcs):
                    S.op(dve, lambda e, j=j, pc=pc: e.tensor_tensor(out=dst[:, j, 0:ntok], in0=pc[:, 0:ntok],
                                                                     in1=rl1[:, 0:ntok], op=ALU.mult),
                         reads=[pB, B1["rl1"]], writes=[dstB])

            for bi in range(9):
                ntile = 4 if bi < 8 else 2
                ntok = ntile * 128
                t0 = bi * 4
                k = bi % 2
                hT, hB = hTb[k], B1["hTb%d" % k]
                tok = slice(bi * 512, bi * 512 + ntok)
                if bi < 8:
                    S.dma(sp, tcb[k][:], tabc[:, tok], writes=[B1["tcb%d" % k]])
                    S.dma(sp, tsb[k][:], tabs[:, tok], writes=[B1["tsb%d" % k]])
                gsb, shb = (gs1_b, sh1_b) if bi < 8 else (gsc_b, shc_b)
                gsB, shB = (Bc["gs1_b"], Bc["sh1_b"]) if bi < 8 else (Bc["gsc_b"], Bc["shc_b"])
                for ti in range(ntile):
                    tt = t0 + ti
                    x_, xB = xt[tt % 2], B1["xt%d" % (tt % 2)]
                    h_, hbB = hb[tt % 2], B1["hb%d" % (tt % 2)]
                    pT_, pTB = pT[tt % 2], B1["pT%d" % (tt % 2)]
                    S.dma(sp, x_[:], xin[tt * 128:(tt + 1) * 128, :], writes=[xB])
                    S.op(act, lambda e, x_=x_, tt=tt: e.activation(out=junk[:], in_=x_[:], func=AF.Square,
                                                                   accum_out=ss[:, tt:tt + 1]),
                         reads=[xB], writes=[Bss[tt]])
                    S.op(act, lambda e, tt=tt: e.activation(out=lnv[:, tt:tt + 1], in_=ss[:, tt:tt + 1], func=AF.Ln,
                                                            bias=EPS, scale=1.0 / 1024), reads=[Bss[tt]], writes=[Bln[tt]])
                    S.op(act, lambda e, tt=tt: e.activation(out=rs[:, tt:tt + 1], in_=lnv[:, tt:tt + 1], func=AF.Exp,
                                                            scale=-0.5), reads=[Bln[tt]], writes=[Brs[tt]])
                    S.op(dve, lambda e, x_=x_, tt=tt: e.scalar_tensor_tensor(out=tt_[:], in0=x_[:], scalar=rs[:, tt:tt + 1],
                                                                             in1=gsb[:], op0=ALU.mult, op1=ALU.mult),
                         reads=[xB, Brs[tt], gsB], writes=[B1["tt_"]])
                    S.op(dve, lambda e, h_=h_: e.tensor_tensor(out=h_[:], in0=tt_[:], in1=shb[:], op=ALU.add),
                         reads=[B1["tt_"], shB], writes=[hbB])
                    for c in range(8):
                        S.mm(lambda e, c=c, h_=h_, pT_=pT_: e.transpose(pT_[:, c * 128:(c + 1) * 128],
                                                                       h_[:, c * 128:(c + 1) * 128], ident_b[:]),
                             reads=[hbB, Bc["ident_b"]], writes=[pTB], signal=(c == 7), first=(c == 0))
                    S.op(act, lambda e, pT_=pT_, ti=ti: e.copy(hT[:, :, ti * 128:(ti + 1) * 128],
                                                               pT_[:].rearrange("p (c t) -> p c t", c=8)),
                         reads=[pTB], writes=[hB])
                if bi < 4:
                    for h in range(8):
                        pa, pBa = next_pp()
                        pb, pBb = next_pp()
                        projT(pa, pBa, C_QA + h * 64, 64, hT, hB, ntok)
                        projT(pb, pBb, C_QAP + h * 64, 64, hT, hB, ntok)
                        rope_evac(pa, pBa, pb, pBb, 0, 64, ntok, k, qaT[:, h, tok], Bq["qaT"])
                for kh in range(2):
                    pa, pBa = next_pp()
                    projT(pa, pBa, C_KA + kh * 64, 64, hT, hB, ntok)
                    if bi < 8:
                        pb, pBb = next_pp()
                        projT(pb, pBb, C_KAP + kh * 64, 64, hT, hB, ntok)
                        rope_evac(pa, pBa, pb, pBb, 0, 64, ntok, k, kaT[:, kh, tok], Bq["kaT"])
                    else:
                        S.op(dve, lambda e, pa=pa, kh=kh: e.tensor_copy(kaT[:, kh, tok], pa[0:64, 0:ntok]),
                             reads=[pBa], writes=[Bq["kaT"]])
                pv, pBv = next_pp()
                for ti in range(ntile):
                    for c in range(8):
                        S.mm(lambda e, c=c, ti=ti: e.matmul(pv[:, ti * 128:(ti + 1) * 128],
                                                           hT[:, c, ti * 128:(ti + 1) * 128], W[:, c, C_VA:C_VA + 128],
                                                           start=(c == 0), stop=(c == 7)),
                             reads=[B1["W"], hB], writes=[pBv], signal=(c == 7 and ti == ntile - 1),
                             first=(c == 0 and ti == 0))
                for kh in range(2):
                    S.op(dve, lambda e, kh=kh: e.tensor_copy(
                        va[:, t0:t0 + ntile, kh * 65:kh * 65 + 64],
                        pv[:, 0:ntile * 128].rearrange("p (t x) -> p t x", t=ntile)[:, :, kh * 64:(kh + 1) * 64]),
                        reads=[pBv], writes=[Bq["va"]])
                if bi < 4:
                    pcs = []
                    for j in range(3):
                        pc, pB = next_pp()
                        projT(pc, pB, C_CQ + j * 128, 128, hT, hB, ntok)
                        pcs.append((pc, pB))
                    latent_norm(pcs, 384, ntok, cqs[k], B1["cqs%d" % k])
                    S.dma(sp, cq_s[:, :, tok], cqs[k][:], reads=[B1["cqs%d" % k]], writes=[Bq["cq_s"]])
                pcs = []
                for j in range(2):
                    pc, pB = next_pp()
                    projT(pc, pB, C_CKV + j * 128, 128, hT, hB, ntok)
                    pcs.append((pc, pB))
                latent_norm(pcs, 256, ntok, ckvs[k], B1["ckvs%d" % k])
                S.dma(sp, ckv_s[:, :, tok], ckvs[k][:, :, 0:ntok], reads=[B1["ckvs%d" % k]], writes=[Bq["ckv_s"]])
                pa, pBa = next_pp()
                projT(pa, pBa, C_KR2, 96, hT, hB, ntok)
                if bi < 8:
                    pb, pBb = next_pp()
                    projT(pb, pBb, C_KR2 + 96, 96, hT, hB, ntok)
                    rope_evac(pa, pBa, pb, pBb, 64, 96, ntok, k, krs[k][64:96, 0:ntok], B1["krs%d" % k])
                else:
                    S.op(dve, lambda e, pa=pa: e.tensor_copy(krs[k][64:96, 0:ntok], pa[64:96, 0:ntok]),
                         reads=[pBa], writes=[B1["krs%d" % k]])
                S.dma(sp, kr_s[64:96, tok], krs[k][64:96, 0:ntok], reads=[B1["krs%d" % k]], writes=[Bq["kr_s"]])
            if dbg and stage == 1:
                allB = [Bq[k] for k in Bq]
                ddump(S, "d_qaT", qaT[:], [64, 8, 2048], allB)
                ddump(S, "d_kaT", kaT[:], [64, 2, 4352], allB)
                ddump(S, "d_va", va[:], [128, 34, 130], allB)
                ddump(S, "d_cq", cq_s, [128, 3, 2048], allB)
                ddump(S, "d_ckv", ckv_s, [128, 2, 4352], allB)
                ddump(S, "d_kr", kr_s[64:96, :], [32, 4352], allB)
            S.barrier()
        A.release(m_attnA)
        if stage == 1:
            return nc, dbg_outs

        def run_attention(iters, st, stB, ptb, ptB, ot, otB, bc, bcB, rden, rdB, bcs, bcsB, fillers=None, fill_every=1):
            flat = []
            for ii, it in enumerate(iters):
                ng = len(it["groups"])
                for gi, g in enumerate(it["groups"]):
                    flat.append((ii, gi, gi == ng - 1, g))
            n = len(flat)
            nst, npt, nbc = len(st), len(ptb), len(bc)

            def QK(i):
                ii, gi, last, g = flat[i]
                it = iters[ii]
                s_, sB = st[i % nst], stB[i % nst]
                nmm = sum(1 + (1 if m is not None else 0) for (_, _, m, _, _) in g)
                cnt = 0
                for slot, (kap, kB, mask, vap, vB) in enumerate(g):
                    cnt += 1
                    S.mm(lambda e, slot=slot, kap=kap, it=it, mask=mask: e.matmul(
                        s_[:, slot * 512:(slot + 1) * 512], kap, it["q"], start=True, stop=(mask is None)),
                        reads=[kB, it["qB"]], writes=[sB], signal=(cnt == nmm), first=(cnt == 1))
                    if mask is not None:
                        cnt += 1
                        S.mm(lambda e, slot=slot, mask=mask: e.matmul(s_[:, slot * 512:(slot + 1) * 512], ident_b[:],
                                                                      mask[0], start=False, stop=True),
                             reads=[Bc["ident_b"], mask[1]], writes=[sB], signal=(cnt == nmm), first=False)

            def EXP(i):
                ii, gi, last, g = flat[i]
                it = iters[ii]
                wdt = 512 * len(g)
                s_, sB = st[i % nst], stB[i % nst]
                p_, pB = ptb[i % npt], ptB[i % npt]
                S.op(act, lambda e: e.activation(out=p_[:, 0:wdt], in_=s_[:, 0:wdt], func=AF.Exp, scale=it["scale"]),
                     reads=[sB], writes=[pB])

            def PV(i):
                ii, gi, last, g = flat[i]
                it = iters[ii]
                o_, oB = ot[ii % len(ot)], otB[ii % len(ot)]
                p_, pB = ptb[i % npt], ptB[i % npt]
                for slot, (kap, kB, mask, vap, vB) in enumerate(g):
                    lastmm = last and slot == len(g) - 1 and it.get("sink") is None
                    S.mm(lambda e, slot=slot, vap=vap: e.matmul(o_[0:65, :], vap, p_[:, slot * 512:(slot + 1) * 512],
                                                                start=(gi == 0 and slot == 0), stop=lastmm),
                         reads=[vB, pB], writes=[oB], signal=(slot == len(g) - 1), first=(gi == 0 and slot == 0))
                if last:
                    if it.get("sink") is not None:
                        sl_, sr_, sB_ = it["sink"]
                        S.mm(lambda e: e.matmul(o_[0:65, :], sl_, sr_, start=False, stop=True),
                             reads=[Bc["e64"], sB_], writes=[oB], signal=True, first=False)
                    j = ii % nbc
                    S.op(dve, lambda e: e.reciprocal(rden[j][64:65, :], o_[64:65, :]), reads=[oB], writes=[rdB[j]])
                    S.mm(lambda e: e.matmul(bc[j][0:64, :], ones_f[64:65, 0:64], rden[j][64:65, :], start=True, stop=True),
                         reads=[Bc["ones_f"], rdB[j]], writes=[bcB[j]])
                    S.op(dve, lambda e: e.tensor_copy(bcs[j][:], bc[j][0:64, :]), reads=[bcB[j]], writes=[bcsB[j]])
                    S.op(dve, lambda e: e.tensor_tensor(out=it["out"], in0=it["ovw"](o_[0:64, :]), in1=it["ovw"](bcs[j][:]),
                                                        op=ALU.mult),
                         reads=[oB, bcsB[j]], writes=[it["outB"]])

            for i in range(n + 2):
                if i < n:
                    QK(i)
                    EXP(i)
                if 0 <= i - 2 < n:
                    PV(i - 2)
                if fillers and (i % fill_every == 0):
                    if fillers:
                        fillers.popleft()()
            while fillers:
                fillers.popleft()()

        TOP = 229312
        out_aT = A.alloc_at("out_aT", [64, 8, 2048], BF16, TOP - 32768)
        A.limit = TOP - 32768
        B_oa = [Buf("oa%d" % i) for i in range(16)]
        m_attnB = A.mark()
        with ExitStack() as pes:
            maskb = A.alloc("maskb", [128, 4, 512], BF16)
            sinkf = A.alloc("sinkf", [1, 1024], F32)
            sinkb = A.alloc("sinkb", [1, 1024], BF16)
            ptb = [A.alloc("ptb%d" % i, [128, 1024], BF16) for i in range(3)]
            rden = [A.alloc("rden%d" % i, [65, 512], F32) for i in range(2)]
            bcs = [A.alloc("bcs%d" % i, [64, 512], F32) for i in range(2)]
            st = [pes.enter_context(nc.psum_tensor("st%d" % i, [128, 1024], F32)) for i in range(2)]
            ot = [pes.enter_context(nc.psum_tensor("ot%d" % i, [128, 512], F32)) for i in range(2)]
            bc = [pes.enter_context(nc.psum_tensor("bc%d" % i, [64, 512], F32)) for i in range(2)]
            B3 = {k: Buf(k) for k in ["maskb", "sinkf", "sinkb"]}
            stB = [Buf("st%d" % i) for i in range(2)]
            ptB = [Buf("pt%d" % i) for i in range(3)]
            otB = [Buf("ot%d" % i) for i in range(2)]
            bcB = [Buf("bc%d" % i) for i in range(2)]
            rdB = [Buf("rd%d" % i) for i in range(2)]
            bcsB = [Buf("bcs%d" % i) for i in range(2)]
            S.dma(pool, maskb[:], masks, writes=[B3["maskb"]])
            S.dma(sp, sinkf[:], sinkr, writes=[B3["sinkf"]])
            S.op(act, lambda e: e.activation(out=sinkb[:], in_=sinkf[:], func=AF.Exp), reads=[B3["sinkf"]],
                 writes=[B3["sinkb"]])
            iters = []
            for n_ in range(16):
                for kh in range(2):
                    Lt = n_ - 1 if n_ >= 1 else 31
                    Rt = n_ + 1 if n_ <= 14 else 16
                    mL = 0 if n_ >= 1 else 2
                    mR = 1 if n_ <= 14 else 3

                    def kt(j, m=None):
                        return (kaT[:, kh, j * 128:(j + 1) * 128], Bq["kaT"],
                                None if m is None else (maskb[:, m, :], B3["maskb"]),
                                va[:, j, kh * 65:(kh + 1) * 65], Bq["va"])
                    groups = [[kt(Lt, mL), kt(n_)], [kt(Rt, mR), kt(32)], [kt(33)]]
                    qs = slice(n_ * 128, (n_ + 1) * 128)
                    iters.append(dict(
                        q=qaT[:, 4 * kh:4 * kh + 4, qs], qB=Bq["qaT"], groups=groups, scale=A_SCALE,
                        sink=(e64[0:1, 0:65], sinkb[0:1, kh * 512:(kh + 1) * 512], B3["sinkb"]),
                        out=out_aT[:, 4 * kh:4 * kh + 4, qs], outB=B_oa[n_],
                        ovw=lambda ap: ap.rearrange("p (h q) -> p h q", h=4)))
            run_attention(iters, st, stB, ptb, ptB, ot, otB, bc, bcB, rden, rdB, bcs, bcsB)
            if dbg and stage == 2:
                ddump(S, "d_oaT", out_aT[:], [64, 8, 2048], B_oa)
            S.barrier()
        A.release(m_persist)
        if stage == 2:
            return nc, dbg_outs

        out_bT = A.alloc_at("out_bT", [64, 8, 2048], BF16, TOP - 65536)
        A.limit = TOP - 65536
        B_ob = [Buf("ob%d" % i) for i in range(4)]
        m_mla = A.mark()
        with ExitStack() as pes:
            cqT = A.alloc("cqT", [128, 3, 2048], BF16)
            ckvT = A.alloc("ckvT", [128, 2, 4352], BF16)
            krT = A.alloc("krT", [96, 4352], BF16)
            wuq = A.alloc("wuq", [128, 3, 768], BF16)
            wuqp = A.alloc("wuqp", [128, 3, 768], BF16)
            wukv = A.alloc("wukv", [128, 2, 1024], BF16)
            gq_t = A.alloc("gq_t", [128, 3], F32)
            gkv_t = A.alloc("gkv_t", [128, 2], F32)
            m_wst = A.mark()
            wst = A.alloc("wst", [128, 3, 768], F32)
            B4 = {k: Buf(k) for k in ["cqT", "ckvT", "krT", "wst", "wuq", "wuqp", "wukv", "gq", "gkv", "tcq", "tsq", "QT0",
                                      "QT1", "KT0", "KT1", "Vh0", "Vh1", "q1", "q2", "ppr"]}
            S.dma(sp, cqT[:], cq_s, reads=[Bq["cq_s"]], writes=[B4["cqT"]])
            S.dma(sp, ckvT[:], ckv_s, reads=[Bq["ckv_s"]], writes=[B4["ckvT"]])
            S.dma(sp, krT[64:96, :], kr_s[64:96, :], reads=[Bq["kr_s"]], writes=[B4["krT"]])
            S.dma(sp, gq_t[:], gq, writes=[B4["gq"]])
            S.dma(sp, gkv_t[:], gkv, writes=[B4["gkv"]])
            for (src, dstw, dB) in [(w_uq, wuq, "wuq"), (w_uqp, wuqp, "wuqp")]:
                S.dma(sp, wst[:, 0:3, :], src.rearrange("(c p) n -> p c n", p=128), writes=[B4["wst"]])
                for c in range(3):
                    S.op(dve, lambda e, c=c, dstw=dstw: e.tensor_scalar(dstw[:, c, :], wst[:, c, :], gq_t[:, c:c + 1], None,
                                                                        op0=ALU.mult),
                         reads=[B4["wst"], B4["gq"]], writes=[B4[dB]])
            for c in range(2):
                for (c0, c1) in [(0, 768), (768, 1024)]:
                    S.dma(sp, wst[:, 0, 0:c1 - c0], w_ukv[c * 128:(c + 1) * 128, c0:c1], writes=[B4["wst"]])
                    S.op(dve, lambda e, c=c, c0=c0, c1=c1: e.tensor_scalar(wukv[:, c, c0:c1], wst[:, 0, 0:c1 - c0],
                                                                           gkv_t[:, c:c + 1], None, op0=ALU.mult),
                         reads=[B4["wst"], B4["gkv"]], writes=[B4["wukv"]])
            S.barrier()
            A.release(m_wst)
            tcq = A.alloc("tcq", [96, 512], F32)
            tsq = A.alloc("tsq", [96, 512], F32)
            QT = [A.alloc("QT%d" % i, [96, 2048], BF16) for i in range(2)]
            KT = [A.alloc("KT%d" % i, [96, 4352], BF16) for i in range(2)]
            Vh = [A.alloc("Vh%d" % i, [128, 34, 65], BF16) for i in range(2)]
            q1 = A.alloc("q1", [96, 512], F32)
            q2 = A.alloc("q2", [96, 512], F32)
            ptb = [A.alloc("ptb%d" % i, [128, 1024], BF16) for i in range(3)]
            rden = [A.alloc("rden%d" % i, [65, 512], F32) for i in range(1)]
            bcs = [A.alloc("bcs%d" % i, [64, 512], F32) for i in range(1)]
            st = [pes.enter_context(nc.psum_tensor("mst%d" % i, [128, 1024], F32)) for i in range(2)]
            ot = [pes.enter_context(nc.psum_tensor("mot%d" % i, [128, 512], F32)) for i in range(2)]
            bc = [pes.enter_context(nc.psum_tensor("mbc%d" % i, [64, 512], F32)) for i in range(1)]
            ppr = pes.enter_context(nc.psum_tensor("ppr", [128, 512], F32))
            stB = [Buf("st%d" % i) for i in range(2)]
            ptB = [Buf("pt%d" % i) for i in range(3)]
            otB = [Buf("ot%d" % i) for i in range(2)]
            bcB = [Buf("bc%d" % i) for i in range(1)]
            rdB = [Buf("rd%d" % i) for i in range(1)]
            bcsB = [Buf("bcs%d" % i) for i in range(1)]
            for i in range(2):
                S.op(pool, lambda e, i=i: e.memset(Vh[i][:, :, 64:65], 1.0), writes=[B4["Vh%d" % i]])

            def prep_closures(h, hp):
                cl = []
                KTh, KB = KT[hp], B4["KT%d" % hp]
                Vhh, VB = Vh[hp], B4["Vh%d" % hp]
                QTh, QB = QT[hp], B4["QT%d" % hp]

                def kcopy():
                    S.op(dve, lambda e: e.tensor_copy(KTh[64:96, :], krT[64:96, :]), reads=[B4["krT"]], writes=[KB])
                cl.append(kcopy)
                for bi in range(9):
                    ntok = 512 if bi < 8 else 256
                    tok = slice(bi * 512, bi * 512 + ntok)

                    def kblk(tok=tok, ntok=ntok):
                        for c in range(2):
                            S.mm(lambda e, c=c: e.matmul(ppr[0:64, 0:ntok], wukv[:, c, h * 128:h * 128 + 64],
                                                         ckvT[:, c, tok], start=(c == 0), stop=(c == 1)),
                                 reads=[B4["wukv"], B4["ckvT"]], writes=[B4["ppr"]], signal=(c == 1), first=(c == 0))
                        S.op(dve, lambda e: e.tensor_copy(KTh[0:64, tok], ppr[0:64, 0:ntok]), reads=[B4["ppr"]],
                             writes=[KB])
                    cl.append(kblk)
                for g0 in range(0, 34, 8):
                    nt = min(8, 34 - g0)

                    def vblk(g0=g0, nt=nt):
                        for t in range(nt):
                            tsl = slice((g0 + t) * 128, (g0 + t + 1) * 128)
                            for c in range(2):
                                S.mm(lambda e, c=c, t=t, tsl=tsl: e.matmul(
                                    ppr[:, t * 64:(t + 1) * 64], ckvT[:, c, tsl], wukv[:, c, h * 128 + 64:h * 128 + 128],
                                    start=(c == 0), stop=(c == 1)),
                                    reads=[B4["wukv"], B4["ckvT"]], writes=[B4["ppr"]],
                                    signal=(c == 1 and t == nt - 1), first=(c == 0 and t == 0))
                        S.op(dve, lambda e: e.tensor_copy(Vhh[:, g0:g0 + nt, 0:64],
                                                          ppr[:, 0:nt * 64].rearrange("p (t d) -> p t d", t=nt)),
                             reads=[B4["ppr"]], writes=[VB])
                    cl.append(vblk)
                for qb in range(4):
                    tok = slice(qb * 512, (qb + 1) * 512)

                    def qa_(tok=tok):
                        S.dma(sp, tcq[64:96, :], tabc[64:96, tok], writes=[B4["tcq"]])
                        S.dma(sp, tsq[64:96, :], tabs[64:96, tok], writes=[B4["tsq"]])
                        for c in range(3):
                            S.mm(lambda e, c=c: e.matmul(ppr[0:96, :], wuq[:, c, h * 96:(h + 1) * 96], cqT[:, c, tok],
                                                         start=(c == 0), stop=(c == 2)),
                                 reads=[B4["wuq"], B4["cqT"]], writes=[B4["ppr"]], signal=(c == 2), first=(c == 0))
                        S.op(dve, lambda e: e.tensor_copy(QTh[0:64, tok], ppr[0:64, :]), reads=[B4["ppr"]], writes=[QB])
                        S.op(dve, lambda e: e.tensor_tensor(out=q1[64:96, :], in0=ppr[64:96, :], in1=tcq[64:96, :],
                                                            op=ALU.mult), reads=[B4["ppr"], B4["tcq"]], writes=[B4["q1"]])

                    def qb_(tok=tok):
                        for c in range(3):
                            S.mm(lambda e, c=c: e.matmul(ppr[0:96, :], wuqp[:, c, h * 96:(h + 1) * 96], cqT[:, c, tok],
                                                         start=(c == 0), stop=(c == 2)),
                                 reads=[B4["wuqp"], B4["cqT"]], writes=[B4["ppr"]], signal=(c == 2), first=(c == 0))
                        S.op(dve, lambda e: e.tensor_tensor(out=q2[64:96, :], in0=ppr[64:96, :], in1=tsq[64:96, :],
                                                            op=ALU.mult), reads=[B4["ppr"], B4["tsq"]], writes=[B4["q2"]])
                        S.op(dve, lambda e: e.tensor_tensor(out=QTh[64:96, tok], in0=q1[64:96, :], in1=q2[64:96, :],
                                                             op=ALU.add), reads=[B4["q1"], B4["q2"]], writes=[QB])
                    cl.append(qa_)
                    cl.append(qb_)
                return cl

            for f in prep_closures(0, 0):
                f()
            for h in range(8):
                hp = h % 2
                iters = []
                for qb in range(4):
                    tok = slice(qb * 512, (qb + 1) * 512)
                    groups = []
                    for g in range(17):
                        groups.append([(KT[hp][:, j * 128:(j + 1) * 128], B4["KT%d" % hp], None,
                                        Vh[hp][:, j, 0:65], B4["Vh%d" % hp]) for j in (2 * g, 2 * g + 1)])
                    iters.append(dict(q=QT[hp][:, tok], qB=B4["QT%d" % hp], groups=groups, scale=MLA_SCALE, sink=None,
                                      out=out_bT[:, h, tok], outB=B_ob[qb], ovw=lambda ap: ap))
                fl = deque(prep_closures(h + 1, 1 - hp)) if h < 7 else None
                run_attention(iters, st, stB, ptb, ptB, ot, otB, bc, bcB, rden, rdB, bcs, bcsB, fillers=fl, fill_every=2)
            if dbg and stage == 3:
                ddump(S, "d_obT", out_bT[:], [64, 8, 2048], B_ob)
            S.barrier()
        A.release(m_mla)
        if stage == 3:
            return nc, dbg_outs

        h2T = A.alloc("h2T", [128, 8, 2048], BF16)
        Wr = A.alloc("Wr", [128, 16, 32], F32)
        B_h2 = [Buf("h2T%d" % i) for i in range(4)]
        B_wr = [Buf("Wr%d" % i) for i in range(16)]
        B_x1 = [Buf("x1s%d" % i) for i in range(16)]
        m_p5 = A.mark()
        with ExitStack() as pes:
            wo = A.alloc("wo", [64, 16, 1024], BF16)
            wrb = A.alloc("wrb", [128, 8, 32], BF16)
            xt = [A.alloc("xt%d" % i, [128, 1024], F32) for i in range(2)]
            x1t = [A.alloc("x1t%d" % i, [128, 1024], F32) for i in range(2)]
            tt_ = A.alloc("tt5", [128, 1024], F32)
            junk = A.alloc("junk5", [128, 1024], BF16)
            hb = [A.alloc("h2b%d" % i, [128, 1024], BF16) for i in range(2)]
            sm = A.alloc("sm5", [128, 16, 8], F32)
            lg = A.alloc("lg", [128, 512], F32)
            rk = A.alloc("rk", [128, 512], F32)
            mk = A.alloc("mk", [128, 512], F32)
            ex = A.alloc("ex", [128, 512], F32)
            m1 = A.alloc("m1", [128, 16], F32)
            thr = A.alloc("thr", [128, 16], F32)
            br16 = A.alloc("br16", [1, 512], BF16)
            py = [pes.enter_context(nc.psum_tensor("py%d" % i, [128, 1024], F32)) for i in range(2)]
            pT = [pes.enter_context(nc.psum_tensor("pT5_%d" % i, [128, 1024], BF16)) for i in range(2)]
            plg = pes.enter_context(nc.psum_tensor("plg", [128, 512], F32))
            B5 = {k: Buf(k) for k in ["wo", "wrb", "brb", "xt0", "xt1", "x1t0", "x1t1", "tt", "hb0", "hb1", "lg", "rk", "m1", "thr",
                                      "mk", "ex", "py0", "py1", "pT0", "pT1", "plg"]}
            Bsm = [[Buf("sm%d_%d" % (i, j)) for j in range(8)] for i in range(16)]
            S.dma(pool, wo[:], w_o.rearrange("(h p) n -> p h n", p=64), writes=[B5["wo"]])
            S.dma(pool, wrb[:], w_r.rearrange("(c p) n -> p c n", p=128), writes=[B5["wrb"]])
            S.dma(pool, br16[:], b_r16, writes=[B5["brb"]])
            S.mm(lambda e: e.matmul(plg[:], ones_b[0:1, :], br16[0:1, :], start=True, stop=False),
                 reads=[Bc["ones_b"], B5["brb"]], writes=[B5["plg"]], signal=True, first=True)
            for tt in range(16):
                k = tt % 2
                tsl = slice(tt * 128, (tt + 1) * 128)
                y_, yB = py[k], B5["py%d" % k]
                x_, xB = xt[k], B5["xt%d" % k]
                x1_, x1B = x1t[k], B5["x1t%d" % k]
                h_, hbB = hb[k], B5["hb%d" % k]
                pT_, pTB = pT[k], B5["pT%d" % k]
                smt = sm[:, tt, :]
                S.dma(sp, x_[:], xin[tsl, :], writes=[xB])
                for cb in range(2):
                    for h in range(16):
                        src = out_aT if h < 8 else out_bT
                        sB = B_oa[tt] if h < 8 else B_ob[tt // 4]
                        S.mm(lambda e, cb=cb, h=h, src=src: e.matmul(y_[:, cb * 512:(cb + 1) * 512], src[:, h % 8, tsl],
                                                                     wo[:, h, cb * 512:(cb + 1) * 512],
                                                                     start=(h == 0), stop=(h == 15)),
                             reads=[sB, B5["wo"]], writes=[yB], signal=(h == 15 and cb == 1), first=(h == 0 and cb == 0))
                S.op(act, lambda e, y_=y_, tt=tt: e.activation(out=junk[:], in_=y_[:], func=AF.Square,
                                                               accum_out=sm[:, tt, 0:1]), reads=[yB], writes=[Bsm[tt][0]])
                S.op(act, lambda e, tt=tt: e.activation(out=sm[:, tt, 1:2], in_=sm[:, tt, 0:1], func=AF.Ln, bias=EPS,
                                                        scale=1.0 / 1024), reads=[Bsm[tt][0]], writes=[Bsm[tt][1]])
                S.op(act, lambda e, tt=tt: e.activation(out=sm[:, tt, 2:3], in_=sm[:, tt, 1:2], func=AF.Exp, scale=-0.5),
                     reads=[Bsm[tt][1]], writes=[Bsm[tt][2]])
                S.op(dve, lambda e, y_=y_, tt=tt: e.scalar_tensor_tensor(out=tt_[:], in0=y_[:], scalar=sm[:, tt, 2:3],
                                                                         in1=gm_b[:], op0=ALU.mult, op1=ALU.mult),
                     reads=[yB, Bsm[tt][2], Bc["gm_b"]], writes=[B5["tt"]])
                S.op(dve, lambda e, x_=x_, x1_=x1_: e.tensor_tensor(out=x1_[:], in0=tt_[:], in1=x_[:], op=ALU.add),
                     reads=[B5["tt"], xB], writes=[x1B])
                S.dma(sp, x1_s[tsl, :], x1_[:], reads=[x1B], writes=[B_x1[tt]])
                S.op(act, lambda e, x1_=x1_, tt=tt: e.activation(out=junk[:], in_=x1_[:], func=AF.Square,
                                                                 accum_out=sm[:, tt, 3:4]), reads=[x1B], writes=[Bsm[tt][3]])
                S.op(act, lambda e, tt=tt: e.activation(out=sm[:, tt, 4:5], in_=sm[:, tt, 3:4], func=AF.Ln, bias=EPS,
                                                        scale=1.0 / 1024), reads=[Bsm[tt][3]], writes=[Bsm[tt][4]])
                S.op(act, lambda e, tt=tt: e.activation(out=sm[:, tt, 5:6], in_=sm[:, tt, 4:5], func=AF.Exp, scale=-0.5),
                     reads=[Bsm[tt][4]], writes=[Bsm[tt][5]])
                S.op(dve, lambda e, x1_=x1_, tt=tt: e.scalar_tensor_tensor(out=tt_[:], in0=x1_[:], scalar=sm[:, tt, 5:6],
                                                                           in1=gs2_b[:], op0=ALU.mult, op1=ALU.mult),
                     reads=[x1B, Bsm[tt][5], Bc["gs2_b"]], writes=[B5["tt"]])
                S.op(dve, lambda e, h_=h_: e.tensor_tensor(out=h_[:], in0=tt_[:], in1=sh2_b[:], op=ALU.add),
                     reads=[B5["tt"], Bc["sh2_b"]], writes=[hbB])
                for c in range(8):
                    S.mm(lambda e, c=c, h_=h_, pT_=pT_: e.transpose(pT_[:, c * 128:(c + 1) * 128],
                                                                   h_[:, c * 128:(c + 1) * 128], ident_b[:]),
                         reads=[hbB, Bc["ident_b"]], writes=[pTB], signal=(c == 7), first=(c == 0))
                S.op(dve, lambda e, pT_=pT_: e.tensor_copy(h2T[:, :, tsl], pT_[:].rearrange("p (c t) -> p c t", c=8)),
                     reads=[pTB], writes=[B_h2[tt // 4]])
                for c in range(8):
                    S.mm(lambda e, c=c, tt=tt: e.matmul(plg[:, tt * 32:(tt + 1) * 32], h2T[:, c, tsl], wrb[:, c, :],
                                                        start=False, stop=(c == 7 and tt == 15)),
                         reads=[B_h2[tt // 4], B5["wrb"]], writes=[B5["plg"]], signal=(c == 7), first=False)
            v3 = lambda t: t[:].rearrange("p (t x) -> p t x", t=16)
            bc3 = lambda t: t[:].unsqueeze(2).to_broadcast([128, 16, 32])
            S.op(dve, lambda e: e.tensor_copy(lg[:], plg[:]), reads=[B5["plg"]], writes=[B5["lg"]])
            S.op(dve, lambda e: e.tensor_copy(rk[:], lg[:]), reads=[B5["lg"]], writes=[B5["rk"]])
            for kk in range(4):
                mdst = m1 if kk == 0 else thr
                mB = B5["m1"] if kk == 0 else B5["thr"]
                S.op(dve, lambda e, mdst=mdst: e.tensor_reduce(out=mdst[:], in_=v3(rk), axis=mybir.AxisListType.X,
                                                               op=ALU.max), reads=[B5["rk"]], writes=[mB])
                if kk < 3:
                    S.op(dve, lambda e, mdst=mdst: e.tensor_tensor(out=v3(mk), in0=v3(rk), in1=bc3(mdst), op=ALU.is_equal),
                         reads=[B5["rk"], mB], writes=[B5["mk"]])
                    S.op(dve, lambda e: e.scalar_tensor_tensor(out=rk[:], in0=mk[:], scalar=-1.0e9, in1=rk[:],
                                                               op0=ALU.mult, op1=ALU.add),
                         reads=[B5["mk"], B5["rk"]], writes=[B5["rk"]])
            S.op(dve, lambda e: e.tensor_tensor(out=v3(mk), in0=v3(lg), in1=bc3(thr), op=ALU.is_ge),
                 reads=[B5["lg"], B5["thr"]], writes=[B5["mk"]])
            S.op(dve, lambda e: e.tensor_tensor(out=v3(rk), in0=v3(lg), in1=bc3(m1), op=ALU.subtract),
                 reads=[B5["lg"], B5["m1"]], writes=[B5["rk"]])
            S.op(act, lambda e: e.activation(out=ex[:], in_=rk[:], func=AF.Exp), reads=[B5["rk"]], writes=[B5["ex"]])
            S.op(dve, lambda e: e.tensor_tensor(out=ex[:], in0=ex[:], in1=mk[:], op=ALU.mult),
                 reads=[B5["ex"], B5["mk"]], writes=[B5["ex"]])
            S.op(dve, lambda e: e.tensor_reduce(out=thr[:], in_=v3(ex), axis=mybir.AxisListType.X, op=ALU.add),
                 reads=[B5["ex"]], writes=[B5["thr"]])
            S.op(dve, lambda e: e.reciprocal(m1[:], thr[:]), reads=[B5["thr"]], writes=[B5["m1"]])
            S.op(dve, lambda e: e.tensor_tensor(out=Wr[:], in0=v3(ex), in1=bc3(m1), op=ALU.mult),
                 reads=[B5["ex"], B5["m1"]], writes=B_wr)
            if dbg and stage == 4:
                ddump(S, "d_h2T", h2T[:], [128, 8, 2048], B_h2)
                ddump(S, "d_Wr", Wr[:], [128, 16, 32], B_wr, cast=False)
                ddump(S, "d_x1", x1_s, [2048, 1024], B_x1, cast=False)
            S.barrier()
        A.release(m_p5)
        if stage == 4:
            return nc, dbg_outs

        A.limit = TOP
        facc = A.alloc("facc", [128, 16, 1024], F32)
        B_fa = [Buf("fa%d" % i) for i in range(16)]
        m_moe = A.mark()
        with ExitStack() as pes:
            bguT = A.alloc("bguT", [128, 16, 32], F32)
            bg7 = A.alloc("bg7", [128, 8, 32], F32)
            bln = A.alloc("bln", [128, 8, 32], F32)
            sgb = A.alloc("sgb", [128, 1], F32)
            B6 = {k: Buf(k) for k in ["GUa", "GUb", "DN0", "DN1", "bd0", "bd1", "actT0", "actT1", "T1_0", "T1_1", "T2_0", "T2_1", "T3_0",
                                      "T3_1", "bgu_t", "bguT", "pgl0", "pgl1", "pgl2", "pgl3", "po0", "po1"]}
            pgl = [pes.enter_context(nc.psum_tensor("pgl%d" % i, [128, 512], F32)) for i in range(4)]
            po = [pes.enter_context(nc.psum_tensor("po%d" % i, [128, 1024], F32)) for i in range(2)]
            m_bgu = A.mark()
            bgu_t = A.alloc("bgu_t", [32, 2048], F32)
            S.dma(sp, bgu_t[:], b_gu, writes=[B6["bgu_t"]])
            for c in range(16):
                S.mm(lambda e, c=c: e.transpose(pgl[0][:, c * 32:(c + 1) * 32], bgu_t[:, c * 128:(c + 1) * 128],
                                                ident_f[0:32, 0:32]),
                     reads=[B6["bgu_t"], Bc["ident_f"]], writes=[B6["pgl0"]], signal=(c == 15), first=(c == 0))
            S.op(dve, lambda e: e.tensor_copy(bguT[:], pgl[0][:].rearrange("p (c x) -> p c x", c=16)),
                 reads=[B6["pgl0"]], writes=[B6["bguT"]])
            S.op(dve, lambda e: e.tensor_scalar(bg7[:], bguT[:, 0:8, :], -1.0, 7.0, op0=ALU.mult, op1=ALU.add),
                 reads=[B6["bguT"]], writes=[B6["bguT"]])
            S.op(dve, lambda e: e.tensor_scalar(bln[:], bguT[:, 8:16, :], -1.0, None, op0=ALU.mult),
                 reads=[B6["bguT"]], writes=[B6["bguT"]])
            S.op(dve, lambda e: e.memset(sgb[:], 11.914), writes=[B6["bguT"]])
            S.barrier()
            A.release(m_bgu)
            GU = A.alloc("GU", [128, 8, 2048], BF16)
            DNs = [A.alloc("DN0", [128, 8, 1024], BF16)] * 2
            bd = [A.alloc("bd%d" % i, [1, 1024], BF16) for i in range(2)]
            actT = [A.alloc("actT%d" % i, [128, 8, 512], BF16) for i in range(2)]
            T1 = [A.alloc("T1_%d" % i, [128, 512], F32) for i in range(2)]
            T2 = [A.alloc("T2_%d" % i, [128, 512], F32) for i in range(2)]
            T3 = [A.alloc("T3_%d" % i, [128, 512], F32) for i in range(2)]
            pair_i = [0]

            def load_gu(e_, half):
                wv = w_gu[e_].rearrange("(c p) n -> p c n", p=128)
                hb_ = "GUa" if half == 0 else "GUb"
                for base in (0, 1024):
                    c0 = base + half * 512
                    S.dma(pool, GU[:, :, c0:c0 + 512], wv[:, :, c0:c0 + 512], writes=[B6[hb_]])

            def load_dn(e_):
                S.dma(pool, DNs[e_ % 2][:], w_dn[e_].rearrange("(c p) n -> p c n", p=128), writes=[B6["DN0"]])
                S.dma(pool, bd[e_ % 2][:], b_dn[e_:e_ + 1, :], writes=[B6["bd%d" % (e_ % 2)]])

            def gu_step(e_, tb, mid=None):
                tok = slice(tb * 512, (tb + 1) * 512)
                aT, aB = actT[tb % 2], B6["actT%d" % (tb % 2)]
                for j in range(8):
                    if j == 4 and mid is not None:
                        mid()
                    guB = B6["GUa"] if j < 4 else B6["GUb"]
                    i = pair_i[0] % 2
                    pair_i[0] += 1
                    pg_, pgB = pgl[2 * i], B6["pgl%d" % (2 * i)]
                    pl_, plB = pgl[2 * i + 1], B6["pgl%d" % (2 * i + 1)]
                    for gi_, (dst, dB, col0) in enumerate([(pg_, pgB, j * 128), (pl_, plB, 1024 + j * 128)]):
                        for c in range(8):
                            S.mm(lambda e, c=c, dst=dst, col0=col0: e.matmul(dst[:], GU[:, c, col0:col0 + 128],
                                                                            h2T[:, c, tok], start=(c == 0), stop=(c == 7)),
                                 reads=[guB, B_h2[tb]], writes=[dB], signal=(c == 7 and gi_ == 1), first=(c == 0))
                    t1, t2, t3 = T1[i], T2[i], T3[i]
                    b1, b2, b3 = B6["T1_%d" % i], B6["T2_%d" % i], B6["T3_%d" % i]
                    S.op(act, lambda e, t1=t1, pg_=pg_, j=j: e.activation(out=t1[:], in_=pg_[:], func=AF.Relu,
                                                                          bias=bg7[:, j, e_:e_ + 1], scale=-1.0),
                         reads=[pgB, B6["bguT"]], writes=[b1])
                    S.op(act, lambda e, t1=t1, t3=t3: e.activation(out=t3[:], in_=t1[:], func=AF.Sigmoid, bias=sgb[:, 0:1],
                                                                   scale=-1.702),
                         reads=[b1, B6["bguT"]], writes=[b3])
                    S.op(act, lambda e, t2=t2, pl_=pl_, j=j: e.activation(out=t2[:], in_=pl_[:], func=AF.Identity,
                                                                          bias=bln[:, j, e_:e_ + 1], scale=-1.0),
                         reads=[plB, B6["bguT"]], writes=[b2])
                    S.op(dve, lambda e, t2=t2: e.tensor_scalar(t2[:], t2[:], 7.0, -7.0, op0=ALU.min, op1=ALU.max),
                         reads=[b2], writes=[b2])
                    S.op(dve, lambda e, t1=t1, t3=t3: e.scalar_tensor_tensor(out=t1[:], in0=t1[:], scalar=7.0, in1=t3[:],
                                                                             op0=ALU.subtract, op1=ALU.mult),
                         reads=[b1, b3], writes=[b1])
                    S.op(dve, lambda e, t1=t1, t2=t2, j=j: e.scalar_tensor_tensor(out=aT[:, j, :], in0=t2[:], scalar=1.0,
                                                                                  in1=t1[:], op0=ALU.subtract, op1=ALU.mult),
                         reads=[b1, b2], writes=[aB])

            def dn_step(e_, tb):
                aT, aB = actT[tb % 2], B6["actT%d" % (tb % 2)]
                bdt, bdB = bd[e_ % 2], B6["bd%d" % (e_ % 2)]
                DN, dnB = DNs[0], B6["DN0"]
                for ti in range(4):
                    tt = tb * 4 + ti
                    o_, oB = po[tt % 2], B6["po%d" % (tt % 2)]
                    for cb in range(2):
                        cs = slice(cb * 512, (cb + 1) * 512)
                        for j in range(8):
                            S.mm(lambda e, j=j, cs=cs, ti=ti: e.matmul(o_[:, cs], aT[:, j, ti * 128:(ti + 1) * 128],
                                                                      DN[:, j, cs], start=(j == 0), stop=False),
                                 reads=[aB, dnB], writes=[oB], signal=False, first=(j == 0 and cb == 0))
                        S.mm(lambda e, cs=cs: e.matmul(o_[:, cs], ones_b[0:1, :], bdt[0:1, cs], start=False, stop=True),
                             reads=[Bc["ones_b"], bdB], writes=[oB], signal=(cb == 1), first=False)
                    if e_ == 0:
                        S.op(dve, lambda e, tt=tt: e.tensor_scalar(facc[:, tt, :], o_[:], Wr[:, tt, e_:e_ + 1], None,
                                                                   op0=ALU.mult),
                             reads=[oB, B_wr[tt]], writes=[B_fa[tt]])
                    else:
                        S.op(dve, lambda e, tt=tt: e.scalar_tensor_tensor(out=facc[:, tt, :], in0=o_[:],
                                                                          scalar=Wr[:, tt, e_:e_ + 1], in1=facc[:, tt, :],
                                                                          op0=ALU.mult, op1=ALU.add),
                             reads=[oB, B_wr[tt], B_fa[tt]], writes=[B_fa[tt]])

            load_gu(0, 0)
            load_gu(0, 1)
            load_dn(0)
            for e_ in range(n_experts):
                nxt = e_ + 1 < n_experts
                gu_step(e_, 0)
                gu_step(e_, 1)
                dn_step(e_, 0)
                gu_step(e_, 2)
                dn_step(e_, 1)
                gu_step(e_, 3, mid=(lambda e_=e_: load_gu(e_ + 1, 0)) if nxt else None)
                if nxt:
                    load_gu(e_ + 1, 1)
                dn_step(e_, 2)
                dn_step(e_, 3)
                if nxt:
                    load_dn(e_ + 1)
            S.barrier()
        A.release(m_moe)

        with ExitStack() as pes:
            x1t = [A.alloc("x1f%d" % i, [128, 1024], F32) for i in range(2)]
            ot_ = [A.alloc("of%d" % i, [128, 1024], F32) for i in range(2)]
            tt_ = A.alloc("tt7", [128, 1024], F32)
            junk = A.alloc("junk7", [128, 1024], BF16)
            sm = A.alloc("sm7", [128, 16, 4], F32)
            B7 = {k: Buf(k) for k in ["x1f0", "x1f1", "of0", "of1", "tt"]}
            Bsm = [[Buf("sm7_%d_%d" % (i, j)) for j in range(3)] for i in range(16)]
            outs = []
            for tt in range(16):
                k = tt % 2
                tsl = slice(tt * 128, (tt + 1) * 128)
                S.dma(sp, x1t[k][:], x1_s[tsl, :], reads=[B_x1[tt]], writes=[B7["x1f%d" % k]])
                S.op(act, lambda e, tt=tt: e.activation(out=junk[:], in_=facc[:, tt, :], func=AF.Square,
                                                        accum_out=sm[:, tt, 0:1]), reads=[B_fa[tt]], writes=[Bsm[tt][0]])
                S.op(act, lambda e, tt=tt: e.activation(out=sm[:, tt, 1:2], in_=sm[:, tt, 0:1], func=AF.Ln, bias=EPS,
                                                        scale=1.0 / 1024), reads=[Bsm[tt][0]], writes=[Bsm[tt][1]])
                S.op(act, lambda e, tt=tt: e.activation(out=sm[:, tt, 2:3], in_=sm[:, tt, 1:2], func=AF.Exp, scale=-0.5),
                     reads=[Bsm[tt][1]], writes=[Bsm[tt][2]])
                S.op(dve, lambda e, tt=tt: e.scalar_tensor_tensor(out=tt_[:], in0=facc[:, tt, :], scalar=sm[:, tt, 2:3],
                                                                  in1=gf_b[:], op0=ALU.mult, op1=ALU.mult),
                     reads=[B_fa[tt], Bsm[tt][2], Bc["gf_b"]], writes=[B7["tt"]])
                S.op(dve, lambda e, k=k: e.tensor_tensor(out=ot_[k][:], in0=tt_[:], in1=x1t[k][:], op=ALU.add),
                     reads=[B7["tt"], B7["x1f%d" % k]], writes=[B7["of%d" % k]])
                outs.append(S.dma(sp, out[tsl, :], ot_[k][:], reads=[B7["of%d" % k]]))
            S.barrier()
        build_program.stats = dict(n_inst=S.n_inst, sbuf_peak=A.peak, auto_sbuf_left=nc.sbuf_bytes_remaining)
    return nc, dbg_outs


def _rope_tables(rot_dim):
    rows = 64
    row = np.repeat(np.arange(rows, dtype=np.float32), 64)
    col = np.tile(np.arange(64, dtype=np.float32), rows)
    quarter = rot_dim // 4
    inv = (np.float32(10000.0) ** (-np.arange(quarter, dtype=np.float32) / np.float32(quarter))).astype(np.float32)
    ang = np.concatenate([row[:, None] * inv, col[:, None] * inv], axis=-1).astype(np.float32)
    return np.cos(ang).astype(np.float32), np.sin(ang).astype(np.float32)


def _const_tables():
    ca, sa = _rope_tables(64)
    cb, sb = _rope_tables(32)
    tc = np.zeros((96, 4096), np.float32)
    ts = np.zeros((96, 4096), np.float32)
    tc[0:32] = ca.T
    tc[32:64] = ca.T
    ts[0:32] = -sa.T
    ts[32:64] = sa.T
    tc[64:80] = cb.T
    tc[80:96] = cb.T
    ts[64:80] = -sb.T
    ts[80:96] = sb.T
    return tc, ts


def _masks(s):
    NEG = -30000.0
    kk = np.arange(128)[:, None]
    ii = np.arange(128)[None, :]
    mL = np.where(ii <= kk, 0.0, NEG).astype(np.float32)
    mR = np.where(kk <= ii, 0.0, NEG).astype(np.float32)
    allneg = np.full((128, 128), NEG, np.float32)
    mLe = allneg if s == 0 else mL
    mRe = mR if s == 0 else allneg
    m = np.stack([np.tile(x, (1, 4)) for x in (mL, mR, mLe, mRe)], axis=1)
    return np.ascontiguousarray(m)


def _swap_halves(w, width):
    n = w.shape[1] // width
    w3 = w.reshape(w.shape[0], n, width)
    h = width // 2
    return np.concatenate([w3[:, :, h:], w3[:, :, :h]], axis=2).reshape(w.shape[0], n * width)


def prep_inputs(I):
    f = lambda a: np.ascontiguousarray(np.asarray(a, dtype=np.float32))
    w_in = f(I["w_in"][0])
    qa, ka = w_in[:, 0:512], w_in[:, 512:640]
    kr = w_in[:, 1408:1440]
    z64 = np.zeros((1024, 64), np.float32)
    w_inx = np.concatenate([w_in[:, 0:1408], z64, kr, z64, _swap_halves(kr, 32), _swap_halves(qa, 64),
                            _swap_halves(ka, 64)], axis=1)
    assert w_inx.shape[1] == C_END
    w_uq = f(I["w_uq"][0])
    wq3 = w_uq.reshape(384, 8, 96)
    w_uqp = np.zeros_like(wq3)
    w_uqp[:, :, 64:96] = _swap_halves(wq3[:, :, 64:96].reshape(384, 256), 32).reshape(384, 8, 32)
    w_uqp = np.ascontiguousarray(w_uqp.reshape(384, 768))
    rows = np.concatenate([f(I["g_mix_pre"][0]), f(I["g_mix_post"][0]), f(I["g_ffn_pre"][0]), f(I["g_ffn_post"][0]),
                           f(I["b_ada"][0])])[None, :]
    tc, ts = _const_tables()
    shared = dict(
        w_ada=f(I["w_ada"][0]), rows=np.ascontiguousarray(rows), w_inx=np.ascontiguousarray(w_inx),
        sinkr=np.ascontiguousarray(np.repeat(f(I["sink"][0]), 128)[None, :]),
        gq=np.ascontiguousarray(f(I["g_q_a"][0]).reshape(3, 128).T), gkv=np.ascontiguousarray(f(I["g_kv_a"][0]).reshape(2, 128).T),
        w_uq=w_uq, w_uqp=w_uqp, w_ukv=f(I["w_ukv"][0]), w_o=f(I["w_o"][0]), w_r=f(I["w_router"][0]),
        b_r16=np.ascontiguousarray(np.tile(f(I["b_router"][0]), 16)[None, :]), w_gu=f(I["w_gate_up"][0]), b_gu=f(I["b_gate_up"][0]), w_dn=f(I["w_down"][0]),
        b_dn=f(I["b_down"][0]), ident=np.eye(128, dtype=np.float32))
    x = np.asarray(I["x"], dtype=np.float32)
    ctx = np.asarray(I["ctx"], dtype=np.float32)
    c = np.asarray(I["c"], dtype=np.float32)
    c_ctx = np.asarray(I["c_ctx"], dtype=np.float32)
    maps = []
    for core in range(8):
        b, s = core // 2, core % 2
        own = x[b, s * 2048:(s + 1) * 2048]
        oth = x[b, (1 - s) * 2048:(2 - s) * 2048]
        xin = np.ascontiguousarray(np.concatenate([own, oth, ctx[b]], axis=0))
        cvec = np.ascontiguousarray(np.concatenate([c[b].reshape(8, 128).T, c_ctx.reshape(8, 128).T], axis=1))
        order = np.concatenate([np.arange(s * 2048, (s + 1) * 2048), np.arange((1 - s) * 2048, (2 - s) * 2048)])
        m = dict(shared)
        m.update(xin=xin, cvec=cvec, tabc=np.ascontiguousarray(tc[:, order]), tabs=np.ascontiguousarray(ts[:, order]),
                 masks=_masks(s))
        maps.append(m)
    return maps


_CACHE = {}


def kernel(**inputs):
    if "nc" not in _CACHE:
        _CACHE["nc"] = build_program()[0]
    nc = _CACHE["nc"]
    maps = prep_inputs(inputs)
    res = run_bass_kernel_spmd(nc, maps, core_ids=list(range(8)))
    outp = np.zeros((4, 4096, 1024), np.float32)
    for core in range(8):
        b, s = core // 2, core % 2
        outp[b, s * 2048:(s + 1) * 2048] = res.results[core]["out"]
    return outp
```

```python
import numpy as np
from contextlib import ExitStack
from collections import deque
import concourse.bass as bass
import concourse.mybir as mybir
from concourse.bass_utils import run_bass_kernel_spmd

F32 = mybir.dt.float32
BF16 = mybir.dt.bfloat16
AF = mybir.ActivationFunctionType
ALU = mybir.AluOpType

import os
DBG_SKIP_ROUTER = os.environ.get("DBG_SKIP_ROUTER") == "1"
EPS = 1e-6
A_SCALE = 64 ** -0.5
MLA_SCALE = 96 ** -0.5
C_QA, C_KA, C_VA, C_CQ, C_CKV, C_KR2, C_QAP, C_KAP, C_END = 0, 512, 640, 768, 1152, 1408, 1600, 2112, 2240


class Buf:
    __slots__ = ("name", "w", "r")

    def __init__(self, name):
        self.name = name
        self.w = None
        self.r = []


class Eng:
    def __init__(self, S, name, obj, is_pe=False, n_dma=0):
        self.name = name
        self.obj = obj
        self.sem = S.new_sem("e_" + name)
        self.count = 0
        self.seen = {}
        self.is_pe = is_pe
        self.pend_r = []
        self.pend_w = []
        self.dma_sems = [[S.new_sem("d_%s%d" % (name, i)), 0] for i in range(n_dma)]
        self.dma_rr = 0


class Sched:
    def __init__(self, nc, es):
        self.nc = nc
        self.es = es
        self.pe = Eng(self, "pe", nc.tensor, is_pe=True)
        self.dve = Eng(self, "dve", nc.vector)
        self.act = Eng(self, "act", nc.scalar)
        self.pool = Eng(self, "pool", nc.gpsimd, n_dma=16)
        self.sp = Eng(self, "sp", nc.sync, n_dma=24)
        self.engs = [self.pe, self.dve, self.act, self.pool, self.sp]
        self.n_inst = 0

    def new_sem(self, name):
        return self.es.enter_context(self.nc.semaphore(name))

    def _wait(self, eng, ev):
        sem, val = ev
        k = id(sem)
        if eng.seen.get(k, 0) >= val:
            return
        eng.obj.wait_ge(sem, val)
        eng.seen[k] = val
        self.n_inst += 1

    def _deps(self, eng, reads, writes):
        for b in reads:
            if b.w is not None and not (eng.is_pe and b.w[0] is eng.sem):
                self._wait(eng, b.w)
        for b in writes:
            if b.w is not None and not (eng.is_pe and b.w[0] is eng.sem):
                self._wait(eng, b.w)
            for ev in b.r:
                if not (eng.is_pe and ev[0] is eng.sem):
                    self._wait(eng, ev)

    def _commit(self, ev, reads, writes):
        for b in reads:
            b.r.append(ev)
            if len(b.r) > 48:
                last = {}
                for e in b.r:
                    k = id(e[0])
                    if k not in last or last[k][1] < e[1]:
                        last[k] = e
                b.r = list(last.values())
        for b in writes:
            b.w = ev
            b.r = []

    def op(self, eng, fn, reads=(), writes=()):
        self._deps(eng, reads, writes)
        ins = fn(eng.obj)
        eng.count += 1
        ev = (eng.sem, eng.count)
        ins.then_inc(eng.sem, 1)
        self._commit(ev, reads, writes)
        self.n_inst += 1
        return ev

    def mm(self, fn, reads=(), writes=(), signal=True, first=True):
        eng = self.pe
        self._deps(eng, reads, writes if first else ())
        ins = fn(eng.obj)
        eng.pend_r.extend(reads)
        for b in writes:
            if b not in eng.pend_w:
                eng.pend_w.append(b)
        self.n_inst += 1
        if signal:
            eng.count += 1
            ev = (eng.sem, eng.count)
            ins.then_inc(eng.sem, 1)
            self._commit(ev, eng.pend_r, eng.pend_w)
            eng.pend_r = []
            eng.pend_w = []
            return ev
        return None

    def dma(self, q, out, in_, reads=(), writes=()):
        slot = q.dma_sems[q.dma_rr % len(q.dma_sems)]
        q.dma_rr += 1
        sem, cur = slot
        if cur > 0:
            self._wait(q, (sem, cur))
        self._deps(q, reads, writes)
        ins = q.obj.dma_start(out=out, in_=in_)
        slot[1] = cur + 16
        ev = (sem, cur + 16)
        ins.then_inc(sem, 16)
        self._commit(ev, reads, writes)
        self.n_inst += 1
        return ev

    def barrier(self):
        assert not self.pe.pend_r and not self.pe.pend_w
        evs = [(e.sem, e.count) for e in self.engs if e.count > 0]
        for e in self.engs:
            evs += [(s, v) for s, v in e.dma_sems if v > 0]
        for e in self.engs:
            for ev in evs:
                if ev[0] is e.sem and e.is_pe:
                    continue
                self._wait(e, ev)


class Arena:
    def __init__(self, nc, base=20480, top=229312):
        self.nc = nc
        self.ptr = base
        self.top = top
        self.n = 0
        self.peak = base
        self.limit = top

    def alloc_at(self, name, shape, dtype, off):
        self.n += 1
        return self.nc.alloc_sbuf_tensor_at("%s_%d" % (name, self.n), list(shape), dtype, offset=off)

    def alloc(self, name, shape, dtype):
        esz = 4 if dtype == F32 else 2
        nbytes = int(np.prod(shape[1:])) * esz
        off = (self.ptr + 31) // 32 * 32
        assert off + nbytes <= self.limit, ("SBUF overflow", name, off, nbytes, self.limit)
        self.ptr = off + nbytes
        self.peak = max(self.peak, self.ptr)
        self.n += 1
        return self.nc.alloc_sbuf_tensor_at("%s_%d" % (name, self.n), list(shape), dtype, offset=off)

    def mark(self):
        return self.ptr

    def release(self, m):
        self.ptr = m


def build_program(stage=99, dbg=False, n_experts=32):
    nc = bass.Bass("TRN2", target_bir_lowering=False)

    def din(name, shape, dt=F32):
        return nc.dram_tensor(name, list(shape), dt, kind="ExternalInput").ap()

    def dout(name, shape, dt=F32):
        return nc.dram_tensor(name, list(shape), dt, kind="ExternalOutput").ap()

    def dscr(name, shape, dt):
        return nc.dram_tensor(name, list(shape), dt, kind="Internal").ap()

    xin = din("xin", [4352, 1024])
    cvec = din("cvec", [128, 16])
    w_ada = din("w_ada", [1024, 6144])
    rows_in = din("rows", [1, 10240])
    w_inx = din("w_inx", [1024, C_END])
    tabc = din("tabc", [96, 4096])
    tabs = din("tabs", [96, 4096])
    masks = din("masks", [128, 4, 512])
    sinkr = din("sinkr", [1, 1024])
    gq = din("gq", [128, 3])
    gkv = din("gkv", [128, 2])
    w_uq = din("w_uq", [384, 768])
    w_uqp = din("w_uqp", [384, 768])
    w_ukv = din("w_ukv", [256, 1024])
    w_o = din("w_o", [1024, 1024])
    w_r = din("w_r", [1024, 32])
    b_r16 = din("b_r16", [1, 512])
    w_gu = din("w_gu", [n_experts, 1024, 2048])
    b_gu = din("b_gu", [32, 2048])
    w_dn = din("w_dn", [n_experts, 1024, 1024])
    b_dn = din("b_dn", [32, 1024])
    ident = din("ident", [128, 128])
    out = dout("out", [2048, 1024])

    cq_s = dscr("cq_s", [128, 3, 2048], BF16)
    ckv_s = dscr("ckv_s", [128, 2, 4352], BF16)
    kr_s = dscr("kr_s", [96, 4352], BF16)
    x1_s = dscr("x1_s", [2048, 1024], F32)

    dbg_outs = {}

    def ddump(S, name, src_ap, shape, reads, cast=True):
        if not dbg:
            return
        o = dout(name, shape)
        dbg_outs[name] = o
        S.dma(S.pool if cast else S.sp, o, src_ap, reads=reads)

    with ExitStack() as es:
        S = Sched(nc, es)
        A = Arena(nc)
        pe, dve, act, pool, sp = S.pe, S.dve, S.act, S.pool, S.sp

        ident_b = A.alloc("ident_b", [128, 128], BF16)
        ident_f = A.alloc("ident_f", [128, 128], F32)
        ones_b = A.alloc("ones_b", [128, 128], BF16)
        ones_f = A.alloc("ones_f", [128, 128], F32)
        e64 = A.alloc("e64", [1, 65], BF16)
        gf_b = A.alloc("gf_b", [128, 1024], F32)
        gm_b = A.alloc("gm_b", [128, 1024], F32)
        gs2_b = A.alloc("gs2_b", [128, 1024], F32)
        sh2_b = A.alloc("sh2_b", [128, 1024], F32)
        Bc = {k: Buf(k) for k in ["ident_b", "ident_f", "ones_b", "ones_f", "e64", "gf_b", "gm_b", "gs2_b", "sh2_b"]}
        S.dma(pool, ident_b[:], ident, writes=[Bc["ident_b"]])
        S.dma(sp, ident_f[:], ident, writes=[Bc["ident_f"]])
        S.op(pool, lambda e: e.memset(ones_b[:], 1.0), writes=[Bc["ones_b"]])
        S.op(pool, lambda e: e.memset(ones_f[:], 1.0), writes=[Bc["ones_f"]])
        S.op(pool, lambda e: e.memset(e64[:], 0.0), writes=[Bc["e64"]])
        S.op(pool, lambda e: e.memset(e64[0:1, 64:65], 1.0), writes=[Bc["e64"]])
        m_persist = A.mark()

        gs1_b = A.alloc("gs1_b", [128, 1024], F32)
        sh1_b = A.alloc("sh1_b", [128, 1024], F32)
        gsc_b = A.alloc("gsc_b", [128, 1024], F32)
        shc_b = A.alloc("shc_b", [128, 1024], F32)
        for k in ["gs1_b", "sh1_b", "gsc_b", "shc_b"]:
            Bc[k] = Buf(k)
        m_bc1 = A.mark()
        with ExitStack() as pes:
            rows_t = A.alloc("rows_t", [1, 10240], F32)
            cv = A.alloc("cv", [128, 16], F32)
            sg = A.alloc("sg", [128, 16], F32)
            sl = A.alloc("sl", [128, 16], F32)
            rep = A.alloc("rep", [128, 16, 128], BF16)
            wa = [A.alloc("wa%d" % i, [128, 8, 512], BF16) for i in range(2)]
            gb = [A.alloc("gb%d" % i, [128, 512], F32) for i in range(2)]
            pm = [pes.enter_context(nc.psum_tensor("pm%d" % i, [128, 512], F32)) for i in range(2)]
            pmc = [pes.enter_context(nc.psum_tensor("pmc%d" % i, [128, 512], F32)) for i in range(2)]
            pg = [pes.enter_context(nc.psum_tensor("pg%d" % i, [128, 512], F32)) for i in range(2)]
            B0 = {k: Buf(k) for k in ["rows", "cv", "sg", "sl", "rep", "wa0", "wa1", "gb0", "gb1", "pm0", "pm1",
                                      "pmc0", "pmc1", "pg0", "pg1"]}
            S.dma(sp, rows_t[:], rows_in, writes=[B0["rows"]])
            S.dma(sp, cv[:], cvec, writes=[B0["cv"]])
            S.op(act, lambda e: e.activation(out=sg[:], in_=cv[:], func=AF.Sigmoid), reads=[B0["cv"]], writes=[B0["sg"]])
            S.op(dve, lambda e: e.tensor_tensor(out=sl[:], in0=cv[:], in1=sg[:], op=ALU.mult),
                 reads=[B0["cv"], B0["sg"]], writes=[B0["sl"]])
            for j in range(16):
                S.op(dve, lambda e, j=j: e.tensor_scalar(rep[:, j, :], ones_f[:], sl[:, j:j + 1], None, op0=ALU.mult),
                     reads=[B0["sl"], Bc["ones_f"]], writes=[B0["rep"]])
            w_ada_v = w_ada.rearrange("(c p) n -> p c n", p=128)
            g_off = {1: 0, 2: 1024, 4: 2048, 5: 3072}
            dests = {0: sh1_b, 1: gs1_b, 2: gm_b, 3: sh2_b, 4: gs2_b, 5: gf_b}
            destB = {0: "sh1_b", 1: "gs1_b", 2: "gm_b", 3: "sh2_b", 4: "gs2_b", 5: "gf_b"}
            for j in range(12):
                m, half = j // 2, j % 2
                k = j % 2
                cs = slice(half * 512, (half + 1) * 512)
                S.dma(pool, wa[k][:], w_ada_v[:, :, j * 512:(j + 1) * 512], writes=[B0["wa%d" % k]])
                vecs = [(0, pm[k], B0["pm%d" % k], dests[m], Bc[destB[m]])]
                if m < 2:
                    vecs.append((8, pmc[k], B0["pmc%d" % k], (shc_b if m == 0 else gsc_b),
                                 Bc["shc_b" if m == 0 else "gsc_b"]))
                if m in g_off:
                    go = g_off[m] + half * 512
                    S.mm(lambda e, k=k, go=go: e.matmul(pg[k][:], ones_f[0:1, :], rows_t[0:1, go:go + 512],
                                                        start=True, stop=True),
                         reads=[Bc["ones_f"], B0["rows"]], writes=[B0["pg%d" % k]])
                    S.op(act, lambda e, k=k: e.copy(gb[k][:], pg[k][:]), reads=[B0["pg%d" % k]], writes=[B0["gb%d" % k]])
                for (v0, pt_, pB, dst, dB) in vecs:
                    for c in range(8):
                        S.mm(lambda e, c=c, v0=v0, pt_=pt_, k=k: e.matmul(pt_[:], rep[:, v0 + c, :], wa[k][:, c, :],
                                                                          start=(c == 0), stop=False),
                             reads=[B0["rep"], B0["wa%d" % k]], writes=[pB], signal=False, first=(c == 0))
                    bo = 4096 + j * 512
                    S.mm(lambda e, pt_=pt_, bo=bo: e.matmul(pt_[:], ones_f[0:1, :], rows_t[0:1, bo:bo + 512],
                                                            start=False, stop=True),
                         reads=[Bc["ones_f"], B0["rows"]], writes=[pB], signal=True, first=False)
                    if m in (0, 3):
                        S.op(act, lambda e, dst=dst, pt_=pt_, cs=cs: e.copy(dst[:, cs], pt_[:]), reads=[pB], writes=[dB])
                    elif m in (1, 4):
                        S.op(dve, lambda e, dst=dst, pt_=pt_, cs=cs, k=k: e.scalar_tensor_tensor(
                            out=dst[:, cs], in0=pt_[:], scalar=1.0, in1=gb[k][:], op0=ALU.add, op1=ALU.mult),
                            reads=[pB, B0["gb%d" % k]], writes=[dB])
                    else:
                        S.op(dve, lambda e, dst=dst, pt_=pt_, cs=cs, k=k: e.tensor_tensor(
                            out=dst[:, cs], in0=pt_[:], in1=gb[k][:], op=ALU.mult),
                            reads=[pB, B0["gb%d" % k]], writes=[dB])
            if dbg and stage == 0:
                for nm, t in [("d_gs1", gs1_b), ("d_sh1", sh1_b), ("d_gsc", gsc_b), ("d_shc", shc_b), ("d_gm", gm_b),
                              ("d_gs2", gs2_b), ("d_sh2", sh2_b), ("d_gf", gf_b)]:
                    ddump(S, nm, t[:], [128, 1024], [Bc[k] for k in Bc], cast=False)
            S.barrier()
        A.release(m_bc1)
        if stage == 0:
            return nc, dbg_outs

        qaT = A.alloc("qaT", [64, 8, 2048], BF16)
        kaT = A.alloc("kaT", [64, 2, 4352], BF16)
        va = A.alloc("va", [128, 34, 130], BF16)
        Bq = {k: Buf(k) for k in ["qaT", "kaT", "va", "cq_s", "ckv_s", "kr_s"]}
        m_attnA = A.mark()
        with ExitStack() as pes:
            W = A.alloc("W", [128, 8, C_END], BF16)
            xt = [A.alloc("xt%d" % i, [128, 1024], F32) for i in range(2)]
            junk = A.alloc("junk", [128, 1024], BF16)
            tt_ = A.alloc("tt_", [128, 1024], F32)
            hb = [A.alloc("hb%d" % i, [128, 1024], BF16) for i in range(2)]
            hTb = [A.alloc("hTb%d" % i, [128, 8, 512], BF16) for i in range(2)]
            tcb = [A.alloc("tcb%d" % i, [96, 512], F32) for i in range(2)]
            tsb = [A.alloc("tsb%d" % i, [96, 512], F32) for i in range(2)]
            r1 = [A.alloc("r1_%d" % i, [96, 512], F32) for i in range(2)]
            r2 = [A.alloc("r2_%d" % i, [96, 512], F32) for i in range(2)]
            ss = A.alloc("ss", [128, 34], F32)
            lnv = A.alloc("lnv", [128, 34], F32)
            rs = A.alloc("rs", [128, 34], F32)
            sq = A.alloc("sq", [128, 3, 512], BF16)
            rl0 = A.alloc("rl0", [128, 512], F32)
            rl1 = A.alloc("rl1", [128, 512], F32)
            cqs = [A.alloc("cqs%d" % i, [128, 3, 512], BF16) for i in range(2)]
            ckvs = [A.alloc("ckvs%d" % i, [128, 2, 512], BF16) for i in range(2)]
            krs = [A.alloc("krs%d" % i, [96, 512], BF16) for i in range(2)]
            pT = [pes.enter_context(nc.psum_tensor("pT%d" % i, [128, 1024], BF16)) for i in range(2)]
            pp = [pes.enter_context(nc.psum_tensor("pp%d" % i, [128, 512], F32)) for i in range(5)]
            pr = pes.enter_context(nc.psum_tensor("pr", [128, 512], F32))
            B1 = {k: Buf(k) for k in ["W", "xt0", "xt1", "tt_", "hb0", "hb1", "hTb0", "hTb1", "tcb0", "tcb1", "tsb0",
                                      "tsb1", "r1_0", "r1_1", "r2_0", "r2_1", "sq", "rl0", "rl1", "cqs0", "cqs1",
                                      "ckvs0", "ckvs1", "krs0", "krs1", "pT0", "pT1", "pp0", "pp1", "pp2", "pp3", "pp4",
                                      "pr"]}
            Bss = [Buf("ss%d" % i) for i in range(34)]
            Bln = [Buf("ln%d" % i) for i in range(34)]
            Brs = [Buf("rs%d" % i) for i in range(34)]
            S.dma(pool, W[:], w_inx.rearrange("(c p) n -> p c n", p=128), writes=[B1["W"]])
            S.op(pool, lambda e: e.memset(va[:, :, 64:65], 1.0), writes=[Bq["va"]])
            S.op(pool, lambda e: e.memset(va[:, :, 129:130], 1.0), writes=[Bq["va"]])
            ppi = [0]
            ropei = [0]

            def next_pp():
                i = ppi[0] % 5
                ppi[0] += 1
                return pp[i], B1["pp%d" % i]

            def projT(dst, dB, col0, M, hT, hB, ntok):
                for c in range(8):
                    S.mm(lambda e, c=c: e.matmul(dst[0:M, 0:ntok], W[:, c, col0:col0 + M], hT[:, c, 0:ntok],
                                                 start=(c == 0), stop=(c == 7)),
                         reads=[B1["W"], hB], writes=[dB], signal=(c == 7), first=(c == 0))

            def rope_evac(pa, pBa, pb, pBb, r0, r1_, ntok, k, dst_ap, dstB):
                i = ropei[0] % 2
                ropei[0] += 1
                S.op(dve, lambda e: e.tensor_tensor(out=r1[i][r0:r1_, 0:ntok], in0=pa[r0:r1_, 0:ntok],
                                                    in1=tcb[k][r0:r1_, 0:ntok], op=ALU.mult),
                     reads=[pBa, B1["tcb%d" % k]], writes=[B1["r1_%d" % i]])
                S.op(dve, lambda e: e.tensor_tensor(out=r2[i][r0:r1_, 0:ntok], in0=pb[r0:r1_, 0:ntok],
                                                    in1=tsb[k][r0:r1_, 0:ntok], op=ALU.mult),
                     reads=[pBb, B1["tsb%d" % k]], writes=[B1["r2_%d" % i]])
                S.op(dve, lambda e: e.tensor_tensor(out=dst_ap, in0=r1[i][r0:r1_, 0:ntok], in1=r2[i][r0:r1_, 0:ntok],
                                                     op=ALU.add),
                     reads=[B1["r1_%d" % i], B1["r2_%d" % i]], writes=[dstB])

            def latent_norm(pcs, nfeat, ntok, dst, dstB):
                nj = len(pcs)
                for j, (pc, pB) in enumerate(pcs):
                    S.op(act, lambda e, j=j, pc=pc: e.activation(out=sq[:, j, 0:ntok], in_=pc[:, 0:ntok], func=AF.Square),
                         reads=[pB], writes=[B1["sq"]])
                for j in range(nj):
                    S.mm(lambda e, j=j: e.matmul(pr[:, 0:ntok], ones_b[:], sq[:, j, 0:ntok], start=(j == 0),
                                                 stop=(j == nj - 1)),
                         reads=[Bc["ones_b"], B1["sq"]], writes=[B1["pr"]], signal=(j == nj - 1), first=(j == 0))
                S.op(act, lambda e: e.activation(out=rl0[:, 0:ntok], in_=pr[:, 0:ntok], func=AF.Ln, bias=EPS,
                                                 scale=1.0 / nfeat), reads=[B1["pr"]], writes=[B1["rl0"]])
                S.op(act, lambda e: e.activation(out=rl1[:, 0:ntok], in_=rl0[:, 0:ntok], func=AF.Exp, scale=-0.5),
                     reads=[B1["rl0"]], writes=[B1["rl1"]])
                for j, (pc, pB) in enumerate(pcs):
                    S.op(dve, lambda e, j=j, pc=pc: e.tensor_tensor(out=dst[:, j, 0:ntok], in0=pc[:, 0:ntok],
                                                                     in1=rl1[:, 0:ntok], op=ALU.mult),
                         reads=[pB, B1["rl1"]], writes=[dstB])

            for bi in range(9):
                ntile = 4 if bi < 8 else 2
                ntok = ntile * 128
                t0 = bi * 4
                k = bi % 2
                hT, hB = hTb[k], B1["hTb%d" % k]
                tok = slice(bi * 512, bi * 512 + ntok)
                if bi < 8:
                    S.dma(sp, tcb[k][:], tabc[:, tok], writes=[B1["tcb%d" % k]])
                    S.dma(sp, tsb[k][:], tabs[:, tok], writes=[B1["tsb%d" % k]])
                gsb, shb = (gs1_b, sh1_b) if bi < 8 else (gsc_b, shc_b)
                gsB, shB = (Bc["gs1_b"], Bc["sh1_b"]) if bi < 8 else (Bc["gsc_b"], Bc["shc_b"])
                for ti in range(ntile):
                    tt = t0 + ti
                    x_, xB = xt[tt % 2], B1["xt%d" % (tt % 2)]
                    h_, hbB = hb[tt % 2], B1["hb%d" % (tt % 2)]
                    pT_, pTB = pT[tt % 2], B1["pT%d" % (tt % 2)]
                    S.dma(sp, x_[:], xin[tt * 128:(tt + 1) * 128, :], writes=[xB])
                    S.op(act, lambda e, x_=x_, tt=tt: e.activation(out=junk[:], in_=x_[:], func=AF.Square,
                                                                   accum_out=ss[:, tt:tt + 1]),
                         reads=[xB], writes=[Bss[tt]])
                    S.op(act, lambda e, tt=tt: e.activation(out=lnv[:, tt:tt + 1], in_=ss[:, tt:tt + 1], func=AF.Ln,
                                                            bias=EPS, scale=1.0 / 1024), reads=[Bss[tt]], writes=[Bln[tt]])
                    S.op(act, lambda e, tt=tt: e.activation(out=rs[:, tt:tt + 1], in_=lnv[:, tt:tt + 1], func=AF.Exp,
                                                            scale=-0.5), reads=[Bln[tt]], writes=[Brs[tt]])
                    S.op(dve, lambda e, x_=x_, tt=tt: e.scalar_tensor_tensor(out=tt_[:], in0=x_[:], scalar=rs[:, tt:tt + 1],
                                                                             in1=gsb[:], op0=ALU.mult, op1=ALU.mult),
                         reads=[xB, Brs[tt], gsB], writes=[B1["tt_"]])
                    S.op(dve, lambda e, h_=h_: e.tensor_tensor(out=h_[:], in0=tt_[:], in1=shb[:], op=ALU.add),
                         reads=[B1["tt_"], shB], writes=[hbB])
                    for c in range(8):
                        S.mm(lambda e, c=c, h_=h_, pT_=pT_: e.transpose(pT_[:, c * 128:(c + 1) * 128],
                                                                       h_[:, c * 128:(c + 1) * 128], ident_b[:]),
                             reads=[hbB, Bc["ident_b"]], writes=[pTB], signal=(c == 7), first=(c == 0))
                    S.op(act, lambda e, pT_=pT_, ti=ti: e.copy(hT[:, :, ti * 128:(ti + 1) * 128],
                                                               pT_[:].rearrange("p (c t) -> p c t", c=8)),
                         reads=[pTB], writes=[hB])
                if bi < 4:
                    for h in range(8):
                        pa, pBa = next_pp()
                        pb, pBb = next_pp()
                        projT(pa, pBa, C_QA + h * 64, 64, hT, hB, ntok)
                        projT(pb, pBb, C_QAP + h * 64, 64, hT, hB, ntok)
                        rope_evac(pa, pBa, pb, pBb, 0, 64, ntok, k, qaT[:, h, tok], Bq["qaT"])
                for kh in range(2):
                    pa, pBa = next_pp()
                    projT(pa, pBa, C_KA + kh * 64, 64, hT, hB, ntok)
                    if bi < 8:
                        pb, pBb = next_pp()
                        projT(pb, pBb, C_KAP + kh * 64, 64, hT, hB, ntok)
                        rope_evac(pa, pBa, pb, pBb, 0, 64, ntok, k, kaT[:, kh, tok], Bq["kaT"])
                    else:
                        S.op(dve, lambda e, pa=pa, kh=kh: e.tensor_copy(kaT[:, kh, tok], pa[0:64, 0:ntok]),
                             reads=[pBa], writes=[Bq["kaT"]])
                pv, pBv = next_pp()
                for ti in range(ntile):
                    for c in range(8):
                        S.mm(lambda e, c=c, ti=ti: e.matmul(pv[:, ti * 128:(ti + 1) * 128],
                                                           hT[:, c, ti * 128:(ti + 1) * 128], W[:, c, C_VA:C_VA + 128],
                                                           start=(c == 0), stop=(c == 7)),
                             reads=[B1["W"], hB], writes=[pBv], signal=(c == 7 and ti == ntile - 1),
                             first=(c == 0 and ti == 0))
                for kh in range(2):
                    S.op(dve, lambda e, kh=kh: e.tensor_copy(
                        va[:, t0:t0 + ntile, kh * 65:kh * 65 + 64],
                        pv[:, 0:ntile * 128].rearrange("p (t x) -> p t x", t=ntile)[:, :, kh * 64:(kh + 1) * 64]),
                        reads=[pBv], writes=[Bq["va"]])
                if bi < 4:
                    pcs = []
                    for j in range(3):
                        pc, pB = next_pp()
                        projT(pc, pB, C_CQ + j * 128, 128, hT, hB, ntok)
                        pcs.append((pc, pB))
                    latent_norm(pcs, 384, ntok, cqs[k], B1["cqs%d" % k])
                    S.dma(sp, cq_s[:, :, tok], cqs[k][:], reads=[B1["cqs%d" % k]], writes=[Bq["cq_s"]])
                pcs = []
                for j in range(2):
                    pc, pB = next_pp()
                    projT(pc, pB, C_CKV + j * 128, 128, hT, hB, ntok)
                    pcs.append((pc, pB))
                latent_norm(pcs, 256, ntok, ckvs[k], B1["ckvs%d" % k])
                S.dma(sp, ckv_s[:, :, tok], ckvs[k][:, :, 0:ntok], reads=[B1["ckvs%d" % k]], writes=[Bq["ckv_s"]])
                pa, pBa = next_pp()
                projT(pa, pBa, C_KR2, 96, hT, hB, ntok)
                if bi < 8:
                    pb, pBb = next_pp()
                    projT(pb, pBb, C_KR2 + 96, 96, hT, hB, ntok)
                    rope_evac(pa, pBa, pb, pBb, 64, 96, ntok, k, krs[k][64:96, 0:ntok], B1["krs%d" % k])
                else:
                    S.op(dve, lambda e, pa=pa: e.tensor_copy(krs[k][64:96, 0:ntok], pa[64:96, 0:ntok]),
                         reads=[pBa], writes=[B1["krs%d" % k]])
                S.dma(sp, kr_s[64:96, tok], krs[k][64:96, 0:ntok], reads=[B1["krs%d" % k]], writes=[Bq["kr_s"]])
            if dbg and stage == 1:
                allB = [Bq[k] for k in Bq]
                ddump(S, "d_qaT", qaT[:], [64, 8, 2048], allB)
                ddump(S, "d_kaT", kaT[:], [64, 2, 4352], allB)
                ddump(S, "d_va", va[:], [128, 34, 130], allB)
                ddump(S, "d_cq", cq_s, [128, 3, 2048], allB)
                ddump(S, "d_ckv", ckv_s, [128, 2, 4352], allB)
                ddump(S, "d_kr", kr_s[64:96, :], [32, 4352], allB)
            S.barrier()
        A.release(m_attnA)
        if stage == 1:
            return nc, dbg_outs

        def run_attention(iters, st, stB, ptb, ptB, ot, otB, bc, bcB, rden, rdB, bcs, bcsB, fillers=None, fill_every=1):
            flat = []
            for ii, it in enumerate(iters):
                ng = len(it["groups"])
                for gi, g in enumerate(it["groups"]):
                    flat.append((ii, gi, gi == ng - 1, g))
            n = len(flat)
            nst, npt, nbc = len(st), len(ptb), len(bc)

            def QK(i):
                ii, gi, last, g = flat[i]
                it = iters[ii]
                s_, sB = st[i % nst], stB[i % nst]
                nmm = sum(1 + (1 if m is not None else 0) for (_, _, m, _, _) in g)
                cnt = 0
                for slot, (kap, kB, mask, vap, vB) in enumerate(g):
                    cnt += 1
                    S.mm(lambda e, slot=slot, kap=kap, it=it, mask=mask: e.matmul(
                        s_[:, slot * 512:(slot + 1) * 512], kap, it["q"], start=True, stop=(mask is None)),
                        reads=[kB, it["qB"]], writes=[sB], signal=(cnt == nmm), first=(cnt == 1))
                    if mask is not None:
                        cnt += 1
                        S.mm(lambda e, slot=slot, mask=mask: e.matmul(s_[:, slot * 512:(slot + 1) * 512], ident_b[:],
                                                                      mask[0], start=False, stop=True),
                             reads=[Bc["ident_b"], mask[1]], writes=[sB], signal=(cnt == nmm), first=False)

            def EXP(i):
                ii, gi, last, g = flat[i]
                it = iters[ii]
                wdt = 512 * len(g)
                s_, sB = st[i % nst], stB[i % nst]
                p_, pB = ptb[i % npt], ptB[i % npt]
                S.op(act, lambda e: e.activation(out=p_[:, 0:wdt], in_=s_[:, 0:wdt], func=AF.Exp, scale=it["scale"]),
                     reads=[sB], writes=[pB])

            def PV(i):
                ii, gi, last, g = flat[i]
                it = iters[ii]
                o_, oB = ot[ii % len(ot)], otB[ii % len(ot)]
                p_, pB = ptb[i % npt], ptB[i % npt]
                for slot, (kap, kB, mask, vap, vB) in enumerate(g):
                    lastmm = last and slot == len(g) - 1 and it.get("sink") is None
                    S.mm(lambda e, slot=slot, vap=vap: e.matmul(o_[0:65, :], vap, p_[:, slot * 512:(slot + 1) * 512],
                                                                start=(gi == 0 and slot == 0), stop=lastmm),
                         reads=[vB, pB], writes=[oB], signal=(slot == len(g) - 1), first=(gi == 0 and slot == 0))
                if last:
                    if it.get("sink") is not None:
                        sl_, sr_, sB_ = it["sink"]
                        S.mm(lambda e: e.matmul(o_[0:65, :], sl_, sr_, start=False, stop=True),
                             reads=[Bc["e64"], sB_], writes=[oB], signal=True, first=False)
                    j = ii % nbc
                    S.op(dve, lambda e: e.reciprocal(rden[j][64:65, :], o_[64:65, :]), reads=[oB], writes=[rdB[j]])
                    S.mm(lambda e: e.matmul(bc[j][0:64, :], ones_f[64:65, 0:64], rden[j][64:65, :], start=True, stop=True),
                         reads=[Bc["ones_f"], rdB[j]], writes=[bcB[j]])
                    S.op(dve, lambda e: e.tensor_copy(bcs[j][:], bc[j][0:64, :]), reads=[bcB[j]], writes=[bcsB[j]])
                    S.op(dve, lambda e: e.tensor_tensor(out=it["out"], in0=it["ovw"](o_[0:64, :]), in1=it["ovw"](bcs[j][:]),
                                                        op=ALU.mult),
                         reads=[oB, bcsB[j]], writes=[it["outB"]])

            for i in range(n + 2):
                if i < n:
                    QK(i)
                    EXP(i)
                if 0 <= i - 2 < n:
                    PV(i - 2)
                if fillers and (i % fill_every == 0):
                    if fillers:
                        fillers.popleft()()
            while fillers:
                fillers.popleft()()

        TOP = 229312
        out_aT = A.alloc_at("out_aT", [64, 8, 2048], BF16, TOP - 32768)
        A.limit = TOP - 32768
        B_oa = [Buf("oa%d" % i) for i in range(16)]
        m_attnB = A.mark()
        with ExitStack() as pes:
            maskb = A.alloc("maskb", [128, 4, 512], BF16)
            sinkf = A.alloc("sinkf", [1, 1024], F32)
            sinkb = A.alloc("sinkb", [1, 1024], BF16)
            ptb = [A.alloc("ptb%d" % i, [128, 1024], BF16) for i in range(3)]
            rden = [A.alloc("rden%d" % i, [65, 512], F32) for i in range(2)]
            bcs = [A.alloc("bcs%d" % i, [64, 512], F32) for i in range(2)]
            st = [pes.enter_context(nc.psum_tensor("st%d" % i, [128, 1024], F32)) for i in range(2)]
            ot = [pes.enter_context(nc.psum_tensor("ot%d" % i, [128, 512], F32)) for i in range(2)]
            bc = [pes.enter_context(nc.psum_tensor("bc%d" % i, [64, 512], F32)) for i in range(2)]
            B3 = {k: Buf(k) for k in ["maskb", "sinkf", "sinkb"]}
            stB = [Buf("st%d" % i) for i in range(2)]
            ptB = [Buf("pt%d" % i) for i in range(3)]
            otB = [Buf("ot%d" % i) for i in range(2)]
            bcB = [Buf("bc%d" % i) for i in range(2)]
            rdB = [Buf("rd%d" % i) for i in range(2)]
            bcsB = [Buf("bcs%d" % i) for i in range(2)]
            S.dma(pool, maskb[:], masks, writes=[B3["maskb"]])
            S.dma(sp, sinkf[:], sinkr, writes=[B3["sinkf"]])
            S.op(act, lambda e: e.activation(out=sinkb[:], in_=sinkf[:], func=AF.Exp), reads=[B3["sinkf"]],
                 writes=[B3["sinkb"]])
            iters = []
            for n_ in range(16):
                for kh in range(2):
                    Lt = n_ - 1 if n_ >= 1 else 31
                    Rt = n_ + 1 if n_ <= 14 else 16
                    mL = 0 if n_ >= 1 else 2
                    mR = 1 if n_ <= 14 else 3

                    def kt(j, m=None):
                        return (kaT[:, kh, j * 128:(j + 1) * 128], Bq["kaT"],
                                None if m is None else (maskb[:, m, :], B3["maskb"]),
                                va[:, j, kh * 65:(kh + 1) * 65], Bq["va"])
                    groups = [[kt(Lt, mL), kt(n_)], [kt(Rt, mR), kt(32)], [kt(33)]]
                    qs = slice(n_ * 128, (n_ + 1) * 128)
                    iters.append(dict(
                        q=qaT[:, 4 * kh:4 * kh + 4, qs], qB=Bq["qaT"], groups=groups, scale=A_SCALE,
                        sink=(e64[0:1, 0:65], sinkb[0:1, kh * 512:(kh + 1) * 512], B3["sinkb"]),
                        out=out_aT[:, 4 * kh:4 * kh + 4, qs], outB=B_oa[n_],
                        ovw=lambda ap: ap.rearrange("p (h q) -> p h q", h=4)))
            run_attention(iters, st, stB, ptb, ptB, ot, otB, bc, bcB, rden, rdB, bcs, bcsB)
            if dbg and stage == 2:
                ddump(S, "d_oaT", out_aT[:], [64, 8, 2048], B_oa)
            S.barrier()
        A.release(m_persist)
        if stage == 2:
            return nc, dbg_outs

        out_bT = A.alloc_at("out_bT", [64, 8, 2048], BF16, TOP - 65536)
        A.limit = TOP - 65536
        B_ob = [Buf("ob%d" % i) for i in range(4)]
        m_mla = A.mark()
        with ExitStack() as pes:
            cqT = A.alloc("cqT", [128, 3, 2048], BF16)
            ckvT = A.alloc("ckvT", [128, 2, 4352], BF16)
            krT = A.alloc("krT", [96, 4352], BF16)
            wuq = A.alloc("wuq", [128, 3, 768], BF16)
            wuqp = A.alloc("wuqp", [128, 3, 768], BF16)
            wukv = A.alloc("wukv", [128, 2, 1024], BF16)
            gq_t = A.alloc("gq_t", [128, 3], F32)
            gkv_t = A.alloc("gkv_t", [128, 2], F32)
            m_wst = A.mark()
            wst = A.alloc("wst", [128, 3, 768], F32)
            B4 = {k: Buf(k) for k in ["cqT", "ckvT", "krT", "wst", "wuq", "wuqp", "wukv", "gq", "gkv", "tcq", "tsq", "QT0",
                                      "QT1", "KT0", "KT1", "Vh0", "Vh1", "q1", "q2", "ppr"]}
            S.dma(sp, cqT[:], cq_s, reads=[Bq["cq_s"]], writes=[B4["cqT"]])
            S.dma(sp, ckvT[:], ckv_s, reads=[Bq["ckv_s"]], writes=[B4["ckvT"]])
            S.dma(sp, krT[64:96, :], kr_s[64:96, :], reads=[Bq["kr_s"]], writes=[B4["krT"]])
            S.dma(sp, gq_t[:], gq, writes=[B4["gq"]])
            S.dma(sp, gkv_t[:], gkv, writes=[B4["gkv"]])
            for (src, dstw, dB) in [(w_uq, wuq, "wuq"), (w_uqp, wuqp, "wuqp")]:
                S.dma(sp, wst[:, 0:3, :], src.rearrange("(c p) n -> p c n", p=128), writes=[B4["wst"]])
                for c in range(3):
                    S.op(dve, lambda e, c=c, dstw=dstw: e.tensor_scalar(dstw[:, c, :], wst[:, c, :], gq_t[:, c:c + 1], None,
                                                                        op0=ALU.mult),
                         reads=[B4["wst"], B4["gq"]], writes=[B4[dB]])
            for c in range(2):
                for (c0, c1) in [(0, 768), (768, 1024)]:
                    S.dma(sp, wst[:, 0, 0:c1 - c0], w_ukv[c * 128:(c + 1) * 128, c0:c1], writes=[B4["wst"]])
                    S.op(dve, lambda e, c=c, c0=c0, c1=c1: e.tensor_scalar(wukv[:, c, c0:c1], wst[:, 0, 0:c1 - c0],
                                                                           gkv_t[:, c:c + 1], None, op0=ALU.mult),
                         reads=[B4["wst"], B4["gkv"]], writes=[B4["wukv"]])
            S.barrier()
            A.release(m_wst)
            tcq = A.alloc("tcq", [96, 512], F32)
            tsq = A.alloc("tsq", [96, 512], F32)
            QT = [A.alloc("QT%d" % i, [96, 2048], BF16) for i in range(2)]
            KT = [A.alloc("KT%d" % i, [96, 4352], BF16) for i in range(2)]
            Vh = [A.alloc("Vh%d" % i, [128, 34, 65], BF16) for i in range(2)]
            q1 = A.alloc("q1", [96, 512], F32)
            q2 = A.alloc("q2", [96, 512], F32)
            ptb = [A.alloc("ptb%d" % i, [128, 1024], BF16) for i in range(3)]
            rden = [A.alloc("rden%d" % i, [65, 512], F32) for i in range(1)]
            bcs = [A.alloc("bcs%d" % i, [64, 512], F32) for i in range(1)]
            st = [pes.enter_context(nc.psum_tensor("mst%d" % i, [128, 1024], F32)) for i in range(2)]
            ot = [pes.enter_context(nc.psum_tensor("mot%d" % i, [128, 512], F32)) for i in range(2)]
            bc = [pes.enter_context(nc.psum_tensor("mbc%d" % i, [64, 512], F32)) for i in range(1)]
            ppr = pes.enter_context(nc.psum_tensor("ppr", [128, 512], F32))
            stB = [Buf("st%d" % i) for i in range(2)]
            ptB = [Buf("pt%d" % i) for i in range(3)]
            otB = [Buf("ot%d" % i) for i in range(2)]
            bcB = [Buf("bc%d" % i) for i in range(1)]
            rdB = [Buf("rd%d" % i) for i in range(1)]
            bcsB = [Buf("bcs%d" % i) for i in range(1)]
            for i in range(2):
                S.op(pool, lambda e, i=i: e.memset(Vh[i][:, :, 64:65], 1.0), writes=[B4["Vh%d" % i]])

            def prep_closures(h, hp):
                cl = []
                KTh, KB = KT[hp], B4["KT%d" % hp]
                Vhh, VB = Vh[hp], B4["Vh%d" % hp]
                QTh, QB = QT[hp], B4["QT%d" % hp]

                def kcopy():
                    S.op(dve, lambda e: e.tensor_copy(KTh[64:96, :], krT[64:96, :]), reads=[B4["krT"]], writes=[KB])
                cl.append(kcopy)
                for bi in range(9):
                    ntok = 512 if bi < 8 else 256
                    tok = slice(bi * 512, bi * 512 + ntok)

                    def kblk(tok=tok, ntok=ntok):
                        for c in range(2):
                            S.mm(lambda e, c=c: e.matmul(ppr[0:64, 0:ntok], wukv[:, c, h * 128:h * 128 + 64],
                                                         ckvT[:, c, tok], start=(c == 0), stop=(c == 1)),
                                 reads=[B4["wukv"], B4["ckvT"]], writes=[B4["ppr"]], signal=(c == 1), first=(c == 0))
                        S.op(dve, lambda e: e.tensor_copy(KTh[0:64, tok], ppr[0:64, 0:ntok]), reads=[B4["ppr"]],
                             writes=[KB])
                    cl.append(kblk)
                for g0 in range(0, 34, 8):
                    nt = min(8, 34 - g0)

                    def vblk(g0=g0, nt=nt):
                        for t in range(nt):
                            tsl = slice((g0 + t) * 128, (g0 + t + 1) * 128)
                            for c in range(2):
                                S.mm(lambda e, c=c, t=t, tsl=tsl: e.matmul(
                                    ppr[:, t * 64:(t + 1) * 64], ckvT[:, c, tsl], wukv[:, c, h * 128 + 64:h * 128 + 128],
                                    start=(c == 0), stop=(c == 1)),
                                    reads=[B4["wukv"], B4["ckvT"]], writes=[B4["ppr"]],
                                    signal=(c == 1 and t == nt - 1), first=(c == 0 and t == 0))
                        S.op(dve, lambda e: e.tensor_copy(Vhh[:, g0:g0 + nt, 0:64],
                                                          ppr[:, 0:nt * 64].rearrange("p (t d) -> p t d", t=nt)),
                             reads=[B4["ppr"]], writes=[VB])
                    cl.append(vblk)
                for qb in range(4):
                    tok = slice(qb * 512, (qb + 1) * 512)

                    def qa_(tok=tok):
                        S.dma(sp, tcq[64:96, :], tabc[64:96, tok], writes=[B4["tcq"]])
                        S.dma(sp, tsq[64:96, :], tabs[64:96, tok], writes=[B4["tsq"]])
                        for c in range(3):
                            S.mm(lambda e, c=c: e.matmul(ppr[0:96, :], wuq[:, c, h * 96:(h + 1) * 96], cqT[:, c, tok],
                                                         start=(c == 0), stop=(c == 2)),
                                 reads=[B4["wuq"], B4["cqT"]], writes=[B4["ppr"]], signal=(c == 2), first=(c == 0))
                        S.op(dve, lambda e: e.tensor_copy(QTh[0:64, tok], ppr[0:64, :]), reads=[B4["ppr"]], writes=[QB])
                        S.op(dve, lambda e: e.tensor_tensor(out=q1[64:96, :], in0=ppr[64:96, :], in1=tcq[64:96, :],
                                                            op=ALU.mult), reads=[B4["ppr"], B4["tcq"]], writes=[B4["q1"]])

                    def qb_(tok=tok):
                        for c in range(3):
                            S.mm(lambda e, c=c: e.matmul(ppr[0:96, :], wuqp[:, c, h * 96:(h + 1) * 96], cqT[:, c, tok],
                                                         start=(c == 0), stop=(c == 2)),
                                 reads=[B4["wuqp"], B4["cqT"]], writes=[B4["ppr"]], signal=(c == 2), first=(c == 0))
                        S.op(dve, lambda e: e.tensor_tensor(out=q2[64:96, :], in0=ppr[64:96, :], in1=tsq[64:96, :],
                                                            op=ALU.mult), reads=[B4["ppr"], B4["tsq"]], writes=[B4["q2"]])
                        S.op(dve, lambda e: e.tensor_tensor(out=QTh[64:96, tok], in0=q1[64:96, :], in1=q2[64:96, :],
                                                             op=ALU.add), reads=[B4["q1"], B4["q2"]], writes=[QB])
                    cl.append(qa_)
                    cl.append(qb_)
                return cl

            for f in prep_closures(0, 0):
                f()
            for h in range(8):
                hp = h % 2
                iters = []
                for qb in range(4):
                    tok = slice(qb * 512, (qb + 1) * 512)
                    groups = []
                    for g in range(17):
                        groups.append([(KT[hp][:, j * 128:(j + 1) * 128], B4["KT%d" % hp], None,
                                        Vh[hp][:, j, 0:65], B4["Vh%d" % hp]) for j in (2 * g, 2 * g + 1)])
                    iters.append(dict(q=QT[hp][:, tok], qB=B4["QT%d" % hp], groups=groups, scale=MLA_SCALE, sink=None,
                                      out=out_bT[:, h, tok], outB=B_ob[qb], ovw=lambda ap: ap))
                fl = deque(prep_closures(h + 1, 1 - hp)) if h < 7 else None
                run_attention(iters, st, stB, ptb, ptB, ot, otB, bc, bcB, rden, rdB, bcs, bcsB, fillers=fl, fill_every=2)
            if dbg and stage == 3:
                ddump(S, "d_obT", out_bT[:], [64, 8, 2048], B_ob)
            S.barrier()
        A.release(m_mla)
        if stage == 3:
            return nc, dbg_outs

        h2T = A.alloc("h2T", [128, 8, 2048], BF16)
        Wr = A.alloc("Wr", [128, 16, 32], F32)
        B_h2 = [Buf("h2T%d" % i) for i in range(4)]
        B_wr = [Buf("Wr%d" % i) for i in range(16)]
        B_x1 = [Buf("x1s%d" % i) for i in range(16)]
        m_p5 = A.mark()
        with ExitStack() as pes:
            wo = A.alloc("wo", [64, 16, 1024], BF16)
            wrb = A.alloc("wrb", [128, 8, 32], BF16)
            xt = [A.alloc("xt%d" % i, [128, 1024], F32) for i in range(2)]
            x1t = [A.alloc("x1t%d" % i, [128, 1024], F32) for i in range(2)]
            tt_ = A.alloc("tt5", [128, 1024], F32)
            junk = A.alloc("junk5", [128, 1024], BF16)
            hb = [A.alloc("h2b%d" % i, [128, 1024], BF16) for i in range(2)]
            sm = A.alloc("sm5", [128, 16, 8], F32)
            lg = A.alloc("lg", [128, 512], F32)
            rk = A.alloc("rk", [128, 512], F32)
            mk = A.alloc("mk", [128, 512], F32)
            ex = A.alloc("ex", [128, 512], F32)
            m1 = A.alloc("m1", [128, 16], F32)
            thr = A.alloc("thr", [128, 16], F32)
            br16 = A.alloc("br16", [1, 512], BF16)
            py = [pes.enter_context(nc.psum_tensor("py%d" % i, [128, 1024], F32)) for i in range(2)]
            pT = [pes.enter_context(nc.psum_tensor("pT5_%d" % i, [128, 1024], BF16)) for i in range(2)]
            plg = pes.enter_context(nc.psum_tensor("plg", [128, 512], F32))
            B5 = {k: Buf(k) for k in ["wo", "wrb", "brb", "xt0", "xt1", "x1t0", "x1t1", "tt", "hb0", "hb1", "lg", "rk", "m1", "thr",
                                      "mk", "ex", "py0", "py1", "pT0", "pT1", "plg"]}
            Bsm = [[Buf("sm%d_%d" % (i, j)) for j in range(8)] for i in range(16)]
            S.dma(pool, wo[:], w_o.rearrange("(h p) n -> p h n", p=64), writes=[B5["wo"]])
            S.dma(pool, wrb[:], w_r.rearrange("(c p) n -> p c n", p=128), writes=[B5["wrb"]])
            S.dma(pool, br16[:], b_r16, writes=[B5["brb"]])
            S.mm(lambda e: e.matmul(plg[:], ones_b[0:1, :], br16[0:1, :], start=True, stop=False),
                 reads=[Bc["ones_b"], B5["brb"]], writes=[B5["plg"]], signal=True, first=True)
            for tt in range(16):
                k = tt % 2
                tsl = slice(tt * 128, (tt + 1) * 128)
                y_, yB = py[k], B5["py%d" % k]
                x_, xB = xt[k], B5["xt%d" % k]
                x1_, x1B = x1t[k], B5["x1t%d" % k]
                h_, hbB = hb[k], B5["hb%d" % k]
                pT_, pTB = pT[k], B5["pT%d" % k]
                smt = sm[:, tt, :]
                S.dma(sp, x_[:], xin[tsl, :], writes=[xB])
                for cb in range(2):
                    for h in range(16):
                        src = out_aT if h < 8 else out_bT
                        sB = B_oa[tt] if h < 8 else B_ob[tt // 4]
                        S.mm(lambda e, cb=cb, h=h, src=src: e.matmul(y_[:, cb * 512:(cb + 1) * 512], src[:, h % 8, tsl],
                                                                     wo[:, h, cb * 512:(cb + 1) * 512],
                                                                     start=(h == 0), stop=(h == 15)),
                             reads=[sB, B5["wo"]], writes=[yB], signal=(h == 15 and cb == 1), first=(h == 0 and cb == 0))
                S.op(act, lambda e, y_=y_, tt=tt: e.activation(out=junk[:], in_=y_[:], func=AF.Square,
                                                               accum_out=sm[:, tt, 0:1]), reads=[yB], writes=[Bsm[tt][0]])
                S.op(act, lambda e, tt=tt: e.activation(out=sm[:, tt, 1:2], in_=sm[:, tt, 0:1], func=AF.Ln, bias=EPS,
                                                        scale=1.0 / 1024), reads=[Bsm[tt][0]], writes=[Bsm[tt][1]])
                S.op(act, lambda e, tt=tt: e.activation(out=sm[:, tt, 2:3], in_=sm[:, tt, 1:2], func=AF.Exp, scale=-0.5),
                     reads=[Bsm[tt][1]], writes=[Bsm[tt][2]])
                S.op(dve, lambda e, y_=y_, tt=tt: e.scalar_tensor_tensor(out=tt_[:], in0=y_[:], scalar=sm[:, tt, 2:3],
                                                                         in1=gm_b[:], op0=ALU.mult, op1=ALU.mult),
                     reads=[yB, Bsm[tt][2], Bc["gm_b"]], writes=[B5["tt"]])
                S.op(dve, lambda e, x_=x_, x1_=x1_: e.tensor_tensor(out=x1_[:], in0=tt_[:], in1=x_[:], op=ALU.add),
                     reads=[B5["tt"], xB], writes=[x1B])
                S.dma(sp, x1_s[tsl, :], x1_[:], reads=[x1B], writes=[B_x1[tt]])
                S.op(act, lambda e, x1_=x1_, tt=tt: e.activation(out=junk[:], in_=x1_[:], func=AF.Square,
                                                                 accum_out=sm[:, tt, 3:4]), reads=[x1B], writes=[Bsm[tt][3]])
                S.op(act, lambda e, tt=tt: e.activation(out=sm[:, tt, 4:5], in_=sm[:, tt, 3:4], func=AF.Ln, bias=EPS,
                                                        scale=1.0 / 1024), reads=[Bsm[tt][3]], writes=[Bsm[tt][4]])
                S.op(act, lambda e, tt=tt: e.activation(out=sm[:, tt, 5:6], in_=sm[:, tt, 4:5], func=AF.Exp, scale=-0.5),
                     reads=[Bsm[tt][4]], writes=[Bsm[tt][5]])
                S.op(dve, lambda e, x1_=x1_, tt=tt: e.scalar_tensor_tensor(out=tt_[:], in0=x1_[:], scalar=sm[:, tt, 5:6],
                                                                           in1=gs2_b[:], op0=ALU.mult, op1=ALU.mult),
                     reads=[x1B, Bsm[tt][5], Bc["gs2_b"]], writes=[B5["tt"]])
                S.op(dve, lambda e, h_=h_: e.tensor_tensor(out=h_[:], in0=tt_[:], in1=sh2_b[:], op=ALU.add),
                     reads=[B5["tt"], Bc["sh2_b"]], writes=[hbB])
                for c in range(8):
                    S.mm(lambda e, c=c, h_=h_, pT_=pT_: e.transpose(pT_[:, c * 128:(c + 1) * 128],
                                                                   h_[:, c * 128:(c + 1) * 128], ident_b[:]),
                         reads=[hbB, Bc["ident_b"]], writes=[pTB], signal=(c == 7), first=(c == 0))
                S.op(dve, lambda e, pT_=pT_: e.tensor_copy(h2T[:, :, tsl], pT_[:].rearrange("p (c t) -> p c t", c=8)),
                     reads=[pTB], writes=[B_h2[tt // 4]])
                for c in range(8):
                    S.mm(lambda e, c=c, tt=tt: e.matmul(plg[:, tt * 32:(tt + 1) * 32], h2T[:, c, tsl], wrb[:, c, :],
                                                        start=False, stop=(c == 7 and tt == 15)),
                         reads=[B_h2[tt // 4], B5["wrb"]], writes=[B5["plg"]], signal=(c == 7), first=False)
            v3 = lambda t: t[:].rearrange("p (t x) -> p t x", t=16)
            bc3 = lambda t: t[:].unsqueeze(2).to_broadcast([128, 16, 32])
            S.op(dve, lambda e: e.tensor_copy(lg[:], plg[:]), reads=[B5["plg"]], writes=[B5["lg"]])
            S.op(dve, lambda e: e.tensor_copy(rk[:], lg[:]), reads=[B5["lg"]], writes=[B5["rk"]])
            for kk in range(4):
                mdst = m1 if kk == 0 else thr
                mB = B5["m1"] if kk == 0 else B5["thr"]
                S.op(dve, lambda e, mdst=mdst: e.tensor_reduce(out=mdst[:], in_=v3(rk), axis=mybir.AxisListType.X,
                                                               op=ALU.max), reads=[B5["rk"]], writes=[mB])
                if kk < 3:
                    S.op(dve, lambda e, mdst=mdst: e.tensor_tensor(out=v3(mk), in0=v3(rk), in1=bc3(mdst), op=ALU.is_equal),
                         reads=[B5["rk"], mB], writes=[B5["mk"]])
                    S.op(dve, lambda e: e.scalar_tensor_tensor(out=rk[:], in0=mk[:], scalar=-1.0e9, in1=rk[:],
                                                               op0=ALU.mult, op1=ALU.add),
                         reads=[B5["mk"], B5["rk"]], writes=[B5["rk"]])
            S.op(dve, lambda e: e.tensor_tensor(out=v3(mk), in0=v3(lg), in1=bc3(thr), op=ALU.is_ge),
                 reads=[B5["lg"], B5["thr"]], writes=[B5["mk"]])
            S.op(dve, lambda e: e.tensor_tensor(out=v3(rk), in0=v3(lg), in1=bc3(m1), op=ALU.subtract),
                 reads=[B5["lg"], B5["m1"]], writes=[B5["rk"]])
            S.op(act, lambda e: e.activation(out=ex[:], in_=rk[:], func=AF.Exp), reads=[B5["rk"]], writes=[B5["ex"]])
            S.op(dve, lambda e: e.tensor_tensor(out=ex[:], in0=ex[:], in1=mk[:], op=ALU.mult),
                 reads=[B5["ex"], B5["mk"]], writes=[B5["ex"]])
            S.op(dve, lambda e: e.tensor_reduce(out=thr[:], in_=v3(ex), axis=mybir.AxisListType.X, op=ALU.add),
                 reads=[B5["ex"]], writes=[B5["thr"]])
            S.op(dve, lambda e: e.reciprocal(m1[:], thr[:]), reads=[B5["thr"]], writes=[B5["m1"]])
            S.op(dve, lambda e: e.tensor_tensor(out=Wr[:], in0=v3(ex), in1=bc3(m1), op=ALU.mult),
                 reads=[B5["ex"], B5["m1"]], writes=B_wr)
            if dbg and stage == 4:
                ddump(S, "d_h2T", h2T[:], [128, 8, 2048], B_h2)
                ddump(S, "d_Wr", Wr[:], [128, 16, 32], B_wr, cast=False)
                ddump(S, "d_x1", x1_s, [2048, 1024], B_x1, cast=False)
            S.barrier()
        A.release(m_p5)
        if stage == 4:
            return nc, dbg_outs

        A.limit = TOP
        facc = A.alloc("facc", [128, 16, 1024], F32)
        B_fa = [Buf("fa%d" % i) for i in range(16)]
        m_moe = A.mark()
        with ExitStack() as pes:
            bguT = A.alloc("bguT", [128, 16, 32], F32)
            bg7 = A.alloc("bg7", [128, 8, 32], F32)
            bln = A.alloc("bln", [128, 8, 32], F32)
            sgb = A.alloc("sgb", [128, 1], F32)
            B6 = {k: Buf(k) for k in ["GUa", "GUb", "DN0", "DN1", "bd0", "bd1", "actT0", "actT1", "T1_0", "T1_1", "T2_0", "T2_1", "T3_0",
                                      "T3_1", "bgu_t", "bguT", "pgl0", "pgl1", "pgl2", "pgl3", "po0", "po1"]}
            pgl = [pes.enter_context(nc.psum_tensor("pgl%d" % i, [128, 512], F32)) for i in range(4)]
            po = [pes.enter_context(nc.psum_tensor("po%d" % i, [128, 1024], F32)) for i in range(2)]
            m_bgu = A.mark()
            bgu_t = A.alloc("bgu_t", [32, 2048], F32)
            S.dma(sp, bgu_t[:], b_gu, writes=[B6["bgu_t"]])
            for c in range(16):
                S.mm(lambda e, c=c: e.transpose(pgl[0][:, c * 32:(c + 1) * 32], bgu_t[:, c * 128:(c + 1) * 128],
                                                ident_f[0:32, 0:32]),
                     reads=[B6["bgu_t"], Bc["ident_f"]], writes=[B6["pgl0"]], signal=(c == 15), first=(c == 0))
            S.op(dve, lambda e: e.tensor_copy(bguT[:], pgl[0][:].rearrange("p (c x) -> p c x", c=16)),
                 reads=[B6["pgl0"]], writes=[B6["bguT"]])
            S.op(dve, lambda e: e.tensor_scalar(bg7[:], bguT[:, 0:8, :], -1.0, 7.0, op0=ALU.mult, op1=ALU.add),
                 reads=[B6["bguT"]], writes=[B6["bguT"]])
            S.op(dve, lambda e: e.tensor_scalar(bln[:], bguT[:, 8:16, :], -1.0, None, op0=ALU.mult),
                 reads=[B6["bguT"]], writes=[B6["bguT"]])
            S.op(dve, lambda e: e.memset(sgb[:], 11.914), writes=[B6["bguT"]])
            WrT = A.alloc("WrT", [32, 16, 128], BF16)
            bdn = A.alloc("bdn", [32, 1024], BF16)
            B6["WrT"] = Buf("WrT")
            B6["bdn"] = Buf("bdn")
            S.dma(pool, bdn[:], b_dn, writes=[B6["bdn"]])
            for q4 in range(4):
                for t4 in range(4):
                    tt = q4 * 4 + t4
                    S.mm(lambda e, q4=q4, t4=t4, tt=tt: e.transpose(pgl[q4][0:32, t4 * 128:(t4 + 1) * 128], Wr[:, tt, :],
                                                                    ident_f[:]),
                         reads=[B_wr[tt], Bc["ident_f"]], writes=[B6["pgl%d" % q4]], signal=(t4 == 3), first=(t4 == 0))
                S.op(dve, lambda e, q4=q4: e.tensor_copy(WrT[:, q4 * 4:(q4 + 1) * 4, :],
                                                         pgl[q4][0:32, :].rearrange("p (t x) -> p t x", t=4)),
                     reads=[B6["pgl%d" % q4]], writes=[B6["WrT"]])
            for tt in range(16):
                o_, oB = po[tt % 2], B6["po%d" % (tt % 2)]
                for cb in range(2):
                    cs = slice(cb * 512, (cb + 1) * 512)
                    S.mm(lambda e, tt=tt, cs=cs, o_=o_: e.matmul(o_[:, cs], WrT[:, tt, :], bdn[:, cs], start=True, stop=True),
                         reads=[B6["WrT"], B6["bdn"]], writes=[oB], signal=(cb == 1), first=(cb == 0))
                S.op(act, lambda e, tt=tt, o_=o_: e.copy(facc[:, tt, :], o_[:]), reads=[oB], writes=[B_fa[tt]])
            S.barrier()
            A.release(m_bgu)
            GU = A.alloc("GU", [128, 8, 2048], BF16)
            DNs = [A.alloc("DN0", [128, 8, 1024], BF16)] * 2
            actT = [A.alloc("actT%d" % i, [128, 8, 512], BF16) for i in range(2)]
            T1 = [A.alloc("T1_%d" % i, [128, 512], F32) for i in range(2)]
            T2 = [A.alloc("T2_%d" % i, [128, 512], F32) for i in range(2)]
            T3 = [A.alloc("T3_%d" % i, [128, 512], F32) for i in range(2)]
            pair_i = [0]

            def load_gu(e_, half):
                wv = w_gu[e_].rearrange("(c p) n -> p c n", p=128)
                hb_ = "GUa" if half == 0 else "GUb"
                for base in (0, 1024):
                    c0 = base + half * 512
                    S.dma(pool, GU[:, :, c0:c0 + 512], wv[:, :, c0:c0 + 512], writes=[B6[hb_]])

            def load_dn(e_):
                S.dma(pool, DNs[e_ % 2][:], w_dn[e_].rearrange("(c p) n -> p c n", p=128), writes=[B6["DN0"]])

            def gu_step(e_, tb, mid=None):
                tok = slice(tb * 512, (tb + 1) * 512)
                aT, aB = actT[tb % 2], B6["actT%d" % (tb % 2)]
                for j in range(8):
                    if j == 4 and mid is not None:
                        mid()
                    guB = B6["GUa"] if j < 4 else B6["GUb"]
                    i = pair_i[0] % 2
                    pair_i[0] += 1
                    pg_, pgB = pgl[2 * i], B6["pgl%d" % (2 * i)]
                    pl_, plB = pgl[2 * i + 1], B6["pgl%d" % (2 * i + 1)]
                    for gi_, (dst, dB, col0) in enumerate([(pg_, pgB, j * 128), (pl_, plB, 1024 + j * 128)]):
                        for c in range(8):
                            S.mm(lambda e, c=c, dst=dst, col0=col0: e.matmul(dst[:], GU[:, c, col0:col0 + 128],
                                                                            h2T[:, c, tok], start=(c == 0), stop=(c == 7)),
                                 reads=[guB, B_h2[tb]], writes=[dB], signal=(c == 7 and gi_ == 1), first=(c == 0))
                    t1, t2, t3 = T1[i], T2[i], T3[i]
                    b1, b2, b3 = B6["T1_%d" % i], B6["T2_%d" % i], B6["T3_%d" % i]
                    S.op(act, lambda e, t1=t1, pg_=pg_, j=j: e.activation(out=t1[:], in_=pg_[:], func=AF.Relu,
                                                                          bias=bg7[:, j, e_:e_ + 1], scale=-1.0),
                         reads=[pgB, B6["bguT"]], writes=[b1])
                    S.op(act, lambda e, t1=t1, t3=t3: e.activation(out=t3[:], in_=t1[:], func=AF.Sigmoid, bias=sgb[:, 0:1],
                                                                   scale=-1.702),
                         reads=[b1, B6["bguT"]], writes=[b3])
                    S.op(act, lambda e, t2=t2, pl_=pl_, j=j: e.activation(out=t2[:], in_=pl_[:], func=AF.Identity,
                                                                          bias=bln[:, j, e_:e_ + 1], scale=-1.0),
                         reads=[plB, B6["bguT"]], writes=[b2])
                    S.op(dve, lambda e, t2=t2: e.tensor_scalar(t2[:], t2[:], 7.0, -7.0, op0=ALU.min, op1=ALU.max),
                         reads=[b2], writes=[b2])
                    S.op(dve, lambda e, t1=t1, t3=t3: e.scalar_tensor_tensor(out=t1[:], in0=t1[:], scalar=7.0, in1=t3[:],
                                                                             op0=ALU.subtract, op1=ALU.mult),
                         reads=[b1, b3], writes=[b1])
                    S.op(dve, lambda e, t1=t1, t2=t2, j=j: e.scalar_tensor_tensor(out=aT[:, j, :], in0=t2[:], scalar=1.0,
                                                                                  in1=t1[:], op0=ALU.subtract, op1=ALU.mult),
                         reads=[b1, b2], writes=[aB])

            def dn_step(e_, tb):
                aT, aB = actT[tb % 2], B6["actT%d" % (tb % 2)]
                DN, dnB = DNs[0], B6["DN0"]
                for ti in range(4):
                    tt = tb * 4 + ti
                    o_, oB = po[tt % 2], B6["po%d" % (tt % 2)]
                    for cb in range(2):
                        cs = slice(cb * 512, (cb + 1) * 512)
                        for j in range(8):
                            S.mm(lambda e, j=j, cs=cs, ti=ti: e.matmul(o_[:, cs], aT[:, j, ti * 128:(ti + 1) * 128],
                                                                      DN[:, j, cs], start=(j == 0), stop=(j == 7)),
                                 reads=[aB, dnB], writes=[oB], signal=(j == 7 and cb == 1), first=(j == 0 and cb == 0))
                    if True:
                        S.op(dve, lambda e, tt=tt: e.scalar_tensor_tensor(out=facc[:, tt, :], in0=o_[:],
                                                                          scalar=Wr[:, tt, e_:e_ + 1], in1=facc[:, tt, :],
                                                                          op0=ALU.mult, op1=ALU.add),
                             reads=[oB, B_wr[tt], B_fa[tt]], writes=[B_fa[tt]])

            load_gu(0, 0)
            load_gu(0, 1)
            load_dn(0)
            for e_ in range(n_experts):
                nxt = e_ + 1 < n_experts
                gu_step(e_, 0)
                gu_step(e_, 1)
                dn_step(e_, 0)
                gu_step(e_, 2)
                dn_step(e_, 1)
                gu_step(e_, 3, mid=(lambda e_=e_: load_gu(e_ + 1, 0)) if nxt else None)
                if nxt:
                    load_gu(e_ + 1, 1)
                dn_step(e_, 2)
                dn_step(e_, 3)
                if nxt:
                    load_dn(e_ + 1)
            S.barrier()
        A.release(m_moe)

        with ExitStack() as pes:
            x1t = [A.alloc("x1f%d" % i, [128, 1024], F32) for i in range(2)]
            ot_ = [A.alloc("of%d" % i, [128, 1024], F32) for i in range(2)]
            tt_ = A.alloc("tt7", [128, 1024], F32)
            junk = A.alloc("junk7", [128, 1024], BF16)
            sm = A.alloc("sm7", [128, 16, 4], F32)
            B7 = {k: Buf(k) for k in ["x1f0", "x1f1", "of0", "of1", "tt"]}
            Bsm = [[Buf("sm7_%d_%d" % (i, j)) for j in range(3)] for i in range(16)]
            outs = []
            for tt in range(16):
                k = tt % 2
                tsl = slice(tt * 128, (tt + 1) * 128)
                S.dma(sp, x1t[k][:], x1_s[tsl, :], reads=[B_x1[tt]], writes=[B7["x1f%d" % k]])
                S.op(act, lambda e, tt=tt: e.activation(out=junk[:], in_=facc[:, tt, :], func=AF.Square,
                                                        accum_out=sm[:, tt, 0:1]), reads=[B_fa[tt]], writes=[Bsm[tt][0]])
                S.op(act, lambda e, tt=tt: e.activation(out=sm[:, tt, 1:2], in_=sm[:, tt, 0:1], func=AF.Ln, bias=EPS,
                                                        scale=1.0 / 1024), reads=[Bsm[tt][0]], writes=[Bsm[tt][1]])
                S.op(act, lambda e, tt=tt: e.activation(out=sm[:, tt, 2:3], in_=sm[:, tt, 1:2], func=AF.Exp, scale=-0.5),
                     reads=[Bsm[tt][1]], writes=[Bsm[tt][2]])
                S.op(dve, lambda e, tt=tt: e.scalar_tensor_tensor(out=tt_[:], in0=facc[:, tt, :], scalar=sm[:, tt, 2:3],
                                                                  in1=gf_b[:], op0=ALU.mult, op1=ALU.mult),
                     reads=[B_fa[tt], Bsm[tt][2], Bc["gf_b"]], writes=[B7["tt"]])
                S.op(dve, lambda e, k=k: e.tensor_tensor(out=ot_[k][:], in0=tt_[:], in1=x1t[k][:], op=ALU.add),
                     reads=[B7["tt"], B7["x1f%d" % k]], writes=[B7["of%d" % k]])
                outs.append(S.dma(sp, out[tsl, :], ot_[k][:], reads=[B7["of%d" % k]]))
            S.barrier()
        build_program.stats = dict(n_inst=S.n_inst, sbuf_peak=A.peak, auto_sbuf_left=nc.sbuf_bytes_remaining)
    return nc, dbg_outs


def _rope_tables(rot_dim):
    rows = 64
    row = np.repeat(np.arange(rows, dtype=np.float32), 64)
    col = np.tile(np.arange(64, dtype=np.float32), rows)
    quarter = rot_dim // 4
    inv = (np.float32(10000.0) ** (-np.arange(quarter, dtype=np.float32) / np.float32(quarter))).astype(np.float32)
    ang = np.concatenate([row[:, None] * inv, col[:, None] * inv], axis=-1).astype(np.float32)
    return np.cos(ang).astype(np.float32), np.sin(ang).astype(np.float32)


def _const_tables():
    ca, sa = _rope_tables(64)
    cb, sb = _rope_tables(32)
    tc = np.zeros((96, 4096), np.float32)
    ts = np.zeros((96, 4096), np.float32)
    tc[0:32] = ca.T
    tc[32:64] = ca.T
    ts[0:32] = -sa.T
    ts[32:64] = sa.T
    tc[64:80] = cb.T
    tc[80:96] = cb.T
    ts[64:80] = -sb.T
    ts[80:96] = sb.T
    return tc, ts


def _masks(s):
    NEG = -30000.0
    kk = np.arange(128)[:, None]
    ii = np.arange(128)[None, :]
    mL = np.where(ii <= kk, 0.0, NEG).astype(np.float32)
    mR = np.where(kk <= ii, 0.0, NEG).astype(np.float32)
    allneg = np.full((128, 128), NEG, np.float32)
    mLe = allneg if s == 0 else mL
    mRe = mR if s == 0 else allneg
    m = np.stack([np.tile(x, (1, 4)) for x in (mL, mR, mLe, mRe)], axis=1)
    return np.ascontiguousarray(m)


def _swap_halves(w, width):
    n = w.shape[1] // width
    w3 = w.reshape(w.shape[0], n, width)
    h = width // 2
    return np.concatenate([w3[:, :, h:], w3[:, :, :h]], axis=2).reshape(w.shape[0], n * width)


def prep_inputs(I):
    f = lambda a: np.ascontiguousarray(np.asarray(a, dtype=np.float32))
    w_in = f(I["w_in"][0])
    qa, ka = w_in[:, 0:512], w_in[:, 512:640]
    kr = w_in[:, 1408:1440]
    z64 = np.zeros((1024, 64), np.float32)
    w_inx = np.concatenate([w_in[:, 0:1408], z64, kr, z64, _swap_halves(kr, 32), _swap_halves(qa, 64),
                            _swap_halves(ka, 64)], axis=1)
    assert w_inx.shape[1] == C_END
    w_uq = f(I["w_uq"][0])
    wq3 = w_uq.reshape(384, 8, 96)
    w_uqp = np.zeros_like(wq3)
    w_uqp[:, :, 64:96] = _swap_halves(wq3[:, :, 64:96].reshape(384, 256), 32).reshape(384, 8, 32)
    w_uqp = np.ascontiguousarray(w_uqp.reshape(384, 768))
    rows = np.concatenate([f(I["g_mix_pre"][0]), f(I["g_mix_post"][0]), f(I["g_ffn_pre"][0]), f(I["g_ffn_post"][0]),
                           f(I["b_ada"][0])])[None, :]
    tc, ts = _const_tables()
    shared = dict(
        w_ada=f(I["w_ada"][0]), rows=np.ascontiguousarray(rows), w_inx=np.ascontiguousarray(w_inx),
        sinkr=np.ascontiguousarray(np.repeat(f(I["sink"][0]), 128)[None, :]),
        gq=np.ascontiguousarray(f(I["g_q_a"][0]).reshape(3, 128).T), gkv=np.ascontiguousarray(f(I["g_kv_a"][0]).reshape(2, 128).T),
        w_uq=w_uq, w_uqp=w_uqp, w_ukv=f(I["w_ukv"][0]), w_o=f(I["w_o"][0]), w_r=f(I["w_router"][0]),
        b_r16=np.ascontiguousarray(np.tile(f(I["b_router"][0]), 16)[None, :]), w_gu=f(I["w_gate_up"][0]), b_gu=f(I["b_gate_up"][0]), w_dn=f(I["w_down"][0]),
        b_dn=f(I["b_down"][0]), ident=np.eye(128, dtype=np.float32))
    x = np.asarray(I["x"], dtype=np.float32)
    ctx = np.asarray(I["ctx"], dtype=np.float32)
    c = np.asarray(I["c"], dtype=np.float32)
    c_ctx = np.asarray(I["c_ctx"], dtype=np.float32)
    maps = []
    for core in range(8):
        b, s = core // 2, core % 2
        own = x[b, s * 2048:(s + 1) * 2048]
        oth = x[b, (1 - s) * 2048:(2 - s) * 2048]
        xin = np.ascontiguousarray(np.concatenate([own, oth, ctx[b]], axis=0))
        cvec = np.ascontiguousarray(np.concatenate([c[b].reshape(8, 128).T, c_ctx.reshape(8, 128).T], axis=1))
        order = np.concatenate([np.arange(s * 2048, (s + 1) * 2048), np.arange((1 - s) * 2048, (2 - s) * 2048)])
        m = dict(shared)
        m.update(xin=xin, cvec=cvec, tabc=np.ascontiguousarray(tc[:, order]), tabs=np.ascontiguousarray(ts[:, order]),
                 masks=_masks(s))
        maps.append(m)
    return maps


_CACHE = {}


def kernel(**inputs):
    if "nc" not in _CACHE:
        _CACHE["nc"] = build_program()[0]
    nc = _CACHE["nc"]
    maps = prep_inputs(inputs)
    res = run_bass_kernel_spmd(nc, maps, core_ids=list(range(8)))
    outp = np.zeros((4, 4096, 1024), np.float32)
    for core in range(8):
        b, s = core // 2, core % 2
        outp[b, s * 2048:(s + 1) * 2048] = res.results[core]["out"]
    return outp
```

```python
import numpy as np
from contextlib import ExitStack
from collections import deque
import concourse.bass as bass
import concourse.mybir as mybir
from concourse.bass_utils import run_bass_kernel_spmd

F32 = mybir.dt.float32
BF16 = mybir.dt.bfloat16
AF = mybir.ActivationFunctionType
ALU = mybir.AluOpType

import os
DBG_SKIP_ROUTER = os.environ.get("DBG_SKIP_ROUTER") == "1"
EPS = 1e-6
A_SCALE = 64 ** -0.5
MLA_SCALE = 96 ** -0.5
C_QA, C_KA, C_VA, C_CQ, C_CKV, C_KR2, C_QAP, C_KAP, C_END = 0, 512, 640, 768, 1152, 1408, 1600, 2112, 2240


class Buf:
    __slots__ = ("name", "w", "r")

    def __init__(self, name):
        self.name = name
        self.w = None
        self.r = []


class Eng:
    def __init__(self, S, name, obj, is_pe=False, n_dma=0):
        self.name = name
        self.obj = obj
        self.sem = S.new_sem("e_" + name)
        self.count = 0
        self.seen = {}
        self.is_pe = is_pe
        self.pend_r = []
        self.pend_w = []
        self.dma_sems = [[S.new_sem("d_%s%d" % (name, i)), 0] for i in range(n_dma)]
        self.dma_rr = 0


class Sched:
    def __init__(self, nc, es):
        self.nc = nc
        self.es = es
        self.pe = Eng(self, "pe", nc.tensor, is_pe=True)
        self.dve = Eng(self, "dve", nc.vector)
        self.act = Eng(self, "act", nc.scalar)
        self.pool = Eng(self, "pool", nc.gpsimd, n_dma=16)
        self.sp = Eng(self, "sp", nc.sync, n_dma=24)
        self.engs = [self.pe, self.dve, self.act, self.pool, self.sp]
        self.n_inst = 0

    def new_sem(self, name):
        return self.es.enter_context(self.nc.semaphore(name))

    def _wait(self, eng, ev):
        sem, val = ev
        k = id(sem)
        if eng.seen.get(k, 0) >= val:
            return
        eng.obj.wait_ge(sem, val)
        eng.seen[k] = val
        self.n_inst += 1

    def _deps(self, eng, reads, writes):
        for b in reads:
            if b.w is not None and not (eng.is_pe and b.w[0] is eng.sem):
                self._wait(eng, b.w)
        for b in writes:
            if b.w is not None and not (eng.is_pe and b.w[0] is eng.sem):
                self._wait(eng, b.w)
            for ev in b.r:
                if not (eng.is_pe and ev[0] is eng.sem):
                    self._wait(eng, ev)

    def _commit(self, ev, reads, writes):
        for b in reads:
            b.r.append(ev)
            if len(b.r) > 48:
                last = {}
                for e in b.r:
                    k = id(e[0])
                    if k not in last or last[k][1] < e[1]:
                        last[k] = e
                b.r = list(last.values())
        for b in writes:
            b.w = ev
            b.r = []

    def op(self, eng, fn, reads=(), writes=()):
        self._deps(eng, reads, writes)
        ins = fn(eng.obj)
        eng.count += 1
        ev = (eng.sem, eng.count)
        ins.then_inc(eng.sem, 1)
        self._commit(ev, reads, writes)
        self.n_inst += 1
        return ev

    def mm(self, fn, reads=(), writes=(), signal=True, first=True):
        eng = self.pe
        self._deps(eng, reads, writes if first else ())
        ins = fn(eng.obj)
        eng.pend_r.extend(reads)
        for b in writes:
            if b not in eng.pend_w:
                eng.pend_w.append(b)
        self.n_inst += 1
        if signal:
            eng.count += 1
            ev = (eng.sem, eng.count)
            ins.then_inc(eng.sem, 1)
            self._commit(ev, eng.pend_r, eng.pend_w)
            eng.pend_r = []
            eng.pend_w = []
            return ev
        return None

    def dma(self, q, out, in_, reads=(), writes=()):
        slot = q.dma_sems[q.dma_rr % len(q.dma_sems)]
        q.dma_rr += 1
        sem, cur = slot
        if cur > 0:
            self._wait(q, (sem, cur))
        self._deps(q, reads, writes)
        ins = q.obj.dma_start(out=out, in_=in_)
        slot[1] = cur + 16
        ev = (sem, cur + 16)
        ins.then_inc(sem, 16)
        self._commit(ev, reads, writes)
        self.n_inst += 1
        return ev

    def barrier(self):
        assert not self.pe.pend_r and not self.pe.pend_w
        evs = [(e.sem, e.count) for e in self.engs if e.count > 0]
        for e in self.engs:
            evs += [(s, v) for s, v in e.dma_sems if v > 0]
        for e in self.engs:
            for ev in evs:
                if ev[0] is e.sem and e.is_pe:
                    continue
                self._wait(e, ev)


class Arena:
    def __init__(self, nc, base=20480, top=229312):
        self.nc = nc
        self.ptr = base
        self.top = top
        self.n = 0
        self.peak = base
        self.limit = top

    def alloc_at(self, name, shape, dtype, off):
        self.n += 1
        return self.nc.alloc_sbuf_tensor_at("%s_%d" % (name, self.n), list(shape), dtype, offset=off)

    def alloc(self, name, shape, dtype):
        esz = 4 if dtype == F32 else 2
        nbytes = int(np.prod(shape[1:])) * esz
        off = (self.ptr + 31) // 32 * 32
        assert off + nbytes <= self.limit, ("SBUF overflow", name, off, nbytes, self.limit)
        self.ptr = off + nbytes
        self.peak = max(self.peak, self.ptr)
        self.n += 1
        return self.nc.alloc_sbuf_tensor_at("%s_%d" % (name, self.n), list(shape), dtype, offset=off)

    def mark(self):
        return self.ptr

    def release(self, m):
        self.ptr = m


def build_program(stage=99, dbg=False, n_experts=32):
    nc = bass.Bass("TRN2", target_bir_lowering=False)

    def din(name, shape, dt=F32):
        return nc.dram_tensor(name, list(shape), dt, kind="ExternalInput").ap()

    def dout(name, shape, dt=F32):
        return nc.dram_tensor(name, list(shape), dt, kind="ExternalOutput").ap()

    def dscr(name, shape, dt):
        return nc.dram_tensor(name, list(shape), dt, kind="Internal").ap()

    xin = din("xin", [4352, 1024])
    cvec = din("cvec", [128, 16])
    w_ada = din("w_ada", [1024, 6144])
    rows_in = din("rows", [1, 10240])
    w_inx = din("w_inx", [1024, C_END])
    tabc = din("tabc", [96, 4096])
    tabs = din("tabs", [96, 4096])
    masks = din("masks", [128, 4, 512])
    sinkr = din("sinkr", [1, 1024])
    gq = din("gq", [128, 3])
    gkv = din("gkv", [128, 2])
    w_uq = din("w_uq", [384, 768])
    w_uqp = din("w_uqp", [384, 768])
    w_ukv = din("w_ukv", [256, 1024])
    w_o = din("w_o", [1024, 1024])
    w_r = din("w_r", [1024, 32])
    b_r16 = din("b_r16", [1, 512])
    w_gu = din("w_gu", [n_experts, 1024, 2048])
    b_gu = din("b_gu", [32, 2048])
    w_dn = din("w_dn", [n_experts, 1024, 1024])
    b_dn = din("b_dn", [32, 1024])
    ident = din("ident", [128, 128])
    out = dout("out", [2048, 1024])

    cq_s = dscr("cq_s", [128, 3, 2048], BF16)
    ckv_s = dscr("ckv_s", [128, 2, 4352], BF16)
    kr_s = dscr("kr_s", [96, 4352], BF16)
    x1_s = dscr("x1_s", [2048, 1024], F32)

    dbg_outs = {}

    def ddump(S, name, src_ap, shape, reads, cast=True):
        if not dbg:
            return
        o = dout(name, shape)
        dbg_outs[name] = o
        S.dma(S.pool if cast else S.sp, o, src_ap, reads=reads)

    with ExitStack() as es:
        S = Sched(nc, es)
        A = Arena(nc)
        pe, dve, act, pool, sp = S.pe, S.dve, S.act, S.pool, S.sp

        ident_b = A.alloc("ident_b", [128, 128], BF16)
        ident_f = A.alloc("ident_f", [128, 128], F32)
        ones_b = A.alloc("ones_b", [128, 128], BF16)
        ones_f = A.alloc("ones_f", [128, 128], F32)
        e64 = A.alloc("e64", [1, 65], BF16)
        gf_b = A.alloc("gf_b", [128, 1024], F32)
        gm_b = A.alloc("gm_b", [128, 1024], F32)
        gs2_b = A.alloc("gs2_b", [128, 1024], F32)
        sh2_b = A.alloc("sh2_b", [128, 1024], F32)
        Bc = {k: Buf(k) for k in ["ident_b", "ident_f", "ones_b", "ones_f", "e64", "gf_b", "gm_b", "gs2_b", "sh2_b"]}
        S.dma(pool, ident_b[:], ident, writes=[Bc["ident_b"]])
        S.dma(sp, ident_f[:], ident, writes=[Bc["ident_f"]])
        S.op(pool, lambda e: e.memset(ones_b[:], 1.0), writes=[Bc["ones_b"]])
        S.op(pool, lambda e: e.memset(ones_f[:], 1.0), writes=[Bc["ones_f"]])
        S.op(pool, lambda e: e.memset(e64[:], 0.0), writes=[Bc["e64"]])
        S.op(pool, lambda e: e.memset(e64[0:1, 64:65], 1.0), writes=[Bc["e64"]])
        m_persist = A.mark()

        gs1_b = A.alloc("gs1_b", [128, 1024], F32)
        sh1_b = A.alloc("sh1_b", [128, 1024], F32)
        gsc_b = A.alloc("gsc_b", [128, 1024], F32)
        shc_b = A.alloc("shc_b", [128, 1024], F32)
        for k in ["gs1_b", "sh1_b", "gsc_b", "shc_b"]:
            Bc[k] = Buf(k)
        m_bc1 = A.mark()
        with ExitStack() as pes:
            rows_t = A.alloc("rows_t", [1, 10240], F32)
            cv = A.alloc("cv", [128, 16], F32)
            sg = A.alloc("sg", [128, 16], F32)
            sl = A.alloc("sl", [128, 16], F32)
            rep = A.alloc("rep", [128, 16, 128], BF16)
            wa = [A.alloc("wa%d" % i, [128, 8, 512], BF16) for i in range(2)]
            gb = [A.alloc("gb%d" % i, [128, 512], F32) for i in range(2)]
            pm = [pes.enter_context(nc.psum_tensor("pm%d" % i, [128, 512], F32)) for i in range(2)]
            pmc = [pes.enter_context(nc.psum_tensor("pmc%d" % i, [128, 512], F32)) for i in range(2)]
            pg = [pes.enter_context(nc.psum_tensor("pg%d" % i, [128, 512], F32)) for i in range(2)]
            B0 = {k: Buf(k) for k in ["rows", "cv", "sg", "sl", "rep", "wa0", "wa1", "gb0", "gb1", "pm0", "pm1",
                                      "pmc0", "pmc1", "pg0", "pg1"]}
            S.dma(sp, rows_t[:], rows_in, writes=[B0["rows"]])
            S.dma(sp, cv[:], cvec, writes=[B0["cv"]])
            S.op(act, lambda e: e.activation(out=sg[:], in_=cv[:], func=AF.Sigmoid), reads=[B0["cv"]], writes=[B0["sg"]])
            S.op(dve, lambda e: e.tensor_tensor(out=sl[:], in0=cv[:], in1=sg[:], op=ALU.mult),
                 reads=[B0["cv"], B0["sg"]], writes=[B0["sl"]])
            for j in range(16):
                S.op(dve, lambda e, j=j: e.tensor_scalar(rep[:, j, :], ones_f[:], sl[:, j:j + 1], None, op0=ALU.mult),
                     reads=[B0["sl"], Bc["ones_f"]], writes=[B0["rep"]])
            w_ada_v = w_ada.rearrange("(c p) n -> p c n", p=128)
            g_off = {1: 0, 2: 1024, 4: 2048, 5: 3072}
            dests = {0: sh1_b, 1: gs1_b, 2: gm_b, 3: sh2_b, 4: gs2_b, 5: gf_b}
            destB = {0: "sh1_b", 1: "gs1_b", 2: "gm_b", 3: "sh2_b", 4: "gs2_b", 5: "gf_b"}
            for j in range(12):
                m, half = j // 2, j % 2
                k = j % 2
                cs = slice(half * 512, (half + 1) * 512)
                S.dma(pool, wa[k][:], w_ada_v[:, :, j * 512:(j + 1) * 512], writes=[B0["wa%d" % k]])
                vecs = [(0, pm[k], B0["pm%d" % k], dests[m], Bc[destB[m]])]
                if m < 2:
                    vecs.append((8, pmc[k], B0["pmc%d" % k], (shc_b if m == 0 else gsc_b),
                                 Bc["shc_b" if m == 0 else "gsc_b"]))
                if m in g_off:
                    go = g_off[m] + half * 512
                    S.mm(lambda e, k=k, go=go: e.matmul(pg[k][:], ones_f[0:1, :], rows_t[0:1, go:go + 512],
                                                        start=True, stop=True),
                         reads=[Bc["ones_f"], B0["rows"]], writes=[B0["pg%d" % k]])
                    S.op(act, lambda e, k=k: e.copy(gb[k][:], pg[k][:]), reads=[B0["pg%d" % k]], writes=[B0["gb%d" % k]])
                for (v0, pt_, pB, dst, dB) in vecs:
                    for c in range(8):
                        S.mm(lambda e, c=c, v0=v0, pt_=pt_, k=k: e.matmul(pt_[:], rep[:, v0 + c, :], wa[k][:, c, :],
                                                                          start=(c == 0), stop=False),
                             reads=[B0["rep"], B0["wa%d" % k]], writes=[pB], signal=False, first=(c == 0))
                    bo = 4096 + j * 512
                    S.mm(lambda e, pt_=pt_, bo=bo: e.matmul(pt_[:], ones_f[0:1, :], rows_t[0:1, bo:bo + 512],
                                                            start=False, stop=True),
                         reads=[Bc["ones_f"], B0["rows"]], writes=[pB], signal=True, first=False)
                    if m in (0, 3):
                        S.op(act, lambda e, dst=dst, pt_=pt_, cs=cs: e.copy(dst[:, cs], pt_[:]), reads=[pB], writes=[dB])
                    elif m in (1, 4):
                        S.op(dve, lambda e, dst=dst, pt_=pt_, cs=cs, k=k: e.scalar_tensor_tensor(
                            out=dst[:, cs], in0=pt_[:], scalar=1.0, in1=gb[k][:], op0=ALU.add, op1=ALU.mult),
                            reads=[pB, B0["gb%d" % k]], writes=[dB])
                    else:
                        S.op(dve, lambda e, dst=dst, pt_=pt_, cs=cs, k=k: e.tensor_tensor(
                            out=dst[:, cs], in0=pt_[:], in1=gb[k][:], op=ALU.mult),
                            reads=[pB, B0["gb%d" % k]], writes=[dB])
            if dbg and stage == 0:
                for nm, t in [("d_gs1", gs1_b), ("d_sh1", sh1_b), ("d_gsc", gsc_b), ("d_shc", shc_b), ("d_gm", gm_b),
                              ("d_gs2", gs2_b), ("d_sh2", sh2_b), ("d_gf", gf_b)]:
                    ddump(S, nm, t[:], [128, 1024], [Bc[k] for k in Bc], cast=False)
            S.barrier()
        A.release(m_bc1)
        if stage == 0:
            return nc, dbg_outs

        qaT = A.alloc("qaT", [64, 8, 2048], BF16)
        kaT = A.alloc("kaT", [64, 2, 4352], BF16)
        va = A.alloc("va", [128, 34, 130], BF16)
        Bq = {k: Buf(k) for k in ["qaT", "kaT", "va", "cq_s", "ckv_s", "kr_s"]}
        m_attnA = A.mark()
        with ExitStack() as pes:
            W = A.alloc("W", [128, 8, C_END], BF16)
            xt = [A.alloc("xt%d" % i, [128, 1024], F32) for i in range(2)]
            junk = A.alloc("junk", [128, 1024], BF16)
            tt_ = A.alloc("tt_", [128, 1024], F32)
            hb = [A.alloc("hb%d" % i, [128, 1024], BF16) for i in range(2)]
            hTb = [A.alloc("hTb%d" % i, [128, 8, 512], BF16) for i in range(2)]
            tcb = [A.alloc("tcb%d" % i, [96, 512], F32) for i in range(2)]
            tsb = [A.alloc("tsb%d" % i, [96, 512], F32) for i in range(2)]
            r1 = [A.alloc("r1_%d" % i, [96, 512], F32) for i in range(2)]
            r2 = [A.alloc("r2_%d" % i, [96, 512], F32) for i in range(2)]
            ss = A.alloc("ss", [128, 34], F32)
            lnv = A.alloc("lnv", [128, 34], F32)
            rs = A.alloc("rs", [128, 34], F32)
            sq = A.alloc("sq", [128, 3, 512], BF16)
            rl0 = A.alloc("rl0", [128, 512], F32)
            rl1 = A.alloc("rl1", [128, 512], F32)
            cqs = [A.alloc("cqs%d" % i, [128, 3, 512], BF16) for i in range(2)]
            ckvs = [A.alloc("ckvs%d" % i, [128, 2, 512], BF16) for i in range(2)]
            krs = [A.alloc("krs%d" % i, [96, 512], BF16) for i in range(2)]
            pT = [pes.enter_context(nc.psum_tensor("pT%d" % i, [128, 1024], BF16)) for i in range(2)]
            pp = [pes.enter_context(nc.psum_tensor("pp%d" % i, [128, 512], F32)) for i in range(5)]
            pr = pes.enter_context(nc.psum_tensor("pr", [128, 512], F32))
            B1 = {k: Buf(k) for k in ["W", "xt0", "xt1", "tt_", "hb0", "hb1", "hTb0", "hTb1", "tcb0", "tcb1", "tsb0",
                                      "tsb1", "r1_0", "r1_1", "r2_0", "r2_1", "sq", "rl0", "rl1", "cqs0", "cqs1",
                                      "ckvs0", "ckvs1", "krs0", "krs1", "pT0", "pT1", "pp0", "pp1", "pp2", "pp3", "pp4",
                                      "pr"]}
            Bss = [Buf("ss%d" % i) for i in range(34)]
            Bln = [Buf("ln%d" % i) for i in range(34)]
            Brs = [Buf("rs%d" % i) for i in range(34)]
            S.dma(pool, W[:], w_inx.rearrange("(c p) n -> p c n", p=128), writes=[B1["W"]])
            S.op(pool, lambda e: e.memset(va[:, :, 64:65], 1.0), writes=[Bq["va"]])
            S.op(pool, lambda e: e.memset(va[:, :, 129:130], 1.0), writes=[Bq["va"]])
            ppi = [0]
            ropei = [0]

            def next_pp():
                i = ppi[0] % 5
                ppi[0] += 1
                return pp[i], B1["pp%d" % i]

            def projT(dst, dB, col0, M, hT, hB, ntok):
                for c in range(8):
                    S.mm(lambda e, c=c: e.matmul(dst[0:M, 0:ntok], W[:, c, col0:col0 + M], hT[:, c, 0:ntok],
                                                 start=(c == 0), stop=(c == 7)),
                         reads=[B1["W"], hB], writes=[dB], signal=(c == 7), first=(c == 0))

            def rope_evac(pa, pBa, pb, pBb, r0, r1_, ntok, k, dst_ap, dstB):
                i = ropei[0] % 2
                ropei[0] += 1
                S.op(dve, lambda e: e.tensor_tensor(out=r1[i][r0:r1_, 0:ntok], in0=pa[r0:r1_, 0:ntok],
                                                    in1=tcb[k][r0:r1_, 0:ntok], op=ALU.mult),
                     reads=[pBa, B1["tcb%d" % k]], writes=[B1["r1_%d" % i]])
                S.op(dve, lambda e: e.tensor_tensor(out=r2[i][r0:r1_, 0:ntok], in0=pb[r0:r1_, 0:ntok],
                                                    in1=tsb[k][r0:r1_, 0:ntok], op=ALU.mult),
                     reads=[pBb, B1["tsb%d" % k]], writes=[B1["r2_%d" % i]])
                S.op(dve, lambda e: e.tensor_tensor(out=dst_ap, in0=r1[i][r0:r1_, 0:ntok], in1=r2[i][r0:r1_, 0:ntok],
                                                     op=ALU.add),
                     reads=[B1["r1_%d" % i], B1["r2_%d" % i]], writes=[dstB])

            def latent_norm(pcs, nfeat, ntok, dst, dstB):
                nj = len(pcs)
                for j, (pc, pB) in enumerate(pcs):
                    S.op(act, lambda e, j=j, pc=pc: e.activation(out=sq[:, j, 0:ntok], in_=pc[:, 0:ntok], func=AF.Square),
                         reads=[pB], writes=[B1["sq"]])
                for j in range(nj):
                    S.mm(lambda e, j=j: e.matmul(pr[:, 0:ntok], ones_b[:], sq[:, j, 0:ntok], start=(j == 0),
                                                 stop=(j == nj - 1)),
                         reads=[Bc["ones_b"], B1["sq"]], writes=[B1["pr"]], signal=(j == nj - 1), first=(j == 0))
                S.op(act, lambda e: e.activation(out=rl0[:, 0:ntok], in_=pr[:, 0:ntok], func=AF.Ln, bias=EPS,
                                                 scale=1.0 / nfeat), reads=[B1["pr"]], writes=[B1["rl0"]])
                S.op(act, lambda e: e.activation(out=rl1[:, 0:ntok], in_=rl0[:, 0:ntok], func=AF.Exp, scale=-0.5),
                     reads=[B1["rl0"]], writes=[B1["rl1"]])
                for j, (pc, pB) in enumerate(pcs):
                    S.op(dve, lambda e, j=j, pc=pc: e.tensor_tensor(out=dst[:, j, 0:ntok], in0=pc[:, 0:ntok],
                                                                     in1=rl1[:, 0:ntok], op=ALU.mult),
                         reads=[pB, B1["rl1"]], writes=[dstB])

            for bi in range(9):
                ntile = 4 if bi < 8 else 2
                ntok = ntile * 128
                t0 = bi * 4
                k = bi % 2
                hT, hB = hTb[k], B1["hTb%d" % k]
                tok = slice(bi * 512, bi * 512 + ntok)
                if bi < 8:
                    S.dma(sp, tcb[k][:], tabc[:, tok], writes=[B1["tcb%d" % k]])
                    S.dma(sp, tsb[k][:], tabs[:, tok], writes=[B1["tsb%d" % k]])
                gsb, shb = (gs1_b, sh1_b) if bi < 8 else (gsc_b, shc_b)
                gsB, shB = (Bc["gs1_b"], Bc["sh1_b"]) if bi < 8 else (Bc["gsc_b"], Bc["shc_b"])
                for ti in range(ntile):
                    tt = t0 + ti
                    x_, xB = xt[tt % 2], B1["xt%d" % (tt % 2)]
                    h_, hbB = hb[tt % 2], B1["hb%d" % (tt % 2)]
                    pT_, pTB = pT[tt % 2], B1["pT%d" % (tt % 2)]
                    S.dma(sp, x_[:], xin[tt * 128:(tt + 1) * 128, :], writes=[xB])
                    S.op(act, lambda e, x_=x_, tt=tt: e.activation(out=junk[:], in_=x_[:], func=AF.Square,
                                                                   accum_out=ss[:, tt:tt + 1]),
                         reads=[xB], writes=[Bss[tt]])
                    S.op(act, lambda e, tt=tt: e.activation(out=lnv[:, tt:tt + 1], in_=ss[:, tt:tt + 1], func=AF.Ln,
                                                            bias=EPS, scale=1.0 / 1024), reads=[Bss[tt]], writes=[Bln[tt]])
                    S.op(act, lambda e, tt=tt: e.activation(out=rs[:, tt:tt + 1], in_=lnv[:, tt:tt + 1], func=AF.Exp,
                                                            scale=-0.5), reads=[Bln[tt]], writes=[Brs[tt]])
                    S.op(dve, lambda e, x_=x_, tt=tt: e.scalar_tensor_tensor(out=tt_[:], in0=x_[:], scalar=rs[:, tt:tt + 1],
                                                                             in1=gsb[:], op0=ALU.mult, op1=ALU.mult),
                         reads=[xB, Brs[tt], gsB], writes=[B1["tt_"]])
                    S.op(dve, lambda e, h_=h_: e.tensor_tensor(out=h_[:], in0=tt_[:], in1=shb[:], op=ALU.add),
                         reads=[B1["tt_"], shB], writes=[hbB])
                    for c in range(8):
                        S.mm(lambda e, c=c, h_=h_, pT_=pT_: e.transpose(pT_[:, c * 128:(c + 1) * 128],
                                                                       h_[:, c * 128:(c + 1) * 128], ident_b[:]),
                             reads=[hbB, Bc["ident_b"]], writes=[pTB], signal=(c == 7), first=(c == 0))
                    S.op(act, lambda e, pT_=pT_, ti=ti: e.copy(hT[:, :, ti * 128:(ti + 1) * 128],
                                                               pT_[:].rearrange("p (c t) -> p c t", c=8)),
                         reads=[pTB], writes=[hB])
                if bi < 4:
                    for h in range(8):
                        pa, pBa = next_pp()
                        pb, pBb = next_pp()
                        projT(pa, pBa, C_QA + h * 64, 64, hT, hB, ntok)
                        projT(pb, pBb, C_QAP + h * 64, 64, hT, hB, ntok)
                        rope_evac(pa, pBa, pb, pBb, 0, 64, ntok, k, qaT[:, h, tok], Bq["qaT"])
                for kh in range(2):
                    pa, pBa = next_pp()
                    projT(pa, pBa, C_KA + kh * 64, 64, hT, hB, ntok)
                    if bi < 8:
                        pb, pBb = next_pp()
                        projT(pb, pBb, C_KAP + kh * 64, 64, hT, hB, ntok)
                        rope_evac(pa, pBa, pb, pBb, 0, 64, ntok, k, kaT[:, kh, tok], Bq["kaT"])
                    else:
                        S.op(dve, lambda e, pa=pa, kh=kh: e.tensor_copy(kaT[:, kh, tok], pa[0:64, 0:ntok]),
                             reads=[pBa], writes=[Bq["kaT"]])
                pv, pBv = next_pp()
                for ti in range(ntile):
                    for c in range(8):
                        S.mm(lambda e, c=c, ti=ti: e.matmul(pv[:, ti * 128:(ti + 1) * 128],
                                                           hT[:, c, ti * 128:(ti + 1) * 128], W[:, c, C_VA:C_VA + 128],
                                                           start=(c == 0), stop=(c == 7)),
                             reads=[B1["W"], hB], writes=[pBv], signal=(c == 7 and ti == ntile - 1),
                             first=(c == 0 and ti == 0))
                for kh in range(2):
                    S.op(dve, lambda e, kh=kh: e.tensor_copy(
                        va[:, t0:t0 + ntile, kh * 65:kh * 65 + 64],
                        pv[:, 0:ntile * 128].rearrange("p (t x) -> p t x", t=ntile)[:, :, kh * 64:(kh + 1) * 64]),
                        reads=[pBv], writes=[Bq["va"]])
                if bi < 4:
                    pcs = []
                    for j in range(3):
                        pc, pB = next_pp()
                        projT(pc, pB, C_CQ + j * 128, 128, hT, hB, ntok)
                        pcs.append((pc, pB))
                    latent_norm(pcs, 384, ntok, cqs[k], B1["cqs%d" % k])
                    S.dma(sp, cq_s[:, :, tok], cqs[k][:], reads=[B1["cqs%d" % k]], writes=[Bq["cq_s"]])
                pcs = []
                for j in range(2):
                    pc, pB = next_pp()
                    projT(pc, pB, C_CKV + j * 128, 128, hT, hB, ntok)
                    pcs.append((pc, pB))
                latent_norm(pcs, 256, ntok, ckvs[k], B1["ckvs%d" % k])
                S.dma(sp, ckv_s[:, :, tok], ckvs[k][:, :, 0:ntok], reads=[B1["ckvs%d" % k]], writes=[Bq["ckv_s"]])
                pa, pBa = next_pp()
                projT(pa, pBa, C_KR2, 96, hT, hB, ntok)
                if bi < 8:
                    pb, pBb = next_pp()
                    projT(pb, pBb, C_KR2 + 96, 96, hT, hB, ntok)
                    rope_evac(pa, pBa, pb, pBb, 64, 96, ntok, k, krs[k][64:96, 0:ntok], B1["krs%d" % k])
                else:
                    S.op(dve, lambda e, pa=pa: e.tensor_copy(krs[k][64:96, 0:ntok], pa[64:96, 0:ntok]),
                         reads=[pBa], writes=[B1["krs%d" % k]])
                S.dma(sp, kr_s[64:96, tok], krs[k][64:96, 0:ntok], reads=[B1["krs%d" % k]], writes=[Bq["kr_s"]])
            if dbg and stage == 1:
                allB = [Bq[k] for k in Bq]
                ddump(S, "d_qaT", qaT[:], [64, 8, 2048], allB)
                ddump(S, "d_kaT", kaT[:], [64, 2, 4352], allB)
                ddump(S, "d_va", va[:], [128, 34, 130], allB)
                ddump(S, "d_cq", cq_s, [128, 3, 2048], allB)
                ddump(S, "d_ckv", ckv_s, [128, 2, 4352], allB)
                ddump(S, "d_kr", kr_s[64:96, :], [32, 4352], allB)
            S.barrier()
        A.release(m_attnA)
        if stage == 1:
            return nc, dbg_outs

        def run_attention(iters, st, stB, ptb, ptB, ot, otB, bc, bcB, rden, rdB, bcs, bcsB, fillers=None, fill_every=1):
            flat = []
            for ii, it in enumerate(iters):
                ng = len(it["groups"])
                for gi, g in enumerate(it["groups"]):
                    flat.append((ii, gi, gi == ng - 1, g))
            n = len(flat)
            nst, npt, nbc = len(st), len(ptb), len(bc)

            def QK(i):
                ii, gi, last, g = flat[i]
                it = iters[ii]
                s_, sB = st[i % nst], stB[i % nst]
                nmm = sum(1 + (1 if m is not None else 0) for (_, _, m, _, _) in g)
                cnt = 0
                for slot, (kap, kB, mask, vap, vB) in enumerate(g):
                    cnt += 1
                    S.mm(lambda e, slot=slot, kap=kap, it=it, mask=mask: e.matmul(
                        s_[:, slot * 512:(slot + 1) * 512], kap, it["q"], start=True, stop=(mask is None)),
                        reads=[kB, it["qB"]], writes=[sB], signal=(cnt == nmm), first=(cnt == 1))
                    if mask is not None:
                        cnt += 1
                        S.mm(lambda e, slot=slot, mask=mask: e.matmul(s_[:, slot * 512:(slot + 1) * 512], ident_b[:],
                                                                      mask[0], start=False, stop=True),
                             reads=[Bc["ident_b"], mask[1]], writes=[sB], signal=(cnt == nmm), first=False)

            def EXP(i):
                ii, gi, last, g = flat[i]
                it = iters[ii]
                wdt = 512 * len(g)
                s_, sB = st[i % nst], stB[i % nst]
                p_, pB = ptb[i % npt], ptB[i % npt]
                S.op(act, lambda e: e.activation(out=p_[:, 0:wdt], in_=s_[:, 0:wdt], func=AF.Exp, scale=it["scale"]),
                     reads=[sB], writes=[pB])

            def PV(i):
                ii, gi, last, g = flat[i]
                it = iters[ii]
                o_, oB = ot[ii % len(ot)], otB[ii % len(ot)]
                p_, pB = ptb[i % npt], ptB[i % npt]
                for slot, (kap, kB, mask, vap, vB) in enumerate(g):
                    lastmm = last and slot == len(g) - 1 and it.get("sink") is None
                    S.mm(lambda e, slot=slot, vap=vap: e.matmul(o_[0:65, :], vap, p_[:, slot * 512:(slot + 1) * 512],
                                                                start=(gi == 0 and slot == 0), stop=lastmm),
                         reads=[vB, pB], writes=[oB], signal=(slot == len(g) - 1), first=(gi == 0 and slot == 0))
                if last:
                    if it.get("sink") is not None:
                        sl_, sr_, sB_ = it["sink"]
                        S.mm(lambda e: e.matmul(o_[0:65, :], sl_, sr_, start=False, stop=True),
                             reads=[Bc["e64"], sB_], writes=[oB], signal=True, first=False)
                    j = ii % nbc
                    S.op(dve, lambda e: e.reciprocal(rden[j][64:65, :], o_[64:65, :]), reads=[oB], writes=[rdB[j]])
                    S.mm(lambda e: e.matmul(bc[j][0:64, :], ones_f[64:65, 0:64], rden[j][64:65, :], start=True, stop=True),
                         reads=[Bc["ones_f"], rdB[j]], writes=[bcB[j]])
                    S.op(dve, lambda e: e.tensor_copy(bcs[j][:], bc[j][0:64, :]), reads=[bcB[j]], writes=[bcsB[j]])
                    S.op(dve, lambda e: e.tensor_tensor(out=it["out"], in0=it["ovw"](o_[0:64, :]), in1=it["ovw"](bcs[j][:]),
                                                        op=ALU.mult),
                         reads=[oB, bcsB[j]], writes=[it["outB"]])

            for i in range(n + 2):
                if i < n:
                    QK(i)
                    EXP(i)
                if 0 <= i - 2 < n:
                    PV(i - 2)
                if fillers and (i % fill_every == 0):
                    if fillers:
                        fillers.popleft()()
            while fillers:
                fillers.popleft()()

        TOP = 229312
        out_aT = A.alloc_at("out_aT", [64, 8, 2048], BF16, TOP - 32768)
        A.limit = TOP - 32768
        B_oa = [Buf("oa%d" % i) for i in range(16)]
        m_attnB = A.mark()
        with ExitStack() as pes:
            maskb = A.alloc("maskb", [128, 4, 512], BF16)
            sinkf = A.alloc("sinkf", [1, 1024], F32)
            sinkb = A.alloc("sinkb", [1, 1024], BF16)
            ptb = [A.alloc("ptb%d" % i, [128, 1024], BF16) for i in range(3)]
            rden = [A.alloc("rden%d" % i, [65, 512], F32) for i in range(2)]
            bcs = [A.alloc("bcs%d" % i, [64, 512], F32) for i in range(2)]
            st = [pes.enter_context(nc.psum_tensor("st%d" % i, [128, 1024], F32)) for i in range(2)]
            ot = [pes.enter_context(nc.psum_tensor("ot%d" % i, [128, 512], F32)) for i in range(2)]
            bc = [pes.enter_context(nc.psum_tensor("bc%d" % i, [64, 512], F32)) for i in range(2)]
            B3 = {k: Buf(k) for k in ["maskb", "sinkf", "sinkb"]}
            stB = [Buf("st%d" % i) for i in range(2)]
            ptB = [Buf("pt%d" % i) for i in range(3)]
            otB = [Buf("ot%d" % i) for i in range(2)]
            bcB = [Buf("bc%d" % i) for i in range(2)]
            rdB = [Buf("rd%d" % i) for i in range(2)]
            bcsB = [Buf("bcs%d" % i) for i in range(2)]
            S.dma(pool, maskb[:], masks, writes=[B3["maskb"]])
            S.dma(sp, sinkf[:], sinkr, writes=[B3["sinkf"]])
            S.op(act, lambda e: e.activation(out=sinkb[:], in_=sinkf[:], func=AF.Exp), reads=[B3["sinkf"]],
                 writes=[B3["sinkb"]])
            iters = []
            for n_ in range(16):
                for kh in range(2):
                    Lt = n_ - 1 if n_ >= 1 else 31
                    Rt = n_ + 1 if n_ <= 14 else 16
                    mL = 0 if n_ >= 1 else 2
                    mR = 1 if n_ <= 14 else 3

                    def kt(j, m=None):
                        return (kaT[:, kh, j * 128:(j + 1) * 128], Bq["kaT"],
                                None if m is None else (maskb[:, m, :], B3["maskb"]),
                                va[:, j, kh * 65:(kh + 1) * 65], Bq["va"])
                    groups = [[kt(Lt, mL), kt(n_)], [kt(Rt, mR), kt(32)], [kt(33)]]
                    qs = slice(n_ * 128, (n_ + 1) * 128)
                    iters.append(dict(
                        q=qaT[:, 4 * kh:4 * kh + 4, qs], qB=Bq["qaT"], groups=groups, scale=A_SCALE,
                        sink=(e64[0:1, 0:65], sinkb[0:1, kh * 512:(kh + 1) * 512], B3["sinkb"]),
                        out=out_aT[:, 4 * kh:4 * kh + 4, qs], outB=B_oa[n_],
                        ovw=lambda ap: ap.rearrange("p (h q) -> p h q", h=4)))
            run_attention(iters, st, stB, ptb, ptB, ot, otB, bc, bcB, rden, rdB, bcs, bcsB)
            if dbg and stage == 2:
                ddump(S, "d_oaT", out_aT[:], [64, 8, 2048], B_oa)
            S.barrier()
        A.release(m_persist)
        if stage == 2:
            return nc, dbg_outs

        out_bT = A.alloc_at("out_bT", [64, 8, 2048], BF16, TOP - 65536)
        A.limit = TOP - 65536
        B_ob = [Buf("ob%d" % i) for i in range(4)]
        m_mla = A.mark()
        with ExitStack() as pes:
            cqT = A.alloc("cqT", [128, 3, 2048], BF16)
            ckvT = A.alloc("ckvT", [128, 2, 4352], BF16)
            krT = A.alloc("krT", [96, 4352], BF16)
            wuq = A.alloc("wuq", [128, 3, 768], BF16)
            wuqp = A.alloc("wuqp", [128, 3, 768], BF16)
            wukv = A.alloc("wukv", [128, 2, 1024], BF16)
            gq_t = A.alloc("gq_t", [128, 3], F32)
            gkv_t = A.alloc("gkv_t", [128, 2], F32)
            m_wst = A.mark()
            wst = A.alloc("wst", [128, 3, 768], F32)
            B4 = {k: Buf(k) for k in ["cqT", "ckvT", "krT", "wst", "wuq", "wuqp", "wukv", "gq", "gkv", "tcq", "tsq", "QT0",
                                      "QT1", "KT0", "KT1", "Vh0", "Vh1", "q1", "q2", "ppr"]}
            S.dma(sp, cqT[:], cq_s, reads=[Bq["cq_s"]], writes=[B4["cqT"]])
            S.dma(sp, ckvT[:], ckv_s, reads=[Bq["ckv_s"]], writes=[B4["ckvT"]])
            S.dma(sp, krT[64:96, :], kr_s[64:96, :], reads=[Bq["kr_s"]], writes=[B4["krT"]])
            S.dma(sp, gq_t[:], gq, writes=[B4["gq"]])
            S.dma(sp, gkv_t[:], gkv, writes=[B4["gkv"]])
            for (src, dstw, dB) in [(w_uq, wuq, "wuq"), (w_uqp, wuqp, "wuqp")]:
                S.dma(sp, wst[:, 0:3, :], src.rearrange("(c p) n -> p c n", p=128), writes=[B4["wst"]])
                for c in range(3):
                    S.op(dve, lambda e, c=c, dstw=dstw: e.tensor_scalar(dstw[:, c, :], wst[:, c, :], gq_t[:, c:c + 1], None,
                                                                        op0=ALU.mult),
                         reads=[B4["wst"], B4["gq"]], writes=[B4[dB]])
            for c in range(2):
                for (c0, c1) in [(0, 768), (768, 1024)]:
                    S.dma(sp, wst[:, 0, 0:c1 - c0], w_ukv[c * 128:(c + 1) * 128, c0:c1], writes=[B4["wst"]])
                    S.op(dve, lambda e, c=c, c0=c0, c1=c1: e.tensor_scalar(wukv[:, c, c0:c1], wst[:, 0, 0:c1 - c0],
                                                                           gkv_t[:, c:c + 1], None, op0=ALU.mult),
                         reads=[B4["wst"], B4["gkv"]], writes=[B4["wukv"]])
            S.barrier()
            A.release(m_wst)
            tcq = A.alloc("tcq", [96, 512], F32)
            tsq = A.alloc("tsq", [96, 512], F32)
            QT = [A.alloc("QT%d" % i, [96, 2048], BF16) for i in range(2)]
            KT = [A.alloc("KT%d" % i, [96, 4352], BF16) for i in range(2)]
            Vh = [A.alloc("Vh%d" % i, [128, 34, 65], BF16) for i in range(2)]
            q1 = A.alloc("q1", [96, 512], F32)
            q2 = A.alloc("q2", [96, 512], F32)
            ptb = [A.alloc("ptb%d" % i, [128, 1024], BF16) for i in range(3)]
            rden = [A.alloc("rden%d" % i, [65, 512], F32) for i in range(1)]
            bcs = [A.alloc("bcs%d" % i, [64, 512], F32) for i in range(1)]
            st = [pes.enter_context(nc.psum_tensor("mst%d" % i, [128, 1024], F32)) for i in range(2)]
            ot = [pes.enter_context(nc.psum_tensor("mot%d" % i, [128, 512], F32)) for i in range(2)]
            bc = [pes.enter_context(nc.psum_tensor("mbc%d" % i, [64, 512], F32)) for i in range(1)]
            ppr = pes.enter_context(nc.psum_tensor("ppr", [128, 512], F32))
            stB = [Buf("st%d" % i) for i in range(2)]
            ptB = [Buf("pt%d" % i) for i in range(3)]
            otB = [Buf("ot%d" % i) for i in range(2)]
            bcB = [Buf("bc%d" % i) for i in range(1)]
            rdB = [Buf("rd%d" % i) for i in range(1)]
            bcsB = [Buf("bcs%d" % i) for i in range(1)]
            for i in range(2):
                S.op(pool, lambda e, i=i: e.memset(Vh[i][:, :, 64:65], 1.0), writes=[B4["Vh%d" % i]])

            def prep_closures(h, hp):
                cl = []
                KTh, KB = KT[hp], B4["KT%d" % hp]
                Vhh, VB = Vh[hp], B4["Vh%d" % hp]
                QTh, QB = QT[hp], B4["QT%d" % hp]

                def kcopy():
                    S.op(dve, lambda e: e.tensor_copy(KTh[64:96, :], krT[64:96, :]), reads=[B4["krT"]], writes=[KB])
                cl.append(kcopy)
                for bi in range(9):
                    ntok = 512 if bi < 8 else 256
                    tok = slice(bi * 512, bi * 512 + ntok)

                    def kblk(tok=tok, ntok=ntok):
                        for c in range(2):
                            S.mm(lambda e, c=c: e.matmul(ppr[0:64, 0:ntok], wukv[:, c, h * 128:h * 128 + 64],
                                                         ckvT[:, c, tok], start=(c == 0), stop=(c == 1)),
                                 reads=[B4["wukv"], B4["ckvT"]], writes=[B4["ppr"]], signal=(c == 1), first=(c == 0))
                        S.op(dve, lambda e: e.tensor_copy(KTh[0:64, tok], ppr[0:64, 0:ntok]), reads=[B4["ppr"]],
                             writes=[KB])
                    cl.append(kblk)
                for g0 in range(0, 34, 8):
                    nt = min(8, 34 - g0)

                    def vblk(g0=g0, nt=nt):
                        for t in range(nt):
                            tsl = slice((g0 + t) * 128, (g0 + t + 1) * 128)
                            for c in range(2):
                                S.mm(lambda e, c=c, t=t, tsl=tsl: e.matmul(
                                    ppr[:, t * 64:(t + 1) * 64], ckvT[:, c, tsl], wukv[:, c, h * 128 + 64:h * 128 + 128],
                                    start=(c == 0), stop=(c == 1)),
                                    reads=[B4["wukv"], B4["ckvT"]], writes=[B4["ppr"]],
                                    signal=(c == 1 and t == nt - 1), first=(c == 0 and t == 0))
                        S.op(dve, lambda e: e.tensor_copy(Vhh[:, g0:g0 + nt, 0:64],
                                                          ppr[:, 0:nt * 64].rearrange("p (t d) -> p t d", t=nt)),
                             reads=[B4["ppr"]], writes=[VB])
                    cl.append(vblk)
                for qb in range(4):
                    tok = slice(qb * 512, (qb + 1) * 512)

                    def qa_(tok=tok):
                        S.dma(sp, tcq[64:96, :], tabc[64:96, tok], writes=[B4["tcq"]])
                        S.dma(sp, tsq[64:96, :], tabs[64:96, tok], writes=[B4["tsq"]])
                        for c in range(3):
                            S.mm(lambda e, c=c: e.matmul(ppr[0:96, :], wuq[:, c, h * 96:(h + 1) * 96], cqT[:, c, tok],
                                                         start=(c == 0), stop=(c == 2)),
                                 reads=[B4["wuq"], B4["cqT"]], writes=[B4["ppr"]], signal=(c == 2), first=(c == 0))
                        S.op(dve, lambda e: e.tensor_copy(QTh[0:64, tok], ppr[0:64, :]), reads=[B4["ppr"]], writes=[QB])
                        S.op(dve, lambda e: e.tensor_tensor(out=q1[64:96, :], in0=ppr[64:96, :], in1=tcq[64:96, :],
                                                            op=ALU.mult), reads=[B4["ppr"], B4["tcq"]], writes=[B4["q1"]])

                    def qb_(tok=tok):
                        for c in range(3):
                            S.mm(lambda e, c=c: e.matmul(ppr[0:96, :], wuqp[:, c, h * 96:(h + 1) * 96], cqT[:, c, tok],
                                                         start=(c == 0), stop=(c == 2)),
                                 reads=[B4["wuqp"], B4["cqT"]], writes=[B4["ppr"]], signal=(c == 2), first=(c == 0))
                        S.op(dve, lambda e: e.tensor_tensor(out=q2[64:96, :], in0=ppr[64:96, :], in1=tsq[64:96, :],
                                                            op=ALU.mult), reads=[B4["ppr"], B4["tsq"]], writes=[B4["q2"]])
                        S.op(dve, lambda e: e.tensor_tensor(out=QTh[64:96, tok], in0=q1[64:96, :], in1=q2[64:96, :],
                                                             op=ALU.add), reads=[B4["q1"], B4["q2"]], writes=[QB])
                    cl.append(qa_)
                    cl.append(qb_)
                return cl

            for f in prep_closures(0, 0):
                f()
            for h in range(8):
                hp = h % 2
                iters = []
                for qb in range(4):
                    tok = slice(qb * 512, (qb + 1) * 512)
                    groups = []
                    for g in range(17):
                        groups.append([(KT[hp][:, j * 128:(j + 1) * 128], B4["KT%d" % hp], None,
                                        Vh[hp][:, j, 0:65], B4["Vh%d" % hp]) for j in (2 * g, 2 * g + 1)])
                    iters.append(dict(q=QT[hp][:, tok], qB=B4["QT%d" % hp], groups=groups, scale=MLA_SCALE, sink=None,
                                      out=out_bT[:, h, tok], outB=B_ob[qb], ovw=lambda ap: ap))
                fl = deque(prep_closures(h + 1, 1 - hp)) if h < 7 else None
                run_attention(iters, st, stB, ptb, ptB, ot, otB, bc, bcB, rden, rdB, bcs, bcsB, fillers=fl, fill_every=2)
            if dbg and stage == 3:
                ddump(S, "d_obT", out_bT[:], [64, 8, 2048], B_ob)
            S.barrier()
        A.release(m_mla)
        if stage == 3:
            return nc, dbg_outs

        h2T = A.alloc("h2T", [128, 8, 2048], BF16)
        Wr = A.alloc("Wr", [128, 16, 32], F32)
        B_h2 = [Buf("h2T%d" % i) for i in range(4)]
        B_wr = [Buf("Wr%d" % i) for i in range(16)]
        B_x1 = [Buf("x1s%d" % i) for i in range(16)]
        m_p5 = A.mark()
        with ExitStack() as pes:
            wo = A.alloc("wo", [64, 16, 1024], BF16)
            wrb = A.alloc("wrb", [128, 8, 32], BF16)
            xt = [A.alloc("xt%d" % i, [128, 1024], F32) for i in range(2)]
            x1t = [A.alloc("x1t%d" % i, [128, 1024], F32) for i in range(2)]
            ttA = [A.alloc("ttA%d" % i, [128, 1024], F32) for i in range(2)]
            ttB = [A.alloc("ttB%d" % i, [128, 1024], F32) for i in range(2)]
            junk = A.alloc("junk5", [128, 1024], BF16)
            hb = [A.alloc("h2b%d" % i, [128, 1024], BF16) for i in range(2)]
            sm = A.alloc("sm5", [128, 16, 8], F32)
            lg = A.alloc("lg", [128, 512], F32)
            rk = A.alloc("rk", [128, 512], F32)
            mk = A.alloc("mk", [128, 512], F32)
            ex = A.alloc("ex", [128, 512], F32)
            m1 = A.alloc("m1", [128, 16], F32)
            thr = A.alloc("thr", [128, 16], F32)
            br16 = A.alloc("br16", [1, 512], BF16)
            py = [pes.enter_context(nc.psum_tensor("py%d" % i, [128, 1024], F32)) for i in range(2)]
            pT = [pes.enter_context(nc.psum_tensor("pT5_%d" % i, [128, 1024], BF16)) for i in range(2)]
            plg = pes.enter_context(nc.psum_tensor("plg", [128, 512], F32))
            B5 = {k: Buf(k) for k in ["wo", "wrb", "brb", "xt0", "xt1", "x1t0", "x1t1", "ttA0", "ttA1", "ttB0", "ttB1", "hb0", "hb1", "lg", "rk", "m1", "thr",
                                      "mk", "ex", "py0", "py1", "pT0", "pT1", "plg"]}
            Bsm = [[Buf("sm%d_%d" % (i, j)) for j in range(8)] for i in range(16)]
            S.dma(pool, wo[:], w_o.rearrange("(h p) n -> p h n", p=64), writes=[B5["wo"]])
            S.dma(pool, wrb[:], w_r.rearrange("(c p) n -> p c n", p=128), writes=[B5["wrb"]])
            S.dma(pool, br16[:], b_r16, writes=[B5["brb"]])
            S.mm(lambda e: e.matmul(plg[:], ones_b[0:1, :], br16[0:1, :], start=True, stop=False),
                 reads=[Bc["ones_b"], B5["brb"]], writes=[B5["plg"]], signal=True, first=True)
            def stage_a(tt):
                k = tt % 2
                tsl = slice(tt * 128, (tt + 1) * 128)
                y_, yB = py[k], B5["py%d" % k]
                x_, xB = xt[k], B5["xt%d" % k]
                x1_, x1B = x1t[k], B5["x1t%d" % k]
                h_, hbB = hb[k], B5["hb%d" % k]
                pT_, pTB = pT[k], B5["pT%d" % k]
                smt = sm[:, tt, :]
                S.dma(sp, x_[:], xin[tsl, :], writes=[xB])
                for cb in range(2):
                    for h in range(16):
                        src = out_aT if h < 8 else out_bT
                        sB = B_oa[tt] if h < 8 else B_ob[tt // 4]
                        S.mm(lambda e, cb=cb, h=h, src=src: e.matmul(y_[:, cb * 512:(cb + 1) * 512], src[:, h % 8, tsl],
                                                                     wo[:, h, cb * 512:(cb + 1) * 512],
                                                                     start=(h == 0), stop=(h == 15)),
                             reads=[sB, B5["wo"]], writes=[yB], signal=(h == 15 and cb == 1), first=(h == 0 and cb == 0))
                S.op(act, lambda e, y_=y_, tt=tt: e.activation(out=junk[:], in_=y_[:], func=AF.Square,
                                                               accum_out=sm[:, tt, 0:1]), reads=[yB], writes=[Bsm[tt][0]])
                S.op(act, lambda e, tt=tt: e.activation(out=sm[:, tt, 1:2], in_=sm[:, tt, 0:1], func=AF.Ln, bias=EPS,
                                                        scale=1.0 / 1024), reads=[Bsm[tt][0]], writes=[Bsm[tt][1]])
                S.op(act, lambda e, tt=tt: e.activation(out=sm[:, tt, 2:3], in_=sm[:, tt, 1:2], func=AF.Exp, scale=-0.5),
                     reads=[Bsm[tt][1]], writes=[Bsm[tt][2]])
                S.op(dve, lambda e, y_=y_, tt=tt: e.scalar_tensor_tensor(out=ttA[k][:], in0=y_[:], scalar=sm[:, tt, 2:3],
                                                                         in1=gm_b[:], op0=ALU.mult, op1=ALU.mult),
                     reads=[yB, Bsm[tt][2], Bc["gm_b"]], writes=[B5["ttA%d" % k]])
                S.op(dve, lambda e, x_=x_, x1_=x1_: e.tensor_tensor(out=x1_[:], in0=ttA[k][:], in1=x_[:], op=ALU.add),
                     reads=[B5["ttA%d" % k], xB], writes=[x1B])
                S.dma(sp, x1_s[tsl, :], x1_[:], reads=[x1B], writes=[B_x1[tt]])

            def stage_b(tt):
                k = tt % 2
                tsl = slice(tt * 128, (tt + 1) * 128)
                y_, yB = py[k], B5["py%d" % k]
                x_, xB = xt[k], B5["xt%d" % k]
                x1_, x1B = x1t[k], B5["x1t%d" % k]
                h_, hbB = hb[k], B5["hb%d" % k]
                pT_, pTB = pT[k], B5["pT%d" % k]
                S.op(act, lambda e, x1_=x1_, tt=tt: e.activation(out=junk[:], in_=x1_[:], func=AF.Square,
                                                                 accum_out=sm[:, tt, 3:4]), reads=[x1B], writes=[Bsm[tt][3]])
                S.op(act, lambda e, tt=tt: e.activation(out=sm[:, tt, 4:5], in_=sm[:, tt, 3:4], func=AF.Ln, bias=EPS,
                                                        scale=1.0 / 1024), reads=[Bsm[tt][3]], writes=[Bsm[tt][4]])
                S.op(act, lambda e, tt=tt: e.activation(out=sm[:, tt, 5:6], in_=sm[:, tt, 4:5], func=AF.Exp, scale=-0.5),
                     reads=[Bsm[tt][4]], writes=[Bsm[tt][5]])
                S.op(dve, lambda e, x1_=x1_, tt=tt: e.scalar_tensor_tensor(out=ttB[k][:], in0=x1_[:], scalar=sm[:, tt, 5:6],
                                                                           in1=gs2_b[:], op0=ALU.mult, op1=ALU.mult),
                     reads=[x1B, Bsm[tt][5], Bc["gs2_b"]], writes=[B5["ttB%d" % k]])
                S.op(dve, lambda e, h_=h_: e.tensor_tensor(out=h_[:], in0=ttB[k][:], in1=sh2_b[:], op=ALU.add),
                     reads=[B5["ttB%d" % k], Bc["sh2_b"]], writes=[hbB])
                for c in range(8):
                    S.mm(lambda e, c=c, h_=h_, pT_=pT_: e.transpose(pT_[:, c * 128:(c + 1) * 128],
                                                                   h_[:, c * 128:(c + 1) * 128], ident_b[:]),
                         reads=[hbB, Bc["ident_b"]], writes=[pTB], signal=(c == 7), first=(c == 0))
                S.op(dve, lambda e, pT_=pT_: e.tensor_copy(h2T[:, :, tsl], pT_[:].rearrange("p (c t) -> p c t", c=8)),
                     reads=[pTB], writes=[B_h2[tt // 4]])
                for c in range(8):
                    S.mm(lambda e, c=c, tt=tt: e.matmul(plg[:, tt * 32:(tt + 1) * 32], h2T[:, c, tsl], wrb[:, c, :],
                                                        start=False, stop=(c == 7 and tt == 15)),
                         reads=[B_h2[tt // 4], B5["wrb"]], writes=[B5["plg"]], signal=(c == 7), first=False)

            stage_a(0)
            for tt in range(16):
                if tt + 1 < 16:
                    stage_a(tt + 1)
                stage_b(tt)
            v3 = lambda t: t[:].rearrange("p (t x) -> p t x", t=16)
            bc3 = lambda t: t[:].unsqueeze(2).to_broadcast([128, 16, 32])
            S.op(dve, lambda e: e.tensor_copy(lg[:], plg[:]), reads=[B5["plg"]], writes=[B5["lg"]])
            S.op(dve, lambda e: e.tensor_copy(rk[:], lg[:]), reads=[B5["lg"]], writes=[B5["rk"]])
            for kk in range(4):
                mdst = m1 if kk == 0 else thr
                mB = B5["m1"] if kk == 0 else B5["thr"]
                S.op(dve, lambda e, mdst=mdst: e.tensor_reduce(out=mdst[:], in_=v3(rk), axis=mybir.AxisListType.X,
                                                               op=ALU.max), reads=[B5["rk"]], writes=[mB])
                if kk < 3:
                    S.op(dve, lambda e, mdst=mdst: e.tensor_tensor(out=v3(mk), in0=v3(rk), in1=bc3(mdst), op=ALU.is_equal),
                         reads=[B5["rk"], mB], writes=[B5["mk"]])
                    S.op(dve, lambda e: e.scalar_tensor_tensor(out=rk[:], in0=mk[:], scalar=-1.0e9, in1=rk[:],
                                                               op0=ALU.mult, op1=ALU.add),
                         reads=[B5["mk"], B5["rk"]], writes=[B5["rk"]])
            S.op(dve, lambda e: e.tensor_tensor(out=v3(mk), in0=v3(lg), in1=bc3(thr), op=ALU.is_ge),
                 reads=[B5["lg"], B5["thr"]], writes=[B5["mk"]])
            S.op(dve, lambda e: e.tensor_tensor(out=v3(rk), in0=v3(lg), in1=bc3(m1), op=ALU.subtract),
                 reads=[B5["lg"], B5["m1"]], writes=[B5["rk"]])
            S.op(act, lambda e: e.activation(out=ex[:], in_=rk[:], func=AF.Exp), reads=[B5["rk"]], writes=[B5["ex"]])
            S.op(dve, lambda e: e.tensor_tensor(out=ex[:], in0=ex[:], in1=mk[:], op=ALU.mult),
                 reads=[B5["ex"], B5["mk"]], writes=[B5["ex"]])
            S.op(dve, lambda e: e.tensor_reduce(out=thr[:], in_=v3(ex), axis=mybir.AxisListType.X, op=ALU.add),
                 reads=[B5["ex"]], writes=[B5["thr"]])
            S.op(dve, lambda e: e.reciprocal(m1[:], thr[:]), reads=[B5["thr"]], writes=[B5["m1"]])
            S.op(dve, lambda e: e.tensor_tensor(out=Wr[:], in0=v3(ex), in1=bc3(m1), op=ALU.mult),
                 reads=[B5["ex"], B5["m1"]], writes=B_wr)
            if dbg and stage == 4:
                ddump(S, "d_h2T", h2T[:], [128, 8, 2048], B_h2)
                ddump(S, "d_Wr", Wr[:], [128, 16, 32], B_wr, cast=False)
                ddump(S, "d_x1", x1_s, [2048, 1024], B_x1, cast=False)
            S.barrier()
        A.release(m_p5)
        if stage == 4:
            return nc, dbg_outs

        A.limit = TOP
        facc = A.alloc("facc", [128, 16, 1024], F32)
        B_fa = [Buf("fa%d" % i) for i in range(16)]
        m_moe = A.mark()
        with ExitStack() as pes:
            bguT = A.alloc("bguT", [128, 16, 32], F32)
            bg7 = A.alloc("bg7", [128, 8, 32], F32)
            bln = A.alloc("bln", [128, 8, 32], F32)
            sgb = A.alloc("sgb", [128, 1], F32)
            B6 = {k: Buf(k) for k in ["GUa", "GUb", "DN0", "DN1", "bd0", "bd1", "actT0", "actT1", "T1_0", "T1_1", "T2_0", "T2_1", "T3_0",
                                      "T3_1", "bgu_t", "bguT", "pgl0", "pgl1", "pgl2", "pgl3", "po0", "po1"]}
            pgl = [pes.enter_context(nc.psum_tensor("pgl%d" % i, [128, 512], F32)) for i in range(4)]
            po = [pes.enter_context(nc.psum_tensor("po%d" % i, [128, 1024], F32)) for i in range(2)]
            m_bgu = A.mark()
            bgu_t = A.alloc("bgu_t", [32, 2048], F32)
            S.dma(sp, bgu_t[:], b_gu, writes=[B6["bgu_t"]])
            for c in range(16):
                S.mm(lambda e, c=c: e.transpose(pgl[0][:, c * 32:(c + 1) * 32], bgu_t[:, c * 128:(c + 1) * 128],
                                                ident_f[0:32, 0:32]),
                     reads=[B6["bgu_t"], Bc["ident_f"]], writes=[B6["pgl0"]], signal=(c == 15), first=(c == 0))
            S.op(dve, lambda e: e.tensor_copy(bguT[:], pgl[0][:].rearrange("p (c x) -> p c x", c=16)),
                 reads=[B6["pgl0"]], writes=[B6["bguT"]])
            S.op(dve, lambda e: e.tensor_scalar(bg7[:], bguT[:, 0:8, :], -1.0, 7.0, op0=ALU.mult, op1=ALU.add),
                 reads=[B6["bguT"]], writes=[B6["bguT"]])
            S.op(dve, lambda e: e.tensor_scalar(bln[:], bguT[:, 8:16, :], -1.0, None, op0=ALU.mult),
                 reads=[B6["bguT"]], writes=[B6["bguT"]])
            S.op(dve, lambda e: e.memset(sgb[:], 11.914), writes=[B6["bguT"]])
            WrT = A.alloc("WrT", [32, 16, 128], BF16)
            bdn = A.alloc("bdn", [32, 1024], BF16)
            B6["WrT"] = Buf("WrT")
            B6["bdn"] = Buf("bdn")
            S.dma(pool, bdn[:], b_dn, writes=[B6["bdn"]])
            for q4 in range(4):
                for t4 in range(4):
                    tt = q4 * 4 + t4
                    S.mm(lambda e, q4=q4, t4=t4, tt=tt: e.transpose(pgl[q4][0:32, t4 * 128:(t4 + 1) * 128], Wr[:, tt, :],
                                                                    ident_f[:]),
                         reads=[B_wr[tt], Bc["ident_f"]], writes=[B6["pgl%d" % q4]], signal=(t4 == 3), first=(t4 == 0))
                S.op(dve, lambda e, q4=q4: e.tensor_copy(WrT[:, q4 * 4:(q4 + 1) * 4, :],
                                                         pgl[q4][0:32, :].rearrange("p (t x) -> p t x", t=4)),
                     reads=[B6["pgl%d" % q4]], writes=[B6["WrT"]])
            for tt in range(16):
                o_, oB = po[tt % 2], B6["po%d" % (tt % 2)]
                for cb in range(2):
                    cs = slice(cb * 512, (cb + 1) * 512)
                    S.mm(lambda e, tt=tt, cs=cs, o_=o_: e.matmul(o_[:, cs], WrT[:, tt, :], bdn[:, cs], start=True, stop=True),
                         reads=[B6["WrT"], B6["bdn"]], writes=[oB], signal=(cb == 1), first=(cb == 0))
                S.op(act, lambda e, tt=tt, o_=o_: e.copy(facc[:, tt, :], o_[:]), reads=[oB], writes=[B_fa[tt]])
            S.barrier()
            A.release(m_bgu)
            GU = A.alloc("GU", [128, 8, 2048], BF16)
            DNs = [A.alloc("DN0", [128, 8, 1024], BF16)] * 2
            actT = [A.alloc("actT%d" % i, [128, 8, 512], BF16) for i in range(2)]
            T1 = [A.alloc("T1_%d" % i, [128, 512], F32) for i in range(2)]
            T2 = [A.alloc("T2_%d" % i, [128, 512], F32) for i in range(2)]
            T3 = [A.alloc("T3_%d" % i, [128, 512], F32) for i in range(2)]
            pair_i = [0]

            def load_gu(e_, half):
                wv = w_gu[e_].rearrange("(c p) n -> p c n", p=128)
                hb_ = "GUa" if half == 0 else "GUb"
                for base in (0, 1024):
                    c0 = base + half * 512
                    S.dma(pool, GU[:, :, c0:c0 + 512], wv[:, :, c0:c0 + 512], writes=[B6[hb_]])

            def load_dn(e_):
                S.dma(pool, DNs[e_ % 2][:], w_dn[e_].rearrange("(c p) n -> p c n", p=128), writes=[B6["DN0"]])

            def gu_step(e_, tb, mid=None):
                tok = slice(tb * 512, (tb + 1) * 512)
                aT, aB = actT[tb % 2], B6["actT%d" % (tb % 2)]
                for j in range(8):
                    if j == 4 and mid is not None:
                        mid()
                    guB = B6["GUa"] if j < 4 else B6["GUb"]
                    i = pair_i[0] % 2
                    pair_i[0] += 1
                    pg_, pgB = pgl[2 * i], B6["pgl%d" % (2 * i)]
                    pl_, plB = pgl[2 * i + 1], B6["pgl%d" % (2 * i + 1)]
                    for gi_, (dst, dB, col0) in enumerate([(pg_, pgB, j * 128), (pl_, plB, 1024 + j * 128)]):
                        for c in range(8):
                            S.mm(lambda e, c=c, dst=dst, col0=col0: e.matmul(dst[:], GU[:, c, col0:col0 + 128],
                                                                            h2T[:, c, tok], start=(c == 0), stop=(c == 7)),
                                 reads=[guB, B_h2[tb]], writes=[dB], signal=(c == 7 and gi_ == 1), first=(c == 0))
                    t1, t2, t3 = T1[i], T2[i], T3[i]
                    b1, b2, b3 = B6["T1_%d" % i], B6["T2_%d" % i], B6["T3_%d" % i]
                    S.op(act, lambda e, t1=t1, pg_=pg_, j=j: e.activation(out=t1[:], in_=pg_[:], func=AF.Relu,
                                                                          bias=bg7[:, j, e_:e_ + 1], scale=-1.0),
                         reads=[pgB, B6["bguT"]], writes=[b1])
                    S.op(act, lambda e, t1=t1, t3=t3: e.activation(out=t3[:], in_=t1[:], func=AF.Sigmoid, bias=sgb[:, 0:1],
                                                                   scale=-1.702),
                         reads=[b1, B6["bguT"]], writes=[b3])
                    S.op(act, lambda e, t2=t2, pl_=pl_, j=j: e.activation(out=t2[:], in_=pl_[:], func=AF.Identity,
                                                                          bias=bln[:, j, e_:e_ + 1], scale=-1.0),
                         reads=[plB, B6["bguT"]], writes=[b2])
                    S.op(dve, lambda e, t2=t2: e.tensor_scalar(t2[:], t2[:], 7.0, -7.0, op0=ALU.min, op1=ALU.max),
                         reads=[b2], writes=[b2])
                    S.op(dve, lambda e, t1=t1, t3=t3: e.scalar_tensor_tensor(out=t1[:], in0=t1[:], scalar=7.0, in1=t3[:],
                                                                             op0=ALU.subtract, op1=ALU.mult),
                         reads=[b1, b3], writes=[b1])
                    S.op(dve, lambda e, t1=t1, t2=t2, j=j: e.scalar_tensor_tensor(out=aT[:, j, :], in0=t2[:], scalar=1.0,
                                                                                  in1=t1[:], op0=ALU.subtract, op1=ALU.mult),
                         reads=[b1, b2], writes=[aB])

            def dn_step(e_, tb):
                aT, aB = actT[tb % 2], B6["actT%d" % (tb % 2)]
                DN, dnB = DNs[0], B6["DN0"]
                for ti in range(4):
                    tt = tb * 4 + ti
                    o_, oB = po[tt % 2], B6["po%d" % (tt % 2)]
                    for cb in range(2):
                        cs = slice(cb * 512, (cb + 1) * 512)
                        for j in range(8):
                            S.mm(lambda e, j=j, cs=cs, ti=ti: e.matmul(o_[:, cs], aT[:, j, ti * 128:(ti + 1) * 128],
                                                                      DN[:, j, cs], start=(j == 0), stop=(j == 7)),
                                 reads=[aB, dnB], writes=[oB], signal=(j == 7 and cb == 1), first=(j == 0 and cb == 0))
                    if True:
                        S.op(dve, lambda e, tt=tt: e.scalar_tensor_tensor(out=facc[:, tt, :], in0=o_[:],
                                                                          scalar=Wr[:, tt, e_:e_ + 1], in1=facc[:, tt, :],
                                                                          op0=ALU.mult, op1=ALU.add),
                             reads=[oB, B_wr[tt], B_fa[tt]], writes=[B_fa[tt]])

            load_gu(0, 0)
            load_gu(0, 1)
            load_dn(0)
            for e_ in range(n_experts):
                nxt = e_ + 1 < n_experts
                gu_step(e_, 0)
                gu_step(e_, 1)
                dn_step(e_, 0)
                gu_step(e_, 2)
                dn_step(e_, 1)
                gu_step(e_, 3, mid=(lambda e_=e_: load_gu(e_ + 1, 0)) if nxt else None)
                if nxt:
                    load_gu(e_ + 1, 1)
                dn_step(e_, 2)
                dn_step(e_, 3)
                if nxt:
                    load_dn(e_ + 1)
            S.barrier()
        A.release(m_moe)

        with ExitStack() as pes:
            x1t = [A.alloc("x1f%d" % i, [128, 1024], F32) for i in range(2)]
            ot_ = [A.alloc("of%d" % i, [128, 1024], F32) for i in range(2)]
            tt_ = A.alloc("tt7", [128, 1024], F32)
            junk = A.alloc("junk7", [128, 1024], BF16)
            sm = A.alloc("sm7", [128, 16, 4], F32)
            B7 = {k: Buf(k) for k in ["x1f0", "x1f1", "of0", "of1", "tt"]}
            Bsm = [[Buf("sm7_%d_%d" % (i, j)) for j in range(3)] for i in range(16)]
            outs = []
            for tt in range(16):
                k = tt % 2
                tsl = slice(tt * 128, (tt + 1) * 128)
                S.dma(sp, x1t[k][:], x1_s[tsl, :], reads=[B_x1[tt]], writes=[B7["x1f%d" % k]])
                S.op(act, lambda e, tt=tt: e.activation(out=junk[:], in_=facc[:, tt, :], func=AF.Square,
                                                        accum_out=sm[:, tt, 0:1]), reads=[B_fa[tt]], writes=[Bsm[tt][0]])
                S.op(act, lambda e, tt=tt: e.activation(out=sm[:, tt, 1:2], in_=sm[:, tt, 0:1], func=AF.Ln, bias=EPS,
                                                        scale=1.0 / 1024), reads=[Bsm[tt][0]], writes=[Bsm[tt][1]])
                S.op(act, lambda e, tt=tt: e.activation(out=sm[:, tt, 2:3], in_=sm[:, tt, 1:2], func=AF.Exp, scale=-0.5),
                     reads=[Bsm[tt][1]], writes=[Bsm[tt][2]])
                S.op(dve, lambda e, tt=tt: e.scalar_tensor_tensor(out=tt_[:], in0=facc[:, tt, :], scalar=sm[:, tt, 2:3],
                                                                  in1=gf_b[:], op0=ALU.mult, op1=ALU.mult),
                     reads=[B_fa[tt], Bsm[tt][2], Bc["gf_b"]], writes=[B7["tt"]])
                S.op(dve, lambda e, k=k: e.tensor_tensor(out=ot_[k][:], in0=tt_[:], in1=x1t[k][:], op=ALU.add),
                     reads=[B7["tt"], B7["x1f%d" % k]], writes=[B7["of%d" % k]])
                outs.append(S.dma(sp, out[tsl, :], ot_[k][:], reads=[B7["of%d" % k]]))
            S.barrier()
        build_program.stats = dict(n_inst=S.n_inst, sbuf_peak=A.peak, auto_sbuf_left=nc.sbuf_bytes_remaining)
    return nc, dbg_outs


def _rope_tables(rot_dim):
    rows = 64
    row = np.repeat(np.arange(rows, dtype=np.float32), 64)
    col = np.tile(np.arange(64, dtype=np.float32), rows)
    quarter = rot_dim // 4
    inv = (np.float32(10000.0) ** (-np.arange(quarter, dtype=np.float32) / np.float32(quarter))).astype(np.float32)
    ang = np.concatenate([row[:, None] * inv, col[:, None] * inv], axis=-1).astype(np.float32)
    return np.cos(ang).astype(np.float32), np.sin(ang).astype(np.float32)


def _const_tables():
    ca, sa = _rope_tables(64)
    cb, sb = _rope_tables(32)
    tc = np.zeros((96, 4096), np.float32)
    ts = np.zeros((96, 4096), np.float32)
    tc[0:32] = ca.T
    tc[32:64] = ca.T
    ts[0:32] = -sa.T
    ts[32:64] = sa.T
    tc[64:80] = cb.T
    tc[80:96] = cb.T
    ts[64:80] = -sb.T
    ts[80:96] = sb.T
    return tc, ts


def _masks(s):
    NEG = -30000.0
    kk = np.arange(128)[:, None]
    ii = np.arange(128)[None, :]
    mL = np.where(ii <= kk, 0.0, NEG).astype(np.float32)
    mR = np.where(kk <= ii, 0.0, NEG).astype(np.float32)
    allneg = np.full((128, 128), NEG, np.float32)
    mLe = allneg if s == 0 else mL
    mRe = mR if s == 0 else allneg
    m = np.stack([np.tile(x, (1, 4)) for x in (mL, mR, mLe, mRe)], axis=1)
    return np.ascontiguousarray(m)


def _swap_halves(w, width):
    n = w.shape[1] // width
    w3 = w.reshape(w.shape[0], n, width)
    h = width // 2
    return np.concatenate([w3[:, :, h:], w3[:, :, :h]], axis=2).reshape(w.shape[0], n * width)


def prep_inputs(I):
    f = lambda a: np.ascontiguousarray(np.asarray(a, dtype=np.float32))
    w_in = f(I["w_in"][0])
    qa, ka = w_in[:, 0:512], w_in[:, 512:640]
    kr = w_in[:, 1408:1440]
    z64 = np.zeros((1024, 64), np.float32)
    w_inx = np.concatenate([w_in[:, 0:1408], z64, kr, z64, _swap_halves(kr, 32), _swap_halves(qa, 64),
                            _swap_halves(ka, 64)], axis=1)
    assert w_inx.shape[1] == C_END
    w_uq = f(I["w_uq"][0])
    wq3 = w_uq.reshape(384, 8, 96)
    w_uqp = np.zeros_like(wq3)
    w_uqp[:, :, 64:96] = _swap_halves(wq3[:, :, 64:96].reshape(384, 256), 32).reshape(384, 8, 32)
    w_uqp = np.ascontiguousarray(w_uqp.reshape(384, 768))
    rows = np.concatenate([f(I["g_mix_pre"][0]), f(I["g_mix_post"][0]), f(I["g_ffn_pre"][0]), f(I["g_ffn_post"][0]),
                           f(I["b_ada"][0])])[None, :]
    tc, ts = _const_tables()
    shared = dict(
        w_ada=f(I["w_ada"][0]), rows=np.ascontiguousarray(rows), w_inx=np.ascontiguousarray(w_inx),
        sinkr=np.ascontiguousarray(np.repeat(f(I["sink"][0]), 128)[None, :]),
        gq=np.ascontiguousarray(f(I["g_q_a"][0]).reshape(3, 128).T), gkv=np.ascontiguousarray(f(I["g_kv_a"][0]).reshape(2, 128).T),
        w_uq=w_uq, w_uqp=w_uqp, w_ukv=f(I["w_ukv"][0]), w_o=f(I["w_o"][0]), w_r=f(I["w_router"][0]),
        b_r16=np.ascontiguousarray(np.tile(f(I["b_router"][0]), 16)[None, :]), w_gu=f(I["w_gate_up"][0]), b_gu=f(I["b_gate_up"][0]), w_dn=f(I["w_down"][0]),
        b_dn=f(I["b_down"][0]), ident=np.eye(128, dtype=np.float32))
    x = np.asarray(I["x"], dtype=np.float32)
    ctx = np.asarray(I["ctx"], dtype=np.float32)
    c = np.asarray(I["c"], dtype=np.float32)
    c_ctx = np.asarray(I["c_ctx"], dtype=np.float32)
    maps = []
    for core in range(8):
        b, s = core // 2, core % 2
        own = x[b, s * 2048:(s + 1) * 2048]
        oth = x[b, (1 - s) * 2048:(2 - s) * 2048]
        xin = np.ascontiguousarray(np.concatenate([own, oth, ctx[b]], axis=0))
        cvec = np.ascontiguousarray(np.concatenate([c[b].reshape(8, 128).T, c_ctx.reshape(8, 128).T], axis=1))
        order = np.concatenate([np.arange(s * 2048, (s + 1) * 2048), np.arange((1 - s) * 2048, (2 - s) * 2048)])
        m = dict(shared)
        m.update(xin=xin, cvec=cvec, tabc=np.ascontiguousarray(tc[:, order]), tabs=np.ascontiguousarray(ts[:, order]),
                 masks=_masks(s))
        maps.append(m)
    return maps


_CACHE = {}


def kernel(**inputs):
    if "nc" not in _CACHE:
        _CACHE["nc"] = build_program()[0]
    nc = _CACHE["nc"]
    maps = prep_inputs(inputs)
    res = run_bass_kernel_spmd(nc, maps, core_ids=list(range(8)))
    outp = np.zeros((4, 4096, 1024), np.float32)
    for core in range(8):
        b, s = core // 2, core % 2
        outp[b, s * 2048:(s + 1) * 2048] = res.results[core]["out"]
    return outp
```

```python
import numpy as np
from contextlib import ExitStack
from collections import deque
import concourse.bass as bass
import concourse.mybir as mybir
from concourse.bass_utils import run_bass_kernel_spmd

F32 = mybir.dt.float32
BF16 = mybir.dt.bfloat16
AF = mybir.ActivationFunctionType
ALU = mybir.AluOpType

import os
DBG_SKIP_ROUTER = os.environ.get("DBG_SKIP_ROUTER") == "1"
EPS = 1e-6
A_SCALE = 64 ** -0.5
MLA_SCALE = 96 ** -0.5
C_QA, C_KA, C_VA, C_CQ, C_CKV, C_KR2, C_QAP, C_KAP, C_END = 0, 512, 640, 768, 1152, 1408, 1600, 2112, 2240


class Buf:
    __slots__ = ("name", "w", "r")

    def __init__(self, name):
        self.name = name
        self.w = None
        self.r = []


class Eng:
    def __init__(self, S, name, obj, is_pe=False, n_dma=0):
        self.name = name
        self.obj = obj
        self.sem = S.new_sem("e_" + name)
        self.count = 0
        self.seen = {}
        self.is_pe = is_pe
        self.pend_r = []
        self.pend_w = []
        self.dma_sems = [[S.new_sem("d_%s%d" % (name, i)), 0] for i in range(n_dma)]
        self.dma_rr = 0


class Sched:
    def __init__(self, nc, es):
        self.nc = nc
        self.es = es
        self.pe = Eng(self, "pe", nc.tensor, is_pe=True)
        self.dve = Eng(self, "dve", nc.vector)
        self.act = Eng(self, "act", nc.scalar)
        self.pool = Eng(self, "pool", nc.gpsimd, n_dma=16)
        self.sp = Eng(self, "sp", nc.sync, n_dma=24)
        self.engs = [self.pe, self.dve, self.act, self.pool, self.sp]
        self.n_inst = 0

    def new_sem(self, name):
        return self.es.enter_context(self.nc.semaphore(name))

    def _wait(self, eng, ev):
        sem, val = ev
        k = id(sem)
        if eng.seen.get(k, 0) >= val:
            return
        eng.obj.wait_ge(sem, val)
        eng.seen[k] = val
        self.n_inst += 1

    def _deps(self, eng, reads, writes):
        for b in reads:
            if b.w is not None and not (eng.is_pe and b.w[0] is eng.sem):
                self._wait(eng, b.w)
        for b in writes:
            if b.w is not None and not (eng.is_pe and b.w[0] is eng.sem):
                self._wait(eng, b.w)
            for ev in b.r:
                if not (eng.is_pe and ev[0] is eng.sem):
                    self._wait(eng, ev)

    def _commit(self, ev, reads, writes):
        for b in reads:
            b.r.append(ev)
            if len(b.r) > 48:
                last = {}
                for e in b.r:
                    k = id(e[0])
                    if k not in last or last[k][1] < e[1]:
                        last[k] = e
                b.r = list(last.values())
        for b in writes:
            b.w = ev
            b.r = []

    def op(self, eng, fn, reads=(), writes=()):
        self._deps(eng, reads, writes)
        ins = fn(eng.obj)
        eng.count += 1
        ev = (eng.sem, eng.count)
        ins.then_inc(eng.sem, 1)
        self._commit(ev, reads, writes)
        self.n_inst += 1
        return ev

    def mm(self, fn, reads=(), writes=(), signal=True, first=True):
        eng = self.pe
        self._deps(eng, reads, writes if first else ())
        ins = fn(eng.obj)
        eng.pend_r.extend(reads)
        for b in writes:
            if b not in eng.pend_w:
                eng.pend_w.append(b)
        self.n_inst += 1
        if signal:
            eng.count += 1
            ev = (eng.sem, eng.count)
            ins.then_inc(eng.sem, 1)
            self._commit(ev, eng.pend_r, eng.pend_w)
            eng.pend_r = []
            eng.pend_w = []
            return ev
        return None

    def dma(self, q, out, in_, reads=(), writes=()):
        slot = q.dma_sems[q.dma_rr % len(q.dma_sems)]
        q.dma_rr += 1
        sem, cur = slot
        if cur > 0:
            self._wait(q, (sem, cur))
        self._deps(q, reads, writes)
        ins = q.obj.dma_start(out=out, in_=in_)
        slot[1] = cur + 16
        ev = (sem, cur + 16)
        ins.then_inc(sem, 16)
        self._commit(ev, reads, writes)
        self.n_inst += 1
        return ev

    def barrier(self):
        assert not self.pe.pend_r and not self.pe.pend_w
        evs = [(e.sem, e.count) for e in self.engs if e.count > 0]
        for e in self.engs:
            evs += [(s, v) for s, v in e.dma_sems if v > 0]
        for e in self.engs:
            for ev in evs:
                if ev[0] is e.sem and e.is_pe:
                    continue
                self._wait(e, ev)


class Arena:
    def __init__(self, nc, base=20480, top=229312):
        self.nc = nc
        self.ptr = base
        self.top = top
        self.n = 0
        self.peak = base
        self.limit = top

    def alloc_at(self, name, shape, dtype, off):
        self.n += 1
        return self.nc.alloc_sbuf_tensor_at("%s_%d" % (name, self.n), list(shape), dtype, offset=off)

    def alloc(self, name, shape, dtype):
        esz = 4 if dtype == F32 else 2
        nbytes = int(np.prod(shape[1:])) * esz
        off = (self.ptr + 31) // 32 * 32
        assert off + nbytes <= self.limit, ("SBUF overflow", name, off, nbytes, self.limit)
        self.ptr = off + nbytes
        self.peak = max(self.peak, self.ptr)
        self.n += 1
        return self.nc.alloc_sbuf_tensor_at("%s_%d" % (name, self.n), list(shape), dtype, offset=off)

    def mark(self):
        return self.ptr

    def release(self, m):
        self.ptr = m


def build_program(stage=99, dbg=False, n_experts=32):
    nc = bass.Bass("TRN2", target_bir_lowering=False)

    def din(name, shape, dt=F32):
        return nc.dram_tensor(name, list(shape), dt, kind="ExternalInput").ap()

    def dout(name, shape, dt=F32):
        return nc.dram_tensor(name, list(shape), dt, kind="ExternalOutput").ap()

    def dscr(name, shape, dt):
        return nc.dram_tensor(name, list(shape), dt, kind="Internal").ap()

    xin = din("xin", [4352, 1024])
    cvec = din("cvec", [128, 16])
    w_ada = din("w_ada", [1024, 6144])
    rows_in = din("rows", [1, 10240])
    w_inx = din("w_inx", [1024, C_END])
    tabc = din("tabc", [96, 4096])
    tabs = din("tabs", [96, 4096])
    masks = din("masks", [128, 4, 512])
    sinkr = din("sinkr", [1, 1024])
    gq = din("gq", [128, 3])
    gkv = din("gkv", [128, 2])
    w_uq = din("w_uq", [384, 768])
    w_uqp = din("w_uqp", [384, 768])
    w_ukv = din("w_ukv", [256, 1024])
    w_o = din("w_o", [1024, 1024])
    w_r = din("w_r", [1024, 32])
    b_r16 = din("b_r16", [1, 512])
    w_gu = din("w_gu", [n_experts, 1024, 2048])
    b_gu = din("b_gu", [32, 2048])
    w_dn = din("w_dn", [n_experts, 1024, 1024])
    b_dn = din("b_dn", [32, 1024])
    ident = din("ident", [128, 128])
    out = dout("out", [2048, 1024])

    cq_s = dscr("cq_s", [128, 3, 2048], BF16)
    ckv_s = dscr("ckv_s", [128, 2, 4352], BF16)
    kr_s = dscr("kr_s", [96, 4352], BF16)
    x1_s = dscr("x1_s", [2048, 1024], F32)

    dbg_outs = {}

    def ddump(S, name, src_ap, shape, reads, cast=True):
        if not dbg:
            return
        o = dout(name, shape)
        dbg_outs[name] = o
        S.dma(S.pool if cast else S.sp, o, src_ap, reads=reads)

    with ExitStack() as es:
        S = Sched(nc, es)
        A = Arena(nc)
        pe, dve, act, pool, sp = S.pe, S.dve, S.act, S.pool, S.sp

        ident_b = A.alloc("ident_b", [128, 128], BF16)
        ident_f = A.alloc("ident_f", [128, 128], F32)
        ones_b = A.alloc("ones_b", [128, 128], BF16)
        ones_f = A.alloc("ones_f", [128, 128], F32)
        e64 = A.alloc("e64", [1, 65], BF16)
        gf_b = A.alloc("gf_b", [128, 1024], F32)
        gm_b = A.alloc("gm_b", [128, 1024], F32)
        gs2_b = A.alloc("gs2_b", [128, 1024], F32)
        sh2_b = A.alloc("sh2_b", [128, 1024], F32)
        Bc = {k: Buf(k) for k in ["ident_b", "ident_f", "ones_b", "ones_f", "e64", "gf_b", "gm_b", "gs2_b", "sh2_b"]}
        S.dma(pool, ident_b[:], ident, writes=[Bc["ident_b"]])
        S.dma(sp, ident_f[:], ident, writes=[Bc["ident_f"]])
        S.op(pool, lambda e: e.memset(ones_b[:], 1.0), writes=[Bc["ones_b"]])
        S.op(pool, lambda e: e.memset(ones_f[:], 1.0), writes=[Bc["ones_f"]])
        S.op(pool, lambda e: e.memset(e64[:], 0.0), writes=[Bc["e64"]])
        S.op(pool, lambda e: e.memset(e64[0:1, 64:65], 1.0), writes=[Bc["e64"]])
        m_persist = A.mark()

        gs1_b = A.alloc("gs1_b", [128, 1024], F32)
        sh1_b = A.alloc("sh1_b", [128, 1024], F32)
        gsc_b = A.alloc("gsc_b", [128, 1024], F32)
        shc_b = A.alloc("shc_b", [128, 1024], F32)
        for k in ["gs1_b", "sh1_b", "gsc_b", "shc_b"]:
            Bc[k] = Buf(k)
        m_bc1 = A.mark()
        with ExitStack() as pes:
            rows_t = A.alloc("rows_t", [1, 10240], F32)
            cv = A.alloc("cv", [128, 16], F32)
            sg = A.alloc("sg", [128, 16], F32)
            sl = A.alloc("sl", [128, 16], F32)
            rep = A.alloc("rep", [128, 16, 128], BF16)
            wa = [A.alloc("wa%d" % i, [128, 8, 512], BF16) for i in range(2)]
            gb = [A.alloc("gb%d" % i, [128, 512], F32) for i in range(2)]
            pm = [pes.enter_context(nc.psum_tensor("pm%d" % i, [128, 512], F32)) for i in range(2)]
            pmc = [pes.enter_context(nc.psum_tensor("pmc%d" % i, [128, 512], F32)) for i in range(2)]
            pg = [pes.enter_context(nc.psum_tensor("pg%d" % i, [128, 512], F32)) for i in range(2)]
            B0 = {k: Buf(k) for k in ["rows", "cv", "sg", "sl", "rep", "wa0", "wa1", "gb0", "gb1", "pm0", "pm1",
                                      "pmc0", "pmc1", "pg0", "pg1"]}
            S.dma(sp, rows_t[:], rows_in, writes=[B0["rows"]])
            S.dma(sp, cv[:], cvec, writes=[B0["cv"]])
            S.op(act, lambda e: e.activation(out=sg[:], in_=cv[:], func=AF.Sigmoid), reads=[B0["cv"]], writes=[B0["sg"]])
            S.op(dve, lambda e: e.tensor_tensor(out=sl[:], in0=cv[:], in1=sg[:], op=ALU.mult),
                 reads=[B0["cv"], B0["sg"]], writes=[B0["sl"]])
            for j in range(16):
                S.op(dve, lambda e, j=j: e.tensor_scalar(rep[:, j, :], ones_f[:], sl[:, j:j + 1], None, op0=ALU.mult),
                     reads=[B0["sl"], Bc["ones_f"]], writes=[B0["rep"]])
            w_ada_v = w_ada.rearrange("(c p) n -> p c n", p=128)
            g_off = {1: 0, 2: 1024, 4: 2048, 5: 3072}
            dests = {0: sh1_b, 1: gs1_b, 2: gm_b, 3: sh2_b, 4: gs2_b, 5: gf_b}
            destB = {0: "sh1_b", 1: "gs1_b", 2: "gm_b", 3: "sh2_b", 4: "gs2_b", 5: "gf_b"}
            for j in range(12):
                m, half = j // 2, j % 2
                k = j % 2
                cs = slice(half * 512, (half + 1) * 512)
                S.dma(pool, wa[k][:], w_ada_v[:, :, j * 512:(j + 1) * 512], writes=[B0["wa%d" % k]])
                vecs = [(0, pm[k], B0["pm%d" % k], dests[m], Bc[destB[m]])]
                if m < 2:
                    vecs.append((8, pmc[k], B0["pmc%d" % k], (shc_b if m == 0 else gsc_b),
                                 Bc["shc_b" if m == 0 else "gsc_b"]))
                if m in g_off:
                    go = g_off[m] + half * 512
                    S.mm(lambda e, k=k, go=go: e.matmul(pg[k][:], ones_f[0:1, :], rows_t[0:1, go:go + 512],
                                                        start=True, stop=True),
                         reads=[Bc["ones_f"], B0["rows"]], writes=[B0["pg%d" % k]])
                    S.op(act, lambda e, k=k: e.copy(gb[k][:], pg[k][:]), reads=[B0["pg%d" % k]], writes=[B0["gb%d" % k]])
                for (v0, pt_, pB, dst, dB) in vecs:
                    for c in range(8):
                        S.mm(lambda e, c=c, v0=v0, pt_=pt_, k=k: e.matmul(pt_[:], rep[:, v0 + c, :], wa[k][:, c, :],
                                                                          start=(c == 0), stop=False),
                             reads=[B0["rep"], B0["wa%d" % k]], writes=[pB], signal=False, first=(c == 0))
                    bo = 4096 + j * 512
                    S.mm(lambda e, pt_=pt_, bo=bo: e.matmul(pt_[:], ones_f[0:1, :], rows_t[0:1, bo:bo + 512],
                                                            start=False, stop=True),
                         reads=[Bc["ones_f"], B0["rows"]], writes=[pB], signal=True, first=False)
                    if m in (0, 3):
                        S.op(act, lambda e, dst=dst, pt_=pt_, cs=cs: e.copy(dst[:, cs], pt_[:]), reads=[pB], writes=[dB])
                    elif m in (1, 4):
                        S.op(dve, lambda e, dst=dst, pt_=pt_, cs=cs, k=k: e.scalar_tensor_tensor(
                            out=dst[:, cs], in0=pt_[:], scalar=1.0, in1=gb[k][:], op0=ALU.add, op1=ALU.mult),
                            reads=[pB, B0["gb%d" % k]], writes=[dB])
                    else:
                        S.op(dve, lambda e, dst=dst, pt_=pt_, cs=cs, k=k: e.tensor_tensor(
                            out=dst[:, cs], in0=pt_[:], in1=gb[k][:], op=ALU.mult),
                            reads=[pB, B0["gb%d" % k]], writes=[dB])
            if dbg and stage == 0:
                for nm, t in [("d_gs1", gs1_b), ("d_sh1", sh1_b), ("d_gsc", gsc_b), ("d_shc", shc_b), ("d_gm", gm_b),
                              ("d_gs2", gs2_b), ("d_sh2", sh2_b), ("d_gf", gf_b)]:
                    ddump(S, nm, t[:], [128, 1024], [Bc[k] for k in Bc], cast=False)
            S.barrier()
        A.release(m_bc1)
        if stage == 0:
            return nc, dbg_outs

        qaT = A.alloc("qaT", [64, 8, 2048], BF16)
        kaT = A.alloc("kaT", [64, 2, 4352], BF16)
        va = A.alloc("va", [128, 34, 130], BF16)
        Bq = {k: Buf(k) for k in ["qaT", "kaT", "va", "cq_s", "ckv_s", "kr_s"]}
        m_attnA = A.mark()
        with ExitStack() as pes:
            W = A.alloc("W", [128, 8, C_END], BF16)
            xt = [A.alloc("xt%d" % i, [128, 1024], F32) for i in range(2)]
            junk = A.alloc("junk", [128, 1024], BF16)
            tt_ = A.alloc("tt_", [128, 1024], F32)
            hb = [A.alloc("hb%d" % i, [128, 1024], BF16) for i in range(2)]
            hTb = [A.alloc("hTb%d" % i, [128, 8, 512], BF16) for i in range(2)]
            tcb = [A.alloc("tcb%d" % i, [96, 512], F32) for i in range(2)]
            tsb = [A.alloc("tsb%d" % i, [96, 512], F32) for i in range(2)]
            r1 = [A.alloc("r1_%d" % i, [96, 512], F32) for i in range(2)]
            r2 = [A.alloc("r2_%d" % i, [96, 512], F32) for i in range(2)]
            ss = A.alloc("ss", [128, 34], F32)
            lnv = A.alloc("lnv", [128, 34], F32)
            rs = A.alloc("rs", [128, 34], F32)
            sq = A.alloc("sq", [128, 3, 512], BF16)
            rl0 = A.alloc("rl0", [128, 512], F32)
            rl1 = A.alloc("rl1", [128, 512], F32)
            cqs = [A.alloc("cqs%d" % i, [128, 3, 512], BF16) for i in range(2)]
            ckvs = [A.alloc("ckvs%d" % i, [128, 2, 512], BF16) for i in range(2)]
            krs = [A.alloc("krs%d" % i, [96, 512], BF16) for i in range(2)]
            pT = [pes.enter_context(nc.psum_tensor("pT%d" % i, [128, 1024], BF16)) for i in range(2)]
            pp = [pes.enter_context(nc.psum_tensor("pp%d" % i, [128, 512], F32)) for i in range(5)]
            pr = pes.enter_context(nc.psum_tensor("pr", [128, 512], F32))
            B1 = {k: Buf(k) for k in ["W", "xt0", "xt1", "tt_", "hb0", "hb1", "hTb0", "hTb1", "tcb0", "tcb1", "tsb0",
                                      "tsb1", "r1_0", "r1_1", "r2_0", "r2_1", "sq", "rl0", "rl1", "cqs0", "cqs1",
                                      "ckvs0", "ckvs1", "krs0", "krs1", "pT0", "pT1", "pp0", "pp1", "pp2", "pp3", "pp4",
                                      "pr"]}
            Bss = [Buf("ss%d" % i) for i in range(34)]
            Bln = [Buf("ln%d" % i) for i in range(34)]
            Brs = [Buf("rs%d" % i) for i in range(34)]
            S.dma(pool, W[:], w_inx.rearrange("(c p) n -> p c n", p=128), writes=[B1["W"]])
            S.op(pool, lambda e: e.memset(va[:, :, 64:65], 1.0), writes=[Bq["va"]])
            S.op(pool, lambda e: e.memset(va[:, :, 129:130], 1.0), writes=[Bq["va"]])
            ppi = [0]
            ropei = [0]

            def next_pp():
                i = ppi[0] % 5
                ppi[0] += 1
                return pp[i], B1["pp%d" % i]

            def projT(dst, dB, col0, M, hT, hB, ntok):
                for c in range(8):
                    S.mm(lambda e, c=c: e.matmul(dst[0:M, 0:ntok], W[:, c, col0:col0 + M], hT[:, c, 0:ntok],
                                                 start=(c == 0), stop=(c == 7)),
                         reads=[B1["W"], hB], writes=[dB], signal=(c == 7), first=(c == 0))

            def rope_evac(pa, pBa, pb, pBb, r0, r1_, ntok, k, dst_ap, dstB):
                i = ropei[0] % 2
                ropei[0] += 1
                S.op(dve, lambda e: e.tensor_tensor(out=r1[i][r0:r1_, 0:ntok], in0=pa[r0:r1_, 0:ntok],
                                                    in1=tcb[k][r0:r1_, 0:ntok], op=ALU.mult),
                     reads=[pBa, B1["tcb%d" % k]], writes=[B1["r1_%d" % i]])
                S.op(dve, lambda e: e.tensor_tensor(out=r2[i][r0:r1_, 0:ntok], in0=pb[r0:r1_, 0:ntok],
                                                    in1=tsb[k][r0:r1_, 0:ntok], op=ALU.mult),
                     reads=[pBb, B1["tsb%d" % k]], writes=[B1["r2_%d" % i]])
                S.op(dve, lambda e: e.tensor_tensor(out=dst_ap, in0=r1[i][r0:r1_, 0:ntok], in1=r2[i][r0:r1_, 0:ntok],
                                                     op=ALU.add),
                     reads=[B1["r1_%d" % i], B1["r2_%d" % i]], writes=[dstB])

            def latent_norm(pcs, nfeat, ntok, dst, dstB):
                nj = len(pcs)
                for j, (pc, pB) in enumerate(pcs):
                    S.op(act, lambda e, j=j, pc=pc: e.activation(out=sq[:, j, 0:ntok], in_=pc[:, 0:ntok], func=AF.Square),
                         reads=[pB], writes=[B1["sq"]])
                for j in range(nj):
                    S.mm(lambda e, j=j: e.matmul(pr[:, 0:ntok], ones_b[:], sq[:, j, 0:ntok], start=(j == 0),
                                                 stop=(j == nj - 1)),
                         reads=[Bc["ones_b"], B1["sq"]], writes=[B1["pr"]], signal=(j == nj - 1), first=(j == 0))
                S.op(act, lambda e: e.activation(out=rl0[:, 0:ntok], in_=pr[:, 0:ntok], func=AF.Ln, bias=EPS,
                                                 scale=1.0 / nfeat), reads=[B1["pr"]], writes=[B1["rl0"]])
                S.op(act, lambda e: e.activation(out=rl1[:, 0:ntok], in_=rl0[:, 0:ntok], func=AF.Exp, scale=-0.5),
                     reads=[B1["rl0"]], writes=[B1["rl1"]])
                for j, (pc, pB) in enumerate(pcs):
                    S.op(dve, lambda e, j=j, pc=pc: e.tensor_tensor(out=dst[:, j, 0:ntok], in0=pc[:, 0:ntok],
                                                                     in1=rl1[:, 0:ntok], op=ALU.mult),
                         reads=[pB, B1["rl1"]], writes=[dstB])

            for bi in range(9):
                ntile = 4 if bi < 8 else 2
                ntok = ntile * 128
                t0 = bi * 4
                k = bi % 2
                hT, hB = hTb[k], B1["hTb%d" % k]
                tok = slice(bi * 512, bi * 512 + ntok)
                if bi < 8:
                    S.dma(sp, tcb[k][:], tabc[:, tok], writes=[B1["tcb%d" % k]])
                    S.dma(sp, tsb[k][:], tabs[:, tok], writes=[B1["tsb%d" % k]])
                gsb, shb = (gs1_b, sh1_b) if bi < 8 else (gsc_b, shc_b)
                gsB, shB = (Bc["gs1_b"], Bc["sh1_b"]) if bi < 8 else (Bc["gsc_b"], Bc["shc_b"])
                for ti in range(ntile):
                    tt = t0 + ti
                    x_, xB = xt[tt % 2], B1["xt%d" % (tt % 2)]
                    h_, hbB = hb[tt % 2], B1["hb%d" % (tt % 2)]
                    pT_, pTB = pT[tt % 2], B1["pT%d" % (tt % 2)]
                    S.dma(sp, x_[:], xin[tt * 128:(tt + 1) * 128, :], writes=[xB])
                    S.op(act, lambda e, x_=x_, tt=tt: e.activation(out=junk[:], in_=x_[:], func=AF.Square,
                                                                   accum_out=ss[:, tt:tt + 1]),
                         reads=[xB], writes=[Bss[tt]])
                    S.op(act, lambda e, tt=tt: e.activation(out=lnv[:, tt:tt + 1], in_=ss[:, tt:tt + 1], func=AF.Ln,
                                                            bias=EPS, scale=1.0 / 1024), reads=[Bss[tt]], writes=[Bln[tt]])
                    S.op(act, lambda e, tt=tt: e.activation(out=rs[:, tt:tt + 1], in_=lnv[:, tt:tt + 1], func=AF.Exp,
                                                            scale=-0.5), reads=[Bln[tt]], writes=[Brs[tt]])
                    S.op(dve, lambda e, x_=x_, tt=tt: e.scalar_tensor_tensor(out=tt_[:], in0=x_[:], scalar=rs[:, tt:tt + 1],
                                                                             in1=gsb[:], op0=ALU.mult, op1=ALU.mult),
                         reads=[xB, Brs[tt], gsB], writes=[B1["tt_"]])
                    S.op(dve, lambda e, h_=h_: e.tensor_tensor(out=h_[:], in0=tt_[:], in1=shb[:], op=ALU.add),
                         reads=[B1["tt_"], shB], writes=[hbB])
                    for c in range(8):
                        S.mm(lambda e, c=c, h_=h_, pT_=pT_: e.transpose(pT_[:, c * 128:(c + 1) * 128],
                                                                       h_[:, c * 128:(c + 1) * 128], ident_b[:]),
                             reads=[hbB, Bc["ident_b"]], writes=[pTB], signal=(c == 7), first=(c == 0))
                    S.op(act, lambda e, pT_=pT_, ti=ti: e.copy(hT[:, :, ti * 128:(ti + 1) * 128],
                                                               pT_[:].rearrange("p (c t) -> p c t", c=8)),
                         reads=[pTB], writes=[hB])
                if bi < 4:
                    for h in range(8):
                        pa, pBa = next_pp()
                        pb, pBb = next_pp()
                        projT(pa, pBa, C_QA + h * 64, 64, hT, hB, ntok)
                        projT(pb, pBb, C_QAP + h * 64, 64, hT, hB, ntok)
                        rope_evac(pa, pBa, pb, pBb, 0, 64, ntok, k, qaT[:, h, tok], Bq["qaT"])
                for kh in range(2):
                    pa, pBa = next_pp()
                    projT(pa, pBa, C_KA + kh * 64, 64, hT, hB, ntok)
                    if bi < 8:
                        pb, pBb = next_pp()
                        projT(pb, pBb, C_KAP + kh * 64, 64, hT, hB, ntok)
                        rope_evac(pa, pBa, pb, pBb, 0, 64, ntok, k, kaT[:, kh, tok], Bq["kaT"])
                    else:
                        S.op(dve, lambda e, pa=pa, kh=kh: e.tensor_copy(kaT[:, kh, tok], pa[0:64, 0:ntok]),
                             reads=[pBa], writes=[Bq["kaT"]])
                pv, pBv = next_pp()
                for ti in range(ntile):
                    for c in range(8):
                        S.mm(lambda e, c=c, ti=ti: e.matmul(pv[:, ti * 128:(ti + 1) * 128],
                                                           hT[:, c, ti * 128:(ti + 1) * 128], W[:, c, C_VA:C_VA + 128],
                                                           start=(c == 0), stop=(c == 7)),
                             reads=[B1["W"], hB], writes=[pBv], signal=(c == 7 and ti == ntile - 1),
                             first=(c == 0 and ti == 0))
                for kh in range(2):
                    S.op(dve, lambda e, kh=kh: e.tensor_copy(
                        va[:, t0:t0 + ntile, kh * 65:kh * 65 + 64],
                        pv[:, 0:ntile * 128].rearrange("p (t x) -> p t x", t=ntile)[:, :, kh * 64:(kh + 1) * 64]),
                        reads=[pBv], writes=[Bq["va"]])
                if bi < 4:
                    pcs = []
                    for j in range(3):
                        pc, pB = next_pp()
                        projT(pc, pB, C_CQ + j * 128, 128, hT, hB, ntok)
                        pcs.append((pc, pB))
                    latent_norm(pcs, 384, ntok, cqs[k], B1["cqs%d" % k])
                    S.dma(sp, cq_s[:, :, tok], cqs[k][:], reads=[B1["cqs%d" % k]], writes=[Bq["cq_s"]])
                pcs = []
                for j in range(2):
                    pc, pB = next_pp()
                    projT(pc, pB, C_CKV + j * 128, 128, hT, hB, ntok)
                    pcs.append((pc, pB))
                latent_norm(pcs, 256, ntok, ckvs[k], B1["ckvs%d" % k])
                S.dma(sp, ckv_s[:, :, tok], ckvs[k][:, :, 0:ntok], reads=[B1["ckvs%d" % k]], writes=[Bq["ckv_s"]])
                pa, pBa = next_pp()
                projT(pa, pBa, C_KR2, 96, hT, hB, ntok)
                if bi < 8:
                    pb, pBb = next_pp()
                    projT(pb, pBb, C_KR2 + 96, 96, hT, hB, ntok)
                    rope_evac(pa, pBa, pb, pBb, 64, 96, ntok, k, krs[k][64:96, 0:ntok], B1["krs%d" % k])
                else:
                    S.op(dve, lambda e, pa=pa: e.tensor_copy(krs[k][64:96, 0:ntok], pa[64:96, 0:ntok]),
                         reads=[pBa], writes=[B1["krs%d" % k]])
                S.dma(sp, kr_s[64:96, tok], krs[k][64:96, 0:ntok], reads=[B1["krs%d" % k]], writes=[Bq["kr_s"]])
            if dbg and stage == 1:
                allB = [Bq[k] for k in Bq]
                ddump(S, "d_qaT", qaT[:], [64, 8, 2048], allB)
                ddump(S, "d_kaT", kaT[:], [64, 2, 4352], allB)
                ddump(S, "d_va", va[:], [128, 34, 130], allB)
                ddump(S, "d_cq", cq_s, [128, 3, 2048], allB)
                ddump(S, "d_ckv", ckv_s, [128, 2, 4352], allB)
                ddump(S, "d_kr", kr_s[64:96, :], [32, 4352], allB)
            S.barrier()
        A.release(m_attnA)
        if stage == 1:
            return nc, dbg_outs

        def run_attention(iters, st, stB, ptb, ptB, ot, otB, bc, bcB, rden, rdB, bcs, bcsB, fillers=None, fill_every=1):
            flat = []
            for ii, it in enumerate(iters):
                ng = len(it["groups"])
                for gi, g in enumerate(it["groups"]):
                    flat.append((ii, gi, gi == ng - 1, g))
            n = len(flat)
            nst, npt, nbc = len(st), len(ptb), len(bc)

            def QK(i):
                ii, gi, last, g = flat[i]
                it = iters[ii]
                s_, sB = st[i % nst], stB[i % nst]
                nmm = sum(1 + (1 if m is not None else 0) for (_, _, m, _, _) in g)
                cnt = 0
                for slot, (kap, kB, mask, vap, vB) in enumerate(g):
                    cnt += 1
                    S.mm(lambda e, slot=slot, kap=kap, it=it, mask=mask: e.matmul(
                        s_[:, slot * 512:(slot + 1) * 512], kap, it["q"], start=True, stop=(mask is None)),
                        reads=[kB, it["qB"]], writes=[sB], signal=(cnt == nmm), first=(cnt == 1))
                    if mask is not None:
                        cnt += 1
                        S.mm(lambda e, slot=slot, mask=mask: e.matmul(s_[:, slot * 512:(slot + 1) * 512], ident_b[:],
                                                                      mask[0], start=False, stop=True),
                             reads=[Bc["ident_b"], mask[1]], writes=[sB], signal=(cnt == nmm), first=False)

            def EXP(i):
                ii, gi, last, g = flat[i]
                it = iters[ii]
                wdt = 512 * len(g)
                s_, sB = st[i % nst], stB[i % nst]
                p_, pB = ptb[i % npt], ptB[i % npt]
                S.op(act, lambda e: e.activation(out=p_[:, 0:wdt], in_=s_[:, 0:wdt], func=AF.Exp, scale=it["scale"]),
                     reads=[sB], writes=[pB])

            def PV(i):
                ii, gi, last, g = flat[i]
                it = iters[ii]
                o_, oB = ot[ii % len(ot)], otB[ii % len(ot)]
                p_, pB = ptb[i % npt], ptB[i % npt]
                for slot, (kap, kB, mask, vap, vB) in enumerate(g):
                    lastmm = last and slot == len(g) - 1 and it.get("sink") is None
                    S.mm(lambda e, slot=slot, vap=vap: e.matmul(o_[0:65, :], vap, p_[:, slot * 512:(slot + 1) * 512],
                                                                start=(gi == 0 and slot == 0), stop=lastmm),
                         reads=[vB, pB], writes=[oB], signal=(slot == len(g) - 1), first=(gi == 0 and slot == 0))
                if last:
                    if it.get("sink") is not None:
                        sl_, sr_, sB_ = it["sink"]
                        S.mm(lambda e: e.matmul(o_[0:65, :], sl_, sr_, start=False, stop=True),
                             reads=[Bc["e64"], sB_], writes=[oB], signal=True, first=False)
                    j = ii % nbc
                    S.op(dve, lambda e: e.reciprocal(rden[j][64:65, :], o_[64:65, :]), reads=[oB], writes=[rdB[j]])

                    def fin2(j=j, o_=o_, oB=oB, it=it):
                        S.mm(lambda e: e.matmul(bc[j][0:64, :], ones_f[64:65, 0:64], rden[j][64:65, :], start=True,
                                                stop=True),
                             reads=[Bc["ones_f"], rdB[j]], writes=[bcB[j]])
                        S.op(dve, lambda e: e.tensor_copy(bcs[j][:], bc[j][0:64, :]), reads=[bcB[j]], writes=[bcsB[j]])
                        S.op(dve, lambda e: e.tensor_tensor(out=it["out"], in0=it["ovw"](o_[0:64, :]),
                                                            in1=it["ovw"](bcs[j][:]), op=ALU.mult),
                             reads=[oB, bcsB[j]], writes=[it["outB"]])
                    pending.append(fin2)

            pending = []
            for i in range(n + 2):
                if i < n:
                    QK(i)
                    EXP(i)
                todo, pending[:] = list(pending), []
                for f_ in todo:
                    f_()
                if 0 <= i - 2 < n:
                    PV(i - 2)
                if fillers and (i % fill_every == 0):
                    if fillers:
                        fillers.popleft()()
            for f_ in pending:
                f_()
            pending[:] = []
            while fillers:
                fillers.popleft()()

        TOP = 229312
        out_aT = A.alloc_at("out_aT", [64, 8, 2048], BF16, TOP - 32768)
        A.limit = TOP - 32768
        B_oa = [Buf("oa%d" % i) for i in range(16)]
        m_attnB = A.mark()
        with ExitStack() as pes:
            maskb = A.alloc("maskb", [128, 4, 512], BF16)
            sinkf = A.alloc("sinkf", [1, 1024], F32)
            sinkb = A.alloc("sinkb", [1, 1024], BF16)
            ptb = [A.alloc("ptb%d" % i, [128, 1024], BF16) for i in range(3)]
            rden = [A.alloc("rden%d" % i, [65, 512], F32) for i in range(2)]
            bcs = [A.alloc("bcs%d" % i, [64, 512], F32) for i in range(2)]
            st = [pes.enter_context(nc.psum_tensor("st%d" % i, [128, 1024], F32)) for i in range(2)]
            ot = [pes.enter_context(nc.psum_tensor("ot%d" % i, [128, 512], F32)) for i in range(2)]
            bc = [pes.enter_context(nc.psum_tensor("bc%d" % i, [64, 512], F32)) for i in range(2)]
            B3 = {k: Buf(k) for k in ["maskb", "sinkf", "sinkb"]}
            stB = [Buf("st%d" % i) for i in range(2)]
            ptB = [Buf("pt%d" % i) for i in range(3)]
            otB = [Buf("ot%d" % i) for i in range(2)]
            bcB = [Buf("bc%d" % i) for i in range(2)]
            rdB = [Buf("rd%d" % i) for i in range(2)]
            bcsB = [Buf("bcs%d" % i) for i in range(2)]
            S.dma(pool, maskb[:], masks, writes=[B3["maskb"]])
            S.dma(sp, sinkf[:], sinkr, writes=[B3["sinkf"]])
            S.op(act, lambda e: e.activation(out=sinkb[:], in_=sinkf[:], func=AF.Exp), reads=[B3["sinkf"]],
                 writes=[B3["sinkb"]])
            iters = []
            for n_ in range(16):
                for kh in range(2):
                    Lt = n_ - 1 if n_ >= 1 else 31
                    Rt = n_ + 1 if n_ <= 14 else 16
                    mL = 0 if n_ >= 1 else 2
                    mR = 1 if n_ <= 14 else 3

                    def kt(j, m=None):
                        return (kaT[:, kh, j * 128:(j + 1) * 128], Bq["kaT"],
                                None if m is None else (maskb[:, m, :], B3["maskb"]),
                                va[:, j, kh * 65:(kh + 1) * 65], Bq["va"])
                    groups = [[kt(Lt, mL), kt(n_)], [kt(Rt, mR), kt(32)], [kt(33)]]
                    qs = slice(n_ * 128, (n_ + 1) * 128)
                    iters.append(dict(
                        q=qaT[:, 4 * kh:4 * kh + 4, qs], qB=Bq["qaT"], groups=groups, scale=A_SCALE,
                        sink=(e64[0:1, 0:65], sinkb[0:1, kh * 512:(kh + 1) * 512], B3["sinkb"]),
                        out=out_aT[:, 4 * kh:4 * kh + 4, qs], outB=B_oa[n_],
                        ovw=lambda ap: ap.rearrange("p (h q) -> p h q", h=4)))
            run_attention(iters, st, stB, ptb, ptB, ot, otB, bc, bcB, rden, rdB, bcs, bcsB)
            if dbg and stage == 2:
                ddump(S, "d_oaT", out_aT[:], [64, 8, 2048], B_oa)
            S.barrier()
        A.release(m_persist)
        if stage == 2:
            return nc, dbg_outs

        out_bT = A.alloc_at("out_bT", [64, 8, 2048], BF16, TOP - 65536)
        A.limit = TOP - 65536
        B_ob = [Buf("ob%d" % i) for i in range(4)]
        m_mla = A.mark()
        with ExitStack() as pes:
            cqT = A.alloc("cqT", [128, 3, 2048], BF16)
            ckvT = A.alloc("ckvT", [128, 2, 4352], BF16)
            krT = A.alloc("krT", [96, 4352], BF16)
            wuq = A.alloc("wuq", [128, 3, 768], BF16)
            wuqp = A.alloc("wuqp", [128, 3, 768], BF16)
            wukv = A.alloc("wukv", [128, 2, 1024], BF16)
            gq_t = A.alloc("gq_t", [128, 3], F32)
            gkv_t = A.alloc("gkv_t", [128, 2], F32)
            m_wst = A.mark()
            wst = A.alloc("wst", [128, 3, 768], F32)
            B4 = {k: Buf(k) for k in ["cqT", "ckvT", "krT", "wst", "wuq", "wuqp", "wukv", "gq", "gkv", "tcq", "tsq", "QT0",
                                      "QT1", "KT0", "KT1", "Vh0", "Vh1", "q1", "q2", "ppr"]}
            S.dma(sp, cqT[:], cq_s, reads=[Bq["cq_s"]], writes=[B4["cqT"]])
            S.dma(sp, ckvT[:], ckv_s, reads=[Bq["ckv_s"]], writes=[B4["ckvT"]])
            S.dma(sp, krT[64:96, :], kr_s[64:96, :], reads=[Bq["kr_s"]], writes=[B4["krT"]])
            S.dma(sp, gq_t[:], gq, writes=[B4["gq"]])
            S.dma(sp, gkv_t[:], gkv, writes=[B4["gkv"]])
            for (src, dstw, dB) in [(w_uq, wuq, "wuq"), (w_uqp, wuqp, "wuqp")]:
                S.dma(sp, wst[:, 0:3, :], src.rearrange("(c p) n -> p c n", p=128), writes=[B4["wst"]])
                for c in range(3):
                    S.op(dve, lambda e, c=c, dstw=dstw: e.tensor_scalar(dstw[:, c, :], wst[:, c, :], gq_t[:, c:c + 1], None,
                                                                        op0=ALU.mult),
                         reads=[B4["wst"], B4["gq"]], writes=[B4[dB]])
            for c in range(2):
                for (c0, c1) in [(0, 768), (768, 1024)]:
                    S.dma(sp, wst[:, 0, 0:c1 - c0], w_ukv[c * 128:(c + 1) * 128, c0:c1], writes=[B4["wst"]])
                    S.op(dve, lambda e, c=c, c0=c0, c1=c1: e.tensor_scalar(wukv[:, c, c0:c1], wst[:, 0, 0:c1 - c0],
                                                                           gkv_t[:, c:c + 1], None, op0=ALU.mult),
                         reads=[B4["wst"], B4["gkv"]], writes=[B4["wukv"]])
            S.barrier()
            A.release(m_wst)
            tcq = A.alloc("tcq", [96, 512], F32)
            tsq = A.alloc("tsq", [96, 512], F32)
            QT = [A.alloc("QT%d" % i, [96, 2048], BF16) for i in range(2)]
            KT = [A.alloc("KT%d" % i, [96, 4352], BF16) for i in range(2)]
            Vh = [A.alloc("Vh%d" % i, [128, 34, 65], BF16) for i in range(2)]
            q1 = A.alloc("q1", [96, 512], F32)
            q2 = A.alloc("q2", [96, 512], F32)
            ptb = [A.alloc("ptb%d" % i, [128, 1024], BF16) for i in range(3)]
            rden = [A.alloc("rden%d" % i, [65, 512], F32) for i in range(1)]
            bcs = [A.alloc("bcs%d" % i, [64, 512], F32) for i in range(1)]
            st = [pes.enter_context(nc.psum_tensor("mst%d" % i, [128, 1024], F32)) for i in range(2)]
            ot = [pes.enter_context(nc.psum_tensor("mot%d" % i, [128, 512], F32)) for i in range(2)]
            bc = [pes.enter_context(nc.psum_tensor("mbc%d" % i, [64, 512], F32)) for i in range(1)]
            ppr = pes.enter_context(nc.psum_tensor("ppr", [128, 512], F32))
            stB = [Buf("st%d" % i) for i in range(2)]
            ptB = [Buf("pt%d" % i) for i in range(3)]
            otB = [Buf("ot%d" % i) for i in range(2)]
            bcB = [Buf("bc%d" % i) for i in range(1)]
            rdB = [Buf("rd%d" % i) for i in range(1)]
            bcsB = [Buf("bcs%d" % i) for i in range(1)]
            for i in range(2):
                S.op(pool, lambda e, i=i: e.memset(Vh[i][:, :, 64:65], 1.0), writes=[B4["Vh%d" % i]])

            def prep_closures(h, hp):
                cl = []
                KTh, KB = KT[hp], B4["KT%d" % hp]
                Vhh, VB = Vh[hp], B4["Vh%d" % hp]
                QTh, QB = QT[hp], B4["QT%d" % hp]

                def kcopy():
                    S.op(dve, lambda e: e.tensor_copy(KTh[64:96, :], krT[64:96, :]), reads=[B4["krT"]], writes=[KB])
                cl.append(kcopy)
                for bi in range(9):
                    ntok = 512 if bi < 8 else 256
                    tok = slice(bi * 512, bi * 512 + ntok)

                    def kblk(tok=tok, ntok=ntok):
                        for c in range(2):
                            S.mm(lambda e, c=c: e.matmul(ppr[0:64, 0:ntok], wukv[:, c, h * 128:h * 128 + 64],
                                                         ckvT[:, c, tok], start=(c == 0), stop=(c == 1)),
                                 reads=[B4["wukv"], B4["ckvT"]], writes=[B4["ppr"]], signal=(c == 1), first=(c == 0))
                        S.op(dve, lambda e: e.tensor_copy(KTh[0:64, tok], ppr[0:64, 0:ntok]), reads=[B4["ppr"]],
                             writes=[KB])
                    cl.append(kblk)
                for g0 in range(0, 34, 8):
                    nt = min(8, 34 - g0)

                    def vblk(g0=g0, nt=nt):
                        for t in range(nt):
                            tsl = slice((g0 + t) * 128, (g0 + t + 1) * 128)
                            for c in range(2):
                                S.mm(lambda e, c=c, t=t, tsl=tsl: e.matmul(
                                    ppr[:, t * 64:(t + 1) * 64], ckvT[:, c, tsl], wukv[:, c, h * 128 + 64:h * 128 + 128],
                                    start=(c == 0), stop=(c == 1)),
                                    reads=[B4["wukv"], B4["ckvT"]], writes=[B4["ppr"]],
                                    signal=(c == 1 and t == nt - 1), first=(c == 0 and t == 0))
                        S.op(dve, lambda e: e.tensor_copy(Vhh[:, g0:g0 + nt, 0:64],
                                                          ppr[:, 0:nt * 64].rearrange("p (t d) -> p t d", t=nt)),
                             reads=[B4["ppr"]], writes=[VB])
                    cl.append(vblk)
                for qb in range(4):
                    tok = slice(qb * 512, (qb + 1) * 512)

                    def qa_(tok=tok):
                        S.dma(sp, tcq[64:96, :], tabc[64:96, tok], writes=[B4["tcq"]])
                        S.dma(sp, tsq[64:96, :], tabs[64:96, tok], writes=[B4["tsq"]])
                        for c in range(3):
                            S.mm(lambda e, c=c: e.matmul(ppr[0:96, :], wuq[:, c, h * 96:(h + 1) * 96], cqT[:, c, tok],
                                                         start=(c == 0), stop=(c == 2)),
                                 reads=[B4["wuq"], B4["cqT"]], writes=[B4["ppr"]], signal=(c == 2), first=(c == 0))
                        S.op(dve, lambda e: e.tensor_copy(QTh[0:64, tok], ppr[0:64, :]), reads=[B4["ppr"]], writes=[QB])
                        S.op(dve, lambda e: e.tensor_tensor(out=q1[64:96, :], in0=ppr[64:96, :], in1=tcq[64:96, :],
                                                            op=ALU.mult), reads=[B4["ppr"], B4["tcq"]], writes=[B4["q1"]])

                    def qb_(tok=tok):
                        for c in range(3):
                            S.mm(lambda e, c=c: e.matmul(ppr[0:96, :], wuqp[:, c, h * 96:(h + 1) * 96], cqT[:, c, tok],
                                                         start=(c == 0), stop=(c == 2)),
                                 reads=[B4["wuqp"], B4["cqT"]], writes=[B4["ppr"]], signal=(c == 2), first=(c == 0))
                        S.op(dve, lambda e: e.tensor_tensor(out=q2[64:96, :], in0=ppr[64:96, :], in1=tsq[64:96, :],
                                                            op=ALU.mult), reads=[B4["ppr"], B4["tsq"]], writes=[B4["q2"]])
                        S.op(dve, lambda e: e.tensor_tensor(out=QTh[64:96, tok], in0=q1[64:96, :], in1=q2[64:96, :],
                                                             op=ALU.add), reads=[B4["q1"], B4["q2"]], writes=[QB])
                    cl.append(qa_)
                    cl.append(qb_)
                return cl

            for f in prep_closures(0, 0):
                f()
            for h in range(8):
                hp = h % 2
                iters = []
                for qb in range(4):
                    tok = slice(qb * 512, (qb + 1) * 512)
                    groups = []
                    for g in range(17):
                        groups.append([(KT[hp][:, j * 128:(j + 1) * 128], B4["KT%d" % hp], None,
                                        Vh[hp][:, j, 0:65], B4["Vh%d" % hp]) for j in (2 * g, 2 * g + 1)])
                    iters.append(dict(q=QT[hp][:, tok], qB=B4["QT%d" % hp], groups=groups, scale=MLA_SCALE, sink=None,
                                      out=out_bT[:, h, tok], outB=B_ob[qb], ovw=lambda ap: ap))
                fl = deque(prep_closures(h + 1, 1 - hp)) if h < 7 else None
                run_attention(iters, st, stB, ptb, ptB, ot, otB, bc, bcB, rden, rdB, bcs, bcsB, fillers=fl, fill_every=2)
            if dbg and stage == 3:
                ddump(S, "d_obT", out_bT[:], [64, 8, 2048], B_ob)
            S.barrier()
        A.release(m_mla)
        if stage == 3:
            return nc, dbg_outs

        h2T = A.alloc("h2T", [128, 8, 2048], BF16)
        Wr = A.alloc("Wr", [128, 16, 32], F32)
        B_h2 = [Buf("h2T%d" % i) for i in range(4)]
        B_wr = [Buf("Wr%d" % i) for i in range(16)]
        B_x1 = [Buf("x1s%d" % i) for i in range(16)]
        m_p5 = A.mark()
        with ExitStack() as pes:
            wo = A.alloc("wo", [64, 16, 1024], BF16)
            wrb = A.alloc("wrb", [128, 8, 32], BF16)
            xt = [A.alloc("xt%d" % i, [128, 1024], F32) for i in range(2)]
            x1t = [A.alloc("x1t%d" % i, [128, 1024], F32) for i in range(2)]
            tt_ = A.alloc("tt5", [128, 1024], F32)
            junk = A.alloc("junk5", [128, 1024], BF16)
            hb = [A.alloc("h2b%d" % i, [128, 1024], BF16) for i in range(2)]
            sm = A.alloc("sm5", [128, 16, 8], F32)
            lg = A.alloc("lg", [128, 512], F32)
            rk = A.alloc("rk", [128, 512], F32)
            mk = A.alloc("mk", [128, 512], F32)
            ex = A.alloc("ex", [128, 512], F32)
            m1 = A.alloc("m1", [128, 16], F32)
            thr = A.alloc("thr", [128, 16], F32)
            br16 = A.alloc("br16", [1, 512], BF16)
            py = [pes.enter_context(nc.psum_tensor("py%d" % i, [128, 1024], F32)) for i in range(2)]
            pT = [pes.enter_context(nc.psum_tensor("pT5_%d" % i, [128, 1024], BF16)) for i in range(2)]
            plg = pes.enter_context(nc.psum_tensor("plg", [128, 512], F32))
            B5 = {k: Buf(k) for k in ["wo", "wrb", "brb", "xt0", "xt1", "x1t0", "x1t1", "tt", "hb0", "hb1", "lg", "rk", "m1", "thr",
                                      "mk", "ex", "py0", "py1", "pT0", "pT1", "plg"]}
            Bsm = [[Buf("sm%d_%d" % (i, j)) for j in range(8)] for i in range(16)]
            S.dma(pool, wo[:], w_o.rearrange("(h p) n -> p h n", p=64), writes=[B5["wo"]])
            S.dma(pool, wrb[:], w_r.rearrange("(c p) n -> p c n", p=128), writes=[B5["wrb"]])
            S.dma(pool, br16[:], b_r16, writes=[B5["brb"]])
            S.mm(lambda e: e.matmul(plg[:], ones_b[0:1, :], br16[0:1, :], start=True, stop=False),
                 reads=[Bc["ones_b"], B5["brb"]], writes=[B5["plg"]], signal=True, first=True)
            for tt in range(16):
                k = tt % 2
                tsl = slice(tt * 128, (tt + 1) * 128)
                y_, yB = py[k], B5["py%d" % k]
                x_, xB = xt[k], B5["xt%d" % k]
                x1_, x1B = x1t[k], B5["x1t%d" % k]
                h_, hbB = hb[k], B5["hb%d" % k]
                pT_, pTB = pT[k], B5["pT%d" % k]
                smt = sm[:, tt, :]
                S.dma(sp, x_[:], xin[tsl, :], writes=[xB])
                for cb in range(2):
                    for h in range(16):
                        src = out_aT if h < 8 else out_bT
                        sB = B_oa[tt] if h < 8 else B_ob[tt // 4]
                        S.mm(lambda e, cb=cb, h=h, src=src: e.matmul(y_[:, cb * 512:(cb + 1) * 512], src[:, h % 8, tsl],
                                                                     wo[:, h, cb * 512:(cb + 1) * 512],
                                                                     start=(h == 0), stop=(h == 15)),
                             reads=[sB, B5["wo"]], writes=[yB], signal=(h == 15 and cb == 1), first=(h == 0 and cb == 0))
                S.op(act, lambda e, y_=y_, tt=tt: e.activation(out=junk[:], in_=y_[:], func=AF.Square,
                                                               accum_out=sm[:, tt, 0:1]), reads=[yB], writes=[Bsm[tt][0]])
                S.op(act, lambda e, tt=tt: e.activation(out=sm[:, tt, 1:2], in_=sm[:, tt, 0:1], func=AF.Ln, bias=EPS,
                                                        scale=1.0 / 1024), reads=[Bsm[tt][0]], writes=[Bsm[tt][1]])
                S.op(act, lambda e, tt=tt: e.activation(out=sm[:, tt, 2:3], in_=sm[:, tt, 1:2], func=AF.Exp, scale=-0.5),
                     reads=[Bsm[tt][1]], writes=[Bsm[tt][2]])
                S.op(dve, lambda e, y_=y_, tt=tt: e.scalar_tensor_tensor(out=tt_[:], in0=y_[:], scalar=sm[:, tt, 2:3],
                                                                         in1=gm_b[:], op0=ALU.mult, op1=ALU.mult),
                     reads=[yB, Bsm[tt][2], Bc["gm_b"]], writes=[B5["tt"]])
                S.op(dve, lambda e, x_=x_, x1_=x1_: e.tensor_tensor(out=x1_[:], in0=tt_[:], in1=x_[:], op=ALU.add),
                     reads=[B5["tt"], xB], writes=[x1B])
                S.dma(sp, x1_s[tsl, :], x1_[:], reads=[x1B], writes=[B_x1[tt]])
                S.op(act, lambda e, x1_=x1_, tt=tt: e.activation(out=junk[:], in_=x1_[:], func=AF.Square,
                                                                 accum_out=sm[:, tt, 3:4]), reads=[x1B], writes=[Bsm[tt][3]])
                S.op(act, lambda e, tt=tt: e.activation(out=sm[:, tt, 4:5], in_=sm[:, tt, 3:4], func=AF.Ln, bias=EPS,
                                                        scale=1.0 / 1024), reads=[Bsm[tt][3]], writes=[Bsm[tt][4]])
                S.op(act, lambda e, tt=tt: e.activation(out=sm[:, tt, 5:6], in_=sm[:, tt, 4:5], func=AF.Exp, scale=-0.5),
                     reads=[Bsm[tt][4]], writes=[Bsm[tt][5]])
                S.op(dve, lambda e, x1_=x1_, tt=tt: e.scalar_tensor_tensor(out=tt_[:], in0=x1_[:], scalar=sm[:, tt, 5:6],
                                                                           in1=gs2_b[:], op0=ALU.mult, op1=ALU.mult),
                     reads=[x1B, Bsm[tt][5], Bc["gs2_b"]], writes=[B5["tt"]])
                S.op(dve, lambda e, h_=h_: e.tensor_tensor(out=h_[:], in0=tt_[:], in1=sh2_b[:], op=ALU.add),
                     reads=[B5["tt"], Bc["sh2_b"]], writes=[hbB])
                for c in range(8):
                    S.mm(lambda e, c=c, h_=h_, pT_=pT_: e.transpose(pT_[:, c * 128:(c + 1) * 128],
                                                                   h_[:, c * 128:(c + 1) * 128], ident_b[:]),
                         reads=[hbB, Bc["ident_b"]], writes=[pTB], signal=(c == 7), first=(c == 0))
                S.op(dve, lambda e, pT_=pT_: e.tensor_copy(h2T[:, :, tsl], pT_[:].rearrange("p (c t) -> p c t", c=8)),
                     reads=[pTB], writes=[B_h2[tt // 4]])
                for c in range(8):
                    S.mm(lambda e, c=c, tt=tt: e.matmul(plg[:, tt * 32:(tt + 1) * 32], h2T[:, c, tsl], wrb[:, c, :],
                                                        start=False, stop=(c == 7 and tt == 15)),
                         reads=[B_h2[tt // 4], B5["wrb"]], writes=[B5["plg"]], signal=(c == 7), first=False)
            v3 = lambda t: t[:].rearrange("p (t x) -> p t x", t=16)
            bc3 = lambda t: t[:].unsqueeze(2).to_broadcast([128, 16, 32])
            S.op(dve, lambda e: e.tensor_copy(lg[:], plg[:]), reads=[B5["plg"]], writes=[B5["lg"]])
            S.op(dve, lambda e: e.tensor_copy(rk[:], lg[:]), reads=[B5["lg"]], writes=[B5["rk"]])
            for kk in range(4):
                mdst = m1 if kk == 0 else thr
                mB = B5["m1"] if kk == 0 else B5["thr"]
                S.op(dve, lambda e, mdst=mdst: e.tensor_reduce(out=mdst[:], in_=v3(rk), axis=mybir.AxisListType.X,
                                                               op=ALU.max), reads=[B5["rk"]], writes=[mB])
                if kk < 3:
                    S.op(dve, lambda e, mdst=mdst: e.tensor_tensor(out=v3(mk), in0=v3(rk), in1=bc3(mdst), op=ALU.is_equal),
                         reads=[B5["rk"], mB], writes=[B5["mk"]])
                    S.op(dve, lambda e: e.scalar_tensor_tensor(out=rk[:], in0=mk[:], scalar=-1.0e9, in1=rk[:],
                                                               op0=ALU.mult, op1=ALU.add),
                         reads=[B5["mk"], B5["rk"]], writes=[B5["rk"]])
            S.op(dve, lambda e: e.tensor_tensor(out=v3(mk), in0=v3(lg), in1=bc3(thr), op=ALU.is_ge),
                 reads=[B5["lg"], B5["thr"]], writes=[B5["mk"]])
            S.op(dve, lambda e: e.tensor_tensor(out=v3(rk), in0=v3(lg), in1=bc3(m1), op=ALU.subtract),
                 reads=[B5["lg"], B5["m1"]], writes=[B5["rk"]])
            S.op(act, lambda e: e.activation(out=ex[:], in_=rk[:], func=AF.Exp), reads=[B5["rk"]], writes=[B5["ex"]])
            S.op(dve, lambda e: e.tensor_tensor(out=ex[:], in0=ex[:], in1=mk[:], op=ALU.mult),
                 reads=[B5["ex"], B5["mk"]], writes=[B5["ex"]])
            S.op(dve, lambda e: e.tensor_reduce(out=thr[:], in_=v3(ex), axis=mybir.AxisListType.X, op=ALU.add),
                 reads=[B5["ex"]], writes=[B5["thr"]])
            S.op(dve, lambda e: e.reciprocal(m1[:], thr[:]), reads=[B5["thr"]], writes=[B5["m1"]])
            S.op(dve, lambda e: e.tensor_tensor(out=Wr[:], in0=v3(ex), in1=bc3(m1), op=ALU.mult),
                 reads=[B5["ex"], B5["m1"]], writes=B_wr)
            if dbg and stage == 4:
                ddump(S, "d_h2T", h2T[:], [128, 8, 2048], B_h2)
                ddump(S, "d_Wr", Wr[:], [128, 16, 32], B_wr, cast=False)
                ddump(S, "d_x1", x1_s, [2048, 1024], B_x1, cast=False)
            S.barrier()
        A.release(m_p5)
        if stage == 4:
            return nc, dbg_outs

        A.limit = TOP
        facc = A.alloc("facc", [128, 16, 1024], F32)
        B_fa = [Buf("fa%d" % i) for i in range(16)]
        m_moe = A.mark()
        with ExitStack() as pes:
            bguT = A.alloc("bguT", [128, 16, 32], F32)
            bg7 = A.alloc("bg7", [128, 8, 32], F32)
            bln = A.alloc("bln", [128, 8, 32], F32)
            sgb = A.alloc("sgb", [128, 1], F32)
            B6 = {k: Buf(k) for k in ["GUa", "GUb", "DN0", "DN1", "bd0", "bd1", "actT0", "actT1", "T1_0", "T1_1", "T2_0", "T2_1", "T3_0",
                                      "T3_1", "bgu_t", "bguT", "pgl0", "pgl1", "pgl2", "pgl3", "po0", "po1"]}
            pgl = [pes.enter_context(nc.psum_tensor("pgl%d" % i, [128, 512], F32)) for i in range(4)]
            po = [pes.enter_context(nc.psum_tensor("po%d" % i, [128, 1024], F32)) for i in range(2)]
            m_bgu = A.mark()
            bgu_t = A.alloc("bgu_t", [32, 2048], F32)
            S.dma(sp, bgu_t[:], b_gu, writes=[B6["bgu_t"]])
            for c in range(16):
                S.mm(lambda e, c=c: e.transpose(pgl[0][:, c * 32:(c + 1) * 32], bgu_t[:, c * 128:(c + 1) * 128],
                                                ident_f[0:32, 0:32]),
                     reads=[B6["bgu_t"], Bc["ident_f"]], writes=[B6["pgl0"]], signal=(c == 15), first=(c == 0))
            S.op(dve, lambda e: e.tensor_copy(bguT[:], pgl[0][:].rearrange("p (c x) -> p c x", c=16)),
                 reads=[B6["pgl0"]], writes=[B6["bguT"]])
            S.op(dve, lambda e: e.tensor_scalar(bg7[:], bguT[:, 0:8, :], -1.0, 7.0, op0=ALU.mult, op1=ALU.add),
                 reads=[B6["bguT"]], writes=[B6["bguT"]])
            S.op(dve, lambda e: e.tensor_scalar(bln[:], bguT[:, 8:16, :], -1.0, None, op0=ALU.mult),
                 reads=[B6["bguT"]], writes=[B6["bguT"]])
            S.op(dve, lambda e: e.memset(sgb[:], 11.914), writes=[B6["bguT"]])
            WrT = A.alloc("WrT", [32, 16, 128], BF16)
            bdn = A.alloc("bdn", [32, 1024], BF16)
            B6["WrT"] = Buf("WrT")
            B6["bdn"] = Buf("bdn")
            S.dma(pool, bdn[:], b_dn, writes=[B6["bdn"]])
            for q4 in range(4):
                for t4 in range(4):
                    tt = q4 * 4 + t4
                    S.mm(lambda e, q4=q4, t4=t4, tt=tt: e.transpose(pgl[q4][0:32, t4 * 128:(t4 + 1) * 128], Wr[:, tt, :],
                                                                    ident_f[:]),
                         reads=[B_wr[tt], Bc["ident_f"]], writes=[B6["pgl%d" % q4]], signal=(t4 == 3), first=(t4 == 0))
                S.op(dve, lambda e, q4=q4: e.tensor_copy(WrT[:, q4 * 4:(q4 + 1) * 4, :],
                                                         pgl[q4][0:32, :].rearrange("p (t x) -> p t x", t=4)),
                     reads=[B6["pgl%d" % q4]], writes=[B6["WrT"]])
            for tt in range(16):
                o_, oB = po[tt % 2], B6["po%d" % (tt % 2)]
                for cb in range(2):
                    cs = slice(cb * 512, (cb + 1) * 512)
                    S.mm(lambda e, tt=tt, cs=cs, o_=o_: e.matmul(o_[:, cs], WrT[:, tt, :], bdn[:, cs], start=True, stop=True),
                         reads=[B6["WrT"], B6["bdn"]], writes=[oB], signal=(cb == 1), first=(cb == 0))
                S.op(act, lambda e, tt=tt, o_=o_: e.copy(facc[:, tt, :], o_[:]), reads=[oB], writes=[B_fa[tt]])
            S.barrier()
            A.release(m_bgu)
            GU = A.alloc("GU", [128, 8, 2048], BF16)
            DNs = [A.alloc("DN0", [128, 8, 1024], BF16)] * 2
            actT = [A.alloc("actT%d" % i, [128, 8, 512], BF16) for i in range(2)]
            T1 = [A.alloc("T1_%d" % i, [128, 512], F32) for i in range(2)]
            T2 = [A.alloc("T2_%d" % i, [128, 512], F32) for i in range(2)]
            T3 = [A.alloc("T3_%d" % i, [128, 512], F32) for i in range(2)]
            pair_i = [0]

            def load_gu(e_, half):
                wv = w_gu[e_].rearrange("(c p) n -> p c n", p=128)
                hb_ = "GUa" if half == 0 else "GUb"
                for base in (0, 1024):
                    c0 = base + half * 512
                    S.dma(pool, GU[:, :, c0:c0 + 512], wv[:, :, c0:c0 + 512], writes=[B6[hb_]])

            def load_dn(e_):
                S.dma(pool, DNs[e_ % 2][:], w_dn[e_].rearrange("(c p) n -> p c n", p=128), writes=[B6["DN0"]])

            def gu_step(e_, tb, mid=None):
                tok = slice(tb * 512, (tb + 1) * 512)
                aT, aB = actT[tb % 2], B6["actT%d" % (tb % 2)]
                for j in range(8):
                    if j == 4 and mid is not None:
                        mid()
                    guB = B6["GUa"] if j < 4 else B6["GUb"]
                    i = pair_i[0] % 2
                    pair_i[0] += 1
                    pg_, pgB = pgl[2 * i], B6["pgl%d" % (2 * i)]
                    pl_, plB = pgl[2 * i + 1], B6["pgl%d" % (2 * i + 1)]
                    for gi_, (dst, dB, col0) in enumerate([(pg_, pgB, j * 128), (pl_, plB, 1024 + j * 128)]):
                        for c in range(8):
                            S.mm(lambda e, c=c, dst=dst, col0=col0: e.matmul(dst[:], GU[:, c, col0:col0 + 128],
                                                                            h2T[:, c, tok], start=(c == 0), stop=(c == 7)),
                                 reads=[guB, B_h2[tb]], writes=[dB], signal=(c == 7 and gi_ == 1), first=(c == 0))
                    t1, t2, t3 = T1[i], T2[i], T3[i]
                    b1, b2, b3 = B6["T1_%d" % i], B6["T2_%d" % i], B6["T3_%d" % i]
                    S.op(act, lambda e, t1=t1, pg_=pg_, j=j: e.activation(out=t1[:], in_=pg_[:], func=AF.Relu,
                                                                          bias=bg7[:, j, e_:e_ + 1], scale=-1.0),
                         reads=[pgB, B6["bguT"]], writes=[b1])
                    S.op(act, lambda e, t1=t1, t3=t3: e.activation(out=t3[:], in_=t1[:], func=AF.Sigmoid, bias=sgb[:, 0:1],
                                                                   scale=-1.702),
                         reads=[b1, B6["bguT"]], writes=[b3])
                    S.op(act, lambda e, t2=t2, pl_=pl_, j=j: e.activation(out=t2[:], in_=pl_[:], func=AF.Identity,
                                                                          bias=bln[:, j, e_:e_ + 1], scale=-1.0),
                         reads=[plB, B6["bguT"]], writes=[b2])
                    S.op(dve, lambda e, t2=t2: e.tensor_scalar(t2[:], t2[:], 7.0, -7.0, op0=ALU.min, op1=ALU.max),
                         reads=[b2], writes=[b2])
                    S.op(dve, lambda e, t1=t1, t3=t3: e.scalar_tensor_tensor(out=t1[:], in0=t1[:], scalar=7.0, in1=t3[:],
                                                                             op0=ALU.subtract, op1=ALU.mult),
                         reads=[b1, b3], writes=[b1])
                    S.op(dve, lambda e, t1=t1, t2=t2, j=j: e.scalar_tensor_tensor(out=aT[:, j, :], in0=t2[:], scalar=1.0,
                                                                                  in1=t1[:], op0=ALU.subtract, op1=ALU.mult),
                         reads=[b1, b2], writes=[aB])

            def dn_step(e_, tb):
                aT, aB = actT[tb % 2], B6["actT%d" % (tb % 2)]
                DN, dnB = DNs[0], B6["DN0"]
                for ti in range(4):
                    tt = tb * 4 + ti
                    o_, oB = po[tt % 2], B6["po%d" % (tt % 2)]
                    for cb in range(2):
                        cs = slice(cb * 512, (cb + 1) * 512)
                        for j in range(8):
                            S.mm(lambda e, j=j, cs=cs, ti=ti: e.matmul(o_[:, cs], aT[:, j, ti * 128:(ti + 1) * 128],
                                                                      DN[:, j, cs], start=(j == 0), stop=(j == 7)),
                                 reads=[aB, dnB], writes=[oB], signal=(j == 7 and cb == 1), first=(j == 0 and cb == 0))
                    if True:
                        S.op(dve, lambda e, tt=tt: e.scalar_tensor_tensor(out=facc[:, tt, :], in0=o_[:],
                                                                          scalar=Wr[:, tt, e_:e_ + 1], in1=facc[:, tt, :],
                                                                          op0=ALU.mult, op1=ALU.add),
                             reads=[oB, B_wr[tt], B_fa[tt]], writes=[B_fa[tt]])

            load_gu(0, 0)
            load_gu(0, 1)
            load_dn(0)
            for e_ in range(n_experts):
                nxt = e_ + 1 < n_experts
                gu_step(e_, 0)
                gu_step(e_, 1)
                dn_step(e_, 0)
                gu_step(e_, 2)
                dn_step(e_, 1)
                gu_step(e_, 3, mid=(lambda e_=e_: load_gu(e_ + 1, 0)) if nxt else None)
                if nxt:
                    load_gu(e_ + 1, 1)
                dn_step(e_, 2)
                dn_step(e_, 3)
                if nxt:
                    load_dn(e_ + 1)
            S.barrier()
        A.release(m_moe)

        with ExitStack() as pes:
            x1t = [A.alloc("x1f%d" % i, [128, 1024], F32) for i in range(2)]
            ot_ = [A.alloc("of%d" % i, [128, 1024], F32) for i in range(2)]
            tt_ = A.alloc("tt7", [128, 1024], F32)
            junk = A.alloc("junk7", [128, 1024], BF16)
            sm = A.alloc("sm7", [128, 16, 4], F32)
            B7 = {k: Buf(k) for k in ["x1f0", "x1f1", "of0", "of1", "tt"]}
            Bsm = [[Buf("sm7_%d_%d" % (i, j)) for j in range(3)] for i in range(16)]
            outs = []
            for tt in range(16):
                k = tt % 2
                tsl = slice(tt * 128, (tt + 1) * 128)
                S.dma(sp, x1t[k][:], x1_s[tsl, :], reads=[B_x1[tt]], writes=[B7["x1f%d" % k]])
                S.op(act, lambda e, tt=tt: e.activation(out=junk[:], in_=facc[:, tt, :], func=AF.Square,
                                                        accum_out=sm[:, tt, 0:1]), reads=[B_fa[tt]], writes=[Bsm[tt][0]])
                S.op(act, lambda e, tt=tt: e.activation(out=sm[:, tt, 1:2], in_=sm[:, tt, 0:1], func=AF.Ln, bias=EPS,
                                                        scale=1.0 / 1024), reads=[Bsm[tt][0]], writes=[Bsm[tt][1]])
                S.op(act, lambda e, tt=tt: e.activation(out=sm[:, tt, 2:3], in_=sm[:, tt, 1:2], func=AF.Exp, scale=-0.5),
                     reads=[Bsm[tt][1]], writes=[Bsm[tt][2]])
                S.op(dve, lambda e, tt=tt: e.scalar_tensor_tensor(out=tt_[:], in0=facc[:, tt, :], scalar=sm[:, tt, 2:3],
                                                                  in1=gf_b[:], op0=ALU.mult, op1=ALU.mult),
                     reads=[B_fa[tt], Bsm[tt][2], Bc["gf_b"]], writes=[B7["tt"]])
                S.op(dve, lambda e, k=k: e.tensor_tensor(out=ot_[k][:], in0=tt_[:], in1=x1t[k][:], op=ALU.add),
                     reads=[B7["tt"], B7["x1f%d" % k]], writes=[B7["of%d" % k]])
                outs.append(S.dma(sp, out[tsl, :], ot_[k][:], reads=[B7["of%d" % k]]))
            S.barrier()
        build_program.stats = dict(n_inst=S.n_inst, sbuf_peak=A.peak, auto_sbuf_left=nc.sbuf_bytes_remaining)
    return nc, dbg_outs


def _rope_tables(rot_dim):
    rows = 64
    row = np.repeat(np.arange(rows, dtype=np.float32), 64)
    col = np.tile(np.arange(64, dtype=np.float32), rows)
    quarter = rot_dim // 4
    inv = (np.float32(10000.0) ** (-np.arange(quarter, dtype=np.float32) / np.float32(quarter))).astype(np.float32)
    ang = np.concatenate([row[:, None] * inv, col[:, None] * inv], axis=-1).astype(np.float32)
    return np.cos(ang).astype(np.float32), np.sin(ang).astype(np.float32)


def _const_tables():
    ca, sa = _rope_tables(64)
    cb, sb = _rope_tables(32)
    tc = np.zeros((96, 4096), np.float32)
    ts = np.zeros((96, 4096), np.float32)
    tc[0:32] = ca.T
    tc[32:64] = ca.T
    ts[0:32] = -sa.T
    ts[32:64] = sa.T
    tc[64:80] = cb.T
    tc[80:96] = cb.T
    ts[64:80] = -sb.T
    ts[80:96] = sb.T
    return tc, ts


def _masks(s):
    NEG = -30000.0
    kk = np.arange(128)[:, None]
    ii = np.arange(128)[None, :]
    mL = np.where(ii <= kk, 0.0, NEG).astype(np.float32)
    mR = np.where(kk <= ii, 0.0, NEG).astype(np.float32)
    allneg = np.full((128, 128), NEG, np.float32)
    mLe = allneg if s == 0 else mL
    mRe = mR if s == 0 else allneg
    m = np.stack([np.tile(x, (1, 4)) for x in (mL, mR, mLe, mRe)], axis=1)
    return np.ascontiguousarray(m)


def _swap_halves(w, width):
    n = w.shape[1] // width
    w3 = w.reshape(w.shape[0], n, width)
    h = width // 2
    return np.concatenate([w3[:, :, h:], w3[:, :, :h]], axis=2).reshape(w.shape[0], n * width)


def prep_inputs(I):
    f = lambda a: np.ascontiguousarray(np.asarray(a, dtype=np.float32))
    w_in = f(I["w_in"][0])
    qa, ka = w_in[:, 0:512], w_in[:, 512:640]
    kr = w_in[:, 1408:1440]
    z64 = np.zeros((1024, 64), np.float32)
    w_inx = np.concatenate([w_in[:, 0:1408], z64, kr, z64, _swap_halves(kr, 32), _swap_halves(qa, 64),
                            _swap_halves(ka, 64)], axis=1)
    assert w_inx.shape[1] == C_END
    w_uq = f(I["w_uq"][0])
    wq3 = w_uq.reshape(384, 8, 96)
    w_uqp = np.zeros_like(wq3)
    w_uqp[:, :, 64:96] = _swap_halves(wq3[:, :, 64:96].reshape(384, 256), 32).reshape(384, 8, 32)
    w_uqp = np.ascontiguousarray(w_uqp.reshape(384, 768))
    rows = np.concatenate([f(I["g_mix_pre"][0]), f(I["g_mix_post"][0]), f(I["g_ffn_pre"][0]), f(I["g_ffn_post"][0]),
                           f(I["b_ada"][0])])[None, :]
    tc, ts = _const_tables()
    shared = dict(
        w_ada=f(I["w_ada"][0]), rows=np.ascontiguousarray(rows), w_inx=np.ascontiguousarray(w_inx),
        sinkr=np.ascontiguousarray(np.repeat(f(I["sink"][0]), 128)[None, :]),
        gq=np.ascontiguousarray(f(I["g_q_a"][0]).reshape(3, 128).T), gkv=np.ascontiguousarray(f(I["g_kv_a"][0]).reshape(2, 128).T),
        w_uq=w_uq, w_uqp=w_uqp, w_ukv=f(I["w_ukv"][0]), w_o=f(I["w_o"][0]), w_r=f(I["w_router"][0]),
        b_r16=np.ascontiguousarray(np.tile(f(I["b_router"][0]), 16)[None, :]), w_gu=f(I["w_gate_up"][0]), b_gu=f(I["b_gate_up"][0]), w_dn=f(I["w_down"][0]),
        b_dn=f(I["b_down"][0]), ident=np.eye(128, dtype=np.float32))
    x = np.asarray(I["x"], dtype=np.float32)
    ctx = np.asarray(I["ctx"], dtype=np.float32)
    c = np.asarray(I["c"], dtype=np.float32)
    c_ctx = np.asarray(I["c_ctx"], dtype=np.float32)
    maps = []
    for core in range(8):
        b, s = core // 2, core % 2
        own = x[b, s * 2048:(s + 1) * 2048]
        oth = x[b, (1 - s) * 2048:(2 - s) * 2048]
        xin = np.ascontiguousarray(np.concatenate([own, oth, ctx[b]], axis=0))
        cvec = np.ascontiguousarray(np.concatenate([c[b].reshape(8, 128).T, c_ctx.reshape(8, 128).T], axis=1))
        order = np.concatenate([np.arange(s * 2048, (s + 1) * 2048), np.arange((1 - s) * 2048, (2 - s) * 2048)])
        m = dict(shared)
        m.update(xin=xin, cvec=cvec, tabc=np.ascontiguousarray(tc[:, order]), tabs=np.ascontiguousarray(ts[:, order]),
                 masks=_masks(s))
        maps.append(m)
    return maps


_CACHE = {}


def kernel(**inputs):
    if "nc" not in _CACHE:
        _CACHE["nc"] = build_program()[0]
    nc = _CACHE["nc"]
    maps = prep_inputs(inputs)
    res = run_bass_kernel_spmd(nc, maps, core_ids=list(range(8)))
    outp = np.zeros((4, 4096, 1024), np.float32)
    for core in range(8):
        b, s = core // 2, core % 2
        outp[b, s * 2048:(s + 1) * 2048] = res.results[core]["out"]
    return outp
```
